# Optimizing a Trainium2 kernel written in Bass

```python
import math
import jax, jax.numpy as jnp
from jax import lax
import numpy as np

D_MODEL = 2048
BATCH = 4
SEQ = 4096
DEPTH = 4

D_MIX = D_MODEL
NORM_EPS = 1e-6
ML_HEADS = 4
ML_HEAD_DIM = 128
ML_WIDTH = ML_HEADS * ML_HEAD_DIM
ML_CONV = 4
ML_CHUNK = 64
MLA_HEADS = 8
MLA_NOPE = 128
MLA_ROPE = 64
MLA_V = 128
MLA_WIDTH = MLA_HEADS * MLA_V
MLA_Q_RANK = 512
MLA_KV_RANK = 256
ROPE_BASE = 10000.0
Q_BLOCK = 128
RW_HEAD_DIM = 64
RW_WIDTH = D_MIX - ML_WIDTH - MLA_WIDTH
RW_HEADS = RW_WIDTH // RW_HEAD_DIM
RW_DECAY_RANK = 64
RW_AAA_RANK = 64
RW_MV_RANK = 32
RW_GN_EPS = 64e-5
RW_MIX_WIDTH = 3 * RW_WIDTH + RW_DECAY_RANK + RW_AAA_RANK

IN_SIZES = (2 * ML_WIDTH, ML_WIDTH, ML_HEADS, ML_HEADS, ML_WIDTH, ML_WIDTH,
            MLA_Q_RANK, MLA_KV_RANK, MLA_ROPE, MLA_WIDTH,
            RW_MIX_WIDTH, RW_WIDTH)
D_IN = (5 * ML_WIDTH + 2 * ML_HEADS + MLA_Q_RANK + MLA_KV_RANK + MLA_ROPE + MLA_WIDTH
        + RW_MIX_WIDTH + RW_WIDTH)

kernel_name = 'hymba_style_mlstm_mla_rwkv7_trunk'


def split_offsets():
    return [int(o) for o in np.cumsum(np.array(IN_SIZES))[:-1]]


def rms_norm(x, g, eps=NORM_EPS):
    xf = x.astype(jnp.float32)
    y = xf * lax.rsqrt(jnp.mean(xf * xf, axis=-1, keepdims=True) + eps)
    return (y * g.astype(jnp.float32)).astype(x.dtype)


def head_layer_norm(x, g, b, eps):
    xf = x.astype(jnp.float32)
    mu = jnp.mean(xf, axis=-1, keepdims=True)
    var = jnp.mean(jnp.square(xf - mu), axis=-1, keepdims=True)
    y = (xf - mu) * lax.rsqrt(var + eps) * g.astype(jnp.float32)
    if b is not None:
        y = y + b.astype(jnp.float32)
    return y


def token_shift(x):
    return jnp.pad(x, ((0, 0), (1, 0), (0, 0)))[:, :-1]


def causal_conv(x, w, b):
    T = x.shape[1]
    K = w.shape[0]
    xp = jnp.pad(x, ((0, 0), (K - 1, 0), (0, 0)))
    y = b
    for j in range(K):
        y = y + w[j] * xp[:, j:j + T]
    return y


def mlstm_chunkwise(q, k, v, log_i, log_f):
    B, H, T, d = q.shape
    L = ML_CHUNK
    nc = T // L
    q = q * (d ** -0.5)

    def to_chunks(a):
        return jnp.moveaxis(a.reshape((B, H, nc, L) + a.shape[3:]), 2, 0)

    causal = jnp.tril(jnp.ones((L, L), dtype=bool))

    def step(carry, inp):
        C, n, m = carry
        qc, kc, vc, li, lf = inp
        b = jnp.cumsum(lf, axis=-1)
        m_inter = b + m[..., None]
        D = jnp.where(causal, b[..., :, None] - b[..., None, :] + li[..., None, :], -jnp.inf)
        m_t = jnp.maximum(m_inter, jnp.max(D, axis=-1))
        inter = jnp.exp(m_inter - m_t)
        S = jnp.einsum('bhld,bhsd->bhls', qc, kc) * jnp.exp(D - m_t[..., None])
        num = inter[..., None] * jnp.einsum('bhvk,bhlk->bhlv', C, qc) + jnp.einsum('bhls,bhsv->bhlv', S, vc)
        den = inter * jnp.einsum('bhk,bhlk->bhl', n, qc) + jnp.sum(S, axis=-1)
        h = num / jnp.maximum(jnp.abs(den), jnp.exp(-m_t))[..., None]
        b_last = b[..., -1]
        g_s = b_last[..., None] - b + li
        m_new = jnp.maximum(b_last + m, jnp.max(g_s, axis=-1))
        decay = jnp.exp(b_last + m - m_new)
        w_s = jnp.exp(g_s - m_new[..., None])
        C = decay[..., None, None] * C + jnp.einsum('bhs,bhsv,bhsk->bhvk', w_s, vc, kc)
        n = decay[..., None] * n + jnp.einsum('bhs,bhsk->bhk', w_s, kc)
        return (C, n, m_new), h

    init = (jnp.zeros((B, H, d, d), jnp.float32), jnp.zeros((B, H, d), jnp.float32),
            jnp.zeros((B, H), jnp.float32))
    _, h = lax.scan(step, init, (to_chunks(q), to_chunks(k), to_chunks(v),
                                 to_chunks(log_i), to_chunks(log_f)))
    return jnp.moveaxis(h, 0, 2).reshape(B, H, T, d)


def mlstm_branch(ml_qk, ml_v, ml_i, ml_f, ml_o, conv_w, conv_b, i_bias, f_bias, norm_g):
    B, T, _ = ml_qk.shape
    qk = jax.nn.silu(causal_conv(ml_qk, conv_w, conv_b))
    q, k = jnp.split(qk, 2, axis=-1)

    def to_heads(t):
        return t.astype(jnp.float32).reshape(B, T, ML_HEADS, ML_HEAD_DIM).transpose(0, 2, 1, 3)

    log_i = (ml_i + i_bias).astype(jnp.float32).transpose(0, 2, 1)
    log_f = jax.nn.log_sigmoid((ml_f + f_bias).astype(jnp.float32)).transpose(0, 2, 1)
    h = mlstm_chunkwise(to_heads(q), to_heads(k), to_heads(ml_v), log_i, log_f)
    h = head_layer_norm(h.transpose(0, 2, 1, 3), norm_g.reshape(ML_HEADS, ML_HEAD_DIM), None, NORM_EPS)
    return jax.nn.sigmoid(ml_o.astype(jnp.float32)) * h.reshape(B, T, ML_WIDTH)


def rope_cos_sin(positions):
    inv_freq = jnp.power(ROPE_BASE, -jnp.arange(0, MLA_ROPE, 2, dtype=jnp.float32) / MLA_ROPE)
    ang = positions.astype(jnp.float32)[..., None] * inv_freq
    return jnp.cos(ang)[:, :, None, :], jnp.sin(ang)[:, :, None, :]


def apply_rope(x, cos, sin):
    half = x.shape[-1] // 2
    x1, x2 = x[..., :half], x[..., half:]
    cos = cos.astype(x.dtype)
    sin = sin.astype(x.dtype)
    return jnp.concatenate([x1 * cos - x2 * sin, x2 * cos + x1 * sin], axis=-1)


def mla_branch(c_q, c_kv, k_rope, cos, sin, q_norm_g, w_uq, kv_norm_g, w_ukv):
    B, T, _ = c_q.shape
    q = (rms_norm(c_q, q_norm_g) @ w_uq).reshape(B, T, MLA_HEADS, MLA_NOPE + MLA_ROPE)
    q_nope, q_rope = q[..., :MLA_NOPE], apply_rope(q[..., MLA_NOPE:], cos, sin)
    kv = (rms_norm(c_kv, kv_norm_g) @ w_ukv).reshape(B, T, MLA_HEADS, MLA_NOPE + MLA_V)
    k_nope, v = kv[..., :MLA_NOPE], kv[..., MLA_NOPE:]
    k_r = apply_rope(k_rope[:, :, None, :], cos, sin)[:, :, 0]
    scale = (MLA_NOPE + MLA_ROPE) ** -0.5
    outs = []
    for blk in range(T // Q_BLOCK):
        s0 = blk * Q_BLOCK
        s1 = s0 + Q_BLOCK
        scores = (jnp.einsum('bqhd,bkhd->bhqk', q_nope[:, s0:s1], k_nope[:, :s1])
                  + jnp.einsum('bqhr,bkr->bhqk', q_rope[:, s0:s1], k_r[:, :s1]))
        scores = scores.astype(jnp.float32) * scale
        mask = (s0 + jnp.arange(Q_BLOCK))[:, None] >= jnp.arange(s1)[None, :]
        p = jax.nn.softmax(jnp.where(mask, scores, -jnp.inf), axis=-1).astype(v.dtype)
        outs.append(jnp.einsum('bhqk,bkhv->bqhv', p, v[:, :s1]))
    return jnp.concatenate(outs, axis=1).reshape(B, T, MLA_WIDTH)


def rwkv7_scan(r, w, k, v, a_vec, b_vec):
    B, T, H, N = r.shape

    def step(S, inp):
        rt, wt, kt, vt, at, bt = inp
        sa = jnp.einsum('bhvk,bhk->bhv', S, at)
        S = S * wt[:, :, None, :] + sa[..., None] * bt[:, :, None, :] + vt[..., None] * kt[:, :, None, :]
        return S, jnp.einsum('bhvk,bhk->bhv', S, rt)

    xs = tuple(jnp.moveaxis(t, 1, 0) for t in (r, w, k, v, a_vec, b_vec))
    _, y = lax.scan(step, jnp.zeros((B, H, N, N), jnp.float32), xs)
    return jnp.moveaxis(y, 0, 1)


def rwkv7_branch(rw_mix, v_first, vres, mu, w0, w2, a0, a2, k_k, k_a, r_k, ln_g, ln_b):
    B, T, _ = rw_mix.shape
    xm = rw_mix.astype(jnp.float32)
    xs = xm + mu * (token_shift(xm) - xm)
    r, k, v, xw, xa = jnp.split(xs, [RW_WIDTH, 2 * RW_WIDTH, 3 * RW_WIDTH,
                                     3 * RW_WIDTH + RW_DECAY_RANK], axis=-1)
    log_w = -jax.nn.softplus(-(w0 + jnp.tanh(xw) @ w2)) - 0.5
    decay = jnp.exp(-jnp.exp(log_w))
    a = jax.nn.sigmoid(a0 + xa @ a2)
    if vres is None:
        v_first = v
    else:
        v0, v1, v2 = vres
        v = v + (v_first - v) * jax.nn.sigmoid(v0 + (v @ v1) @ v2)

    def heads(t):
        return t.reshape(B, T, RW_HEADS, RW_HEAD_DIM)

    kk = heads(k * k_k)
    kk = kk / jnp.maximum(jnp.sqrt(jnp.sum(kk * kk, axis=-1, keepdims=True)), 1e-12)
    a_h = heads(a)
    k_h = heads(k * (1.0 + (a - 1.0) * k_a))
    r_h, v_h = heads(r), heads(v)
    y = rwkv7_scan(r_h, heads(decay), k_h, v_h, -kk, kk * a_h)
    y = head_layer_norm(y, ln_g.reshape(RW_HEADS, RW_HEAD_DIM), ln_b.reshape(RW_HEADS, RW_HEAD_DIM), RW_GN_EPS)
    y = y + jnp.sum(r_h * k_h * r_k, axis=-1, keepdims=True) * v_h
    return y.reshape(B, T, RW_WIDTH), v_first


def setup_inputs(seed: int = 0) -> dict:
    key = jax.random.key(seed)
    ks = jax.random.split(key, 32)

    def nrm(k, shape, scale):
        return scale * jax.random.normal(k, shape, jnp.float32)

    def uni(k, shape, lo, hi):
        return jax.random.uniform(k, shape, jnp.float32, lo, hi)

    x = jax.random.normal(ks[0], (BATCH, SEQ, D_MODEL), jnp.float32)
    offs = jax.random.randint(ks[1], (BATCH, 1), 0, 1024)
    positions = (jnp.arange(SEQ)[None, :] + offs).astype(jnp.int32)
    return {
        'x': x,
        'positions': positions,
        'norm_g': 1.0 + nrm(ks[2], (DEPTH, D_MODEL), 0.02),
        'w_in': nrm(ks[3], (DEPTH, D_MODEL, D_IN), D_MODEL ** -0.5),
        'ml_conv_w': nrm(ks[4], (DEPTH, ML_CONV, 2 * ML_WIDTH), 0.5),
        'ml_conv_b': nrm(ks[5], (DEPTH, 2 * ML_WIDTH), 0.01),
        'ml_i_bias': nrm(ks[6], (DEPTH, ML_HEADS), 0.1),
        'ml_f_bias': uni(ks[7], (DEPTH, ML_HEADS), 3.0, 6.0),
        'ml_norm_g': 1.0 + nrm(ks[8], (DEPTH, ML_WIDTH), 0.02),
        'mla_q_norm_g': 1.0 + nrm(ks[9], (DEPTH, MLA_Q_RANK), 0.02),
        'mla_w_uq': nrm(ks[10], (DEPTH, MLA_Q_RANK, MLA_HEADS * (MLA_NOPE + MLA_ROPE)), MLA_Q_RANK ** -0.5),
        'mla_kv_norm_g': 1.0 + nrm(ks[11], (DEPTH, MLA_KV_RANK), 0.02),
        'mla_w_ukv': nrm(ks[12], (DEPTH, MLA_KV_RANK, MLA_HEADS * (MLA_NOPE + MLA_V)), MLA_KV_RANK ** -0.5),
        'rw_mu': uni(ks[13], (DEPTH, RW_MIX_WIDTH), 0.0, 1.0),
        'rw_w0': uni(ks[14], (DEPTH, RW_WIDTH), -4.0, 0.0),
        'rw_w2': nrm(ks[15], (DEPTH, RW_DECAY_RANK, RW_WIDTH), 0.5 * RW_DECAY_RANK ** -0.5),
        'rw_a0': nrm(ks[16], (DEPTH, RW_WIDTH), 0.1),
        'rw_a2': nrm(ks[17], (DEPTH, RW_AAA_RANK, RW_WIDTH), 0.5 * RW_AAA_RANK ** -0.5),
        'rw_v0': nrm(ks[18], (DEPTH - 1, RW_WIDTH), 0.1),
        'rw_v1': nrm(ks[19], (DEPTH - 1, RW_WIDTH, RW_MV_RANK), RW_WIDTH ** -0.5),
        'rw_v2': nrm(ks[20], (DEPTH - 1, RW_MV_RANK, RW_WIDTH), 0.5 * RW_MV_RANK ** -0.5),
        'rw_k_k': 0.85 + nrm(ks[21], (DEPTH, RW_WIDTH), 0.05),
        'rw_k_a': 1.0 + nrm(ks[22], (DEPTH, RW_WIDTH), 0.05),
        'rw_r_k': nrm(ks[23], (DEPTH, RW_HEADS, RW_HEAD_DIM), 0.1),
        'rw_ln_g': 1.0 + nrm(ks[24], (DEPTH, RW_WIDTH), 0.02),
        'rw_ln_b': nrm(ks[25], (DEPTH, RW_WIDTH), 0.01),
        'w_out': nrm(ks[26], (DEPTH, D_MIX, D_MODEL), 0.5 * D_MIX ** -0.5),
        'final_norm_g': 1.0 + nrm(ks[27], (D_MODEL,), 0.02),
    }


def reference(x, positions, norm_g, w_in, ml_conv_w, ml_conv_b, ml_i_bias, ml_f_bias, ml_norm_g,
              mla_q_norm_g, mla_w_uq, mla_kv_norm_g, mla_w_ukv,
              rw_mu, rw_w0, rw_w2, rw_a0, rw_a2, rw_v0, rw_v1, rw_v2, rw_k_k, rw_k_a, rw_r_k,
              rw_ln_g, rw_ln_b, w_out, final_norm_g):
    cos, sin = rope_cos_sin(positions)
    offsets = split_offsets()
    v_first = None
    for l in range(DEPTH):
        h = rms_norm(x, norm_g[l])
        proj = h @ w_in[l]
        (ml_qk, ml_v, ml_i, ml_f, ml_o, ml_z, mla_cq, mla_ckv, mla_kr, mla_z,
         rw_mix, rw_z) = jnp.split(proj, offsets, axis=-1)
        y_ml = mlstm_branch(ml_qk, ml_v, ml_i, ml_f, ml_o, ml_conv_w[l], ml_conv_b[l],
                            ml_i_bias[l], ml_f_bias[l], ml_norm_g[l])
        y_mla = mla_branch(mla_cq, mla_ckv, mla_kr, cos, sin, mla_q_norm_g[l], mla_w_uq[l],
                           mla_kv_norm_g[l], mla_w_ukv[l])
        vres = None if l == 0 else (rw_v0[l - 1], rw_v1[l - 1], rw_v2[l - 1])
        y_rw, v_first = rwkv7_branch(rw_mix, v_first, vres, rw_mu[l], rw_w0[l], rw_w2[l], rw_a0[l],
                                     rw_a2[l], rw_k_k[l], rw_k_a[l], rw_r_k[l], rw_ln_g[l], rw_ln_b[l])
        mixed = jnp.concatenate([jax.nn.silu(ml_z) * y_ml.astype(x.dtype),
                                 jax.nn.silu(mla_z) * y_mla.astype(x.dtype),
                                 jax.nn.silu(rw_z) * y_rw.astype(x.dtype)], axis=-1)
        x = x + mixed @ w_out[l]
    return rms_norm(x, final_norm_g)
```

```python
import numpy as np
from contextlib import ExitStack
import concourse.bass as bass
import concourse.mybir as mybir
from concourse.bass_utils import run_bass_kernel_spmd

F32 = mybir.dt.float32
I32 = mybir.dt.int32
AF = mybir.ActivationFunctionType
ALU = mybir.AluOpType
AX = mybir.AxisListType

D = 2048
DIN = 6600
EPS = 1e-6
FM_RANGES = [(0, 1024), (2568, 3336), (4424, 6088)]
TM_RANGES = [(1024, 2568), (3336, 4424), (6088, 6600)]
TM_V, TM_I, TM_F, TM_O, TM_Z = 0, 512, 516, 520, 1032
TM_KR, TM_MZ, TM_RZ = 1544, 1608, 2632
NTM = 3144
FM_QK, FM_CQ, FM_CKV, FM_RW = 0, 8, 12, 14
NFM = 27


_UID = [0]


def sbt(nc, name, shape, dt):
    _UID[0] += 1
    return nc.sbuf_tensor("%s_u%d" % (name, _UID[0]), shape, dt)


def pst(nc, name, shape, dt):
    _UID[0] += 1
    return nc.psum_tensor("%s_u%d" % (name, _UID[0]), shape, dt)


class Buf:
    __slots__ = ("w", "r")

    def __init__(self):
        self.w = None
        self.r = {}


class KB:
    NDS = 24

    def __init__(self, nc):
        self.nc = nc
        self.E = {"pe": nc.tensor, "act": nc.scalar, "dve": nc.vector, "pool": nc.gpsimd, "sp": nc.sync}
        self.sem = {e: nc.alloc_semaphore("s_" + e) for e in ("pe", "act", "dve", "pool")}
        self.cnt = {e: 0 for e in self.sem}
        self.dsem = [nc.alloc_semaphore("d%d" % i) for i in range(self.NDS)]
        self.dcnt = [0] * self.NDS
        self.dnext = 0
        self.bar = nc.alloc_semaphore("bar")
        self.nbar = 0
        self.seen = {e: {} for e in self.E}

    def _semh(self, key):
        return self.sem[key] if isinstance(key, str) else self.dsem[key[1]]

    def _wait(self, e, key, val, same_ok=False):
        if val <= 0:
            return
        if same_ok and key == e:
            return
        if self.seen[e].get(key, 0) >= val:
            return
        self.E[e].wait_ge(self._semh(key), val)
        self.seen[e][key] = val

    def _deps(self, e, reads, writes):
        for b in reads:
            if b.w is not None:
                self._wait(e, b.w[0], b.w[1])
        for b in writes:
            if b.w is not None:
                self._wait(e, b.w[0], b.w[1])
            for key, val in b.r.items():
                self._wait(e, key, val, same_ok=True)

    def _mark(self, ev, reads, writes):
        for b in reads:
            if b.r.get(ev[0], 0) < ev[1]:
                b.r[ev[0]] = ev[1]
        for b in writes:
            b.w = ev
            b.r = {}

    def op(self, e, fn, R=(), W=()):
        self._deps(e, R, W)
        ins = fn()
        self.cnt[e] += 1
        ins.then_inc(self.sem[e], 1)
        self._mark((e, self.cnt[e]), R, W)

    def dma(self, q, out, in_, R=(), W=(), slow=False):
        i = self.dnext
        self.dnext = (i + 1) % self.NDS
        key = ("d", i)
        self._wait(q, key, self.dcnt[i])
        self._deps(q, R, W)
        if slow:
            ins = self.E[q].dma_start(out=out, in_=in_, allow_slow_non_contiguous=True)
        else:
            ins = self.E[q].dma_start(out=out, in_=in_)
        self.dcnt[i] += 16
        ins.then_inc(self.dsem[i], 16)
        self._mark((key, self.dcnt[i]), R, W)

    def barrier(self):
        sp = self.E["sp"]
        for e in self.sem:
            self._wait("sp", e, self.cnt[e])
        for i in range(self.NDS):
            self._wait("sp", ("d", i), self.dcnt[i])
        self.nbar += 1
        sp.sem_inc(self.bar, 1)
        for e in ("pe", "act", "dve", "pool"):
            self.E[e].wait_ge(self.bar, self.nbar)
            for k2 in self.sem:
                self.seen[e][k2] = self.cnt[k2]
            for i in range(self.NDS):
                self.seen[e][("d", i)] = self.dcnt[i]


class Ring:
    def __init__(self, es, nc, name, shape, dtype, n, psum=False):
        self.items = []
        for i in range(n):
            if psum:
                t = es.enter_context(pst(nc, "%s%d" % (name, i), shape, dtype))
            else:
                t = es.enter_context(sbt(nc, "%s%d" % (name, i), shape, dtype))
            self.items.append((t, Buf()))
        self.i = 0

    def next(self):
        it = self.items[self.i]
        self.i = (self.i + 1) % len(self.items)
        return it


def split_blocks(ranges, maxw=512):
    out = []
    for (a, b) in ranges:
        c = a
        while c < b:
            w = min(maxw, b - c)
            out.append((c, w))
            c += w
    return out


def make_consts():
    c = np.zeros((128, 1280), np.float32)
    c[:, 0:128] = np.eye(128, dtype=np.float32)
    j = np.arange(128)
    c[:, 128:256] = (j[:, None] <= j[None, :]).astype(np.float32)
    c[:, 256:384] = 1.0
    same = (j[:, None] // 64) == (j[None, :] // 64)
    c[:, 384:512] = ((j[:, None] <= j[None, :]) & same)
    c[:, 512:640] = ((j[:, None] < j[None, :]) & same)
    invf = np.power(10000.0, -np.arange(0, 64, 2, dtype=np.float32) / 64).astype(np.float32)
    c[:, 640:672] = invf[None, :]
    c[:, 672:674] = (j[:, None] // 64 == np.arange(2)[None, :])
    c[:, 700] = 1e-6
    c[:, 701] = 64e-5
    c[:, 702] = 1.0
    c[:, 703] = -0.5
    c[:, 704] = 0.0
    c[:, 705] = -np.pi
    c[:, 706] = 1e-24
    c[:, 707] = 1.0 / 128
    i64 = np.arange(64)
    c[:64, 768:832] = (i64[:, None] < i64[None, :])
    c[:64, 832:896] = (i64[:, None] <= i64[None, :])
    c[:64, 896:960] = (i64[:, None] > i64[None, :])
    for jj in range(4):
        c[:, 960 + jj * 8:968 + jj * 8] = ((2 * jj + j[:, None] // 64) == np.arange(8)[None, :])
    c[:, 1024:1152] = same
    return c


C_ID, C_TRI, C_ONE, C_BI, C_BS, C_IF, C_CI = 0, 128, 256, 384, 512, 640, 672
C_M1, C_M3, C_HS, C_BD = 768, 896, 960, 1024
C_EPS, C_GNEPS, C_1, C_MH, C_0, C_MPI, C_TINY, C_R128 = 700, 701, 702, 703, 704, 705, 706, 707


def build(T, depth, dbg=()):
    assert T % 128 == 0
    NT = T // 128
    SG = min(T, 1024)
    NSG = T // SG
    G = min(T, 512)
    NG = T // G
    nc = bass.Bass("TRN2", target_bir_lowering=False)

    def din(name, shape, dt=F32):
        return nc.dram_tensor(name, list(shape), dt, kind="ExternalInput").ap()

    def dscr(name, shape, dt=F32):
        kind = "ExternalOutput" if name in dbg else "Internal"
        return nc.dram_tensor(name, list(shape), dt, kind=kind).ap()

    x_in = din("x", [T, D])
    pos_in = din("positions", [T], I32)
    consts_in = din("consts", [128, 1280])
    norm_g = din("norm_g", [depth, D])
    w_in = din("w_in", [depth, D, DIN])
    w_out = din("w_out", [depth, D, D])
    final_g = din("final_norm_g", [D])
    ml_conv_w = din("ml_conv_w", [depth, 4, 1024]); ml_conv_b = din("ml_conv_b", [depth, 1024])
    mla_q_norm_g = din("mla_q_norm_g", [depth, 512]); mla_w_uq = din("mla_w_uq", [depth, 512, 1536])
    mla_kv_norm_g = din("mla_kv_norm_g", [depth, 256]); mla_w_ukv = din("mla_w_ukv", [depth, 256, 2048])
    rw_mu = din("rw_mu", [depth, 1664]); rw_w0 = din("rw_w0", [depth, 512]); rw_w2 = din("rw_w2", [depth, 64, 512])
    rw_a0 = din("rw_a0", [depth, 512]); rw_a2 = din("rw_a2", [depth, 64, 512])
    rw_v0 = din("rw_v0", [3, 512]); rw_v1 = din("rw_v1", [3, 512, 32]); rw_v2 = din("rw_v2", [3, 32, 512])
    rw_k_k = din("rw_k_k", [depth, 512]); rw_k_a = din("rw_k_a", [depth, 512]); rw_r_k = din("rw_r_k", [depth, 8, 64])
    rw_ln_g = din("rw_ln_g", [depth, 512]); rw_ln_b = din("rw_ln_b", [depth, 512])
    ml_i_bias = din("ml_i_bias", [depth, 4]); ml_f_bias = din("ml_f_bias", [depth, 4]); ml_norm_g = din("ml_norm_g", [depth, 512])
    out = nc.dram_tensor("out", [T, D], F32, kind="ExternalOutput").ap()

    xT = dscr("xT", [D, T])
    pFM = dscr("pFM", [NFM * 128, T])
    pTM = dscr("pTM", [T, NTM])
    mixT = dscr("mixT", [D, T])
    csd = dscr("cs", [T, 64])
    qnT = dscr("qnT", [1024, T]); knT = dscr("knT", [1024, T]); qrT = dscr("qrT", [512, T]); krT = dscr("krT", [64, T])
    vaug = dscr("vaug", [8, T, 129])
    rwA = dscr("rwA", [512, T]); rwR = dscr("rwR", [512, T]); rwB = dscr("rwB", [512, T]); rwK = dscr("rwK", [512, T])
    rwBp = dscr("rwBp", [T, 512]); rwKp = dscr("rwKp", [T, 512]); rwV = dscr("rwV", [T, 512]); rwY = dscr("rwY", [T, 512])
    rwGL = dscr("rwGL", [512, T // 64]); rwRKR = dscr("rwRKR", [T, 8]); vfirst = dscr("vfirst", [512, T])

    fm_blocks = split_blocks(FM_RANGES)
    tm_blocks = split_blocks(TM_RANGES)

    with ExitStack() as top:
        k = KB(nc)
        cst = top.enter_context(sbt(nc, "cst", [128, 1280], F32))
        cstb = Buf()
        k.dma("sp", cst[:], consts_in, W=[cstb])
        ident = cst[:, C_ID:C_ID + 128]
        ones = cst[:, C_ONE:C_ONE + 128]
        onec = cst[:, C_ONE:C_ONE + 1]

        def rstd(o, i, scale, epscol, R, W, np_=128):
            k.op("act", lambda: nc.scalar.activation(out=o, in_=i, func=AF.Sqrt, bias=cst[:np_, epscol:epscol + 1], scale=scale), R=list(R) + [cstb], W=W)
            k.op("dve", lambda: nc.vector.reciprocal(out=o, in_=o), R=W, W=W)

        with ExitStack() as es:
            xin = Ring(es, nc, "i_x", [128, D], F32, 2)
            ps = Ring(es, nc, "i_ps", [128, 512], F32, 4, psum=True)
            stg = Ring(es, nc, "i_st", [128, 16, 128], F32, 2)
            for t in range(NT):
                xt, xb = xin.next()
                k.dma("sp", xt[:], x_in[t * 128:(t + 1) * 128, :], W=[xb])
                st, sb = stg.next()
                for q in range(4):
                    p, pb = ps.next()
                    for j in range(4):
                        kc = q * 4 + j
                        k.op("pe", lambda: nc.tensor.transpose(p[:, j * 128:(j + 1) * 128], xt[:, kc * 128:(kc + 1) * 128], ident),
                             R=[xb, cstb], W=[pb])
                    eng = "act" if q % 2 else "dve"
                    if eng == "act":
                        k.op("act", lambda: nc.scalar.copy(out=st[:, q * 4:(q + 1) * 4, :], in_=p[:].rearrange("p (a b) -> p a b", a=4)), R=[pb], W=[sb])
                    else:
                        k.op("dve", lambda: nc.vector.tensor_copy(out=st[:, q * 4:(q + 1) * 4, :], in_=p[:].rearrange("p (a b) -> p a b", a=4)), R=[pb], W=[sb])
                k.dma("pool", xT.rearrange("(kc p) t -> p kc t", p=128)[:, :, t * 128:(t + 1) * 128], st[:], R=[sb])
            posi = es.enter_context(sbt(nc, "i_pi", [128, NT], I32))
            posf = es.enter_context(sbt(nc, "i_pf", [128, NT], F32))
            pob = Buf()
            k.dma("sp", posi[:], pos_in.rearrange("(n p) -> p n", p=128), W=[pob], slow=True)
            k.op("dve", lambda: nc.vector.tensor_copy(out=posf[:], in_=posi[:]), R=[pob], W=[pob])
            angr = Ring(es, nc, "i_ang", [128, 64], F32, 2)
            tir = Ring(es, nc, "i_ti", [128, 64], I32, 2)
            tfr = Ring(es, nc, "i_tf", [128, 64], F32, 2)
            TWO_PI = float(2 * np.pi)
            C1 = 6.28125
            C2 = float(2 * np.pi - 6.28125)
            for t in range(NT):
                an, anb = angr.next()
                ti, tib = tir.next()
                tf, tfb = tfr.next()
                k.op("dve", lambda: nc.vector.tensor_scalar(out=an[:, 0:32], in0=cst[:, C_IF:C_IF + 32], scalar1=posf[:, t:t + 1], scalar2=None, op0=ALU.mult), R=[pob, cstb], W=[anb])
                k.op("dve", lambda: nc.vector.tensor_scalar(out=an[:, 32:64], in0=an[:, 0:32], scalar1=float(np.pi / 2), scalar2=None, op0=ALU.add), R=[anb], W=[anb])
                k.op("dve", lambda: nc.vector.tensor_scalar(out=tf[:], in0=an[:], scalar1=float(1 / (2 * np.pi)), scalar2=None, op0=ALU.mult), R=[anb], W=[tfb])
                k.op("dve", lambda: nc.vector.tensor_copy(out=ti[:], in_=tf[:]), R=[tfb], W=[tib])
                k.op("dve", lambda: nc.vector.tensor_copy(out=tf[:], in_=ti[:]), R=[tib], W=[tfb])
                k.op("dve", lambda: nc.vector.scalar_tensor_tensor(out=an[:], in0=tf[:], scalar=-C1, in1=an[:], op0=ALU.mult, op1=ALU.add), R=[tfb, anb], W=[anb])
                k.op("dve", lambda: nc.vector.scalar_tensor_tensor(out=an[:], in0=tf[:], scalar=-C2, in1=an[:], op0=ALU.mult, op1=ALU.add), R=[tfb, anb], W=[anb])
                k.op("dve", lambda: nc.vector.tensor_scalar(out=tf[:], in0=an[:], scalar1=float(np.pi), scalar2=-TWO_PI, op0=ALU.is_gt, op1=ALU.mult), R=[anb], W=[tfb])
                k.op("dve", lambda: nc.vector.tensor_tensor(out=an[:], in0=an[:], in1=tf[:], op=ALU.add), R=[tfb, anb], W=[anb])
                k.op("dve", lambda: nc.vector.tensor_scalar(out=tf[:], in0=an[:], scalar1=float(-np.pi), scalar2=TWO_PI, op0=ALU.is_lt, op1=ALU.mult), R=[anb], W=[tfb])
                k.op("dve", lambda: nc.vector.tensor_tensor(out=an[:], in0=an[:], in1=tf[:], op=ALU.add), R=[tfb, anb], W=[anb])
                k.op("act", lambda: nc.scalar.activation(out=an[:], in_=an[:], func=AF.Sin), R=[anb], W=[anb])
                k.dma("pool", csd[t * 128:(t + 1) * 128, :], an[:], R=[anb])
            k.barrier()

        for l in range(depth):
            with ExitStack() as es:
                gcol = es.enter_context(sbt(nc, "p1_g", [128, 16], F32))
                gb = Buf()
                k.dma("sp", gcol[:], norm_g[l].rearrange("(kc p) -> p kc", p=128), W=[gb], slow=True)
                hT = es.enter_context(sbt(nc, "p1_hT", [128, 16, SG], F32))
                hb = Buf()
                xs = Ring(es, nc, "p1_xs", [128, SG], F32, 2)
                sqr = Ring(es, nc, "p1_sq", [128, SG], F32, 2)
                wr = Ring(es, nc, "p1_w", [128, 16, 512], F32, 2)
                stg = Ring(es, nc, "p1_st", [128, 512], F32, 4)
                rbc = es.enter_context(sbt(nc, "p1_rbc", [128, SG], F32))
                rbcb = Buf()
                rtm = es.enter_context(sbt(nc, "p1_rtm", [128, SG // 128], F32))
                rtmb = Buf()
                ps = Ring(es, nc, "p1_ps", [128, 512], F32, 4, psum=True)
                pbc = Ring(es, nc, "p1_pbc", [128, 512], F32, SG // G, psum=True)
                ptm = es.enter_context(pst(nc, "p1_ptm", [128, 512], F32))
                ptmb = Buf()
                for sg in range(NSG):
                    c0 = sg * SG
                    pbcs = [pbc.next() for _ in range(SG // G)]
                    for kc in range(16):
                        xt, xb = xs.next()
                        k.dma("sp", xt[:], xT[kc * 128:(kc + 1) * 128, c0:c0 + SG], W=[xb])
                        sq, sqb = sqr.next()
                        k.op("act", lambda: nc.scalar.activation(out=sq[:], in_=xt[:], func=AF.Square), R=[xb], W=[sqb])
                        k.op("dve", lambda: nc.vector.tensor_scalar(out=hT[:, kc, :], in0=xt[:], scalar1=gcol[:, kc:kc + 1], scalar2=None, op0=ALU.mult),
                             R=[xb, gb], W=[hb])
                        for s in range(SG // G):
                            pp, ppb = pbcs[s]
                            k.op("pe", lambda: nc.tensor.matmul(pp[:, :G], lhsT=ones, rhs=sq[:, s * G:(s + 1) * G], start=(kc == 0), stop=(kc == 15)),
                                 R=[sqb, cstb], W=[ppb])
                    for s in range(SG // G):
                        pp, ppb = pbcs[s]
                        rstd(rbc[:, s * G:(s + 1) * G], pp[:, :G], 1.0 / D, C_EPS, [ppb], [rbcb])
                    for t in range(SG // 128):
                        k.op("pe", lambda: nc.tensor.matmul(ptm[:, t:t + 1], lhsT=rbc[:, t * 128:(t + 1) * 128], rhs=cst[:, C_R128:C_R128 + 1], start=True, stop=True),
                             R=[rbcb, cstb], W=[ptmb])
                    k.op("dve", lambda: nc.vector.tensor_copy(out=rtm[:], in_=ptm[:, :SG // 128]), R=[ptmb], W=[rtmb])
                    fmrow = 0
                    for (cc0, nb) in fm_blocks:
                        wt, wb = wr.next()
                        k.dma("sp", wt[:, :, :nb], w_in[l].rearrange("(kc p) n -> p kc n", p=128)[:, :, cc0:cc0 + nb], W=[wb])
                        for j in range(nb // 128):
                            for s in range(SG // G):
                                p, pb = ps.next()
                                for kc in range(16):
                                    k.op("pe", lambda: nc.tensor.matmul(p[:, :G], lhsT=wt[:, kc, j * 128:(j + 1) * 128], rhs=hT[:, kc, s * G:(s + 1) * G],
                                                                        start=(kc == 0), stop=(kc == 15)), R=[wb, hb], W=[pb])
                                st, sb = stg.next()
                                k.op("dve", lambda: nc.vector.tensor_tensor(out=st[:, :G], in0=p[:, :G], in1=rbc[:, s * G:(s + 1) * G], op=ALU.mult),
                                     R=[pb, rbcb], W=[sb])
                                k.dma("pool", pFM[fmrow * 128:(fmrow + 1) * 128, c0 + s * G:c0 + (s + 1) * G], st[:, :G], R=[sb])
                            fmrow += 1
                    tmcol = 0
                    for (cc0, nb) in tm_blocks:
                        wt, wb = wr.next()
                        k.dma("sp", wt[:, :, :nb], w_in[l].rearrange("(kc p) n -> p kc n", p=128)[:, :, cc0:cc0 + nb], W=[wb])
                        for t in range(SG // 128):
                            p, pb = ps.next()
                            for kc in range(16):
                                k.op("pe", lambda: nc.tensor.matmul(p[:, :nb], lhsT=hT[:, kc, t * 128:(t + 1) * 128], rhs=wt[:, kc, :nb],
                                                                    start=(kc == 0), stop=(kc == 15)), R=[wb, hb], W=[pb])
                            st, sb = stg.next()
                            k.op("act", lambda: nc.scalar.activation(out=st[:, :nb], in_=p[:, :nb], func=AF.Copy, scale=rtm[:, t:t + 1]),
                                 R=[pb, rtmb], W=[sb])
                            k.dma("pool", pTM[c0 + t * 128:c0 + (t + 1) * 128, tmcol:tmcol + nb], st[:, :nb], R=[sb])
                        tmcol += nb
                k.barrier()


            with ExitStack() as es:
                SB = lambda name, shape: es.enter_context(sbt(nc, name, shape, F32))
                PS = lambda name: es.enter_context(pst(nc, name, [128, 512], F32))
                tri = cst[:, C_TRI:C_TRI + 128]
                cw = SB("m_cw", [128, 4, 8]); cb = SB("m_cb", [128, 8]); ibfb = SB("m_ib", [128, 8]); gbc = SB("m_g", [128, 512])
                pb_ = Buf()
                for j in range(4):
                    k.dma("sp", cw[:, j, :], ml_conv_w[l, j].rearrange("(c p) -> p c", p=128), W=[pb_], slow=True)
                k.dma("sp", cb[:], ml_conv_b[l].rearrange("(c p) -> p c", p=128), W=[pb_], slow=True)
                k.dma("sp", ibfb[:, 0:4], ml_i_bias[l].partition_broadcast(128), W=[pb_])
                k.dma("sp", ibfb[:, 4:8], ml_f_bias[l].partition_broadcast(128), W=[pb_])
                k.dma("sp", gbc[:], ml_norm_g[l].partition_broadcast(128), W=[pb_])
                CT = [SB("m_CT%d" % h, [128, 129]) for h in range(4)]
                CTb = [Buf() for h in range(4)]
                for h in range(4):
                    k.op("pool", lambda: nc.gpsimd.memset(CT[h][:], 0.0), W=[CTb[h]])
                qkr = Ring(es, nc, "m_qkr", [128, 8, 131], F32, 2)
                accr = Ring(es, nc, "m_acc", [128, 8, 128], F32, 2)
                qkt = Ring(es, nc, "m_qk", [128, 8, 128], F32, 2)
                vr = Ring(es, nc, "m_v", [128, 4, 129], F32, 2)
                for (vt, vb) in vr.items:
                    k.op("pool", lambda: nc.gpsimd.memset(vt[:, :, 128:129], 1.0), W=[vb])
                gtr = Ring(es, nc, "m_gt", [128, 8], F32, 2)
                Gr = Ring(es, nc, "m_G", [128, 32], F32, 2)
                ozr = Ring(es, nc, "m_oz", [128, 1024], F32, 2)
                rhr = Ring(es, nc, "m_rh", [128, 128], F32, 2)
                Er = Ring(es, nc, "m_E", [128, 128], F32, 2)
                STr = Ring(es, nc, "m_ST", [128, 128], F32, 2)
                n1r = Ring(es, nc, "m_n1", [128, 129], F32, 2)
                n2r = Ring(es, nc, "m_n2", [128, 129], F32, 2)
                smr = Ring(es, nc, "m_sm", [128, 16], F32, 2)
                hhr = Ring(es, nc, "m_hh", [128, 128], F32, 2)
                kwr = Ring(es, nc, "m_kw", [128, 128], F32, 2)
                yr = Ring(es, nc, "m_y", [128, 512], F32, 2)
                ggr = Ring(es, nc, "m_gg", [128, 1024], F32, 2)
                mtr = Ring(es, nc, "m_mt", [128, 4, 128], F32, 2)
                pgA = PS("m_pgA"); pgB = PS("m_pgB"); pBbc = PS("m_pB"); pQK = PS("m_pQK")
                pN1 = PS("m_pN1"); pN2 = PS("m_pN2"); pKT = PS("m_pKT"); pdC = PS("m_pdC")
                pgAb, pgBb, pBbcb, pQKb, pN1b, pN2b, pKTb, pdCb = [Buf() for _ in range(8)]
                NSD = nc.vector.BN_STATS_DIM
                for c in range(NT):
                    t0 = c * 128
                    X, Xb = qkr.next()
                    src = pFM.rearrange("(ch p) t -> p ch t", p=128)
                    if c == 0:
                        k.op("pool", lambda: nc.gpsimd.memset(X[:, :, 0:3], 0.0), W=[Xb])
                        k.dma("sp", X[:, :, 3:131], src[:, 0:8, 0:128], W=[Xb])
                    else:
                        k.dma("sp", X[:], src[:, 0:8, t0 - 3:t0 + 128], W=[Xb])
                    vt, vb = vr.next()
                    k.dma("sp", vt[:, :, 0:128], pTM[t0:t0 + 128, TM_V:TM_V + 512].rearrange("p (h d) -> p h d", h=4), W=[vb])
                    gt, gtb = gtr.next()
                    k.dma("sp", gt[:], pTM[t0:t0 + 128, TM_I:TM_I + 8], W=[gtb])
                    oz, ozb = ozr.next()
                    k.dma("sp", oz[:], pTM[t0:t0 + 128, TM_O:TM_O + 1024], W=[ozb])
                    acc, accb = accr.next()
                    qk, qkb = qkt.next()
                    for ch in range(8):
                        eng, E_ = ("dve", nc.vector)
                        k.op(eng, lambda: E_.tensor_scalar(out=acc[:, ch, :], in0=X[:, ch, 0:128], scalar1=cw[:, 0, ch:ch + 1], scalar2=cb[:, ch:ch + 1], op0=ALU.mult, op1=ALU.add),
                             R=[Xb, pb_], W=[accb])
                        for j in range(1, 4):
                            k.op(eng, lambda: E_.scalar_tensor_tensor(out=acc[:, ch, :], in0=X[:, ch, j:j + 128], scalar=cw[:, j, ch:ch + 1], in1=acc[:, ch, :], op0=ALU.mult, op1=ALU.add),
                                 R=[Xb, pb_, accb], W=[accb])
                    k.op("act", lambda: nc.scalar.activation(out=qk[:], in_=acc[:], func=AF.Silu), R=[accb], W=[qkb])
                    k.op("pool", lambda: nc.gpsimd.tensor_scalar(out=qk[:, 0:4, :], in0=qk[:, 0:4, :], scalar1=float(128 ** -0.5), scalar2=None, op0=ALU.mult), R=[qkb], W=[qkb])
                    Gt, Gb = Gr.next()
                    k.op("dve", lambda: nc.vector.tensor_tensor(out=Gt[:, 0:8], in0=gt[:], in1=ibfb[:], op=ALU.add), R=[gtb, pb_], W=[Gb])
                    k.op("act", lambda: nc.scalar.activation(out=Gt[:, 8:12], in_=Gt[:, 4:8], func=AF.Exp, scale=-1.0), R=[Gb], W=[Gb])
                    k.op("act", lambda: nc.scalar.activation(out=Gt[:, 12:16], in_=Gt[:, 8:12], func=AF.Ln, bias=cst[:, C_1:C_1 + 1]), R=[Gb, cstb], W=[Gb])
                    k.op("dve", lambda: nc.vector.tensor_scalar(out=Gt[:, 12:16], in0=Gt[:, 12:16], scalar1=-1.0, scalar2=None, op0=ALU.mult), R=[Gb], W=[Gb])
                    k.op("pe", lambda: nc.tensor.matmul(pgA[:, 0:4], lhsT=tri, rhs=Gt[:, 12:16], start=True, stop=True), R=[Gb, cstb], W=[pgAb])
                    k.op("pe", lambda: nc.tensor.matmul(pgB[:, 0:4], lhsT=ones, rhs=Gt[:, 12:16], start=True, stop=True), R=[Gb, cstb], W=[pgBb])
                    k.op("dve", lambda: nc.vector.tensor_tensor(out=Gt[:, 16:20], in0=Gt[:, 0:4], in1=pgA[:, 0:4], op=ALU.subtract), R=[Gb, pgAb], W=[Gb])
                    k.op("dve", lambda: nc.vector.tensor_tensor(out=Gt[:, 20:24], in0=Gt[:, 16:20], in1=pgB[:, 0:4], op=ALU.add), R=[Gb, pgBb], W=[Gb])
                    k.op("act", lambda: nc.scalar.activation(out=Gt[:, 20:24], in_=Gt[:, 20:24], func=AF.Exp), R=[Gb], W=[Gb])
                    k.op("act", lambda: nc.scalar.activation(out=Gt[:, 24:28], in_=pgB[:, 0:4], func=AF.Exp), R=[pgBb], W=[Gb])
                    k.op("act", lambda: nc.scalar.activation(out=Gt[:, 28:32], in_=pgA[:, 0:4], func=AF.Exp), R=[pgAb], W=[Gb])
                    y, yb = yr.next()
                    for h in range(4):
                        rh, rhb = rhr.next()
                        k.op("pool", lambda: nc.gpsimd.tensor_scalar(out=rh[:], in0=tri, scalar1=Gt[:, 12 + h:13 + h], scalar2=None, op0=ALU.mult), R=[Gb, cstb], W=[rhb])
                        k.op("pe", lambda: nc.tensor.matmul(pBbc[:, 0:128], lhsT=ones, rhs=rh[:], start=True, stop=True), R=[rhb, cstb], W=[pBbcb])
                        Et, Eb = Er.next()
                        k.op("act", lambda: nc.scalar.activation(out=Et[:], in_=pBbc[:, 0:128], func=AF.Exp, bias=Gt[:, 16 + h:17 + h]), R=[pBbcb, Gb], W=[Eb])
                        k.op("pool", lambda: nc.gpsimd.tensor_tensor(out=Et[:], in0=Et[:], in1=tri, op=ALU.mult), R=[Eb, cstb], W=[Eb])
                        k.op("pe", lambda: nc.tensor.matmul(pQK[:, 0:128], lhsT=qk[:, 4 + h, :], rhs=qk[:, h, :], start=True, stop=True), R=[qkb], W=[pQKb])
                        ST, STb = STr.next()
                        k.op("dve", lambda: nc.vector.tensor_tensor(out=ST[:], in0=pQK[:, 0:128], in1=Et[:], op=ALU.mult), R=[pQKb, Eb], W=[STb])
                        k.op("pe", lambda: nc.tensor.matmul(pN1[:, 0:129], lhsT=ST[:], rhs=vt[:, h, :], start=True, stop=True), R=[STb, vb], W=[pN1b])
                        k.op("pe", lambda: nc.tensor.matmul(pN2[:, 0:129], lhsT=qk[:, h, :], rhs=CT[h][:], start=True, stop=True), R=[qkb, CTb[h]], W=[pN2b])
                        n1, n1b = n1r.next()
                        k.op("act", lambda: nc.scalar.activation(out=n1[:], in_=pN2[:, 0:129], func=AF.Copy, scale=Gt[:, 28 + h:29 + h]), R=[pN2b, Gb], W=[n1b])
                        n2, n2b = n2r.next()
                        k.op("dve", lambda: nc.vector.tensor_tensor(out=n2[:], in0=pN1[:, 0:129], in1=n1[:], op=ALU.add), R=[pN1b, n1b], W=[n2b])
                        sm, smb = smr.next()
                        k.op("act", lambda: nc.scalar.activation(out=sm[:, 0:1], in_=n2[:, 128:129], func=AF.Abs), R=[n2b], W=[smb])
                        k.op("dve", lambda: nc.vector.tensor_scalar_max(out=sm[:, 0:1], in0=sm[:, 0:1], scalar1=1.0), R=[smb], W=[smb])
                        k.op("dve", lambda: nc.vector.reciprocal(out=sm[:, 0:1], in_=sm[:, 0:1]), R=[smb], W=[smb])
                        hh, hhb = hhr.next()
                        k.op("dve", lambda: nc.vector.tensor_scalar(out=hh[:], in0=n2[:, 0:128], scalar1=sm[:, 0:1], scalar2=None, op0=ALU.mult), R=[n2b, smb], W=[hhb])
                        k.op("dve", lambda: nc.vector.bn_stats(out=sm[:, 2:2 + NSD], in_=hh[:]), R=[hhb], W=[smb])
                        k.op("dve", lambda: nc.vector.bn_aggr(out=sm[:, 10:12], in_=sm[:, 2:2 + NSD]), R=[smb], W=[smb])
                        rstd(sm[:, 12:13], sm[:, 11:12], 1.0, C_EPS, [smb], [smb])
                        k.op("dve", lambda: nc.vector.tensor_scalar(out=y[:, h * 128:(h + 1) * 128], in0=hh[:], scalar1=sm[:, 10:11], scalar2=sm[:, 12:13], op0=ALU.subtract, op1=ALU.mult),
                             R=[hhb, smb], W=[yb])
                        k.op("pe", lambda: nc.tensor.transpose(pKT[:, 0:128], qk[:, 4 + h, :], ident), R=[qkb, cstb], W=[pKTb])
                        kw, kwb = kwr.next()
                        k.op("act", lambda: nc.scalar.activation(out=kw[:], in_=pKT[:, 0:128], func=AF.Copy, scale=Gt[:, 20 + h:21 + h]), R=[pKTb, Gb], W=[kwb])
                        k.op("pe", lambda: nc.tensor.matmul(pdC[:, 0:129], lhsT=kw[:], rhs=vt[:, h, :], start=True, stop=True), R=[kwb, vb], W=[pdCb])
                        k.op("dve", lambda: nc.vector.scalar_tensor_tensor(out=CT[h][:], in0=CT[h][:], scalar=Gt[:, 24 + h:25 + h], in1=pdC[:, 0:129], op0=ALU.mult, op1=ALU.add),
                             R=[CTb[h], Gb, pdCb], W=[CTb[h]])
                    gg, ggb = ggr.next()
                    k.op("act", lambda: nc.scalar.activation(out=gg[:, 0:512], in_=oz[:, 0:512], func=AF.Sigmoid), R=[ozb], W=[ggb])
                    k.op("act", lambda: nc.scalar.activation(out=gg[:, 512:1024], in_=oz[:, 512:1024], func=AF.Silu), R=[ozb], W=[ggb])
                    k.op("pool", lambda: nc.gpsimd.tensor_tensor(out=gg[:, 0:512], in0=gg[:, 0:512], in1=gg[:, 512:1024], op=ALU.mult), R=[ggb], W=[ggb])
                    k.op("pool", lambda: nc.gpsimd.tensor_tensor(out=gg[:, 0:512], in0=gg[:, 0:512], in1=gbc[:], op=ALU.mult), R=[ggb, pb_], W=[ggb])
                    k.op("dve", lambda: nc.vector.tensor_tensor(out=y[:], in0=y[:], in1=gg[:, 0:512], op=ALU.mult), R=[yb, ggb], W=[yb])
                    mt, mtb = mtr.next()
                    for j in range(4):
                        k.op("pe", lambda: nc.tensor.transpose(pKT[:, 128 + j * 64:128 + j * 64 + 64] if False else pKT[:, 0:128], y[:, j * 128:(j + 1) * 128], ident), R=[yb, cstb], W=[pKTb])
                        k.op("act", lambda: nc.scalar.copy(out=mt[:, j, :], in_=pKT[:, 0:128]), R=[pKTb], W=[mtb])
                    k.dma("pool", mixT.rearrange("(cc p) t -> p cc t", p=128)[:, 0:4, t0:t0 + 128], mt[:], R=[mtb])
                k.barrier()

            with ExitStack() as es:
                SB = lambda name, shape: es.enter_context(sbt(nc, name, shape, F32))
                PS = lambda name: es.enter_context(pst(nc, name, [128, 512], F32))
                tri = cst[:, C_TRI:C_TRI + 128]
                wuq = SB("a_wuq", [128, 4, 1536]); wukv = SB("a_wukv", [128, 2, 2048]); gq = SB("a_gq", [128, 6])
                wb_ = Buf()
                k.dma("sp", wuq[:], mla_w_uq[l].rearrange("(c p) n -> p c n", p=128), W=[wb_])
                k.dma("sp", wukv[:], mla_w_ukv[l].rearrange("(c p) n -> p c n", p=128), W=[wb_])
                k.dma("sp", gq[:, 0:4], mla_q_norm_g[l].rearrange("(c p) -> p c", p=128), W=[wb_], slow=True)
                k.dma("sp", gq[:, 4:6], mla_kv_norm_g[l].rearrange("(c p) -> p c", p=128), W=[wb_], slow=True)
                xcr = Ring(es, nc, "a_xc", [128, 6, 128], F32, 2)
                sqr = Ring(es, nc, "a_sq", [128, 6, 128], F32, 2)
                rbr = Ring(es, nc, "a_rb", [128, 2, 128], F32, 2)
                cnr = Ring(es, nc, "a_cn", [128, 6, 128], F32, 2)
                csr = Ring(es, nc, "a_cs", [128, 64], F32, 2)
                kxr = Ring(es, nc, "a_kx", [128, 64], F32, 2)
                stq = Ring(es, nc, "a_stq", [128, 4, 128], F32, 4)
                tmr = Ring(es, nc, "a_tm", [128, 8, 32], F32, 4)
                qrr = Ring(es, nc, "a_qr", [128, 8, 64], F32, 2)
                krr = Ring(es, nc, "a_kr", [128, 64], F32, 2)
                kst = Ring(es, nc, "a_kst", [64, 128], F32, 2)
                var_ = Ring(es, nc, "a_va", [128, 8, 129], F32, 2)
                for (vt, vb) in var_.items:
                    k.op("pool", lambda: nc.gpsimd.memset(vt[:, :, 128:129], 1.0), W=[vb])
                pS1 = PS("a_pS1"); pS2 = PS("a_pS2"); pS1b = Buf(); pS2b = Buf()
                pq = Ring(es, nc, "a_pq", [128, 512], F32, 4, psum=True)
                qn_v = qnT.rearrange("(h p) t -> p h t", p=128)
                kn_v = knT.rearrange("(h p) t -> p h t", p=128)
                qr_v = qrT.rearrange("(b p) t -> p b t", p=128)
                for c in range(NT):
                    t0 = c * 128
                    xc, xcb = xcr.next()
                    k.dma("sp", xc[:], pFM.rearrange("(ch p) t -> p ch t", p=128)[:, FM_CQ:FM_CQ + 6, t0:t0 + 128], W=[xcb])
                    cs_, csb = csr.next()
                    k.dma("sp", cs_[:], csd[t0:t0 + 128, :], W=[csb])
                    kx, kxb = kxr.next()
                    k.dma("sp", kx[:], pTM[t0:t0 + 128, TM_KR:TM_KR + 64], W=[kxb])
                    sq, sqb = sqr.next()
                    k.op("act", lambda: nc.scalar.activation(out=sq[:], in_=xc[:], func=AF.Square), R=[xcb], W=[sqb])
                    for ch in range(4):
                        k.op("pe", lambda: nc.tensor.matmul(pS1[:, 0:128], lhsT=ones, rhs=sq[:, ch, :], start=(ch == 0), stop=(ch == 3)), R=[sqb, cstb], W=[pS1b])
                    for ch in range(2):
                        k.op("pe", lambda: nc.tensor.matmul(pS2[:, 0:128], lhsT=ones, rhs=sq[:, 4 + ch, :], start=(ch == 0), stop=(ch == 1)), R=[sqb, cstb], W=[pS2b])
                    rb, rbb = rbr.next()
                    rstd(rb[:, 0, :], pS1[:, 0:128], 1.0 / 512, C_EPS, [pS1b], [rbb])
                    rstd(rb[:, 1, :], pS2[:, 0:128], 1.0 / 256, C_EPS, [pS2b], [rbb])
                    cn, cnb = cnr.next()
                    for ch in range(6):
                        k.op("dve", lambda: nc.vector.scalar_tensor_tensor(out=cn[:, ch, :], in0=xc[:, ch, :], scalar=gq[:, ch:ch + 1], in1=rb[:, 0 if ch < 4 else 1, :], op0=ALU.mult, op1=ALU.mult),
                             R=[xcb, wb_, rbb], W=[cnb])
                    for (W_, nch, c0_, hs, dst) in ((wuq, 4, 0, 192, qn_v), (wukv, 2, 4, 256, kn_v)):
                        for b in range(2):
                            p, pb = pq.next()
                            for hh_ in range(4):
                                h = b * 4 + hh_
                                for ch in range(nch):
                                    k.op("pe", lambda: nc.tensor.matmul(p[:, hh_ * 128:(hh_ + 1) * 128], lhsT=W_[:, ch, h * hs:h * hs + 128], rhs=cn[:, c0_ + ch, :],
                                                                        start=(ch == 0), stop=(ch == nch - 1)), R=[wb_, cnb], W=[pb])
                            st, sb = stq.next()
                            k.op("act", lambda: nc.scalar.copy(out=st[:], in_=p[:].rearrange("p (a b) -> p a b", a=4)), R=[pb], W=[sb])
                            k.dma("pool", dst[:, b * 4:(b + 1) * 4, t0:t0 + 128], st[:], R=[sb])
                    va, vab = var_.next()
                    for b in range(2):
                        p, pb = pq.next()
                        for ch in range(2):
                            k.op("pe", lambda: nc.tensor.matmul(p[:], lhsT=cn[:, 4 + ch, :], rhs=wukv[:, ch, :].rearrange("p (h d) -> p h d", d=256)[:, b * 4:(b + 1) * 4, 128:256],
                                                                start=(ch == 0), stop=(ch == 1)), R=[wb_, cnb], W=[pb])
                        k.op("act", lambda: nc.scalar.copy(out=va[:, b * 4:(b + 1) * 4, 0:128], in_=p[:].rearrange("p (a b) -> p a b", a=4)), R=[pb], W=[vab])
                    k.dma("pool", vaug.rearrange("h t d -> t h d")[t0:t0 + 128], va[:], R=[vab])
                    p, pb = pq.next()
                    for ch in range(4):
                        k.op("pe", lambda: nc.tensor.matmul(p[:], lhsT=cn[:, ch, :], rhs=wuq[:, ch, :].rearrange("p (h d) -> p h d", d=192)[:, :, 128:192],
                                                            start=(ch == 0), stop=(ch == 3)), R=[wb_, cnb], W=[pb])
                    pv = p[:].rearrange("p (h d) -> p h d", d=64)
                    sin8 = cs_[:, 0:32].unsqueeze(1).to_broadcast([128, 8, 32])
                    cos8 = cs_[:, 32:64].unsqueeze(1).to_broadcast([128, 8, 32])
                    qr, qrb = qrr.next()
                    t1, t1b = tmr.next(); t2, t2b = tmr.next(); t3, t3b = tmr.next(); t4, t4b = tmr.next()
                    k.op("dve", lambda: nc.vector.tensor_tensor(out=t1[:], in0=pv[:, :, 0:32], in1=cos8, op=ALU.mult), R=[pb, csb], W=[t1b])
                    k.op("dve", lambda: nc.vector.tensor_tensor(out=t2[:], in0=pv[:, :, 32:64], in1=sin8, op=ALU.mult), R=[pb, csb], W=[t2b])
                    k.op("dve", lambda: nc.vector.tensor_tensor(out=t3[:], in0=pv[:, :, 32:64], in1=cos8, op=ALU.mult), R=[pb, csb], W=[t3b])
                    k.op("dve", lambda: nc.vector.tensor_tensor(out=t4[:], in0=pv[:, :, 0:32], in1=sin8, op=ALU.mult), R=[pb, csb], W=[t4b])
                    k.op("pool", lambda: nc.gpsimd.tensor_tensor(out=qr[:, :, 0:32], in0=t1[:], in1=t2[:], op=ALU.subtract), R=[t1b, t2b], W=[qrb])
                    k.op("pool", lambda: nc.gpsimd.tensor_tensor(out=qr[:, :, 32:64], in0=t3[:], in1=t4[:], op=ALU.add), R=[t3b, t4b], W=[qrb])
                    p, pb = pq.next()
                    for b in range(4):
                        k.op("pe", lambda: nc.tensor.transpose(p[:, b * 128:(b + 1) * 128], qr[:, 2 * b:2 * b + 2, :].rearrange("p a d -> p (a d)"), ident), R=[qrb, cstb], W=[pb])
                    st, sb = stq.next()
                    k.op("act", lambda: nc.scalar.copy(out=st[:], in_=p[:].rearrange("p (a b) -> p a b", a=4)), R=[pb], W=[sb])
                    k.dma("pool", qr_v[:, :, t0:t0 + 128], st[:], R=[sb])
                    kr_, krb = krr.next()
                    t1, t1b = tmr.next(); t2, t2b = tmr.next()
                    k.op("dve", lambda: nc.vector.tensor_tensor(out=t1[:, 0, :], in0=kx[:, 0:32], in1=cs_[:, 32:64], op=ALU.mult), R=[kxb, csb], W=[t1b])
                    k.op("dve", lambda: nc.vector.tensor_tensor(out=t1[:, 1, :], in0=kx[:, 32:64], in1=cs_[:, 0:32], op=ALU.mult), R=[kxb, csb], W=[t1b])
                    k.op("dve", lambda: nc.vector.tensor_tensor(out=t2[:, 0, :], in0=kx[:, 32:64], in1=cs_[:, 32:64], op=ALU.mult), R=[kxb, csb], W=[t2b])
                    k.op("dve", lambda: nc.vector.tensor_tensor(out=t2[:, 1, :], in0=kx[:, 0:32], in1=cs_[:, 0:32], op=ALU.mult), R=[kxb, csb], W=[t2b])
                    k.op("pool", lambda: nc.gpsimd.tensor_tensor(out=kr_[:, 0:32], in0=t1[:, 0, :], in1=t1[:, 1, :], op=ALU.subtract), R=[t1b], W=[krb])
                    k.op("pool", lambda: nc.gpsimd.tensor_tensor(out=kr_[:, 32:64], in0=t2[:, 0, :], in1=t2[:, 1, :], op=ALU.add), R=[t2b], W=[krb])
                    p, pb = pq.next()
                    k.op("pe", lambda: nc.tensor.transpose(p[0:64, 0:128], kr_[:], ident), R=[krb, cstb], W=[pb])
                    ks, ksb = kst.next()
                    k.op("act", lambda: nc.scalar.copy(out=ks[:], in_=p[0:64, 0:128]), R=[pb], W=[ksb])
                    k.dma("pool", krT[:, t0:t0 + 128], ks[:], R=[ksb])
                k.barrier()

            with ExitStack() as es:
                SB = lambda name, shape: es.enter_context(sbt(nc, name, shape, F32))
                tri = cst[:, C_TRI:C_TRI + 128]
                krt = SB("b_krt", [64, T]); krtb = Buf()
                k.dma("sp", krt[:], krT, W=[krtb])
                knh = SB("b_kn", [128, T]); qnh = SB("b_qn", [128, T]); qrh = SB("b_qr", [64, T])
                vah = SB("b_va", [128, NT, 129]); zh = SB("b_z", [128, NT, 128])
                knb, qnb, qrb, vahb, zhb = [Buf() for _ in range(5)]
                Ptr = Ring(es, nc, "b_P", [128, 512], F32, 3)
                smr = Ring(es, nc, "b_sm", [128, 2], F32, 2)
                ytr = Ring(es, nc, "b_y", [128, 128], F32, 2)
                szr = Ring(es, nc, "b_sz", [128, 128], F32, 2)
                sty = Ring(es, nc, "b_sty", [128, 128], F32, 3)
                pST = Ring(es, nc, "b_pST", [128, 512], F32, 3, psum=True)
                pO = Ring(es, nc, "b_pO", [128, 512], F32, 2, psum=True)
                pX = Ring(es, nc, "b_pX", [128, 512], F32, 2, psum=True)
                SCL = float(192 ** -0.5)
                for h in range(8):
                    k.dma("sp", knh[:], knT[h * 128:(h + 1) * 128, :], W=[knb])
                    k.dma("sp", qnh[:], qnT[h * 128:(h + 1) * 128, :], W=[qnb])
                    k.dma("sp", qrh[:], qrT[h * 64:(h + 1) * 64, :], W=[qrb])
                    k.dma("sp", vah[:], vaug[h].rearrange("(n p) d -> p n d", p=128), W=[vahb])
                    k.dma("sp", zh[:], pTM[:, TM_MZ + h * 128:TM_MZ + (h + 1) * 128].rearrange("(n p) d -> p n d", p=128), W=[zhb])
                    for qt in range(NT):
                        qs = slice(qt * 128, (qt + 1) * 128)
                        po, pob_ = pO.next()
                        for kb in range(0, qt + 1, 4):
                            nk = min(4, qt + 1 - kb)
                            ps_, psb = pST.next()
                            for i in range(nk):
                                j = kb + i
                                js = slice(j * 128, (j + 1) * 128)
                                k.op("pe", lambda: nc.tensor.matmul(ps_[:, i * 128:(i + 1) * 128], lhsT=knh[:, js], rhs=qnh[:, qs], start=True, stop=False), R=[knb, qnb], W=[psb])
                                k.op("pe", lambda: nc.tensor.matmul(ps_[:, i * 128:(i + 1) * 128], lhsT=krt[:, js], rhs=qrh[:, qs], start=False, stop=True), R=[krtb, qrb], W=[psb])
                            Pt, Ptb = Ptr.next()
                            k.op("act", lambda: nc.scalar.activation(out=Pt[:, 0:nk * 128], in_=ps_[:, 0:nk * 128], func=AF.Exp, scale=SCL), R=[psb], W=[Ptb])
                            if kb + nk - 1 == qt:
                                i = nk - 1
                                k.op("pool", lambda: nc.gpsimd.tensor_tensor(out=Pt[:, i * 128:(i + 1) * 128], in0=Pt[:, i * 128:(i + 1) * 128], in1=tri, op=ALU.mult), R=[Ptb, cstb], W=[Ptb])
                            for i in range(nk):
                                j = kb + i
                                k.op("pe", lambda: nc.tensor.matmul(po[:, 0:129], lhsT=Pt[:, i * 128:(i + 1) * 128], rhs=vah[:, j, :], start=(j == 0), stop=(j == qt)), R=[Ptb, vahb], W=[pob_])
                        sm, smb = smr.next()
                        k.op("dve", lambda: nc.vector.reciprocal(out=sm[:, 0:1], in_=po[:, 128:129]), R=[pob_], W=[smb])
                        yt, ytb = ytr.next()
                        k.op("dve", lambda: nc.vector.tensor_scalar(out=yt[:], in0=po[:, 0:128], scalar1=sm[:, 0:1], scalar2=None, op0=ALU.mult), R=[pob_, smb], W=[ytb])
                        sz, szb = szr.next()
                        k.op("act", lambda: nc.scalar.activation(out=sz[:], in_=zh[:, qt, :], func=AF.Silu), R=[zhb], W=[szb])
                        k.op("pool", lambda: nc.gpsimd.tensor_tensor(out=yt[:], in0=yt[:], in1=sz[:], op=ALU.mult), R=[ytb, szb], W=[ytb])
                        px, pxb = pX.next()
                        k.op("pe", lambda: nc.tensor.transpose(px[:, 0:128], yt[:], ident), R=[ytb, cstb], W=[pxb])
                        st, sb = sty.next()
                        k.op("dve", lambda: nc.vector.tensor_copy(out=st[:], in_=px[:, 0:128]), R=[pxb], W=[sb])
                        k.dma("pool", mixT[512 + h * 128:512 + (h + 1) * 128, qs], st[:], R=[sb])
                k.barrier()

            NCH = T // 64
            with ExitStack() as es:
                SB = lambda name, shape: es.enter_context(sbt(nc, name, shape, F32))
                mu = SB("r_mu", [128, 13]); prm = SB("r_prm", [128, 6, 4]); w0bc = SB("r_w0", [128, 512])
                w2t = SB("r_w2", [64, 512]); a2t = SB("r_a2", [128, 512])
                v1t = SB("r_v1", [128, 4, 32]); v2t = SB("r_v2", [32, 512])
                prb = Buf()
                k.dma("sp", mu[:], rw_mu[l].rearrange("(c p) -> p c", p=128), W=[prb], slow=True)
                plist = [rw_a0[l], rw_k_k[l], rw_k_a[l], rw_r_k[l].rearrange("h d -> (h d)")]
                if l > 0:
                    plist.append(rw_v0[l - 1])
                for i_, src in enumerate(plist):
                    k.dma("sp", prm[:, i_, :], src.rearrange("(c p) -> p c", p=128), W=[prb], slow=True)
                k.dma("sp", w0bc[:], rw_w0[l].partition_broadcast(128), W=[prb])
                k.dma("sp", w2t[:], rw_w2[l], W=[prb])
                k.dma("sp", a2t[64:128, :], rw_a2[l], W=[prb])
                if l > 0:
                    k.dma("sp", v1t[:], rw_v1[l - 1].rearrange("(c p) n -> p c n", p=128), W=[prb])
                    k.dma("sp", v2t[:], rw_v2[l - 1], W=[prb])
                k.op("dve", lambda: nc.vector.tensor_scalar(out=prm[:, 5, :], in0=prm[:, 2, :], scalar1=-1.0, scalar2=1.0, op0=ALU.mult, op1=ALU.add), R=[prb], W=[prb])
                bc = lambda ap_: ap_.unsqueeze(2).to_broadcast([128, 4, 128])
                Xr = Ring(es, nc, "r_X", [128, 13, 129], F32, 2)
                dr = Ring(es, nc, "r_d", [128, 13, 128], F32, 1)
                xsr = Ring(es, nc, "r_xs", [128, 13, 128], F32, 2)
                twr = Ring(es, nc, "r_tw", [64, 128], F32, 2)
                ldr = Ring(es, nc, "r_ld", [128, 512], F32, 2)
                T4 = lambda name, n=1: Ring(es, nc, name, [128, 4, 128], F32, n)
                gir, ger, aar, kkr_, khr, bvr = T4("r_gi"), T4("r_ge"), T4("r_aa"), T4("r_kk"), T4("r_kh"), T4("r_bv")
                e1r, e2r, e3r, e4r = T4("r_e1"), T4("r_e2"), T4("r_e3"), T4("r_e4")
                tmpr = T4("r_tmp", 3)
                outr = T4("r_out", 4)
                vfr = T4("r_vf", 2)
                m1r = Ring(es, nc, "r_m1", [32, 128], F32, 2)
                tmo = Ring(es, nc, "r_tmo", [128, 512], F32, 3)
                rkro = Ring(es, nc, "r_rkr", [128, 8], F32, 2)
                glo = Ring(es, nc, "r_glo", [128, 4, 2], F32, 2)
                pr = Ring(es, nc, "r_ps", [128, 512], F32, 8, psum=True)
                fmv = lambda dt_: dt_.rearrange("(j p) t -> p j t", p=128)
                for c in range(NT):
                    t0 = c * 128
                    X, Xb = Xr.next()
                    src = pFM.rearrange("(ch p) t -> p ch t", p=128)
                    if c == 0:
                        k.op("pool", lambda: nc.gpsimd.memset(X[:, :, 0:1], 0.0), W=[Xb])
                        k.dma("sp", X[:, :, 1:129], src[:, FM_RW:FM_RW + 13, 0:128], W=[Xb])
                    else:
                        k.dma("sp", X[:], src[:, FM_RW:FM_RW + 13, t0 - 1:t0 + 128], W=[Xb])
                    d_, db = dr.next()
                    k.op("dve", lambda: nc.vector.tensor_tensor(out=d_[:], in0=X[:, :, 0:128], in1=X[:, :, 1:129], op=ALU.subtract), R=[Xb], W=[db])
                    xs_, xsb = xsr.next()
                    for ch in range(13):
                        k.op("dve", lambda: nc.vector.scalar_tensor_tensor(out=xs_[:, ch, :], in0=d_[:, ch, :], scalar=mu[:, ch:ch + 1], in1=X[:, ch, 1:129], op0=ALU.mult, op1=ALU.add),
                             R=[db, Xb, prb], W=[xsb])
                    rr = xs_[:, 0:4, :]; kx = xs_[:, 4:8, :]; vv = xs_[:, 8:12, :]
                    tw, twb = twr.next()
                    k.op("act", lambda: nc.scalar.activation(out=tw[:], in_=xs_[0:64, 12, :], func=AF.Tanh), R=[xsb], W=[twb])
                    pz, pzb = pr.next()
                    k.op("pe", lambda: nc.tensor.matmul(pz[:], lhsT=tw[:], rhs=w2t[:], start=True, stop=True), R=[twb, prb], W=[pzb])
                    ld, ldb = ldr.next()
                    k.op("dve", lambda: nc.vector.tensor_tensor(out=ld[:], in0=pz[:], in1=w0bc[:], op=ALU.add), R=[pzb, prb], W=[ldb])
                    k.op("act", lambda: nc.scalar.activation(out=ld[:], in_=ld[:], func=AF.Sigmoid), R=[ldb], W=[ldb])
                    k.op("pool", lambda: nc.gpsimd.tensor_scalar(out=ld[:], in0=ld[:], scalar1=float(-np.exp(-0.5)), scalar2=None, op0=ALU.mult), R=[ldb], W=[ldb])
                    pgi, pgib = pr.next(); pge, pgeb = pr.next()
                    for j in range(4):
                        k.op("pe", lambda: nc.tensor.matmul(pgi[:, j * 128:(j + 1) * 128], lhsT=ld[:, j * 128:(j + 1) * 128], rhs=cst[:, C_BI:C_BI + 128], start=True, stop=True), R=[ldb, cstb], W=[pgib])
                        k.op("pe", lambda: nc.tensor.matmul(pge[:, j * 128:(j + 1) * 128], lhsT=ld[:, j * 128:(j + 1) * 128], rhs=cst[:, C_BS:C_BS + 128], start=True, stop=True), R=[ldb, cstb], W=[pgeb])
                    gi, gib = gir.next(); ge, geb = ger.next()
                    k.op("act", lambda: nc.scalar.copy(out=gi[:], in_=pgi[:].rearrange("p (a b) -> p a b", a=4)), R=[pgib], W=[gib])
                    e1, e1b = e1r.next(); e2, e2b = e2r.next(); e3, e3b = e3r.next(); e4, e4b = e4r.next()
                    k.op("act", lambda: nc.scalar.activation(out=e1[:], in_=gi[:], func=AF.Exp), R=[gib], W=[e1b])
                    k.op("act", lambda: nc.scalar.activation(out=e2[:], in_=pge[:].rearrange("p (a b) -> p a b", a=4), func=AF.Exp), R=[pgeb], W=[e2b])
                    k.op("act", lambda: nc.scalar.activation(out=e3[:], in_=gi[:], func=AF.Exp, scale=-1.0), R=[gib], W=[e3b])
                    for j in range(4):
                        for hf in range(2):
                            k.op("act", lambda: nc.scalar.activation(out=e4[:, j, hf * 64:(hf + 1) * 64], in_=gi[:, j, hf * 64:(hf + 1) * 64], func=AF.Exp, scale=-1.0,
                                                                     bias=gi[:, j, hf * 64 + 63:hf * 64 + 64]), R=[gib], W=[e4b])
                    pa, pab = pr.next()
                    for j in range(4):
                        k.op("pe", lambda: nc.tensor.matmul(pa[:, j * 128:(j + 1) * 128], lhsT=a2t[64:128, j * 128:(j + 1) * 128], rhs=xs_[64:128, 12, :], start=True, stop=True), R=[xsb, prb], W=[pab])
                    aa, aab = aar.next()
                    for j in range(4):
                        k.op("act", lambda: nc.scalar.activation(out=aa[:, j, :], in_=pa[:, j * 128:(j + 1) * 128], func=AF.Sigmoid, bias=prm[:, 0, j:j + 1]), R=[pab, prb], W=[aab])
                    if l > 0:
                        pm, pmb = pr.next()
                        for ch in range(4):
                            k.op("pe", lambda: nc.tensor.matmul(pm[0:32, 0:128], lhsT=v1t[:, ch, :], rhs=xs_[:, 8 + ch, :], start=(ch == 0), stop=(ch == 3)), R=[xsb, prb], W=[pmb])
                        m1, m1b = m1r.next()
                        k.op("act", lambda: nc.scalar.copy(out=m1[:], in_=pm[0:32, 0:128]), R=[pmb], W=[m1b])
                        pm2, pm2b = pr.next()
                        for j in range(4):
                            k.op("pe", lambda: nc.tensor.matmul(pm2[:, j * 128:(j + 1) * 128], lhsT=v2t[:, j * 128:(j + 1) * 128], rhs=m1[:], start=True, stop=True), R=[m1b, prb], W=[pm2b])
                        gt_, gtb_ = tmpr.next()
                        for j in range(4):
                            k.op("act", lambda: nc.scalar.activation(out=gt_[:, j, :], in_=pm2[:, j * 128:(j + 1) * 128], func=AF.Sigmoid, bias=prm[:, 4, j:j + 1]), R=[pm2b, prb], W=[gtb_])
                        vf, vfb = vfr.next()
                        k.dma("sp", vf[:], fmv(vfirst)[:, :, t0:t0 + 128], W=[vfb])
                        k.op("dve", lambda: nc.vector.tensor_tensor(out=vf[:], in0=vf[:], in1=vv, op=ALU.subtract), R=[vfb, xsb], W=[vfb])
                        k.op("pool", lambda: nc.gpsimd.tensor_tensor(out=vf[:], in0=vf[:], in1=gt_[:], op=ALU.mult), R=[vfb, gtb_], W=[vfb])
                        k.op("dve", lambda: nc.vector.tensor_tensor(out=xs_[:, 8:12, :], in0=vv, in1=vf[:], op=ALU.add), R=[vfb, xsb], W=[xsb])
                    else:
                        k.dma("pool", fmv(vfirst)[:, :, t0:t0 + 128], vv, R=[xsb])
                    kk, kkb = kkr_.next()
                    k.op("dve", lambda: nc.vector.tensor_tensor(out=kk[:], in0=kx, in1=bc(prm[:, 1, :]), op=ALU.mult), R=[xsb, prb], W=[kkb])
                    sq_, sqb_ = tmpr.next()
                    k.op("act", lambda: nc.scalar.activation(out=sq_[:], in_=kk[:], func=AF.Square), R=[kkb], W=[sqb_])
                    pn, pnb = pr.next()
                    for j in range(4):
                        k.op("pe", lambda: nc.tensor.matmul(pn[:, j * 128:(j + 1) * 128], lhsT=cst[:, C_BD:C_BD + 128], rhs=sq_[:, j, :], start=True, stop=True), R=[sqb_, cstb], W=[pnb])
                    rn, rnb = tmpr.next()
                    k.op("act", lambda: nc.scalar.activation(out=rn[:], in_=pn[:].rearrange("p (a b) -> p a b", a=4), func=AF.Sqrt), R=[pnb], W=[rnb])
                    k.op("dve", lambda: nc.vector.tensor_scalar_max(out=rn[:], in0=rn[:], scalar1=1e-12), R=[rnb], W=[rnb])
                    k.op("dve", lambda: nc.vector.reciprocal(out=rn[:], in_=rn[:]), R=[rnb], W=[rnb])
                    k.op("pool", lambda: nc.gpsimd.tensor_tensor(out=kk[:], in0=kk[:], in1=rn[:], op=ALU.mult), R=[kkb, rnb], W=[kkb])
                    kh, khb = khr.next()
                    k.op("dve", lambda: nc.vector.tensor_tensor(out=kh[:], in0=aa[:], in1=bc(prm[:, 2, :]), op=ALU.mult), R=[aab, prb], W=[khb])
                    k.op("dve", lambda: nc.vector.tensor_tensor(out=kh[:], in0=kh[:], in1=bc(prm[:, 5, :]), op=ALU.add), R=[khb, prb], W=[khb])
                    k.op("dve", lambda: nc.vector.tensor_tensor(out=kh[:], in0=kh[:], in1=kx, op=ALU.mult), R=[khb, xsb], W=[khb])
                    bv, bvb = bvr.next()
                    k.op("pool", lambda: nc.gpsimd.tensor_tensor(out=bv[:], in0=kk[:], in1=aa[:], op=ALU.mult), R=[kkb, aab], W=[bvb])
                    def emit(dst, in0, in1, neg=False, R=()):
                        o, ob = outr.next()
                        k.op("dve", lambda: nc.vector.tensor_tensor(out=o[:], in0=in0, in1=in1, op=ALU.mult), R=list(R), W=[ob])
                        if neg:
                            k.op("pool", lambda: nc.gpsimd.tensor_scalar(out=o[:], in0=o[:], scalar1=-1.0, scalar2=None, op0=ALU.mult), R=[ob], W=[ob])
                        k.dma("pool", fmv(dst)[:, :, t0:t0 + 128], o[:], R=[ob])
                        return o, ob
                    emit(rwA, kk[:], e2[:], neg=True, R=[kkb, e2b])
                    emit(rwR, rr, e1[:], R=[xsb, e1b])
                    emit(rwB, bv[:], e3[:], R=[bvb, e3b])
                    emit(rwK, kh[:], e3[:], R=[khb, e3b])
                    for (dst, a_, ab_) in ((rwBp, bv, bvb), (rwKp, kh, khb), (rwV, None, None)):
                        if a_ is not None:
                            o, ob = outr.next()
                            k.op("dve", lambda: nc.vector.tensor_tensor(out=o[:], in0=a_[:], in1=e4[:], op=ALU.mult), R=[ab_, e4b], W=[ob])
                            srcv = o
                        else:
                            srcv, ob = xs_[:, 8:12, :], xsb
                        pt_, ptb_ = pr.next()
                        for j in range(4):
                            k.op("pe", lambda: nc.tensor.transpose(pt_[:, j * 128:(j + 1) * 128], srcv[:, j, :], ident), R=[ob, cstb], W=[ptb_])
                        to, tob = tmo.next()
                        k.op("act", lambda: nc.scalar.copy(out=to[:], in_=pt_[:]), R=[ptb_], W=[tob])
                        k.dma("pool", dst[t0:t0 + 128, :], to[:], R=[tob])
                    go_, gob = glo.next()
                    k.op("pool", lambda: nc.gpsimd.tensor_copy(out=go_[:, :, 0:1], in_=e1[:, :, 63:64]), R=[e1b], W=[gob])
                    k.op("pool", lambda: nc.gpsimd.tensor_copy(out=go_[:, :, 1:2], in_=e1[:, :, 127:128]), R=[e1b], W=[gob])
                    k.dma("pool", rwGL.rearrange("(j p) c -> p j c", p=128)[:, :, 2 * c:2 * c + 2], go_[:], R=[gob], slow=True)
                    pd_, pdb_ = tmpr.next()
                    k.op("dve", lambda: nc.vector.tensor_tensor(out=pd_[:], in0=rr, in1=kh[:], op=ALU.mult), R=[xsb, khb], W=[pdb_])
                    k.op("pool", lambda: nc.gpsimd.tensor_tensor(out=pd_[:], in0=pd_[:], in1=bc(prm[:, 3, :]), op=ALU.mult), R=[pdb_, prb], W=[pdb_])
                    pk, pkb = pr.next()
                    for j in range(4):
                        k.op("pe", lambda: nc.tensor.matmul(pk[:, 0:8], lhsT=pd_[:, j, :], rhs=cst[:, C_HS + 8 * j:C_HS + 8 * j + 8], start=(j == 0), stop=(j == 3)), R=[pdb_, cstb], W=[pkb])
                    ro, rob = rkro.next()
                    k.op("act", lambda: nc.scalar.copy(out=ro[:], in_=pk[:, 0:8]), R=[pkb], W=[rob])
                    k.dma("pool", rwRKR[t0:t0 + 128, :], ro[:], R=[rob])
                k.barrier()

            with ExitStack() as es:
                SB = lambda name, shape: es.enter_context(sbt(nc, name, shape, F32))
                GLt = SB("c_GL", [64, 8, NCH]); glb = Buf()
                k.dma("sp", GLt[:], rwGL.rearrange("(h q) c -> q h c", q=64), W=[glb])
                hv = lambda dt_: dt_.rearrange("(h q) t -> q h t", q=64)
                ARr = Ring(es, nc, "c_AR", [64, 8, 2, 64], F32, 3)
                BKr = Ring(es, nc, "c_BK", [64, 8, 2, 64], F32, 3)
                TMr = Ring(es, nc, "c_TM", [64, 3, 512], F32, 3)
                Hr = Ring(es, nc, "c_H", [64, 8, 64], F32, 2)
                A1r = Ring(es, nc, "c_A1", [64, 8, 128], F32, 2)
                A2r = Ring(es, nc, "c_A2", [64, 8, 128], F32, 2)
                Pr_ = Ring(es, nc, "c_P", [64, 8, 64], F32, 3)
                Ptr_ = Ring(es, nc, "c_Pt", [64, 8, 64], F32, 3)
                Lr = Ring(es, nc, "c_L", [64, 8, 64], F32, 3)
                Xsr = Ring(es, nc, "c_Xs", [64, 8, 64], F32, 2)
                Usr = Ring(es, nc, "c_Us", [64, 8, 64], F32, 2)
                Yr = Ring(es, nc, "c_Y", [64, 512], F32, 3)
                pr = Ring(es, nc, "c_ps", [128, 512], F32, 8, psum=True)
                m1 = cst[0:64, C_M1:C_M1 + 128].unsqueeze(1).to_broadcast([64, 4, 128])
                m3 = cst[0:64, C_M3:C_M3 + 64].unsqueeze(1).to_broadcast([64, 8, 64])
                i64b = cst[0:64, 0:64].unsqueeze(1).to_broadcast([64, 8, 64])
                H, Hb = Hr.next()
                k.op("pool", lambda: nc.gpsimd.memset(H[:], 0.0), W=[Hb])
                v8 = lambda p_: p_[0:64, :].rearrange("p (h d) -> p h d", h=8)
                for c in range(NCH):
                    cs_ = slice(c * 64, (c + 1) * 64)
                    AR, ARb = ARr.next(); BK, BKb = BKr.next(); TM_, TMb = TMr.next()
                    k.dma("sp", AR[:, :, 0, :], hv(rwA)[:, :, cs_], W=[ARb])
                    k.dma("sp", AR[:, :, 1, :], hv(rwR)[:, :, cs_], W=[ARb])
                    k.dma("sp", BK[:, :, 0, :], hv(rwB)[:, :, cs_], W=[BKb])
                    k.dma("sp", BK[:, :, 1, :], hv(rwK)[:, :, cs_], W=[BKb])
                    k.dma("sp", TM_[:, 0, :], rwBp[cs_, :], W=[TMb])
                    k.dma("sp", TM_[:, 1, :], rwKp[cs_, :], W=[TMb])
                    k.dma("sp", TM_[:, 2, :], rwV[cs_, :], W=[TMb])
                    A1, A1b = A1r.next(); A2, A2b = A2r.next()
                    for (A_, Ab_, which) in ((A1, A1b, 0), (A2, A2b, 1)):
                        for b in range(2):
                            p, pb = pr.next()
                            for hh_ in range(4):
                                h = b * 4 + hh_
                                k.op("pe", lambda: nc.tensor.matmul(p[0:64, hh_ * 128:(hh_ + 1) * 128], lhsT=BK[:, h, which, :], rhs=AR[:, h, :, :].rearrange("p a d -> p (a d)"), start=True, stop=True),
                                     R=[BKb, ARb], W=[pb])
                            k.op("dve", lambda: nc.vector.tensor_tensor(out=A_[:, b * 4:(b + 1) * 4, :], in0=p[0:64, :].rearrange("p (h d) -> p h d", h=4), in1=m1, op=ALU.mult), R=[pb, cstb], W=[Ab_])
                    p, pb = pr.next()
                    for h in range(8):
                        k.op("pe", lambda: nc.tensor.matmul(p[0:64, h * 64:(h + 1) * 64], lhsT=AR[:, h, 0, :], rhs=BK[:, h, 0, :], start=True, stop=True), R=[ARb, BKb], W=[pb])
                    Pt_, Ptb_ = Ptr_.next()
                    k.op("dve", lambda: nc.vector.tensor_tensor(out=Pt_[:], in0=v8(p), in1=m3, op=ALU.mult), R=[pb, cstb], W=[Ptb_])
                    P_, Pb_ = Pr_.next()
                    k.op("pool", lambda: nc.gpsimd.tensor_copy(out=P_[:], in_=A1[:, :, 0:64]), R=[A1b], W=[Pb_])
                    L_, Lb_ = Lr.next()
                    k.op("pool", lambda: nc.gpsimd.tensor_tensor(out=L_[:], in0=A1[:, :, 0:64], in1=i64b, op=ALU.add), R=[A1b, cstb], W=[Lb_])
                    for lev in range(5):
                        pa, pab = pr.next(); pb2, pb2b = pr.next()
                        for h in range(8):
                            k.op("pe", lambda: nc.tensor.matmul(pa[0:64, h * 64:(h + 1) * 64], lhsT=Pt_[:, h, :], rhs=P_[:, h, :], start=True, stop=True), R=[Ptb_, Pb_], W=[pab])
                        for h in range(8):
                            k.op("pe", lambda: nc.tensor.matmul(pb2[0:64, h * 64:(h + 1) * 64], lhsT=P_[:, h, :], rhs=Pt_[:, h, :], start=True, stop=True), R=[Ptb_, Pb_], W=[pb2b])
                        Pn, Pnb = Pr_.next(); Ptn, Ptnb = Ptr_.next()
                        k.op("act", lambda: nc.scalar.copy(out=Pn[:], in_=v8(pa)), R=[pab], W=[Pnb])
                        k.op("dve", lambda: nc.vector.tensor_copy(out=Ptn[:], in_=v8(pb2)), R=[pb2b], W=[Ptnb])
                        P_, Pb_, Pt_, Ptb_ = Pn, Pnb, Ptn, Ptnb
                        pc, pcb = pr.next()
                        for h in range(8):
                            k.op("pe", lambda: nc.tensor.matmul(pc[0:64, h * 64:(h + 1) * 64], lhsT=Pt_[:, h, :], rhs=L_[:, h, :], start=True, stop=True), R=[Ptb_, Lb_], W=[pcb])
                        Ln, Lnb = Lr.next()
                        k.op("dve", lambda: nc.vector.tensor_tensor(out=Ln[:], in0=L_[:], in1=v8(pc), op=ALU.add), R=[Lb_, pcb], W=[Lnb])
                        L_, Lb_ = Ln, Lnb
                    Vh = lambda h: TM_[:, 2, h * 64:(h + 1) * 64]
                    px, pxb = pr.next()
                    for h in range(8):
                        k.op("pe", lambda: nc.tensor.matmul(px[0:64, h * 64:(h + 1) * 64], lhsT=AR[:, h, 0, :], rhs=H[:, h, :], start=True, stop=False), R=[ARb, Hb], W=[pxb])
                        k.op("pe", lambda: nc.tensor.matmul(px[0:64, h * 64:(h + 1) * 64], lhsT=A2[:, h, 0:64], rhs=Vh(h), start=False, stop=True), R=[A2b, TMb], W=[pxb])
                    Xs, Xsb = Xsr.next()
                    k.op("act", lambda: nc.scalar.copy(out=Xs[:], in_=v8(px)), R=[pxb], W=[Xsb])
                    pu, pub = pr.next()
                    for h in range(8):
                        k.op("pe", lambda: nc.tensor.matmul(pu[0:64, h * 64:(h + 1) * 64], lhsT=L_[:, h, :], rhs=Xs[:, h, :], start=True, stop=True), R=[Lb_, Xsb], W=[pub])
                    Us, Usb = Usr.next()
                    k.op("dve", lambda: nc.vector.tensor_copy(out=Us[:], in_=v8(pu)), R=[pub], W=[Usb])
                    py, pyb = pr.next()
                    for h in range(8):
                        k.op("pe", lambda: nc.tensor.matmul(py[0:64, h * 64:(h + 1) * 64], lhsT=AR[:, h, 1, :], rhs=H[:, h, :], start=True, stop=False), R=[ARb, Hb], W=[pyb])
                        k.op("pe", lambda: nc.tensor.matmul(py[0:64, h * 64:(h + 1) * 64], lhsT=A1[:, h, 64:128], rhs=Us[:, h, :], start=False, stop=False), R=[A1b, Usb], W=[pyb])
                        k.op("pe", lambda: nc.tensor.matmul(py[0:64, h * 64:(h + 1) * 64], lhsT=A2[:, h, 64:128], rhs=Vh(h), start=False, stop=True), R=[A2b, TMb], W=[pyb])
                    Yt, Ytb = Yr.next()
                    k.op("act", lambda: nc.scalar.copy(out=Yt[:], in_=py[0:64, :]), R=[pyb], W=[Ytb])
                    k.dma("pool", rwY[cs_, :], Yt[:], R=[Ytb])
                    ph, phb = pr.next()
                    for h in range(8):
                        k.op("pe", lambda: nc.tensor.matmul(ph[0:64, h * 64:(h + 1) * 64], lhsT=TM_[:, 0, h * 64:(h + 1) * 64], rhs=Us[:, h, :], start=True, stop=False), R=[TMb, Usb], W=[phb])
                        k.op("pe", lambda: nc.tensor.matmul(ph[0:64, h * 64:(h + 1) * 64], lhsT=TM_[:, 1, h * 64:(h + 1) * 64], rhs=Vh(h), start=False, stop=True), R=[TMb], W=[phb])
                    Hn, Hnb = Hr.next()
                    k.op("dve", lambda: nc.vector.tensor_tensor(out=Hn[:], in0=H[:], in1=GLt[:, :, c:c + 1].to_broadcast([64, 8, 64]), op=ALU.mult), R=[Hb, glb], W=[Hnb])
                    k.op("dve", lambda: nc.vector.tensor_tensor(out=Hn[:], in0=Hn[:], in1=v8(ph), op=ALU.add), R=[Hnb, phb], W=[Hnb])
                    H, Hb = Hn, Hnb
                k.barrier()

            with ExitStack() as es:
                SB = lambda name, shape: es.enter_context(sbt(nc, name, shape, F32))
                lng = SB("e_g", [128, 512]); lnb = SB("e_b", [128, 512]); eb = Buf()
                k.dma("sp", lng[:], rw_ln_g[l].partition_broadcast(128), W=[eb])
                k.dma("sp", lnb[:], rw_ln_b[l].partition_broadcast(128), W=[eb])
                inr = Ring(es, nc, "e_in", [128, 3, 512], F32, 2)
                rkr_ = Ring(es, nc, "e_rk", [128, 8], F32, 2)
                sqr = Ring(es, nc, "e_sq", [128, 8, 64], F32, 2)
                str_ = Ring(es, nc, "e_st", [128, 4, 8], F32, 2)
                yr = Ring(es, nc, "e_y", [128, 8, 64], F32, 2)
                mtr = Ring(es, nc, "e_mt", [128, 4, 128], F32, 2)
                pr = Ring(es, nc, "e_ps", [128, 512], F32, 2, psum=True)
                b8 = lambda ap_: ap_.unsqueeze(2).to_broadcast([128, 8, 64])
                v3 = lambda ap_: ap_.rearrange("p (h d) -> p h d", h=8)
                for c in range(NT):
                    t0 = c * 128
                    it, itb = inr.next()
                    k.dma("sp", it[:, 0, :], rwY[t0:t0 + 128, :], W=[itb])
                    k.dma("sp", it[:, 1, :], rwV[t0:t0 + 128, :], W=[itb])
                    k.dma("sp", it[:, 2, :], pTM[t0:t0 + 128, TM_RZ:TM_RZ + 512], W=[itb])
                    rk, rkb = rkr_.next()
                    k.dma("sp", rk[:], rwRKR[t0:t0 + 128, :], W=[rkb])
                    Y3 = v3(it[:, 0, :])
                    sq, sqb = sqr.next()
                    k.op("act", lambda: nc.scalar.activation(out=sq[:], in_=Y3, func=AF.Square), R=[itb], W=[sqb])
                    st, stb = str_.next()
                    k.op("dve", lambda: nc.vector.tensor_reduce(out=st[:, 0, :], in_=Y3, axis=AX.X, op=ALU.add), R=[itb], W=[stb])
                    k.op("dve", lambda: nc.vector.tensor_reduce(out=st[:, 1, :], in_=sq[:], axis=AX.X, op=ALU.add), R=[sqb], W=[stb])
                    k.op("dve", lambda: nc.vector.tensor_scalar(out=st[:, 0, :], in0=st[:, 0, :], scalar1=1.0 / 64, scalar2=None, op0=ALU.mult), R=[stb], W=[stb])
                    k.op("dve", lambda: nc.vector.tensor_tensor(out=st[:, 2, :], in0=st[:, 0, :], in1=st[:, 0, :], op=ALU.mult), R=[stb], W=[stb])
                    k.op("dve", lambda: nc.vector.scalar_tensor_tensor(out=st[:, 1, :], in0=st[:, 1, :], scalar=1.0 / 64, in1=st[:, 2, :], op0=ALU.mult, op1=ALU.subtract), R=[stb], W=[stb])
                    rstd(st[:, 3, :], st[:, 1, :], 1.0, C_GNEPS, [stb], [stb])
                    y, yb = yr.next()
                    k.op("dve", lambda: nc.vector.tensor_tensor(out=y[:], in0=Y3, in1=b8(st[:, 0, :]), op=ALU.subtract), R=[itb, stb], W=[yb])
                    k.op("dve", lambda: nc.vector.tensor_tensor(out=y[:], in0=y[:], in1=b8(st[:, 3, :]), op=ALU.mult), R=[yb, stb], W=[yb])
                    k.op("pool", lambda: nc.gpsimd.tensor_tensor(out=y[:], in0=y[:], in1=v3(lng[:]), op=ALU.mult), R=[yb, eb], W=[yb])
                    k.op("pool", lambda: nc.gpsimd.tensor_tensor(out=y[:], in0=y[:], in1=v3(lnb[:]), op=ALU.add), R=[yb, eb], W=[yb])
                    k.op("dve", lambda: nc.vector.tensor_tensor(out=sq[:], in0=v3(it[:, 1, :]), in1=b8(rk[:]), op=ALU.mult), R=[itb, rkb, sqb], W=[sqb])
                    k.op("dve", lambda: nc.vector.tensor_tensor(out=y[:], in0=y[:], in1=sq[:], op=ALU.add), R=[yb, sqb], W=[yb])
                    k.op("act", lambda: nc.scalar.activation(out=it[:, 2, :], in_=it[:, 2, :], func=AF.Silu), R=[itb], W=[itb])
                    k.op("dve", lambda: nc.vector.tensor_tensor(out=y[:], in0=y[:], in1=v3(it[:, 2, :]), op=ALU.mult), R=[yb, itb], W=[yb])
                    p, pb = pr.next()
                    yf = y[:].rearrange("p h d -> p (h d)")
                    for j in range(4):
                        k.op("pe", lambda: nc.tensor.transpose(p[:, j * 128:(j + 1) * 128], yf[:, j * 128:(j + 1) * 128], ident), R=[yb, cstb], W=[pb])
                    mt, mtb = mtr.next()
                    k.op("act", lambda: nc.scalar.copy(out=mt[:], in_=p[:].rearrange("p (a b) -> p a b", a=4)), R=[pb], W=[mtb])
                    k.dma("pool", mixT.rearrange("(cc p) t -> p cc t", p=128)[:, 12:16, t0:t0 + 128], mt[:], R=[mtb])
                k.barrier()

            with ExitStack() as es:
                mx = Ring(es, nc, "p5_m", [128, 16, G], F32, 2)
                wr = Ring(es, nc, "p5_w", [128, 16, 512], F32, 2)
                xr = Ring(es, nc, "p5_x", [128, G], F32, 3)
                xo = Ring(es, nc, "p5_o", [128, G], F32, 3)
                ps = Ring(es, nc, "p5_ps", [128, 512], F32, 4, psum=True)
                for g in range(NG):
                    mt, mb = mx.next()
                    k.dma("sp", mt[:], mixT.rearrange("(cc p) t -> p cc t", p=128)[:, :, g * G:(g + 1) * G], W=[mb])
                    for nb in range(4):
                        wt, wb = wr.next()
                        k.dma("sp", wt[:], w_out[l].rearrange("(cc p) n -> p cc n", p=128)[:, :, nb * 512:(nb + 1) * 512], W=[wb])
                        for j in range(4):
                            n0 = nb * 512 + j * 128
                            xt, xb = xr.next()
                            k.dma("sp", xt[:], xT[n0:n0 + 128, g * G:(g + 1) * G], W=[xb])
                            p, pb = ps.next()
                            for cc in range(16):
                                k.op("pe", lambda: nc.tensor.matmul(p[:, :G], lhsT=wt[:, cc, j * 128:(j + 1) * 128], rhs=mt[:, cc, :],
                                                                    start=(cc == 0), stop=(cc == 15)), R=[wb, mb], W=[pb])
                            ot, ob = xo.next()
                            k.op("dve", lambda: nc.vector.tensor_tensor(out=ot[:], in0=p[:, :G], in1=xt[:], op=ALU.add), R=[pb, xb], W=[ob])
                            k.dma("pool", xT[n0:n0 + 128, g * G:(g + 1) * G], ot[:], R=[ob])
                k.barrier()

        with ExitStack() as es:
            gbc = es.enter_context(sbt(nc, "f_g", [128, D], F32))
            gbb = Buf()
            k.dma("sp", gbc[:], final_g.partition_broadcast(128), W=[gbb])
            xin = Ring(es, nc, "f_x", [128, 16, 128], F32, 2)
            xtm = Ring(es, nc, "f_t", [128, D], F32, 2)
            junk = es.enter_context(sbt(nc, "f_j", [128, D], F32))
            jb = Buf()
            ssq = Ring(es, nc, "f_s", [128, 2], F32, 2)
            ot = Ring(es, nc, "f_o", [128, D], F32, 2)
            ps = Ring(es, nc, "f_ps", [128, 512], F32, 8, psum=True)
            for t in range(NT):
                xt, xb = xin.next()
                k.dma("sp", xt[:], xT.rearrange("(kc p) t -> p kc t", p=128)[:, :, t * 128:(t + 1) * 128], W=[xb])
                xm, xmb = xtm.next()
                for q in range(4):
                    p, pb = ps.next()
                    for j in range(4):
                        kc = q * 4 + j
                        k.op("pe", lambda: nc.tensor.transpose(p[:, j * 128:(j + 1) * 128], xt[:, kc, :], ident), R=[xb, cstb], W=[pb])
                    if q % 2:
                        k.op("act", lambda: nc.scalar.copy(out=xm[:, q * 512:(q + 1) * 512], in_=p[:]), R=[pb], W=[xmb])
                    else:
                        k.op("dve", lambda: nc.vector.tensor_copy(out=xm[:, q * 512:(q + 1) * 512], in_=p[:]), R=[pb], W=[xmb])
                sq, sqb = ssq.next()
                k.op("act", lambda: nc.scalar.activation(out=junk[:], in_=xm[:], func=AF.Square, accum_out=sq[:, 0:1]), R=[xmb], W=[jb, sqb])
                rstd(sq[:, 1:2], sq[:, 0:1], 1.0 / D, C_EPS, [sqb], [sqb])
                o, ob = ot.next()
                k.op("dve", lambda: nc.vector.scalar_tensor_tensor(out=o[:], in0=xm[:], scalar=sq[:, 1:2], in1=gbc[:], op0=ALU.mult, op1=ALU.mult),
                     R=[xmb, sqb, gbb], W=[ob])
                k.dma("pool", out[t * 128:(t + 1) * 128, :], o[:], R=[ob])
            k.barrier()
    return nc


def MIXERS(env):
    pass


_CACHE = {}


WNAMES = ("norm_g", "w_in", "w_out", "final_norm_g", "ml_conv_w", "ml_conv_b", "ml_i_bias", "ml_f_bias", "ml_norm_g",
          "mla_q_norm_g", "mla_w_uq", "mla_kv_norm_g", "mla_w_ukv",
          "rw_mu", "rw_w0", "rw_w2", "rw_a0", "rw_a2", "rw_k_k", "rw_k_a", "rw_r_k", "rw_ln_g", "rw_ln_b")


def make_maps(inputs, T, depth):
    consts = make_consts()
    shared = {}
    for name in WNAMES:
        a = inputs[name]
        shared[name] = np.ascontiguousarray(a if name == "final_norm_g" else a[:depth])
    for name in ("rw_v0", "rw_v1", "rw_v2"):
        shared[name] = np.ascontiguousarray(inputs[name])
    in_maps = []
    for c in range(8):
        b = c % 4
        m = {"x": np.ascontiguousarray(inputs["x"][b, :T]), "positions": np.ascontiguousarray(inputs["positions"][b, :T]),
             "consts": consts}
        m.update(shared)
        in_maps.append(m)
    return in_maps


def kernel(**inputs):
    T = 4096
    depth = 4
    if "nc" not in _CACHE:
        _CACHE["nc"] = build(T, depth)
    nc = _CACHE["nc"]
    in_maps = make_maps(inputs, T, depth)
    res = run_bass_kernel_spmd(nc, in_maps, core_ids=list(range(8)))
    return np.stack([res.results[b]["out"] for b in range(4)], axis=0)
```

```python
import numpy as np
from contextlib import ExitStack
import concourse.bass as bass
import concourse.mybir as mybir
from concourse.bass_utils import run_bass_kernel_spmd

F32 = mybir.dt.float32
BF16 = mybir.dt.bfloat16
I32 = mybir.dt.int32
AF = mybir.ActivationFunctionType
ALU = mybir.AluOpType
AX = mybir.AxisListType

D = 2048
DIN = 6600
EPS = 1e-6
FM_RANGES = [(0, 1024), (2568, 3336), (4424, 6088)]
TM_RANGES = [(1024, 2568), (3336, 4424), (6088, 6600)]
TM_V, TM_I, TM_F, TM_O, TM_Z = 0, 512, 516, 520, 1032
TM_KR, TM_MZ, TM_RZ = 1544, 1608, 2632
NTM = 3144
FM_QK, FM_CQ, FM_CKV, FM_RW = 0, 8, 12, 14
NFM = 27


_UID = [0]


def sbt(nc, name, shape, dt):
    _UID[0] += 1
    return nc.sbuf_tensor("%s_u%d" % (name, _UID[0]), shape, dt)


def pst(nc, name, shape, dt):
    _UID[0] += 1
    return nc.psum_tensor("%s_u%d" % (name, _UID[0]), shape, dt)


class Buf:
    __slots__ = ("w", "r")

    def __init__(self):
        self.w = None
        self.r = {}


class KB:
    NDS = 24

    def __init__(self, nc):
        self.nc = nc
        self.E = {"pe": nc.tensor, "act": nc.scalar, "dve": nc.vector, "pool": nc.gpsimd, "sp": nc.sync}
        self.sem = {e: nc.alloc_semaphore("s_" + e) for e in ("pe", "act", "dve", "pool")}
        self.cnt = {e: 0 for e in self.sem}
        self.dsem = [nc.alloc_semaphore("d%d" % i) for i in range(self.NDS)]
        self.dcnt = [0] * self.NDS
        self.dnext = 0
        self.bar = nc.alloc_semaphore("bar")
        self.nbar = 0
        self.seen = {e: {} for e in self.E}

    def _semh(self, key):
        return self.sem[key] if isinstance(key, str) else self.dsem[key[1]]

    def _wait(self, e, key, val, same_ok=False):
        if val <= 0:
            return
        if same_ok and key == e:
            return
        if self.seen[e].get(key, 0) >= val:
            return
        self.E[e].wait_ge(self._semh(key), val)
        self.seen[e][key] = val

    def _deps(self, e, reads, writes):
        for b in reads:
            if b.w is not None:
                self._wait(e, b.w[0], b.w[1])
        for b in writes:
            if b.w is not None:
                self._wait(e, b.w[0], b.w[1])
            for key, val in b.r.items():
                self._wait(e, key, val, same_ok=True)

    def _mark(self, ev, reads, writes):
        for b in reads:
            if b.r.get(ev[0], 0) < ev[1]:
                b.r[ev[0]] = ev[1]
        for b in writes:
            b.w = ev
            b.r = {}

    def op(self, e, fn, R=(), W=()):
        self._deps(e, R, W)
        ins = fn()
        self.cnt[e] += 1
        ins.then_inc(self.sem[e], 1)
        self._mark((e, self.cnt[e]), R, W)

    def dma(self, q, out, in_, R=(), W=(), slow=False):
        i = self.dnext
        self.dnext = (i + 1) % self.NDS
        key = ("d", i)
        self._wait(q, key, self.dcnt[i])
        self._deps(q, R, W)
        if slow:
            ins = self.E[q].dma_start(out=out, in_=in_, allow_slow_non_contiguous=True)
        else:
            ins = self.E[q].dma_start(out=out, in_=in_)
        self.dcnt[i] += 16
        ins.then_inc(self.dsem[i], 16)
        self._mark((key, self.dcnt[i]), R, W)

    def barrier(self):
        sp = self.E["sp"]
        for e in self.sem:
            self._wait("sp", e, self.cnt[e])
        for i in range(self.NDS):
            self._wait("sp", ("d", i), self.dcnt[i])
        self.nbar += 1
        sp.sem_inc(self.bar, 1)
        for e in ("pe", "act", "dve", "pool"):
            self.E[e].wait_ge(self.bar, self.nbar)
            for k2 in self.sem:
                self.seen[e][k2] = self.cnt[k2]
            for i in range(self.NDS):
                self.seen[e][("d", i)] = self.dcnt[i]


class Ring:
    def __init__(self, es, nc, name, shape, dtype, n, psum=False):
        self.items = []
        for i in range(n):
            if psum:
                t = es.enter_context(pst(nc, "%s%d" % (name, i), shape, dtype))
            else:
                t = es.enter_context(sbt(nc, "%s%d" % (name, i), shape, dtype))
            self.items.append((t, Buf()))
        self.i = 0

    def next(self):
        it = self.items[self.i]
        self.i = (self.i + 1) % len(self.items)
        return it


def split_blocks(ranges, maxw=512):
    out = []
    for (a, b) in ranges:
        c = a
        while c < b:
            w = min(maxw, b - c)
            out.append((c, w))
            c += w
    return out


def make_consts():
    c = np.zeros((128, 1280), np.float32)
    c[:, 0:128] = np.eye(128, dtype=np.float32)
    j = np.arange(128)
    c[:, 128:256] = (j[:, None] <= j[None, :]).astype(np.float32)
    c[:, 256:384] = 1.0
    same = (j[:, None] // 64) == (j[None, :] // 64)
    c[:, 384:512] = ((j[:, None] <= j[None, :]) & same)
    c[:, 512:640] = ((j[:, None] < j[None, :]) & same)
    invf = np.power(10000.0, -np.arange(0, 64, 2, dtype=np.float32) / 64).astype(np.float32)
    c[:, 640:672] = invf[None, :]
    c[:, 672:674] = (j[:, None] // 64 == np.arange(2)[None, :])
    c[:, 700] = 1e-6
    c[:, 701] = 64e-5
    c[:, 702] = 1.0
    c[:, 703] = -0.5
    c[:, 704] = 0.0
    c[:, 705] = -np.pi
    c[:, 706] = 1e-24
    c[:, 707] = 1.0 / 128
    i64 = np.arange(64)
    c[:64, 768:832] = (i64[:, None] < i64[None, :])
    c[:64, 832:896] = (i64[:, None] <= i64[None, :])
    c[:64, 896:960] = (i64[:, None] > i64[None, :])
    for jj in range(4):
        c[:, 960 + jj * 8:968 + jj * 8] = ((2 * jj + j[:, None] // 64) == np.arange(8)[None, :])
    c[:, 1024:1152] = same
    return c


C_ID, C_TRI, C_ONE, C_BI, C_BS, C_IF, C_CI = 0, 128, 256, 384, 512, 640, 672
C_M1, C_M3, C_HS, C_BD = 768, 896, 960, 1024
C_EPS, C_GNEPS, C_1, C_MH, C_0, C_MPI, C_TINY, C_R128 = 700, 701, 702, 703, 704, 705, 706, 707


def build(T, depth, dbg=()):
    assert T % 128 == 0
    NT = T // 128
    SG = min(T, 1024)
    NSG = T // SG
    G = min(T, 512)
    NG = T // G
    nc = bass.Bass("TRN2", target_bir_lowering=False)

    def din(name, shape, dt=F32):
        return nc.dram_tensor(name, list(shape), dt, kind="ExternalInput").ap()

    def dscr(name, shape, dt=F32):
        kind = "ExternalOutput" if name in dbg else "Internal"
        return nc.dram_tensor(name, list(shape), dt, kind=kind).ap()

    x_in = din("x", [T, D])
    pos_in = din("positions", [T], I32)
    consts_in = din("consts", [128, 1280])
    norm_g = din("norm_g", [depth, D])
    w_in = din("w_in", [depth, D, DIN])
    w_out = din("w_out", [depth, D, D])
    final_g = din("final_norm_g", [D])
    ml_conv_w = din("ml_conv_w", [depth, 4, 1024]); ml_conv_b = din("ml_conv_b", [depth, 1024])
    mla_q_norm_g = din("mla_q_norm_g", [depth, 512]); mla_w_uq = din("mla_w_uq", [depth, 512, 1536])
    mla_kv_norm_g = din("mla_kv_norm_g", [depth, 256]); mla_w_ukv = din("mla_w_ukv", [depth, 256, 2048])
    rw_mu = din("rw_mu", [depth, 1664]); rw_w0 = din("rw_w0", [depth, 512]); rw_w2 = din("rw_w2", [depth, 64, 512])
    rw_a0 = din("rw_a0", [depth, 512]); rw_a2 = din("rw_a2", [depth, 64, 512])
    rw_v0 = din("rw_v0", [3, 512]); rw_v1 = din("rw_v1", [3, 512, 32]); rw_v2 = din("rw_v2", [3, 32, 512])
    rw_k_k = din("rw_k_k", [depth, 512]); rw_k_a = din("rw_k_a", [depth, 512]); rw_r_k = din("rw_r_k", [depth, 8, 64])
    rw_ln_g = din("rw_ln_g", [depth, 512]); rw_ln_b = din("rw_ln_b", [depth, 512])
    ml_i_bias = din("ml_i_bias", [depth, 4]); ml_f_bias = din("ml_f_bias", [depth, 4]); ml_norm_g = din("ml_norm_g", [depth, 512])
    out = nc.dram_tensor("out", [T, D], F32, kind="ExternalOutput").ap()

    xT = dscr("xT", [D, T])
    pFM = dscr("pFM", [NFM * 128, T])
    pTM = dscr("pTM", [T, NTM])
    mixT = dscr("mixT", [D, T], BF16)
    csd = dscr("cs", [T, 64])
    qnT = dscr("qnT", [1024, T], BF16); knT = dscr("knT", [1024, T], BF16); qrT = dscr("qrT", [512, T], BF16); krT = dscr("krT", [64, T], BF16)
    vaug = dscr("vaug", [8, T, 129], BF16)
    rwA = dscr("rwA", [512, T]); rwR = dscr("rwR", [512, T]); rwB = dscr("rwB", [512, T]); rwK = dscr("rwK", [512, T])
    rwBp = dscr("rwBp", [T, 512]); rwKp = dscr("rwKp", [T, 512]); rwV = dscr("rwV", [T, 512]); rwY = dscr("rwY", [T, 512])
    rwGL = dscr("rwGL", [512, T // 64]); rwRKR = dscr("rwRKR", [T, 8]); vfirst = dscr("vfirst", [512, T])

    fm_blocks = split_blocks(FM_RANGES)
    tm_blocks = split_blocks(TM_RANGES)

    with ExitStack() as top:
        k = KB(nc)
        cst = top.enter_context(sbt(nc, "cst", [128, 1280], F32))
        cstb = Buf()
        k.dma("sp", cst[:], consts_in, W=[cstb])
        ident = cst[:, C_ID:C_ID + 128]
        ones = cst[:, C_ONE:C_ONE + 128]
        onec = cst[:, C_ONE:C_ONE + 1]

        def rstd(o, i, scale, epscol, R, W, np_=128):
            k.op("act", lambda: nc.scalar.activation(out=o, in_=i, func=AF.Sqrt, bias=cst[:np_, epscol:epscol + 1], scale=scale), R=list(R) + [cstb], W=W)
            k.op("dve", lambda: nc.vector.reciprocal(out=o, in_=o), R=W, W=W)

        with ExitStack() as es:
            xin = Ring(es, nc, "i_x", [128, D], F32, 2)
            ps = Ring(es, nc, "i_ps", [128, 512], F32, 4, psum=True)
            stg = Ring(es, nc, "i_st", [128, 16, 128], F32, 2)
            for t in range(NT):
                xt, xb = xin.next()
                k.dma("sp", xt[:], x_in[t * 128:(t + 1) * 128, :], W=[xb])
                st, sb = stg.next()
                for q in range(4):
                    p, pb = ps.next()
                    for j in range(4):
                        kc = q * 4 + j
                        k.op("pe", lambda: nc.tensor.transpose(p[:, j * 128:(j + 1) * 128], xt[:, kc * 128:(kc + 1) * 128], ident),
                             R=[xb, cstb], W=[pb])
                    eng = "act" if q % 2 else "dve"
                    if eng == "act":
                        k.op("act", lambda: nc.scalar.copy(out=st[:, q * 4:(q + 1) * 4, :], in_=p[:].rearrange("p (a b) -> p a b", a=4)), R=[pb], W=[sb])
                    else:
                        k.op("dve", lambda: nc.vector.tensor_copy(out=st[:, q * 4:(q + 1) * 4, :], in_=p[:].rearrange("p (a b) -> p a b", a=4)), R=[pb], W=[sb])
                k.dma("pool", xT.rearrange("(kc p) t -> p kc t", p=128)[:, :, t * 128:(t + 1) * 128], st[:], R=[sb])
            posi = es.enter_context(sbt(nc, "i_pi", [128, NT], I32))
            posf = es.enter_context(sbt(nc, "i_pf", [128, NT], F32))
            pob = Buf()
            k.dma("sp", posi[:], pos_in.rearrange("(n p) -> p n", p=128), W=[pob], slow=True)
            k.op("dve", lambda: nc.vector.tensor_copy(out=posf[:], in_=posi[:]), R=[pob], W=[pob])
            angr = Ring(es, nc, "i_ang", [128, 64], F32, 2)
            tir = Ring(es, nc, "i_ti", [128, 64], I32, 2)
            tfr = Ring(es, nc, "i_tf", [128, 64], F32, 2)
            TWO_PI = float(2 * np.pi)
            C1 = 6.28125
            C2 = float(2 * np.pi - 6.28125)
            for t in range(NT):
                an, anb = angr.next()
                ti, tib = tir.next()
                tf, tfb = tfr.next()
                k.op("dve", lambda: nc.vector.tensor_scalar(out=an[:, 0:32], in0=cst[:, C_IF:C_IF + 32], scalar1=posf[:, t:t + 1], scalar2=None, op0=ALU.mult), R=[pob, cstb], W=[anb])
                k.op("dve", lambda: nc.vector.tensor_scalar(out=an[:, 32:64], in0=an[:, 0:32], scalar1=float(np.pi / 2), scalar2=None, op0=ALU.add), R=[anb], W=[anb])
                k.op("dve", lambda: nc.vector.tensor_scalar(out=tf[:], in0=an[:], scalar1=float(1 / (2 * np.pi)), scalar2=None, op0=ALU.mult), R=[anb], W=[tfb])
                k.op("dve", lambda: nc.vector.tensor_copy(out=ti[:], in_=tf[:]), R=[tfb], W=[tib])
                k.op("dve", lambda: nc.vector.tensor_copy(out=tf[:], in_=ti[:]), R=[tib], W=[tfb])
                k.op("dve", lambda: nc.vector.scalar_tensor_tensor(out=an[:], in0=tf[:], scalar=-C1, in1=an[:], op0=ALU.mult, op1=ALU.add), R=[tfb, anb], W=[anb])
                k.op("dve", lambda: nc.vector.scalar_tensor_tensor(out=an[:], in0=tf[:], scalar=-C2, in1=an[:], op0=ALU.mult, op1=ALU.add), R=[tfb, anb], W=[anb])
                k.op("dve", lambda: nc.vector.tensor_scalar(out=tf[:], in0=an[:], scalar1=float(np.pi), scalar2=-TWO_PI, op0=ALU.is_gt, op1=ALU.mult), R=[anb], W=[tfb])
                k.op("dve", lambda: nc.vector.tensor_tensor(out=an[:], in0=an[:], in1=tf[:], op=ALU.add), R=[tfb, anb], W=[anb])
                k.op("dve", lambda: nc.vector.tensor_scalar(out=tf[:], in0=an[:], scalar1=float(-np.pi), scalar2=TWO_PI, op0=ALU.is_lt, op1=ALU.mult), R=[anb], W=[tfb])
                k.op("dve", lambda: nc.vector.tensor_tensor(out=an[:], in0=an[:], in1=tf[:], op=ALU.add), R=[tfb, anb], W=[anb])
                k.op("act", lambda: nc.scalar.activation(out=an[:], in_=an[:], func=AF.Sin), R=[anb], W=[anb])
                k.dma("pool", csd[t * 128:(t + 1) * 128, :], an[:], R=[anb])
            k.barrier()

        for l in range(depth):
            with ExitStack() as es:
                gcol = es.enter_context(sbt(nc, "p1_g", [128, 16], F32))
                gb = Buf()
                k.dma("sp", gcol[:], norm_g[l].rearrange("(kc p) -> p kc", p=128), W=[gb], slow=True)
                hT = es.enter_context(sbt(nc, "p1_hT", [128, 16, SG], BF16))
                hb = Buf()
                xs = Ring(es, nc, "p1_xs", [128, SG], F32, 2)
                sqr = Ring(es, nc, "p1_sq", [128, SG], F32, 2)
                wr = Ring(es, nc, "p1_w", [128, 16, 512], F32, 2)
                wcr = Ring(es, nc, "p1_wc", [128, 16, 512], BF16, 2)
                wcn = [0]
                stg = Ring(es, nc, "p1_st", [128, 512], F32, 4)
                rbc = es.enter_context(sbt(nc, "p1_rbc", [128, SG], F32))
                rbcb = Buf()
                rtm = es.enter_context(sbt(nc, "p1_rtm", [128, SG // 128], F32))
                rtmb = Buf()
                ps = Ring(es, nc, "p1_ps", [128, 512], F32, 4, psum=True)
                pbc = Ring(es, nc, "p1_pbc", [128, 512], F32, SG // G, psum=True)
                ptm = es.enter_context(pst(nc, "p1_ptm", [128, 512], F32))
                ptmb = Buf()
                def p1_loadw(cc0, nb):
                    wf, wfb = wr.next()
                    k.dma("sp", wf[:, :, :nb], w_in[l].rearrange("(kc p) n -> p kc n", p=128)[:, :, cc0:cc0 + nb], W=[wfb])
                    wc, wcb = wcr.next()
                    wcn[0] += 1
                    for hf in range(2):
                        if (wcn[0] + hf) % 2:
                            k.op("act", lambda: nc.scalar.copy(out=wc[:, hf * 8:(hf + 1) * 8, :nb], in_=wf[:, hf * 8:(hf + 1) * 8, :nb]), R=[wfb], W=[wcb])
                        else:
                            k.op("dve", lambda: nc.vector.tensor_copy(out=wc[:, hf * 8:(hf + 1) * 8, :nb], in_=wf[:, hf * 8:(hf + 1) * 8, :nb]), R=[wfb], W=[wcb])
                    return wc, wcb

                for sg in range(NSG):
                    c0 = sg * SG
                    pbcs = [pbc.next() for _ in range(SG // G)]
                    for kc in range(16):
                        xt, xb = xs.next()
                        k.dma("sp", xt[:], xT[kc * 128:(kc + 1) * 128, c0:c0 + SG], W=[xb])
                        sq, sqb = sqr.next()
                        k.op("act", lambda: nc.scalar.activation(out=sq[:], in_=xt[:], func=AF.Square), R=[xb], W=[sqb])
                        k.op("dve", lambda: nc.vector.tensor_scalar(out=hT[:, kc, :], in0=xt[:], scalar1=gcol[:, kc:kc + 1], scalar2=None, op0=ALU.mult),
                             R=[xb, gb], W=[hb])
                        for s in range(SG // G):
                            pp, ppb = pbcs[s]
                            k.op("pe", lambda: nc.tensor.matmul(pp[:, :G], lhsT=ones, rhs=sq[:, s * G:(s + 1) * G], start=(kc == 0), stop=(kc == 15)),
                                 R=[sqb, cstb], W=[ppb])
                    for s in range(SG // G):
                        pp, ppb = pbcs[s]
                        rstd(rbc[:, s * G:(s + 1) * G], pp[:, :G], 1.0 / D, C_EPS, [ppb], [rbcb])
                    for t in range(SG // 128):
                        k.op("pe", lambda: nc.tensor.matmul(ptm[:, t:t + 1], lhsT=rbc[:, t * 128:(t + 1) * 128], rhs=cst[:, C_R128:C_R128 + 1], start=True, stop=True),
                             R=[rbcb, cstb], W=[ptmb])
                    k.op("dve", lambda: nc.vector.tensor_copy(out=rtm[:], in_=ptm[:, :SG // 128]), R=[ptmb], W=[rtmb])
                    fmrow = 0
                    for (cc0, nb) in fm_blocks:
                        wt, wb = p1_loadw(cc0, nb)
                        for j in range(nb // 128):
                            for s in range(SG // G):
                                p, pb = ps.next()
                                for kc in range(16):
                                    k.op("pe", lambda: nc.tensor.matmul(p[:, :G], lhsT=wt[:, kc, j * 128:(j + 1) * 128], rhs=hT[:, kc, s * G:(s + 1) * G],
                                                                        start=(kc == 0), stop=(kc == 15)), R=[wb, hb], W=[pb])
                                st, sb = stg.next()
                                k.op("dve", lambda: nc.vector.tensor_tensor(out=st[:, :G], in0=p[:, :G], in1=rbc[:, s * G:(s + 1) * G], op=ALU.mult),
                                     R=[pb, rbcb], W=[sb])
                                k.dma("pool", pFM[fmrow * 128:(fmrow + 1) * 128, c0 + s * G:c0 + (s + 1) * G], st[:, :G], R=[sb])
                            fmrow += 1
                    tmcol = 0
                    for (cc0, nb) in tm_blocks:
                        wt, wb = p1_loadw(cc0, nb)
                        for t in range(SG // 128):
                            p, pb = ps.next()
                            for kc in range(16):
                                k.op("pe", lambda: nc.tensor.matmul(p[:, :nb], lhsT=hT[:, kc, t * 128:(t + 1) * 128], rhs=wt[:, kc, :nb],
                                                                    start=(kc == 0), stop=(kc == 15)), R=[wb, hb], W=[pb])
                            st, sb = stg.next()
                            k.op("act", lambda: nc.scalar.activation(out=st[:, :nb], in_=p[:, :nb], func=AF.Copy, scale=rtm[:, t:t + 1]),
                                 R=[pb, rtmb], W=[sb])
                            k.dma("pool", pTM[c0 + t * 128:c0 + (t + 1) * 128, tmcol:tmcol + nb], st[:, :nb], R=[sb])
                        tmcol += nb
                k.barrier()


            with ExitStack() as es:
                SB = lambda name, shape: es.enter_context(sbt(nc, name, shape, F32))
                PS = lambda name: es.enter_context(pst(nc, name, [128, 512], F32))
                tri = cst[:, C_TRI:C_TRI + 128]
                cw = SB("m_cw", [128, 4, 8]); cb = SB("m_cb", [128, 8]); ibfb = SB("m_ib", [128, 8]); gbc = SB("m_g", [128, 512])
                pb_ = Buf()
                for j in range(4):
                    k.dma("sp", cw[:, j, :], ml_conv_w[l, j].rearrange("(c p) -> p c", p=128), W=[pb_], slow=True)
                k.dma("sp", cb[:], ml_conv_b[l].rearrange("(c p) -> p c", p=128), W=[pb_], slow=True)
                k.dma("sp", ibfb[:, 0:4], ml_i_bias[l].partition_broadcast(128), W=[pb_])
                k.dma("sp", ibfb[:, 4:8], ml_f_bias[l].partition_broadcast(128), W=[pb_])
                k.dma("sp", gbc[:], ml_norm_g[l].partition_broadcast(128), W=[pb_])
                CT = [SB("m_CT%d" % h, [128, 129]) for h in range(4)]
                CTb = [Buf() for h in range(4)]
                for h in range(4):
                    k.op("pool", lambda: nc.gpsimd.memset(CT[h][:], 0.0), W=[CTb[h]])
                qkr = Ring(es, nc, "m_qkr", [128, 8, 131], F32, 2)
                accr = Ring(es, nc, "m_acc", [128, 8, 128], F32, 2)
                qkt = Ring(es, nc, "m_qk", [128, 8, 128], F32, 2)
                vr = Ring(es, nc, "m_v", [128, 4, 129], F32, 2)
                for (vt, vb) in vr.items:
                    k.op("pool", lambda: nc.gpsimd.memset(vt[:, :, 128:129], 1.0), W=[vb])
                gtr = Ring(es, nc, "m_gt", [128, 8], F32, 2)
                Gr = Ring(es, nc, "m_G", [128, 32], F32, 2)
                ozr = Ring(es, nc, "m_oz", [128, 1024], F32, 2)
                rhr = Ring(es, nc, "m_rh", [128, 128], F32, 2)
                Er = Ring(es, nc, "m_E", [128, 128], F32, 2)
                STr = Ring(es, nc, "m_ST", [128, 128], F32, 2)
                n1r = Ring(es, nc, "m_n1", [128, 129], F32, 2)
                n2r = Ring(es, nc, "m_n2", [128, 129], F32, 2)
                smr = Ring(es, nc, "m_sm", [128, 16], F32, 2)
                hhr = Ring(es, nc, "m_hh", [128, 128], F32, 2)
                kwr = Ring(es, nc, "m_kw", [128, 128], F32, 2)
                yr = Ring(es, nc, "m_y", [128, 512], F32, 2)
                ggr = Ring(es, nc, "m_gg", [128, 1024], F32, 2)
                mtr = Ring(es, nc, "m_mt", [128, 4, 128], BF16, 2)
                pgA = PS("m_pgA"); pgB = PS("m_pgB"); pBbc = PS("m_pB"); pQK = PS("m_pQK")
                pN1 = PS("m_pN1"); pN2 = PS("m_pN2"); pKT = PS("m_pKT"); pdC = PS("m_pdC")
                pgAb, pgBb, pBbcb, pQKb, pN1b, pN2b, pKTb, pdCb = [Buf() for _ in range(8)]
                NSD = nc.vector.BN_STATS_DIM
                for c in range(NT):
                    t0 = c * 128
                    X, Xb = qkr.next()
                    src = pFM.rearrange("(ch p) t -> p ch t", p=128)
                    if c == 0:
                        k.op("pool", lambda: nc.gpsimd.memset(X[:, :, 0:3], 0.0), W=[Xb])
                        k.dma("sp", X[:, :, 3:131], src[:, 0:8, 0:128], W=[Xb])
                    else:
                        k.dma("sp", X[:], src[:, 0:8, t0 - 3:t0 + 128], W=[Xb])
                    vt, vb = vr.next()
                    k.dma("sp", vt[:, :, 0:128], pTM[t0:t0 + 128, TM_V:TM_V + 512].rearrange("p (h d) -> p h d", h=4), W=[vb])
                    gt, gtb = gtr.next()
                    k.dma("sp", gt[:], pTM[t0:t0 + 128, TM_I:TM_I + 8], W=[gtb])
                    oz, ozb = ozr.next()
                    k.dma("sp", oz[:], pTM[t0:t0 + 128, TM_O:TM_O + 1024], W=[ozb])
                    acc, accb = accr.next()
                    qk, qkb = qkt.next()
                    for ch in range(8):
                        eng, E_ = ("dve", nc.vector)
                        k.op(eng, lambda: E_.tensor_scalar(out=acc[:, ch, :], in0=X[:, ch, 0:128], scalar1=cw[:, 0, ch:ch + 1], scalar2=cb[:, ch:ch + 1], op0=ALU.mult, op1=ALU.add),
                             R=[Xb, pb_], W=[accb])
                        for j in range(1, 4):
                            k.op(eng, lambda: E_.scalar_tensor_tensor(out=acc[:, ch, :], in0=X[:, ch, j:j + 128], scalar=cw[:, j, ch:ch + 1], in1=acc[:, ch, :], op0=ALU.mult, op1=ALU.add),
                                 R=[Xb, pb_, accb], W=[accb])
                    k.op("act", lambda: nc.scalar.activation(out=qk[:], in_=acc[:], func=AF.Silu), R=[accb], W=[qkb])
                    k.op("pool", lambda: nc.gpsimd.tensor_scalar(out=qk[:, 0:4, :], in0=qk[:, 0:4, :], scalar1=float(128 ** -0.5), scalar2=None, op0=ALU.mult), R=[qkb], W=[qkb])
                    Gt, Gb = Gr.next()
                    k.op("dve", lambda: nc.vector.tensor_tensor(out=Gt[:, 0:8], in0=gt[:], in1=ibfb[:], op=ALU.add), R=[gtb, pb_], W=[Gb])
                    k.op("act", lambda: nc.scalar.activation(out=Gt[:, 8:12], in_=Gt[:, 4:8], func=AF.Exp, scale=-1.0), R=[Gb], W=[Gb])
                    k.op("act", lambda: nc.scalar.activation(out=Gt[:, 12:16], in_=Gt[:, 8:12], func=AF.Ln, bias=cst[:, C_1:C_1 + 1]), R=[Gb, cstb], W=[Gb])
                    k.op("dve", lambda: nc.vector.tensor_scalar(out=Gt[:, 12:16], in0=Gt[:, 12:16], scalar1=-1.0, scalar2=None, op0=ALU.mult), R=[Gb], W=[Gb])
                    k.op("pe", lambda: nc.tensor.matmul(pgA[:, 0:4], lhsT=tri, rhs=Gt[:, 12:16], start=True, stop=True), R=[Gb, cstb], W=[pgAb])
                    k.op("pe", lambda: nc.tensor.matmul(pgB[:, 0:4], lhsT=ones, rhs=Gt[:, 12:16], start=True, stop=True), R=[Gb, cstb], W=[pgBb])
                    k.op("dve", lambda: nc.vector.tensor_tensor(out=Gt[:, 16:20], in0=Gt[:, 0:4], in1=pgA[:, 0:4], op=ALU.subtract), R=[Gb, pgAb], W=[Gb])
                    k.op("dve", lambda: nc.vector.tensor_tensor(out=Gt[:, 20:24], in0=Gt[:, 16:20], in1=pgB[:, 0:4], op=ALU.add), R=[Gb, pgBb], W=[Gb])
                    k.op("act", lambda: nc.scalar.activation(out=Gt[:, 20:24], in_=Gt[:, 20:24], func=AF.Exp), R=[Gb], W=[Gb])
                    k.op("act", lambda: nc.scalar.activation(out=Gt[:, 24:28], in_=pgB[:, 0:4], func=AF.Exp), R=[pgBb], W=[Gb])
                    k.op("act", lambda: nc.scalar.activation(out=Gt[:, 28:32], in_=pgA[:, 0:4], func=AF.Exp), R=[pgAb], W=[Gb])
                    y, yb = yr.next()
                    for h in range(4):
                        rh, rhb = rhr.next()
                        k.op("pool", lambda: nc.gpsimd.tensor_scalar(out=rh[:], in0=tri, scalar1=Gt[:, 12 + h:13 + h], scalar2=None, op0=ALU.mult), R=[Gb, cstb], W=[rhb])
                        k.op("pe", lambda: nc.tensor.matmul(pBbc[:, 0:128], lhsT=ones, rhs=rh[:], start=True, stop=True), R=[rhb, cstb], W=[pBbcb])
                        Et, Eb = Er.next()
                        k.op("act", lambda: nc.scalar.activation(out=Et[:], in_=pBbc[:, 0:128], func=AF.Exp, bias=Gt[:, 16 + h:17 + h]), R=[pBbcb, Gb], W=[Eb])
                        k.op("pool", lambda: nc.gpsimd.tensor_tensor(out=Et[:], in0=Et[:], in1=tri, op=ALU.mult), R=[Eb, cstb], W=[Eb])
                        k.op("pe", lambda: nc.tensor.matmul(pQK[:, 0:128], lhsT=qk[:, 4 + h, :], rhs=qk[:, h, :], start=True, stop=True), R=[qkb], W=[pQKb])
                        ST, STb = STr.next()
                        k.op("dve", lambda: nc.vector.tensor_tensor(out=ST[:], in0=pQK[:, 0:128], in1=Et[:], op=ALU.mult), R=[pQKb, Eb], W=[STb])
                        k.op("pe", lambda: nc.tensor.matmul(pN1[:, 0:129], lhsT=ST[:], rhs=vt[:, h, :], start=True, stop=True), R=[STb, vb], W=[pN1b])
                        k.op("pe", lambda: nc.tensor.matmul(pN2[:, 0:129], lhsT=qk[:, h, :], rhs=CT[h][:], start=True, stop=True), R=[qkb, CTb[h]], W=[pN2b])
                        n1, n1b = n1r.next()
                        k.op("act", lambda: nc.scalar.activation(out=n1[:], in_=pN2[:, 0:129], func=AF.Copy, scale=Gt[:, 28 + h:29 + h]), R=[pN2b, Gb], W=[n1b])
                        n2, n2b = n2r.next()
                        k.op("dve", lambda: nc.vector.tensor_tensor(out=n2[:], in0=pN1[:, 0:129], in1=n1[:], op=ALU.add), R=[pN1b, n1b], W=[n2b])
                        sm, smb = smr.next()
                        k.op("act", lambda: nc.scalar.activation(out=sm[:, 0:1], in_=n2[:, 128:129], func=AF.Abs), R=[n2b], W=[smb])
                        k.op("dve", lambda: nc.vector.tensor_scalar_max(out=sm[:, 0:1], in0=sm[:, 0:1], scalar1=1.0), R=[smb], W=[smb])
                        k.op("dve", lambda: nc.vector.reciprocal(out=sm[:, 0:1], in_=sm[:, 0:1]), R=[smb], W=[smb])
                        hh, hhb = hhr.next()
                        k.op("dve", lambda: nc.vector.tensor_scalar(out=hh[:], in0=n2[:, 0:128], scalar1=sm[:, 0:1], scalar2=None, op0=ALU.mult), R=[n2b, smb], W=[hhb])
                        k.op("dve", lambda: nc.vector.bn_stats(out=sm[:, 2:2 + NSD], in_=hh[:]), R=[hhb], W=[smb])
                        k.op("dve", lambda: nc.vector.bn_aggr(out=sm[:, 10:12], in_=sm[:, 2:2 + NSD]), R=[smb], W=[smb])
                        rstd(sm[:, 12:13], sm[:, 11:12], 1.0, C_EPS, [smb], [smb])
                        k.op("dve", lambda: nc.vector.tensor_scalar(out=y[:, h * 128:(h + 1) * 128], in0=hh[:], scalar1=sm[:, 10:11], scalar2=sm[:, 12:13], op0=ALU.subtract, op1=ALU.mult),
                             R=[hhb, smb], W=[yb])
                        k.op("pe", lambda: nc.tensor.transpose(pKT[:, 0:128], qk[:, 4 + h, :], ident), R=[qkb, cstb], W=[pKTb])
                        kw, kwb = kwr.next()
                        k.op("act", lambda: nc.scalar.activation(out=kw[:], in_=pKT[:, 0:128], func=AF.Copy, scale=Gt[:, 20 + h:21 + h]), R=[pKTb, Gb], W=[kwb])
                        k.op("pe", lambda: nc.tensor.matmul(pdC[:, 0:129], lhsT=kw[:], rhs=vt[:, h, :], start=True, stop=True), R=[kwb, vb], W=[pdCb])
                        k.op("dve", lambda: nc.vector.scalar_tensor_tensor(out=CT[h][:], in0=CT[h][:], scalar=Gt[:, 24 + h:25 + h], in1=pdC[:, 0:129], op0=ALU.mult, op1=ALU.add),
                             R=[CTb[h], Gb, pdCb], W=[CTb[h]])
                    gg, ggb = ggr.next()
                    k.op("act", lambda: nc.scalar.activation(out=gg[:, 0:512], in_=oz[:, 0:512], func=AF.Sigmoid), R=[ozb], W=[ggb])
                    k.op("act", lambda: nc.scalar.activation(out=gg[:, 512:1024], in_=oz[:, 512:1024], func=AF.Silu), R=[ozb], W=[ggb])
                    k.op("pool", lambda: nc.gpsimd.tensor_tensor(out=gg[:, 0:512], in0=gg[:, 0:512], in1=gg[:, 512:1024], op=ALU.mult), R=[ggb], W=[ggb])
                    k.op("pool", lambda: nc.gpsimd.tensor_tensor(out=gg[:, 0:512], in0=gg[:, 0:512], in1=gbc[:], op=ALU.mult), R=[ggb, pb_], W=[ggb])
                    k.op("dve", lambda: nc.vector.tensor_tensor(out=y[:], in0=y[:], in1=gg[:, 0:512], op=ALU.mult), R=[yb, ggb], W=[yb])
                    mt, mtb = mtr.next()
                    for j in range(4):
                        k.op("pe", lambda: nc.tensor.transpose(pKT[:, 128 + j * 64:128 + j * 64 + 64] if False else pKT[:, 0:128], y[:, j * 128:(j + 1) * 128], ident), R=[yb, cstb], W=[pKTb])
                        k.op("act", lambda: nc.scalar.copy(out=mt[:, j, :], in_=pKT[:, 0:128]), R=[pKTb], W=[mtb])
                    k.dma("pool", mixT.rearrange("(cc p) t -> p cc t", p=128)[:, 0:4, t0:t0 + 128], mt[:], R=[mtb])
                k.barrier()

            with ExitStack() as es:
                SB = lambda name, shape: es.enter_context(sbt(nc, name, shape, F32))
                PS = lambda name: es.enter_context(pst(nc, name, [128, 512], F32))
                tri = cst[:, C_TRI:C_TRI + 128]
                wuq = SB("a_wuq", [128, 4, 1536]); wukv = SB("a_wukv", [128, 2, 2048]); gq = SB("a_gq", [128, 6])
                wb_ = Buf()
                k.dma("sp", wuq[:], mla_w_uq[l].rearrange("(c p) n -> p c n", p=128), W=[wb_])
                k.dma("sp", wukv[:], mla_w_ukv[l].rearrange("(c p) n -> p c n", p=128), W=[wb_])
                k.dma("sp", gq[:, 0:4], mla_q_norm_g[l].rearrange("(c p) -> p c", p=128), W=[wb_], slow=True)
                k.dma("sp", gq[:, 4:6], mla_kv_norm_g[l].rearrange("(c p) -> p c", p=128), W=[wb_], slow=True)
                xcr = Ring(es, nc, "a_xc", [128, 6, 128], F32, 2)
                sqr = Ring(es, nc, "a_sq", [128, 6, 128], F32, 2)
                rbr = Ring(es, nc, "a_rb", [128, 2, 128], F32, 2)
                cnr = Ring(es, nc, "a_cn", [128, 6, 128], F32, 2)
                csr = Ring(es, nc, "a_cs", [128, 64], F32, 2)
                kxr = Ring(es, nc, "a_kx", [128, 64], F32, 2)
                stq = Ring(es, nc, "a_stq", [128, 4, 128], BF16, 4)
                tmr = Ring(es, nc, "a_tm", [128, 8, 32], F32, 4)
                qrr = Ring(es, nc, "a_qr", [128, 8, 64], F32, 2)
                krr = Ring(es, nc, "a_kr", [128, 64], F32, 2)
                kst = Ring(es, nc, "a_kst", [64, 128], BF16, 2)
                var_ = Ring(es, nc, "a_va", [128, 8, 129], BF16, 2)
                for (vt, vb) in var_.items:
                    k.op("pool", lambda: nc.gpsimd.memset(vt[:, :, 128:129], 1.0), W=[vb])
                pS1 = PS("a_pS1"); pS2 = PS("a_pS2"); pS1b = Buf(); pS2b = Buf()
                pq = Ring(es, nc, "a_pq", [128, 512], F32, 4, psum=True)
                qn_v = qnT.rearrange("(h p) t -> p h t", p=128)
                kn_v = knT.rearrange("(h p) t -> p h t", p=128)
                qr_v = qrT.rearrange("(b p) t -> p b t", p=128)
                for c in range(NT):
                    t0 = c * 128
                    xc, xcb = xcr.next()
                    k.dma("sp", xc[:], pFM.rearrange("(ch p) t -> p ch t", p=128)[:, FM_CQ:FM_CQ + 6, t0:t0 + 128], W=[xcb])
                    cs_, csb = csr.next()
                    k.dma("sp", cs_[:], csd[t0:t0 + 128, :], W=[csb])
                    kx, kxb = kxr.next()
                    k.dma("sp", kx[:], pTM[t0:t0 + 128, TM_KR:TM_KR + 64], W=[kxb])
                    sq, sqb = sqr.next()
                    k.op("act", lambda: nc.scalar.activation(out=sq[:], in_=xc[:], func=AF.Square), R=[xcb], W=[sqb])
                    for ch in range(4):
                        k.op("pe", lambda: nc.tensor.matmul(pS1[:, 0:128], lhsT=ones, rhs=sq[:, ch, :], start=(ch == 0), stop=(ch == 3)), R=[sqb, cstb], W=[pS1b])
                    for ch in range(2):
                        k.op("pe", lambda: nc.tensor.matmul(pS2[:, 0:128], lhsT=ones, rhs=sq[:, 4 + ch, :], start=(ch == 0), stop=(ch == 1)), R=[sqb, cstb], W=[pS2b])
                    rb, rbb = rbr.next()
                    rstd(rb[:, 0, :], pS1[:, 0:128], 1.0 / 512, C_EPS, [pS1b], [rbb])
                    rstd(rb[:, 1, :], pS2[:, 0:128], 1.0 / 256, C_EPS, [pS2b], [rbb])
                    cn, cnb = cnr.next()
                    for ch in range(6):
                        k.op("dve", lambda: nc.vector.scalar_tensor_tensor(out=cn[:, ch, :], in0=xc[:, ch, :], scalar=gq[:, ch:ch + 1], in1=rb[:, 0 if ch < 4 else 1, :], op0=ALU.mult, op1=ALU.mult),
                             R=[xcb, wb_, rbb], W=[cnb])
                    for (W_, nch, c0_, hs, dst) in ((wuq, 4, 0, 192, qn_v), (wukv, 2, 4, 256, kn_v)):
                        for b in range(2):
                            p, pb = pq.next()
                            for hh_ in range(4):
                                h = b * 4 + hh_
                                for ch in range(nch):
                                    k.op("pe", lambda: nc.tensor.matmul(p[:, hh_ * 128:(hh_ + 1) * 128], lhsT=W_[:, ch, h * hs:h * hs + 128], rhs=cn[:, c0_ + ch, :],
                                                                        start=(ch == 0), stop=(ch == nch - 1)), R=[wb_, cnb], W=[pb])
                            st, sb = stq.next()
                            k.op("act", lambda: nc.scalar.copy(out=st[:], in_=p[:].rearrange("p (a b) -> p a b", a=4)), R=[pb], W=[sb])
                            k.dma("pool", dst[:, b * 4:(b + 1) * 4, t0:t0 + 128], st[:], R=[sb])
                    va, vab = var_.next()
                    for b in range(2):
                        p, pb = pq.next()
                        for ch in range(2):
                            k.op("pe", lambda: nc.tensor.matmul(p[:], lhsT=cn[:, 4 + ch, :], rhs=wukv[:, ch, :].rearrange("p (h d) -> p h d", d=256)[:, b * 4:(b + 1) * 4, 128:256],
                                                                start=(ch == 0), stop=(ch == 1)), R=[wb_, cnb], W=[pb])
                        k.op("act", lambda: nc.scalar.copy(out=va[:, b * 4:(b + 1) * 4, 0:128], in_=p[:].rearrange("p (a b) -> p a b", a=4)), R=[pb], W=[vab])
                    k.dma("pool", vaug.rearrange("h t d -> t h d")[t0:t0 + 128], va[:], R=[vab])
                    p, pb = pq.next()
                    for ch in range(4):
                        k.op("pe", lambda: nc.tensor.matmul(p[:], lhsT=cn[:, ch, :], rhs=wuq[:, ch, :].rearrange("p (h d) -> p h d", d=192)[:, :, 128:192],
                                                            start=(ch == 0), stop=(ch == 3)), R=[wb_, cnb], W=[pb])
                    pv = p[:].rearrange("p (h d) -> p h d", d=64)
                    sin8 = cs_[:, 0:32].unsqueeze(1).to_broadcast([128, 8, 32])
                    cos8 = cs_[:, 32:64].unsqueeze(1).to_broadcast([128, 8, 32])
                    qr, qrb = qrr.next()
                    t1, t1b = tmr.next(); t2, t2b = tmr.next(); t3, t3b = tmr.next(); t4, t4b = tmr.next()
                    k.op("dve", lambda: nc.vector.tensor_tensor(out=t1[:], in0=pv[:, :, 0:32], in1=cos8, op=ALU.mult), R=[pb, csb], W=[t1b])
                    k.op("dve", lambda: nc.vector.tensor_tensor(out=t2[:], in0=pv[:, :, 32:64], in1=sin8, op=ALU.mult), R=[pb, csb], W=[t2b])
                    k.op("dve", lambda: nc.vector.tensor_tensor(out=t3[:], in0=pv[:, :, 32:64], in1=cos8, op=ALU.mult), R=[pb, csb], W=[t3b])
                    k.op("dve", lambda: nc.vector.tensor_tensor(out=t4[:], in0=pv[:, :, 0:32], in1=sin8, op=ALU.mult), R=[pb, csb], W=[t4b])
                    k.op("pool", lambda: nc.gpsimd.tensor_tensor(out=qr[:, :, 0:32], in0=t1[:], in1=t2[:], op=ALU.subtract), R=[t1b, t2b], W=[qrb])
                    k.op("pool", lambda: nc.gpsimd.tensor_tensor(out=qr[:, :, 32:64], in0=t3[:], in1=t4[:], op=ALU.add), R=[t3b, t4b], W=[qrb])
                    p, pb = pq.next()
                    for b in range(4):
                        k.op("pe", lambda: nc.tensor.transpose(p[:, b * 128:(b + 1) * 128], qr[:, 2 * b:2 * b + 2, :].rearrange("p a d -> p (a d)"), ident), R=[qrb, cstb], W=[pb])
                    st, sb = stq.next()
                    k.op("act", lambda: nc.scalar.copy(out=st[:], in_=p[:].rearrange("p (a b) -> p a b", a=4)), R=[pb], W=[sb])
                    k.dma("pool", qr_v[:, :, t0:t0 + 128], st[:], R=[sb])
                    kr_, krb = krr.next()
                    t1, t1b = tmr.next(); t2, t2b = tmr.next()
                    k.op("dve", lambda: nc.vector.tensor_tensor(out=t1[:, 0, :], in0=kx[:, 0:32], in1=cs_[:, 32:64], op=ALU.mult), R=[kxb, csb], W=[t1b])
                    k.op("dve", lambda: nc.vector.tensor_tensor(out=t1[:, 1, :], in0=kx[:, 32:64], in1=cs_[:, 0:32], op=ALU.mult), R=[kxb, csb], W=[t1b])
                    k.op("dve", lambda: nc.vector.tensor_tensor(out=t2[:, 0, :], in0=kx[:, 32:64], in1=cs_[:, 32:64], op=ALU.mult), R=[kxb, csb], W=[t2b])
                    k.op("dve", lambda: nc.vector.tensor_tensor(out=t2[:, 1, :], in0=kx[:, 0:32], in1=cs_[:, 0:32], op=ALU.mult), R=[kxb, csb], W=[t2b])
                    k.op("pool", lambda: nc.gpsimd.tensor_tensor(out=kr_[:, 0:32], in0=t1[:, 0, :], in1=t1[:, 1, :], op=ALU.subtract), R=[t1b], W=[krb])
                    k.op("pool", lambda: nc.gpsimd.tensor_tensor(out=kr_[:, 32:64], in0=t2[:, 0, :], in1=t2[:, 1, :], op=ALU.add), R=[t2b], W=[krb])
                    p, pb = pq.next()
                    k.op("pe", lambda: nc.tensor.transpose(p[0:64, 0:128], kr_[:], ident), R=[krb, cstb], W=[pb])
                    ks, ksb = kst.next()
                    k.op("act", lambda: nc.scalar.copy(out=ks[:], in_=p[0:64, 0:128]), R=[pb], W=[ksb])
                    k.dma("pool", krT[:, t0:t0 + 128], ks[:], R=[ksb])
                k.barrier()

            with ExitStack() as es:
                SB = lambda name, shape: es.enter_context(sbt(nc, name, shape, F32))
                tri = cst[:, C_TRI:C_TRI + 128]
                SBh = lambda name, shape: es.enter_context(sbt(nc, name, shape, BF16))
                krt = SBh("b_krt", [64, T]); krtb = Buf()
                k.dma("sp", krt[:], krT, W=[krtb])
                knh = SBh("b_kn", [128, T]); qnh = SBh("b_qn", [128, T]); qrh = SBh("b_qr", [64, T])
                vah = SBh("b_va", [128, NT, 129]); zh = SB("b_z", [128, NT, 128])
                trib = SBh("b_tri", [128, 128]); tribb = Buf()
                k.op("dve", lambda: nc.vector.tensor_copy(out=trib[:], in_=tri), R=[cstb], W=[tribb])
                knb, qnb, qrb, vahb, zhb = [Buf() for _ in range(5)]
                Ptr = Ring(es, nc, "b_P", [128, 512], BF16, 3)
                smr = Ring(es, nc, "b_sm", [128, 2], F32, 2)
                ytr = Ring(es, nc, "b_y", [128, 128], F32, 2)
                szr = Ring(es, nc, "b_sz", [128, 128], F32, 2)
                sty = Ring(es, nc, "b_sty", [128, 128], BF16, 3)
                pST = Ring(es, nc, "b_pST", [128, 512], F32, 3, psum=True)
                pO = Ring(es, nc, "b_pO", [128, 512], F32, 2, psum=True)
                pX = Ring(es, nc, "b_pX", [128, 512], F32, 2, psum=True)
                SCL = float(192 ** -0.5)
                for h in range(8):
                    k.dma("sp", knh[:], knT[h * 128:(h + 1) * 128, :], W=[knb])
                    k.dma("sp", qnh[:], qnT[h * 128:(h + 1) * 128, :], W=[qnb])
                    k.dma("sp", qrh[:], qrT[h * 64:(h + 1) * 64, :], W=[qrb])
                    k.dma("sp", vah[:], vaug[h].rearrange("(n p) d -> p n d", p=128), W=[vahb])
                    k.dma("sp", zh[:], pTM[:, TM_MZ + h * 128:TM_MZ + (h + 1) * 128].rearrange("(n p) d -> p n d", p=128), W=[zhb])
                    for qt in range(NT):
                        qs = slice(qt * 128, (qt + 1) * 128)
                        po, pob_ = pO.next()
                        for kb in range(0, qt + 1, 4):
                            nk = min(4, qt + 1 - kb)
                            ps_, psb = pST.next()
                            for i in range(nk):
                                j = kb + i
                                js = slice(j * 128, (j + 1) * 128)
                                k.op("pe", lambda: nc.tensor.matmul(ps_[:, i * 128:(i + 1) * 128], lhsT=knh[:, js], rhs=qnh[:, qs], start=True, stop=False), R=[knb, qnb], W=[psb])
                                k.op("pe", lambda: nc.tensor.matmul(ps_[:, i * 128:(i + 1) * 128], lhsT=krt[:, js], rhs=qrh[:, qs], start=False, stop=True), R=[krtb, qrb], W=[psb])
                            Pt, Ptb = Ptr.next()
                            k.op("act", lambda: nc.scalar.activation(out=Pt[:, 0:nk * 128], in_=ps_[:, 0:nk * 128], func=AF.Exp, scale=SCL), R=[psb], W=[Ptb])
                            if kb + nk - 1 == qt:
                                i = nk - 1
                                k.op("pool", lambda: nc.gpsimd.tensor_tensor(out=Pt[:, i * 128:(i + 1) * 128], in0=Pt[:, i * 128:(i + 1) * 128], in1=trib[:], op=ALU.mult), R=[Ptb, tribb], W=[Ptb])
                            for i in range(nk):
                                j = kb + i
                                k.op("pe", lambda: nc.tensor.matmul(po[:, 0:129], lhsT=Pt[:, i * 128:(i + 1) * 128], rhs=vah[:, j, :], start=(j == 0), stop=(j == qt)), R=[Ptb, vahb], W=[pob_])
                        sm, smb = smr.next()
                        k.op("dve", lambda: nc.vector.reciprocal(out=sm[:, 0:1], in_=po[:, 128:129]), R=[pob_], W=[smb])
                        yt, ytb = ytr.next()
                        k.op("dve", lambda: nc.vector.tensor_scalar(out=yt[:], in0=po[:, 0:128], scalar1=sm[:, 0:1], scalar2=None, op0=ALU.mult), R=[pob_, smb], W=[ytb])
                        sz, szb = szr.next()
                        k.op("act", lambda: nc.scalar.activation(out=sz[:], in_=zh[:, qt, :], func=AF.Silu), R=[zhb], W=[szb])
                        k.op("pool", lambda: nc.gpsimd.tensor_tensor(out=yt[:], in0=yt[:], in1=sz[:], op=ALU.mult), R=[ytb, szb], W=[ytb])
                        px, pxb = pX.next()
                        k.op("pe", lambda: nc.tensor.transpose(px[:, 0:128], yt[:], ident), R=[ytb, cstb], W=[pxb])
                        st, sb = sty.next()
                        k.op("dve", lambda: nc.vector.tensor_copy(out=st[:], in_=px[:, 0:128]), R=[pxb], W=[sb])
                        k.dma("pool", mixT[512 + h * 128:512 + (h + 1) * 128, qs], st[:], R=[sb])
                k.barrier()

            NCH = T // 64
            with ExitStack() as es:
                SB = lambda name, shape: es.enter_context(sbt(nc, name, shape, F32))
                mu = SB("r_mu", [128, 13]); prm = SB("r_prm", [128, 6, 4]); w0bc = SB("r_w0", [128, 512])
                w2t = SB("r_w2", [64, 512]); a2t = SB("r_a2", [128, 512])
                v1t = SB("r_v1", [128, 4, 32]); v2t = SB("r_v2", [32, 512])
                prb = Buf()
                k.dma("sp", mu[:], rw_mu[l].rearrange("(c p) -> p c", p=128), W=[prb], slow=True)
                plist = [rw_a0[l], rw_k_k[l], rw_k_a[l], rw_r_k[l].rearrange("h d -> (h d)")]
                if l > 0:
                    plist.append(rw_v0[l - 1])
                for i_, src in enumerate(plist):
                    k.dma("sp", prm[:, i_, :], src.rearrange("(c p) -> p c", p=128), W=[prb], slow=True)
                k.dma("sp", w0bc[:], rw_w0[l].partition_broadcast(128), W=[prb])
                k.dma("sp", w2t[:], rw_w2[l], W=[prb])
                k.dma("sp", a2t[64:128, :], rw_a2[l], W=[prb])
                if l > 0:
                    k.dma("sp", v1t[:], rw_v1[l - 1].rearrange("(c p) n -> p c n", p=128), W=[prb])
                    k.dma("sp", v2t[:], rw_v2[l - 1], W=[prb])
                k.op("dve", lambda: nc.vector.tensor_scalar(out=prm[:, 5, :], in0=prm[:, 2, :], scalar1=-1.0, scalar2=1.0, op0=ALU.mult, op1=ALU.add), R=[prb], W=[prb])
                bc = lambda ap_: ap_.unsqueeze(2).to_broadcast([128, 4, 128])
                Xr = Ring(es, nc, "r_X", [128, 13, 129], F32, 2)
                dr = Ring(es, nc, "r_d", [128, 13, 128], F32, 1)
                xsr = Ring(es, nc, "r_xs", [128, 13, 128], F32, 2)
                twr = Ring(es, nc, "r_tw", [64, 128], F32, 2)
                ldr = Ring(es, nc, "r_ld", [128, 512], F32, 2)
                T4 = lambda name, n=1: Ring(es, nc, name, [128, 4, 128], F32, n)
                gir, ger, aar, kkr_, khr, bvr = T4("r_gi"), T4("r_ge"), T4("r_aa"), T4("r_kk"), T4("r_kh"), T4("r_bv")
                e1r, e2r, e3r, e4r = T4("r_e1"), T4("r_e2"), T4("r_e3"), T4("r_e4")
                tmpr = T4("r_tmp", 3)
                outr = T4("r_out", 4)
                vfr = T4("r_vf", 2)
                m1r = Ring(es, nc, "r_m1", [32, 128], F32, 2)
                tmo = Ring(es, nc, "r_tmo", [128, 512], F32, 3)
                rkro = Ring(es, nc, "r_rkr", [128, 8], F32, 2)
                glo = Ring(es, nc, "r_glo", [128, 4, 2], F32, 2)
                pr = Ring(es, nc, "r_ps", [128, 512], F32, 8, psum=True)
                fmv = lambda dt_: dt_.rearrange("(j p) t -> p j t", p=128)
                for c in range(NT):
                    t0 = c * 128
                    X, Xb = Xr.next()
                    src = pFM.rearrange("(ch p) t -> p ch t", p=128)
                    if c == 0:
                        k.op("pool", lambda: nc.gpsimd.memset(X[:, :, 0:1], 0.0), W=[Xb])
                        k.dma("sp", X[:, :, 1:129], src[:, FM_RW:FM_RW + 13, 0:128], W=[Xb])
                    else:
                        k.dma("sp", X[:], src[:, FM_RW:FM_RW + 13, t0 - 1:t0 + 128], W=[Xb])
                    d_, db = dr.next()
                    k.op("dve", lambda: nc.vector.tensor_tensor(out=d_[:], in0=X[:, :, 0:128], in1=X[:, :, 1:129], op=ALU.subtract), R=[Xb], W=[db])
                    xs_, xsb = xsr.next()
                    for ch in range(13):
                        k.op("dve", lambda: nc.vector.scalar_tensor_tensor(out=xs_[:, ch, :], in0=d_[:, ch, :], scalar=mu[:, ch:ch + 1], in1=X[:, ch, 1:129], op0=ALU.mult, op1=ALU.add),
                             R=[db, Xb, prb], W=[xsb])
                    rr = xs_[:, 0:4, :]; kx = xs_[:, 4:8, :]; vv = xs_[:, 8:12, :]
                    tw, twb = twr.next()
                    k.op("act", lambda: nc.scalar.activation(out=tw[:], in_=xs_[0:64, 12, :], func=AF.Tanh), R=[xsb], W=[twb])
                    pz, pzb = pr.next()
                    k.op("pe", lambda: nc.tensor.matmul(pz[:], lhsT=tw[:], rhs=w2t[:], start=True, stop=True), R=[twb, prb], W=[pzb])
                    ld, ldb = ldr.next()
                    k.op("dve", lambda: nc.vector.tensor_tensor(out=ld[:], in0=pz[:], in1=w0bc[:], op=ALU.add), R=[pzb, prb], W=[ldb])
                    k.op("act", lambda: nc.scalar.activation(out=ld[:], in_=ld[:], func=AF.Sigmoid), R=[ldb], W=[ldb])
                    k.op("pool", lambda: nc.gpsimd.tensor_scalar(out=ld[:], in0=ld[:], scalar1=float(-np.exp(-0.5)), scalar2=None, op0=ALU.mult), R=[ldb], W=[ldb])
                    pgi, pgib = pr.next(); pge, pgeb = pr.next()
                    for j in range(4):
                        k.op("pe", lambda: nc.tensor.matmul(pgi[:, j * 128:(j + 1) * 128], lhsT=ld[:, j * 128:(j + 1) * 128], rhs=cst[:, C_BI:C_BI + 128], start=True, stop=True), R=[ldb, cstb], W=[pgib])
                        k.op("pe", lambda: nc.tensor.matmul(pge[:, j * 128:(j + 1) * 128], lhsT=ld[:, j * 128:(j + 1) * 128], rhs=cst[:, C_BS:C_BS + 128], start=True, stop=True), R=[ldb, cstb], W=[pgeb])
                    gi, gib = gir.next(); ge, geb = ger.next()
                    k.op("act", lambda: nc.scalar.copy(out=gi[:], in_=pgi[:].rearrange("p (a b) -> p a b", a=4)), R=[pgib], W=[gib])
                    e1, e1b = e1r.next(); e2, e2b = e2r.next(); e3, e3b = e3r.next(); e4, e4b = e4r.next()
                    k.op("act", lambda: nc.scalar.activation(out=e1[:], in_=gi[:], func=AF.Exp), R=[gib], W=[e1b])
                    k.op("act", lambda: nc.scalar.activation(out=e2[:], in_=pge[:].rearrange("p (a b) -> p a b", a=4), func=AF.Exp), R=[pgeb], W=[e2b])
                    k.op("act", lambda: nc.scalar.activation(out=e3[:], in_=gi[:], func=AF.Exp, scale=-1.0), R=[gib], W=[e3b])
                    for j in range(4):
                        for hf in range(2):
                            k.op("act", lambda: nc.scalar.activation(out=e4[:, j, hf * 64:(hf + 1) * 64], in_=gi[:, j, hf * 64:(hf + 1) * 64], func=AF.Exp, scale=-1.0,
                                                                     bias=gi[:, j, hf * 64 + 63:hf * 64 + 64]), R=[gib], W=[e4b])
                    pa, pab = pr.next()
                    for j in range(4):
                        k.op("pe", lambda: nc.tensor.matmul(pa[:, j * 128:(j + 1) * 128], lhsT=a2t[64:128, j * 128:(j + 1) * 128], rhs=xs_[64:128, 12, :], start=True, stop=True), R=[xsb, prb], W=[pab])
                    aa, aab = aar.next()
                    for j in range(4):
                        k.op("act", lambda: nc.scalar.activation(out=aa[:, j, :], in_=pa[:, j * 128:(j + 1) * 128], func=AF.Sigmoid, bias=prm[:, 0, j:j + 1]), R=[pab, prb], W=[aab])
                    if l > 0:
                        pm, pmb = pr.next()
                        for ch in range(4):
                            k.op("pe", lambda: nc.tensor.matmul(pm[0:32, 0:128], lhsT=v1t[:, ch, :], rhs=xs_[:, 8 + ch, :], start=(ch == 0), stop=(ch == 3)), R=[xsb, prb], W=[pmb])
                        m1, m1b = m1r.next()
                        k.op("act", lambda: nc.scalar.copy(out=m1[:], in_=pm[0:32, 0:128]), R=[pmb], W=[m1b])
                        pm2, pm2b = pr.next()
                        for j in range(4):
                            k.op("pe", lambda: nc.tensor.matmul(pm2[:, j * 128:(j + 1) * 128], lhsT=v2t[:, j * 128:(j + 1) * 128], rhs=m1[:], start=True, stop=True), R=[m1b, prb], W=[pm2b])
                        gt_, gtb_ = tmpr.next()
                        for j in range(4):
                            k.op("act", lambda: nc.scalar.activation(out=gt_[:, j, :], in_=pm2[:, j * 128:(j + 1) * 128], func=AF.Sigmoid, bias=prm[:, 4, j:j + 1]), R=[pm2b, prb], W=[gtb_])
                        vf, vfb = vfr.next()
                        k.dma("sp", vf[:], fmv(vfirst)[:, :, t0:t0 + 128], W=[vfb])
                        k.op("dve", lambda: nc.vector.tensor_tensor(out=vf[:], in0=vf[:], in1=vv, op=ALU.subtract), R=[vfb, xsb], W=[vfb])
                        k.op("pool", lambda: nc.gpsimd.tensor_tensor(out=vf[:], in0=vf[:], in1=gt_[:], op=ALU.mult), R=[vfb, gtb_], W=[vfb])
                        k.op("dve", lambda: nc.vector.tensor_tensor(out=xs_[:, 8:12, :], in0=vv, in1=vf[:], op=ALU.add), R=[vfb, xsb], W=[xsb])
                    else:
                        k.dma("pool", fmv(vfirst)[:, :, t0:t0 + 128], vv, R=[xsb])
                    kk, kkb = kkr_.next()
                    k.op("dve", lambda: nc.vector.tensor_tensor(out=kk[:], in0=kx, in1=bc(prm[:, 1, :]), op=ALU.mult), R=[xsb, prb], W=[kkb])
                    sq_, sqb_ = tmpr.next()
                    k.op("act", lambda: nc.scalar.activation(out=sq_[:], in_=kk[:], func=AF.Square), R=[kkb], W=[sqb_])
                    pn, pnb = pr.next()
                    for j in range(4):
                        k.op("pe", lambda: nc.tensor.matmul(pn[:, j * 128:(j + 1) * 128], lhsT=cst[:, C_BD:C_BD + 128], rhs=sq_[:, j, :], start=True, stop=True), R=[sqb_, cstb], W=[pnb])
                    rn, rnb = tmpr.next()
                    k.op("act", lambda: nc.scalar.activation(out=rn[:], in_=pn[:].rearrange("p (a b) -> p a b", a=4), func=AF.Sqrt), R=[pnb], W=[rnb])
                    k.op("dve", lambda: nc.vector.tensor_scalar_max(out=rn[:], in0=rn[:], scalar1=1e-12), R=[rnb], W=[rnb])
                    k.op("dve", lambda: nc.vector.reciprocal(out=rn[:], in_=rn[:]), R=[rnb], W=[rnb])
                    k.op("pool", lambda: nc.gpsimd.tensor_tensor(out=kk[:], in0=kk[:], in1=rn[:], op=ALU.mult), R=[kkb, rnb], W=[kkb])
                    kh, khb = khr.next()
                    k.op("dve", lambda: nc.vector.tensor_tensor(out=kh[:], in0=aa[:], in1=bc(prm[:, 2, :]), op=ALU.mult), R=[aab, prb], W=[khb])
                    k.op("dve", lambda: nc.vector.tensor_tensor(out=kh[:], in0=kh[:], in1=bc(prm[:, 5, :]), op=ALU.add), R=[khb, prb], W=[khb])
                    k.op("dve", lambda: nc.vector.tensor_tensor(out=kh[:], in0=kh[:], in1=kx, op=ALU.mult), R=[khb, xsb], W=[khb])
                    bv, bvb = bvr.next()
                    k.op("pool", lambda: nc.gpsimd.tensor_tensor(out=bv[:], in0=kk[:], in1=aa[:], op=ALU.mult), R=[kkb, aab], W=[bvb])
                    def emit(dst, in0, in1, neg=False, R=()):
                        o, ob = outr.next()
                        k.op("dve", lambda: nc.vector.tensor_tensor(out=o[:], in0=in0, in1=in1, op=ALU.mult), R=list(R), W=[ob])
                        if neg:
                            k.op("pool", lambda: nc.gpsimd.tensor_scalar(out=o[:], in0=o[:], scalar1=-1.0, scalar2=None, op0=ALU.mult), R=[ob], W=[ob])
                        k.dma("pool", fmv(dst)[:, :, t0:t0 + 128], o[:], R=[ob])
                        return o, ob
                    emit(rwA, kk[:], e2[:], neg=True, R=[kkb, e2b])
                    emit(rwR, rr, e1[:], R=[xsb, e1b])
                    emit(rwB, bv[:], e3[:], R=[bvb, e3b])
                    emit(rwK, kh[:], e3[:], R=[khb, e3b])
                    for (dst, a_, ab_) in ((rwBp, bv, bvb), (rwKp, kh, khb), (rwV, None, None)):
                        if a_ is not None:
                            o, ob = outr.next()
                            k.op("dve", lambda: nc.vector.tensor_tensor(out=o[:], in0=a_[:], in1=e4[:], op=ALU.mult), R=[ab_, e4b], W=[ob])
                            srcv = o
                        else:
                            srcv, ob = xs_[:, 8:12, :], xsb
                        pt_, ptb_ = pr.next()
                        for j in range(4):
                            k.op("pe", lambda: nc.tensor.transpose(pt_[:, j * 128:(j + 1) * 128], srcv[:, j, :], ident), R=[ob, cstb], W=[ptb_])
                        to, tob = tmo.next()
                        k.op("act", lambda: nc.scalar.copy(out=to[:], in_=pt_[:]), R=[ptb_], W=[tob])
                        k.dma("pool", dst[t0:t0 + 128, :], to[:], R=[tob])
                    go_, gob = glo.next()
                    k.op("pool", lambda: nc.gpsimd.tensor_copy(out=go_[:, :, 0:1], in_=e1[:, :, 63:64]), R=[e1b], W=[gob])
                    k.op("pool", lambda: nc.gpsimd.tensor_copy(out=go_[:, :, 1:2], in_=e1[:, :, 127:128]), R=[e1b], W=[gob])
                    k.dma("pool", rwGL.rearrange("(j p) c -> p j c", p=128)[:, :, 2 * c:2 * c + 2], go_[:], R=[gob], slow=True)
                    pd_, pdb_ = tmpr.next()
                    k.op("dve", lambda: nc.vector.tensor_tensor(out=pd_[:], in0=rr, in1=kh[:], op=ALU.mult), R=[xsb, khb], W=[pdb_])
                    k.op("pool", lambda: nc.gpsimd.tensor_tensor(out=pd_[:], in0=pd_[:], in1=bc(prm[:, 3, :]), op=ALU.mult), R=[pdb_, prb], W=[pdb_])
                    pk, pkb = pr.next()
                    for j in range(4):
                        k.op("pe", lambda: nc.tensor.matmul(pk[:, 0:8], lhsT=pd_[:, j, :], rhs=cst[:, C_HS + 8 * j:C_HS + 8 * j + 8], start=(j == 0), stop=(j == 3)), R=[pdb_, cstb], W=[pkb])
                    ro, rob = rkro.next()
                    k.op("act", lambda: nc.scalar.copy(out=ro[:], in_=pk[:, 0:8]), R=[pkb], W=[rob])
                    k.dma("pool", rwRKR[t0:t0 + 128, :], ro[:], R=[rob])
                k.barrier()

            with ExitStack() as es:
                SB = lambda name, shape: es.enter_context(sbt(nc, name, shape, F32))
                GLt = SB("c_GL", [64, 8, NCH]); glb = Buf()
                k.dma("sp", GLt[:], rwGL.rearrange("(h q) c -> q h c", q=64), W=[glb])
                hv = lambda dt_: dt_.rearrange("(h q) t -> q h t", q=64)
                ARr = Ring(es, nc, "c_AR", [64, 8, 2, 64], F32, 3)
                BKr = Ring(es, nc, "c_BK", [64, 8, 2, 64], F32, 3)
                TMr = Ring(es, nc, "c_TM", [64, 3, 512], F32, 3)
                Hr = Ring(es, nc, "c_H", [64, 8, 64], F32, 2)
                A1r = Ring(es, nc, "c_A1", [64, 8, 128], F32, 2)
                A2r = Ring(es, nc, "c_A2", [64, 8, 128], F32, 2)
                Pr_ = Ring(es, nc, "c_P", [64, 8, 64], F32, 3)
                Ptr_ = Ring(es, nc, "c_Pt", [64, 8, 64], F32, 3)
                Lr = Ring(es, nc, "c_L", [64, 8, 64], F32, 3)
                Xsr = Ring(es, nc, "c_Xs", [64, 8, 64], F32, 2)
                Usr = Ring(es, nc, "c_Us", [64, 8, 64], F32, 2)
                Yr = Ring(es, nc, "c_Y", [64, 512], F32, 3)
                pr = Ring(es, nc, "c_ps", [128, 512], F32, 8, psum=True)
                m1 = cst[0:64, C_M1:C_M1 + 128].unsqueeze(1).to_broadcast([64, 4, 128])
                m3 = cst[0:64, C_M3:C_M3 + 64].unsqueeze(1).to_broadcast([64, 8, 64])
                i64b = cst[0:64, 0:64].unsqueeze(1).to_broadcast([64, 8, 64])
                H, Hb = Hr.next()
                k.op("pool", lambda: nc.gpsimd.memset(H[:], 0.0), W=[Hb])
                v8 = lambda p_: p_[0:64, :].rearrange("p (h d) -> p h d", h=8)
                for c in range(NCH):
                    cs_ = slice(c * 64, (c + 1) * 64)
                    AR, ARb = ARr.next(); BK, BKb = BKr.next(); TM_, TMb = TMr.next()
                    k.dma("sp", AR[:, :, 0, :], hv(rwA)[:, :, cs_], W=[ARb])
                    k.dma("sp", AR[:, :, 1, :], hv(rwR)[:, :, cs_], W=[ARb])
                    k.dma("sp", BK[:, :, 0, :], hv(rwB)[:, :, cs_], W=[BKb])
                    k.dma("sp", BK[:, :, 1, :], hv(rwK)[:, :, cs_], W=[BKb])
                    k.dma("sp", TM_[:, 0, :], rwBp[cs_, :], W=[TMb])
                    k.dma("sp", TM_[:, 1, :], rwKp[cs_, :], W=[TMb])
                    k.dma("sp", TM_[:, 2, :], rwV[cs_, :], W=[TMb])
                    A1, A1b = A1r.next(); A2, A2b = A2r.next()
                    for (A_, Ab_, which) in ((A1, A1b, 0), (A2, A2b, 1)):
                        for b in range(2):
                            p, pb = pr.next()
                            for hh_ in range(4):
                                h = b * 4 + hh_
                                k.op("pe", lambda: nc.tensor.matmul(p[0:64, hh_ * 128:(hh_ + 1) * 128], lhsT=BK[:, h, which, :], rhs=AR[:, h, :, :].rearrange("p a d -> p (a d)"), start=True, stop=True),
                                     R=[BKb, ARb], W=[pb])
                            k.op("dve", lambda: nc.vector.tensor_tensor(out=A_[:, b * 4:(b + 1) * 4, :], in0=p[0:64, :].rearrange("p (h d) -> p h d", h=4), in1=m1, op=ALU.mult), R=[pb, cstb], W=[Ab_])
                    p, pb = pr.next()
                    for h in range(8):
                        k.op("pe", lambda: nc.tensor.matmul(p[0:64, h * 64:(h + 1) * 64], lhsT=AR[:, h, 0, :], rhs=BK[:, h, 0, :], start=True, stop=True), R=[ARb, BKb], W=[pb])
                    Pt_, Ptb_ = Ptr_.next()
                    k.op("dve", lambda: nc.vector.tensor_tensor(out=Pt_[:], in0=v8(p), in1=m3, op=ALU.mult), R=[pb, cstb], W=[Ptb_])
                    P_, Pb_ = Pr_.next()
                    k.op("pool", lambda: nc.gpsimd.tensor_copy(out=P_[:], in_=A1[:, :, 0:64]), R=[A1b], W=[Pb_])
                    L_, Lb_ = Lr.next()
                    k.op("pool", lambda: nc.gpsimd.tensor_tensor(out=L_[:], in0=A1[:, :, 0:64], in1=i64b, op=ALU.add), R=[A1b, cstb], W=[Lb_])
                    for lev in range(5):
                        pa, pab = pr.next(); pb2, pb2b = pr.next()
                        for h in range(8):
                            k.op("pe", lambda: nc.tensor.matmul(pa[0:64, h * 64:(h + 1) * 64], lhsT=Pt_[:, h, :], rhs=P_[:, h, :], start=True, stop=True), R=[Ptb_, Pb_], W=[pab])
                        for h in range(8):
                            k.op("pe", lambda: nc.tensor.matmul(pb2[0:64, h * 64:(h + 1) * 64], lhsT=P_[:, h, :], rhs=Pt_[:, h, :], start=True, stop=True), R=[Ptb_, Pb_], W=[pb2b])
                        Pn, Pnb = Pr_.next(); Ptn, Ptnb = Ptr_.next()
                        k.op("act", lambda: nc.scalar.copy(out=Pn[:], in_=v8(pa)), R=[pab], W=[Pnb])
                        k.op("dve", lambda: nc.vector.tensor_copy(out=Ptn[:], in_=v8(pb2)), R=[pb2b], W=[Ptnb])
                        P_, Pb_, Pt_, Ptb_ = Pn, Pnb, Ptn, Ptnb
                        pc, pcb = pr.next()
                        for h in range(8):
                            k.op("pe", lambda: nc.tensor.matmul(pc[0:64, h * 64:(h + 1) * 64], lhsT=Pt_[:, h, :], rhs=L_[:, h, :], start=True, stop=True), R=[Ptb_, Lb_], W=[pcb])
                        Ln, Lnb = Lr.next()
                        k.op("dve", lambda: nc.vector.tensor_tensor(out=Ln[:], in0=L_[:], in1=v8(pc), op=ALU.add), R=[Lb_, pcb], W=[Lnb])
                        L_, Lb_ = Ln, Lnb
                    Vh = lambda h: TM_[:, 2, h * 64:(h + 1) * 64]
                    px, pxb = pr.next()
                    for h in range(8):
                        k.op("pe", lambda: nc.tensor.matmul(px[0:64, h * 64:(h + 1) * 64], lhsT=AR[:, h, 0, :], rhs=H[:, h, :], start=True, stop=False), R=[ARb, Hb], W=[pxb])
                        k.op("pe", lambda: nc.tensor.matmul(px[0:64, h * 64:(h + 1) * 64], lhsT=A2[:, h, 0:64], rhs=Vh(h), start=False, stop=True), R=[A2b, TMb], W=[pxb])
                    Xs, Xsb = Xsr.next()
                    k.op("act", lambda: nc.scalar.copy(out=Xs[:], in_=v8(px)), R=[pxb], W=[Xsb])
                    pu, pub = pr.next()
                    for h in range(8):
                        k.op("pe", lambda: nc.tensor.matmul(pu[0:64, h * 64:(h + 1) * 64], lhsT=L_[:, h, :], rhs=Xs[:, h, :], start=True, stop=True), R=[Lb_, Xsb], W=[pub])
                    Us, Usb = Usr.next()
                    k.op("dve", lambda: nc.vector.tensor_copy(out=Us[:], in_=v8(pu)), R=[pub], W=[Usb])
                    py, pyb = pr.next()
                    for h in range(8):
                        k.op("pe", lambda: nc.tensor.matmul(py[0:64, h * 64:(h + 1) * 64], lhsT=AR[:, h, 1, :], rhs=H[:, h, :], start=True, stop=False), R=[ARb, Hb], W=[pyb])
                        k.op("pe", lambda: nc.tensor.matmul(py[0:64, h * 64:(h + 1) * 64], lhsT=A1[:, h, 64:128], rhs=Us[:, h, :], start=False, stop=False), R=[A1b, Usb], W=[pyb])
                        k.op("pe", lambda: nc.tensor.matmul(py[0:64, h * 64:(h + 1) * 64], lhsT=A2[:, h, 64:128], rhs=Vh(h), start=False, stop=True), R=[A2b, TMb], W=[pyb])
                    Yt, Ytb = Yr.next()
                    k.op("act", lambda: nc.scalar.copy(out=Yt[:], in_=py[0:64, :]), R=[pyb], W=[Ytb])
                    k.dma("pool", rwY[cs_, :], Yt[:], R=[Ytb])
                    ph, phb = pr.next()
                    for h in range(8):
                        k.op("pe", lambda: nc.tensor.matmul(ph[0:64, h * 64:(h + 1) * 64], lhsT=TM_[:, 0, h * 64:(h + 1) * 64], rhs=Us[:, h, :], start=True, stop=False), R=[TMb, Usb], W=[phb])
                        k.op("pe", lambda: nc.tensor.matmul(ph[0:64, h * 64:(h + 1) * 64], lhsT=TM_[:, 1, h * 64:(h + 1) * 64], rhs=Vh(h), start=False, stop=True), R=[TMb], W=[phb])
                    Hn, Hnb = Hr.next()
                    k.op("dve", lambda: nc.vector.tensor_tensor(out=Hn[:], in0=H[:], in1=GLt[:, :, c:c + 1].to_broadcast([64, 8, 64]), op=ALU.mult), R=[Hb, glb], W=[Hnb])
                    k.op("dve", lambda: nc.vector.tensor_tensor(out=Hn[:], in0=Hn[:], in1=v8(ph), op=ALU.add), R=[Hnb, phb], W=[Hnb])
                    H, Hb = Hn, Hnb
                k.barrier()

            with ExitStack() as es:
                SB = lambda name, shape: es.enter_context(sbt(nc, name, shape, F32))
                lng = SB("e_g", [128, 512]); lnb = SB("e_b", [128, 512]); eb = Buf()
                k.dma("sp", lng[:], rw_ln_g[l].partition_broadcast(128), W=[eb])
                k.dma("sp", lnb[:], rw_ln_b[l].partition_broadcast(128), W=[eb])
                inr = Ring(es, nc, "e_in", [128, 3, 512], F32, 2)
                rkr_ = Ring(es, nc, "e_rk", [128, 8], F32, 2)
                sqr = Ring(es, nc, "e_sq", [128, 8, 64], F32, 2)
                str_ = Ring(es, nc, "e_st", [128, 4, 8], F32, 2)
                yr = Ring(es, nc, "e_y", [128, 8, 64], F32, 2)
                mtr = Ring(es, nc, "e_mt", [128, 4, 128], BF16, 2)
                pr = Ring(es, nc, "e_ps", [128, 512], F32, 2, psum=True)
                b8 = lambda ap_: ap_.unsqueeze(2).to_broadcast([128, 8, 64])
                v3 = lambda ap_: ap_.rearrange("p (h d) -> p h d", h=8)
                for c in range(NT):
                    t0 = c * 128
                    it, itb = inr.next()
                    k.dma("sp", it[:, 0, :], rwY[t0:t0 + 128, :], W=[itb])
                    k.dma("sp", it[:, 1, :], rwV[t0:t0 + 128, :], W=[itb])
                    k.dma("sp", it[:, 2, :], pTM[t0:t0 + 128, TM_RZ:TM_RZ + 512], W=[itb])
                    rk, rkb = rkr_.next()
                    k.dma("sp", rk[:], rwRKR[t0:t0 + 128, :], W=[rkb])
                    Y3 = v3(it[:, 0, :])
                    sq, sqb = sqr.next()
                    k.op("act", lambda: nc.scalar.activation(out=sq[:], in_=Y3, func=AF.Square), R=[itb], W=[sqb])
                    st, stb = str_.next()
                    k.op("dve", lambda: nc.vector.tensor_reduce(out=st[:, 0, :], in_=Y3, axis=AX.X, op=ALU.add), R=[itb], W=[stb])
                    k.op("dve", lambda: nc.vector.tensor_reduce(out=st[:, 1, :], in_=sq[:], axis=AX.X, op=ALU.add), R=[sqb], W=[stb])
                    k.op("dve", lambda: nc.vector.tensor_scalar(out=st[:, 0, :], in0=st[:, 0, :], scalar1=1.0 / 64, scalar2=None, op0=ALU.mult), R=[stb], W=[stb])
                    k.op("dve", lambda: nc.vector.tensor_tensor(out=st[:, 2, :], in0=st[:, 0, :], in1=st[:, 0, :], op=ALU.mult), R=[stb], W=[stb])
                    k.op("dve", lambda: nc.vector.scalar_tensor_tensor(out=st[:, 1, :], in0=st[:, 1, :], scalar=1.0 / 64, in1=st[:, 2, :], op0=ALU.mult, op1=ALU.subtract), R=[stb], W=[stb])
                    rstd(st[:, 3, :], st[:, 1, :], 1.0, C_GNEPS, [stb], [stb])
                    y, yb = yr.next()
                    k.op("dve", lambda: nc.vector.tensor_tensor(out=y[:], in0=Y3, in1=b8(st[:, 0, :]), op=ALU.subtract), R=[itb, stb], W=[yb])
                    k.op("dve", lambda: nc.vector.tensor_tensor(out=y[:], in0=y[:], in1=b8(st[:, 3, :]), op=ALU.mult), R=[yb, stb], W=[yb])
                    k.op("pool", lambda: nc.gpsimd.tensor_tensor(out=y[:], in0=y[:], in1=v3(lng[:]), op=ALU.mult), R=[yb, eb], W=[yb])
                    k.op("pool", lambda: nc.gpsimd.tensor_tensor(out=y[:], in0=y[:], in1=v3(lnb[:]), op=ALU.add), R=[yb, eb], W=[yb])
                    k.op("dve", lambda: nc.vector.tensor_tensor(out=sq[:], in0=v3(it[:, 1, :]), in1=b8(rk[:]), op=ALU.mult), R=[itb, rkb, sqb], W=[sqb])
                    k.op("dve", lambda: nc.vector.tensor_tensor(out=y[:], in0=y[:], in1=sq[:], op=ALU.add), R=[yb, sqb], W=[yb])
                    k.op("act", lambda: nc.scalar.activation(out=it[:, 2, :], in_=it[:, 2, :], func=AF.Silu), R=[itb], W=[itb])
                    k.op("dve", lambda: nc.vector.tensor_tensor(out=y[:], in0=y[:], in1=v3(it[:, 2, :]), op=ALU.mult), R=[yb, itb], W=[yb])
                    p, pb = pr.next()
                    yf = y[:].rearrange("p h d -> p (h d)")
                    for j in range(4):
                        k.op("pe", lambda: nc.tensor.transpose(p[:, j * 128:(j + 1) * 128], yf[:, j * 128:(j + 1) * 128], ident), R=[yb, cstb], W=[pb])
                    mt, mtb = mtr.next()
                    k.op("act", lambda: nc.scalar.copy(out=mt[:], in_=p[:].rearrange("p (a b) -> p a b", a=4)), R=[pb], W=[mtb])
                    k.dma("pool", mixT.rearrange("(cc p) t -> p cc t", p=128)[:, 12:16, t0:t0 + 128], mt[:], R=[mtb])
                k.barrier()

            with ExitStack() as es:
                mx = Ring(es, nc, "p5_m", [128, 16, G], BF16, 2)
                wfr = Ring(es, nc, "p5_wf", [128, 16, 512], F32, 2)
                wr = Ring(es, nc, "p5_w", [128, 16, 512], BF16, 2)
                xr = Ring(es, nc, "p5_x", [128, G], F32, 3)
                xo = Ring(es, nc, "p5_o", [128, G], F32, 3)
                ps = Ring(es, nc, "p5_ps", [128, 512], F32, 4, psum=True)
                for g in range(NG):
                    mt, mb = mx.next()
                    k.dma("sp", mt[:], mixT.rearrange("(cc p) t -> p cc t", p=128)[:, :, g * G:(g + 1) * G], W=[mb])
                    for nb in range(4):
                        wf, wfb = wfr.next()
                        k.dma("sp", wf[:], w_out[l].rearrange("(cc p) n -> p cc n", p=128)[:, :, nb * 512:(nb + 1) * 512], W=[wfb])
                        wt, wb = wr.next()
                        k.op("act", lambda: nc.scalar.copy(out=wt[:, 0:8, :], in_=wf[:, 0:8, :]), R=[wfb], W=[wb])
                        k.op("pool", lambda: nc.gpsimd.tensor_copy(out=wt[:, 8:16, :], in_=wf[:, 8:16, :]), R=[wfb], W=[wb])
                        for j in range(4):
                            n0 = nb * 512 + j * 128
                            xt, xb = xr.next()
                            k.dma("sp", xt[:], xT[n0:n0 + 128, g * G:(g + 1) * G], W=[xb])
                            p, pb = ps.next()
                            for cc in range(16):
                                k.op("pe", lambda: nc.tensor.matmul(p[:, :G], lhsT=wt[:, cc, j * 128:(j + 1) * 128], rhs=mt[:, cc, :],
                                                                    start=(cc == 0), stop=(cc == 15)), R=[wb, mb], W=[pb])
                            ot, ob = xo.next()
                            k.op("dve", lambda: nc.vector.tensor_tensor(out=ot[:], in0=p[:, :G], in1=xt[:], op=ALU.add), R=[pb, xb], W=[ob])
                            k.dma("pool", xT[n0:n0 + 128, g * G:(g + 1) * G], ot[:], R=[ob])
                k.barrier()

        with ExitStack() as es:
            gbc = es.enter_context(sbt(nc, "f_g", [128, D], F32))
            gbb = Buf()
            k.dma("sp", gbc[:], final_g.partition_broadcast(128), W=[gbb])
            xin = Ring(es, nc, "f_x", [128, 16, 128], F32, 2)
            xtm = Ring(es, nc, "f_t", [128, D], F32, 2)
            junk = es.enter_context(sbt(nc, "f_j", [128, D], F32))
            jb = Buf()
            ssq = Ring(es, nc, "f_s", [128, 2], F32, 2)
            ot = Ring(es, nc, "f_o", [128, D], F32, 2)
            ps = Ring(es, nc, "f_ps", [128, 512], F32, 8, psum=True)
            for t in range(NT):
                xt, xb = xin.next()
                k.dma("sp", xt[:], xT.rearrange("(kc p) t -> p kc t", p=128)[:, :, t * 128:(t + 1) * 128], W=[xb])
                xm, xmb = xtm.next()
                for q in range(4):
                    p, pb = ps.next()
                    for j in range(4):
                        kc = q * 4 + j
                        k.op("pe", lambda: nc.tensor.transpose(p[:, j * 128:(j + 1) * 128], xt[:, kc, :], ident), R=[xb, cstb], W=[pb])
                    if q % 2:
                        k.op("act", lambda: nc.scalar.copy(out=xm[:, q * 512:(q + 1) * 512], in_=p[:]), R=[pb], W=[xmb])
                    else:
                        k.op("dve", lambda: nc.vector.tensor_copy(out=xm[:, q * 512:(q + 1) * 512], in_=p[:]), R=[pb], W=[xmb])
                sq, sqb = ssq.next()
                k.op("act", lambda: nc.scalar.activation(out=junk[:], in_=xm[:], func=AF.Square, accum_out=sq[:, 0:1]), R=[xmb], W=[jb, sqb])
                rstd(sq[:, 1:2], sq[:, 0:1], 1.0 / D, C_EPS, [sqb], [sqb])
                o, ob = ot.next()
                k.op("dve", lambda: nc.vector.scalar_tensor_tensor(out=o[:], in0=xm[:], scalar=sq[:, 1:2], in1=gbc[:], op0=ALU.mult, op1=ALU.mult),
                     R=[xmb, sqb, gbb], W=[ob])
                k.dma("pool", out[t * 128:(t + 1) * 128, :], o[:], R=[ob])
            k.barrier()
    return nc


def MIXERS(env):
    pass


_CACHE = {}


WNAMES = ("norm_g", "w_in", "w_out", "final_norm_g", "ml_conv_w", "ml_conv_b", "ml_i_bias", "ml_f_bias", "ml_norm_g",
          "mla_q_norm_g", "mla_w_uq", "mla_kv_norm_g", "mla_w_ukv",
          "rw_mu", "rw_w0", "rw_w2", "rw_a0", "rw_a2", "rw_k_k", "rw_k_a", "rw_r_k", "rw_ln_g", "rw_ln_b")


def make_maps(inputs, T, depth):
    consts = make_consts()
    shared = {}
    for name in WNAMES:
        a = inputs[name]
        shared[name] = np.ascontiguousarray(a if name == "final_norm_g" else a[:depth])
    for name in ("rw_v0", "rw_v1", "rw_v2"):
        shared[name] = np.ascontiguousarray(inputs[name])
    in_maps = []
    for c in range(8):
        b = c % 4
        m = {"x": np.ascontiguousarray(inputs["x"][b, :T]), "positions": np.ascontiguousarray(inputs["positions"][b, :T]),
             "consts": consts}
        m.update(shared)
        in_maps.append(m)
    return in_maps


def kernel(**inputs):
    T = 4096
    depth = 4
    if "nc" not in _CACHE:
        _CACHE["nc"] = build(T, depth)
    nc = _CACHE["nc"]
    in_maps = make_maps(inputs, T, depth)
    res = run_bass_kernel_spmd(nc, in_maps, core_ids=list(range(8)))
    return np.stack([res.results[b]["out"] for b in range(4)], axis=0)
```

```python
import numpy as np
from contextlib import ExitStack
import concourse.bass as bass
import concourse.mybir as mybir
from concourse.bass_utils import run_bass_kernel_spmd

F32 = mybir.dt.float32
BF16 = mybir.dt.bfloat16
I32 = mybir.dt.int32
AF = mybir.ActivationFunctionType
ALU = mybir.AluOpType
AX = mybir.AxisListType

D = 2048
DIN = 6600
EPS = 1e-6
FM_RANGES = [(0, 1024), (2568, 3336), (4424, 6088)]
TM_RANGES = [(1024, 2568), (3336, 4424), (6088, 6600)]
TM_V, TM_I, TM_F, TM_O, TM_Z = 0, 512, 516, 520, 1032
TM_KR, TM_MZ, TM_RZ = 1544, 1608, 2632
NTM = 3144
FM_QK, FM_CQ, FM_CKV, FM_RW = 0, 8, 12, 14
NFM = 27


_UID = [0]


def sbt(nc, name, shape, dt):
    _UID[0] += 1
    return nc.sbuf_tensor("%s_u%d" % (name, _UID[0]), shape, dt)


def pst(nc, name, shape, dt):
    _UID[0] += 1
    return nc.psum_tensor("%s_u%d" % (name, _UID[0]), shape, dt)


class Buf:
    __slots__ = ("w", "r")

    def __init__(self):
        self.w = None
        self.r = {}


class KB:
    NDS = 24

    def __init__(self, nc):
        self.nc = nc
        self.E = {"pe": nc.tensor, "act": nc.scalar, "dve": nc.vector, "pool": nc.gpsimd, "sp": nc.sync}
        self.sem = {e: nc.alloc_semaphore("s_" + e) for e in ("pe", "act", "dve", "pool")}
        self.cnt = {e: 0 for e in self.sem}
        self.dsem = [nc.alloc_semaphore("d%d" % i) for i in range(self.NDS)]
        self.dcnt = [0] * self.NDS
        self.dnext = 0
        self.bar = nc.alloc_semaphore("bar")
        self.nbar = 0
        self.seen = {e: {} for e in self.E}

    def _semh(self, key):
        return self.sem[key] if isinstance(key, str) else self.dsem[key[1]]

    def _wait(self, e, key, val, same_ok=False):
        if val <= 0:
            return
        if key == e and (same_ok or e == "pe"):
            return
        if self.seen[e].get(key, 0) >= val:
            return
        self.E[e].wait_ge(self._semh(key), val)
        self.seen[e][key] = val

    def _deps(self, e, reads, writes):
        for b in reads:
            if b.w is not None:
                self._wait(e, b.w[0], b.w[1])
        for b in writes:
            if b.w is not None:
                self._wait(e, b.w[0], b.w[1])
            for key, val in b.r.items():
                self._wait(e, key, val, same_ok=True)

    def _mark(self, ev, reads, writes):
        for b in reads:
            if b.r.get(ev[0], 0) < ev[1]:
                b.r[ev[0]] = ev[1]
        for b in writes:
            b.w = ev
            b.r = {}

    def op(self, e, fn, R=(), W=()):
        self._deps(e, R, W)
        ins = fn()
        self.cnt[e] += 1
        ins.then_inc(self.sem[e], 1)
        self._mark((e, self.cnt[e]), R, W)

    def dma(self, q, out, in_, R=(), W=(), slow=False):
        i = self.dnext
        self.dnext = (i + 1) % self.NDS
        key = ("d", i)
        self._wait(q, key, self.dcnt[i])
        self._deps(q, R, W)
        if slow:
            ins = self.E[q].dma_start(out=out, in_=in_, allow_slow_non_contiguous=True)
        else:
            ins = self.E[q].dma_start(out=out, in_=in_)
        self.dcnt[i] += 16
        ins.then_inc(self.dsem[i], 16)
        self._mark((key, self.dcnt[i]), R, W)

    def barrier(self):
        sp = self.E["sp"]
        for e in self.sem:
            self._wait("sp", e, self.cnt[e])
        for i in range(self.NDS):
            self._wait("sp", ("d", i), self.dcnt[i])
        self.nbar += 1
        sp.sem_inc(self.bar, 1)
        for e in ("pe", "act", "dve", "pool"):
            self.E[e].wait_ge(self.bar, self.nbar)
            for k2 in self.sem:
                self.seen[e][k2] = self.cnt[k2]
            for i in range(self.NDS):
                self.seen[e][("d", i)] = self.dcnt[i]


class Ring:
    def __init__(self, es, nc, name, shape, dtype, n, psum=False):
        self.items = []
        for i in range(n):
            if psum:
                t = es.enter_context(pst(nc, "%s%d" % (name, i), shape, dtype))
            else:
                t = es.enter_context(sbt(nc, "%s%d" % (name, i), shape, dtype))
            self.items.append((t, Buf()))
        self.i = 0

    def next(self):
        it = self.items[self.i]
        self.i = (self.i + 1) % len(self.items)
        return it


def split_blocks(ranges, maxw=512):
    out = []
    for (a, b) in ranges:
        c = a
        while c < b:
            w = min(maxw, b - c)
            out.append((c, w))
            c += w
    return out


def make_consts():
    c = np.zeros((128, 1280), np.float32)
    c[:, 0:128] = np.eye(128, dtype=np.float32)
    j = np.arange(128)
    c[:, 128:256] = (j[:, None] <= j[None, :]).astype(np.float32)
    c[:, 256:384] = 1.0
    same = (j[:, None] // 64) == (j[None, :] // 64)
    c[:, 384:512] = ((j[:, None] <= j[None, :]) & same)
    c[:, 512:640] = ((j[:, None] < j[None, :]) & same)
    invf = np.power(10000.0, -np.arange(0, 64, 2, dtype=np.float32) / 64).astype(np.float32)
    c[:, 640:672] = invf[None, :]
    c[:, 672:674] = (j[:, None] // 64 == np.arange(2)[None, :])
    c[:, 700] = 1e-6
    c[:, 701] = 64e-5
    c[:, 702] = 1.0
    c[:, 703] = -0.5
    c[:, 704] = 0.0
    c[:, 705] = -np.pi
    c[:, 706] = 1e-24
    c[:, 707] = 1.0 / 128
    i64 = np.arange(64)
    c[:64, 768:832] = (i64[:, None] < i64[None, :])
    c[:64, 832:896] = (i64[:, None] <= i64[None, :])
    c[:64, 896:960] = (i64[:, None] > i64[None, :])
    for jj in range(4):
        c[:, 960 + jj * 8:968 + jj * 8] = ((2 * jj + j[:, None] // 64) == np.arange(8)[None, :])
    c[:, 1024:1152] = same
    return c


C_ID, C_TRI, C_ONE, C_BI, C_BS, C_IF, C_CI = 0, 128, 256, 384, 512, 640, 672
C_M1, C_M3, C_HS, C_BD = 768, 896, 960, 1024
C_EPS, C_GNEPS, C_1, C_MH, C_0, C_MPI, C_TINY, C_R128 = 700, 701, 702, 703, 704, 705, 706, 707


def build(T, depth, dbg=()):
    assert T % 128 == 0
    NT = T // 128
    SG = min(T, 1024)
    NSG = T // SG
    G = min(T, 512)
    NG = T // G
    nc = bass.Bass("TRN2", target_bir_lowering=False)

    def din(name, shape, dt=F32):
        return nc.dram_tensor(name, list(shape), dt, kind="ExternalInput").ap()

    def dscr(name, shape, dt=F32):
        kind = "ExternalOutput" if name in dbg else "Internal"
        return nc.dram_tensor(name, list(shape), dt, kind=kind).ap()

    x_in = din("x", [T, D])
    pos_in = din("positions", [T], I32)
    consts_in = din("consts", [128, 1280])
    norm_g = din("norm_g", [depth, D])
    w_in = din("w_in", [depth, D, DIN])
    w_out = din("w_out", [depth, D, D])
    final_g = din("final_norm_g", [D])
    ml_conv_w = din("ml_conv_w", [depth, 4, 1024]); ml_conv_b = din("ml_conv_b", [depth, 1024])
    mla_q_norm_g = din("mla_q_norm_g", [depth, 512]); mla_w_uq = din("mla_w_uq", [depth, 512, 1536])
    mla_kv_norm_g = din("mla_kv_norm_g", [depth, 256]); mla_w_ukv = din("mla_w_ukv", [depth, 256, 2048])
    rw_mu = din("rw_mu", [depth, 1664]); rw_w0 = din("rw_w0", [depth, 512]); rw_w2 = din("rw_w2", [depth, 64, 512])
    rw_a0 = din("rw_a0", [depth, 512]); rw_a2 = din("rw_a2", [depth, 64, 512])
    rw_v0 = din("rw_v0", [3, 512]); rw_v1 = din("rw_v1", [3, 512, 32]); rw_v2 = din("rw_v2", [3, 32, 512])
    rw_k_k = din("rw_k_k", [depth, 512]); rw_k_a = din("rw_k_a", [depth, 512]); rw_r_k = din("rw_r_k", [depth, 8, 64])
    rw_ln_g = din("rw_ln_g", [depth, 512]); rw_ln_b = din("rw_ln_b", [depth, 512])
    ml_i_bias = din("ml_i_bias", [depth, 4]); ml_f_bias = din("ml_f_bias", [depth, 4]); ml_norm_g = din("ml_norm_g", [depth, 512])
    out = nc.dram_tensor("out", [T, D], F32, kind="ExternalOutput").ap()

    xT = dscr("xT", [D, T])
    pFM = dscr("pFM", [NFM * 128, T])
    pTM = dscr("pTM", [T, NTM])
    mixT = dscr("mixT", [D, T], BF16)
    csd = dscr("cs", [T, 64])
    qnT = dscr("qnT", [1024, T], BF16); knT = dscr("knT", [1024, T], BF16); qrT = dscr("qrT", [512, T], BF16); krT = dscr("krT", [64, T], BF16)
    vaug = dscr("vaug", [8, T, 129], BF16)
    rwA = dscr("rwA", [512, T]); rwR = dscr("rwR", [512, T]); rwB = dscr("rwB", [512, T]); rwK = dscr("rwK", [512, T])
    rwBp = dscr("rwBp", [T, 512]); rwKp = dscr("rwKp", [T, 512]); rwV = dscr("rwV", [T, 512]); rwY = dscr("rwY", [T, 512])
    rwGL = dscr("rwGL", [512, T // 64]); rwRKR = dscr("rwRKR", [T, 8]); vfirst = dscr("vfirst", [512, T])

    fm_blocks = split_blocks(FM_RANGES)
    tm_blocks = split_blocks(TM_RANGES)

    with ExitStack() as top:
        k = KB(nc)
        cst = top.enter_context(sbt(nc, "cst", [128, 1280], F32))
        cstb = Buf()
        k.dma("sp", cst[:], consts_in, W=[cstb])
        ident = cst[:, C_ID:C_ID + 128]
        ones = cst[:, C_ONE:C_ONE + 128]
        onec = cst[:, C_ONE:C_ONE + 1]

        def rstd(o, i, scale, epscol, R, W, np_=128):
            k.op("act", lambda: nc.scalar.activation(out=o, in_=i, func=AF.Sqrt, bias=cst[:np_, epscol:epscol + 1], scale=scale), R=list(R) + [cstb], W=W)
            k.op("dve", lambda: nc.vector.reciprocal(out=o, in_=o), R=W, W=W)

        with ExitStack() as es:
            xin = Ring(es, nc, "i_x", [128, D], F32, 2)
            ps = Ring(es, nc, "i_ps", [128, 512], F32, 4, psum=True)
            stg = Ring(es, nc, "i_st", [128, 16, 128], F32, 2)
            for t in range(NT):
                xt, xb = xin.next()
                k.dma("sp", xt[:], x_in[t * 128:(t + 1) * 128, :], W=[xb])
                st, sb = stg.next()
                for q in range(4):
                    p, pb = ps.next()
                    for j in range(4):
                        kc = q * 4 + j
                        k.op("pe", lambda: nc.tensor.transpose(p[:, j * 128:(j + 1) * 128], xt[:, kc * 128:(kc + 1) * 128], ident),
                             R=[xb, cstb], W=[pb])
                    eng = "act" if q % 2 else "dve"
                    if eng == "act":
                        k.op("act", lambda: nc.scalar.copy(out=st[:, q * 4:(q + 1) * 4, :], in_=p[:].rearrange("p (a b) -> p a b", a=4)), R=[pb], W=[sb])
                    else:
                        k.op("dve", lambda: nc.vector.tensor_copy(out=st[:, q * 4:(q + 1) * 4, :], in_=p[:].rearrange("p (a b) -> p a b", a=4)), R=[pb], W=[sb])
                k.dma("pool", xT.rearrange("(kc p) t -> p kc t", p=128)[:, :, t * 128:(t + 1) * 128], st[:], R=[sb])
            posi = es.enter_context(sbt(nc, "i_pi", [128, NT], I32))
            posf = es.enter_context(sbt(nc, "i_pf", [128, NT], F32))
            pob = Buf()
            k.dma("sp", posi[:], pos_in.rearrange("(n p) -> p n", p=128), W=[pob], slow=True)
            k.op("dve", lambda: nc.vector.tensor_copy(out=posf[:], in_=posi[:]), R=[pob], W=[pob])
            angr = Ring(es, nc, "i_ang", [128, 64], F32, 2)
            tir = Ring(es, nc, "i_ti", [128, 64], I32, 2)
            tfr = Ring(es, nc, "i_tf", [128, 64], F32, 2)
            TWO_PI = float(2 * np.pi)
            C1 = 6.28125
            C2 = float(2 * np.pi - 6.28125)
            for t in range(NT):
                an, anb = angr.next()
                ti, tib = tir.next()
                tf, tfb = tfr.next()
                k.op("dve", lambda: nc.vector.tensor_scalar(out=an[:, 0:32], in0=cst[:, C_IF:C_IF + 32], scalar1=posf[:, t:t + 1], scalar2=None, op0=ALU.mult), R=[pob, cstb], W=[anb])
                k.op("dve", lambda: nc.vector.tensor_scalar(out=an[:, 32:64], in0=an[:, 0:32], scalar1=float(np.pi / 2), scalar2=None, op0=ALU.add), R=[anb], W=[anb])
                k.op("dve", lambda: nc.vector.tensor_scalar(out=tf[:], in0=an[:], scalar1=float(1 / (2 * np.pi)), scalar2=None, op0=ALU.mult), R=[anb], W=[tfb])
                k.op("dve", lambda: nc.vector.tensor_copy(out=ti[:], in_=tf[:]), R=[tfb], W=[tib])
                k.op("dve", lambda: nc.vector.tensor_copy(out=tf[:], in_=ti[:]), R=[tib], W=[tfb])
                k.op("dve", lambda: nc.vector.scalar_tensor_tensor(out=an[:], in0=tf[:], scalar=-C1, in1=an[:], op0=ALU.mult, op1=ALU.add), R=[tfb, anb], W=[anb])
                k.op("dve", lambda: nc.vector.scalar_tensor_tensor(out=an[:], in0=tf[:], scalar=-C2, in1=an[:], op0=ALU.mult, op1=ALU.add), R=[tfb, anb], W=[anb])
                k.op("dve", lambda: nc.vector.tensor_scalar(out=tf[:], in0=an[:], scalar1=float(np.pi), scalar2=-TWO_PI, op0=ALU.is_gt, op1=ALU.mult), R=[anb], W=[tfb])
                k.op("dve", lambda: nc.vector.tensor_tensor(out=an[:], in0=an[:], in1=tf[:], op=ALU.add), R=[tfb, anb], W=[anb])
                k.op("dve", lambda: nc.vector.tensor_scalar(out=tf[:], in0=an[:], scalar1=float(-np.pi), scalar2=TWO_PI, op0=ALU.is_lt, op1=ALU.mult), R=[anb], W=[tfb])
                k.op("dve", lambda: nc.vector.tensor_tensor(out=an[:], in0=an[:], in1=tf[:], op=ALU.add), R=[tfb, anb], W=[anb])
                k.op("act", lambda: nc.scalar.activation(out=an[:], in_=an[:], func=AF.Sin), R=[anb], W=[anb])
                k.dma("pool", csd[t * 128:(t + 1) * 128, :], an[:], R=[anb])
            k.barrier()

        for l in range(depth):
            with ExitStack() as es:
                gcol = es.enter_context(sbt(nc, "p1_g", [128, 16], F32))
                gb = Buf()
                k.dma("sp", gcol[:], norm_g[l].rearrange("(kc p) -> p kc", p=128), W=[gb], slow=True)
                hT = es.enter_context(sbt(nc, "p1_hT", [128, 16, SG], BF16))
                hb = Buf()
                xs = Ring(es, nc, "p1_xs", [128, SG], F32, 2)
                sqr = Ring(es, nc, "p1_sq", [128, SG], F32, 2)
                wr = Ring(es, nc, "p1_w", [128, 16, 512], F32, 2)
                wcr = Ring(es, nc, "p1_wc", [128, 16, 512], BF16, 2)
                wcn = [0]
                stg = Ring(es, nc, "p1_st", [128, 512], F32, 4)
                rbc = es.enter_context(sbt(nc, "p1_rbc", [128, SG], F32))
                rbcb = Buf()
                rtm = es.enter_context(sbt(nc, "p1_rtm", [128, SG // 128], F32))
                rtmb = Buf()
                ps = Ring(es, nc, "p1_ps", [128, 512], F32, 4, psum=True)
                pbc = Ring(es, nc, "p1_pbc", [128, 512], F32, SG // G, psum=True)
                ptm = es.enter_context(pst(nc, "p1_ptm", [128, 512], F32))
                ptmb = Buf()
                def p1_loadw(cc0, nb):
                    wf, wfb = wr.next()
                    k.dma("sp", wf[:, :, :nb], w_in[l].rearrange("(kc p) n -> p kc n", p=128)[:, :, cc0:cc0 + nb], W=[wfb])
                    wc, wcb = wcr.next()
                    wcn[0] += 1
                    for hf in range(2):
                        if (wcn[0] + hf) % 2:
                            k.op("act", lambda: nc.scalar.copy(out=wc[:, hf * 8:(hf + 1) * 8, :nb], in_=wf[:, hf * 8:(hf + 1) * 8, :nb]), R=[wfb], W=[wcb])
                        else:
                            k.op("dve", lambda: nc.vector.tensor_copy(out=wc[:, hf * 8:(hf + 1) * 8, :nb], in_=wf[:, hf * 8:(hf + 1) * 8, :nb]), R=[wfb], W=[wcb])
                    return wc, wcb

                for sg in range(NSG):
                    c0 = sg * SG
                    pbcs = [pbc.next() for _ in range(SG // G)]
                    for kc in range(16):
                        xt, xb = xs.next()
                        k.dma("sp", xt[:], xT[kc * 128:(kc + 1) * 128, c0:c0 + SG], W=[xb])
                        sq, sqb = sqr.next()
                        k.op("act", lambda: nc.scalar.activation(out=sq[:], in_=xt[:], func=AF.Square), R=[xb], W=[sqb])
                        k.op("dve", lambda: nc.vector.tensor_scalar(out=hT[:, kc, :], in0=xt[:], scalar1=gcol[:, kc:kc + 1], scalar2=None, op0=ALU.mult),
                             R=[xb, gb], W=[hb])
                        for s in range(SG // G):
                            pp, ppb = pbcs[s]
                            k.op("pe", lambda: nc.tensor.matmul(pp[:, :G], lhsT=ones, rhs=sq[:, s * G:(s + 1) * G], start=(kc == 0), stop=(kc == 15)),
                                 R=[sqb, cstb], W=[ppb])
                    for s in range(SG // G):
                        pp, ppb = pbcs[s]
                        rstd(rbc[:, s * G:(s + 1) * G], pp[:, :G], 1.0 / D, C_EPS, [ppb], [rbcb])
                    for t in range(SG // 128):
                        k.op("pe", lambda: nc.tensor.matmul(ptm[:, t:t + 1], lhsT=rbc[:, t * 128:(t + 1) * 128], rhs=cst[:, C_R128:C_R128 + 1], start=True, stop=True),
                             R=[rbcb, cstb], W=[ptmb])
                    k.op("dve", lambda: nc.vector.tensor_copy(out=rtm[:], in_=ptm[:, :SG // 128]), R=[ptmb], W=[rtmb])
                    fmrow = 0
                    for (cc0, nb) in fm_blocks:
                        wt, wb = p1_loadw(cc0, nb)
                        for j in range(nb // 128):
                            for s in range(SG // G):
                                p, pb = ps.next()
                                for kc in range(16):
                                    k.op("pe", lambda: nc.tensor.matmul(p[:, :G], lhsT=wt[:, kc, j * 128:(j + 1) * 128], rhs=hT[:, kc, s * G:(s + 1) * G],
                                                                        start=(kc == 0), stop=(kc == 15)), R=[wb, hb], W=[pb])
                                st, sb = stg.next()
                                k.op("dve", lambda: nc.vector.tensor_tensor(out=st[:, :G], in0=p[:, :G], in1=rbc[:, s * G:(s + 1) * G], op=ALU.mult),
                                     R=[pb, rbcb], W=[sb])
                                k.dma("pool", pFM[fmrow * 128:(fmrow + 1) * 128, c0 + s * G:c0 + (s + 1) * G], st[:, :G], R=[sb])
                            fmrow += 1
                    tmcol = 0
                    for (cc0, nb) in tm_blocks:
                        wt, wb = p1_loadw(cc0, nb)
                        for t in range(SG // 128):
                            p, pb = ps.next()
                            for kc in range(16):
                                k.op("pe", lambda: nc.tensor.matmul(p[:, :nb], lhsT=hT[:, kc, t * 128:(t + 1) * 128], rhs=wt[:, kc, :nb],
                                                                    start=(kc == 0), stop=(kc == 15)), R=[wb, hb], W=[pb])
                            st, sb = stg.next()
                            k.op("act", lambda: nc.scalar.activation(out=st[:, :nb], in_=p[:, :nb], func=AF.Copy, scale=rtm[:, t:t + 1]),
                                 R=[pb, rtmb], W=[sb])
                            k.dma("pool", pTM[c0 + t * 128:c0 + (t + 1) * 128, tmcol:tmcol + nb], st[:, :nb], R=[sb])
                        tmcol += nb
                k.barrier()


            with ExitStack() as es:
                SB = lambda name, shape: es.enter_context(sbt(nc, name, shape, F32))
                PS = lambda name: es.enter_context(pst(nc, name, [128, 512], F32))
                tri = cst[:, C_TRI:C_TRI + 128]
                cw = SB("m_cw", [128, 4, 8]); cb = SB("m_cb", [128, 8]); ibfb = SB("m_ib", [128, 8]); gbc = SB("m_g", [128, 512])
                pb_ = Buf()
                for j in range(4):
                    k.dma("sp", cw[:, j, :], ml_conv_w[l, j].rearrange("(c p) -> p c", p=128), W=[pb_], slow=True)
                k.dma("sp", cb[:], ml_conv_b[l].rearrange("(c p) -> p c", p=128), W=[pb_], slow=True)
                k.dma("sp", ibfb[:, 0:4], ml_i_bias[l].partition_broadcast(128), W=[pb_])
                k.dma("sp", ibfb[:, 4:8], ml_f_bias[l].partition_broadcast(128), W=[pb_])
                k.dma("sp", gbc[:], ml_norm_g[l].partition_broadcast(128), W=[pb_])
                CT = [SB("m_CT%d" % h, [128, 129]) for h in range(4)]
                CTb = [Buf() for h in range(4)]
                for h in range(4):
                    k.op("pool", lambda: nc.gpsimd.memset(CT[h][:], 0.0), W=[CTb[h]])
                qkr = Ring(es, nc, "m_qkr", [128, 8, 131], F32, 2)
                accr = Ring(es, nc, "m_acc", [128, 8, 128], F32, 2)
                qkt = Ring(es, nc, "m_qk", [128, 8, 128], F32, 2)
                vr = Ring(es, nc, "m_v", [128, 4, 129], F32, 2)
                for (vt, vb) in vr.items:
                    k.op("pool", lambda: nc.gpsimd.memset(vt[:, :, 128:129], 1.0), W=[vb])
                gtr = Ring(es, nc, "m_gt", [128, 8], F32, 2)
                Gr = Ring(es, nc, "m_G", [128, 32], F32, 2)
                ozr = Ring(es, nc, "m_oz", [128, 1024], F32, 2)
                rhr = Ring(es, nc, "m_rh", [128, 128], F32, 2)
                Er = Ring(es, nc, "m_E", [128, 128], F32, 2)
                STr = Ring(es, nc, "m_ST", [128, 128], F32, 2)
                n1r = Ring(es, nc, "m_n1", [128, 129], F32, 2)
                n2r = Ring(es, nc, "m_n2", [128, 129], F32, 2)
                smr = Ring(es, nc, "m_sm", [128, 16], F32, 2)
                hhr = Ring(es, nc, "m_hh", [128, 128], F32, 2)
                kwr = Ring(es, nc, "m_kw", [128, 128], F32, 2)
                yr = Ring(es, nc, "m_y", [128, 512], F32, 2)
                ggr = Ring(es, nc, "m_gg", [128, 1024], F32, 2)
                mtr = Ring(es, nc, "m_mt", [128, 4, 128], BF16, 2)
                pgA = PS("m_pgA"); pgB = PS("m_pgB"); pBbc = PS("m_pB"); pQK = PS("m_pQK")
                pN1 = PS("m_pN1"); pN2 = PS("m_pN2"); pKT = PS("m_pKT"); pdC = PS("m_pdC")
                pgAb, pgBb, pBbcb, pQKb, pN1b, pN2b, pKTb, pdCb = [Buf() for _ in range(8)]
                NSD = nc.vector.BN_STATS_DIM
                for c in range(NT):
                    t0 = c * 128
                    X, Xb = qkr.next()
                    src = pFM.rearrange("(ch p) t -> p ch t", p=128)
                    if c == 0:
                        k.op("pool", lambda: nc.gpsimd.memset(X[:, :, 0:3], 0.0), W=[Xb])
                        k.dma("sp", X[:, :, 3:131], src[:, 0:8, 0:128], W=[Xb])
                    else:
                        k.dma("sp", X[:], src[:, 0:8, t0 - 3:t0 + 128], W=[Xb])
                    vt, vb = vr.next()
                    k.dma("sp", vt[:, :, 0:128], pTM[t0:t0 + 128, TM_V:TM_V + 512].rearrange("p (h d) -> p h d", h=4), W=[vb])
                    gt, gtb = gtr.next()
                    k.dma("sp", gt[:], pTM[t0:t0 + 128, TM_I:TM_I + 8], W=[gtb])
                    oz, ozb = ozr.next()
                    k.dma("sp", oz[:], pTM[t0:t0 + 128, TM_O:TM_O + 1024], W=[ozb])
                    acc, accb = accr.next()
                    qk, qkb = qkt.next()
                    for ch in range(8):
                        eng, E_ = ("dve", nc.vector)
                        k.op(eng, lambda: E_.tensor_scalar(out=acc[:, ch, :], in0=X[:, ch, 0:128], scalar1=cw[:, 0, ch:ch + 1], scalar2=cb[:, ch:ch + 1], op0=ALU.mult, op1=ALU.add),
                             R=[Xb, pb_], W=[accb])
                        for j in range(1, 4):
                            k.op(eng, lambda: E_.scalar_tensor_tensor(out=acc[:, ch, :], in0=X[:, ch, j:j + 128], scalar=cw[:, j, ch:ch + 1], in1=acc[:, ch, :], op0=ALU.mult, op1=ALU.add),
                                 R=[Xb, pb_, accb], W=[accb])
                    k.op("act", lambda: nc.scalar.activation(out=qk[:], in_=acc[:], func=AF.Silu), R=[accb], W=[qkb])
                    k.op("pool", lambda: nc.gpsimd.tensor_scalar(out=qk[:, 0:4, :], in0=qk[:, 0:4, :], scalar1=float(128 ** -0.5), scalar2=None, op0=ALU.mult), R=[qkb], W=[qkb])
                    Gt, Gb = Gr.next()
                    k.op("dve", lambda: nc.vector.tensor_tensor(out=Gt[:, 0:8], in0=gt[:], in1=ibfb[:], op=ALU.add), R=[gtb, pb_], W=[Gb])
                    k.op("act", lambda: nc.scalar.activation(out=Gt[:, 8:12], in_=Gt[:, 4:8], func=AF.Exp, scale=-1.0), R=[Gb], W=[Gb])
                    k.op("act", lambda: nc.scalar.activation(out=Gt[:, 12:16], in_=Gt[:, 8:12], func=AF.Ln, bias=cst[:, C_1:C_1 + 1]), R=[Gb, cstb], W=[Gb])
                    k.op("dve", lambda: nc.vector.tensor_scalar(out=Gt[:, 12:16], in0=Gt[:, 12:16], scalar1=-1.0, scalar2=None, op0=ALU.mult), R=[Gb], W=[Gb])
                    k.op("pe", lambda: nc.tensor.matmul(pgA[:, 0:4], lhsT=tri, rhs=Gt[:, 12:16], start=True, stop=True), R=[Gb, cstb], W=[pgAb])
                    k.op("pe", lambda: nc.tensor.matmul(pgB[:, 0:4], lhsT=ones, rhs=Gt[:, 12:16], start=True, stop=True), R=[Gb, cstb], W=[pgBb])
                    k.op("dve", lambda: nc.vector.tensor_tensor(out=Gt[:, 16:20], in0=Gt[:, 0:4], in1=pgA[:, 0:4], op=ALU.subtract), R=[Gb, pgAb], W=[Gb])
                    k.op("dve", lambda: nc.vector.tensor_tensor(out=Gt[:, 20:24], in0=Gt[:, 16:20], in1=pgB[:, 0:4], op=ALU.add), R=[Gb, pgBb], W=[Gb])
                    k.op("act", lambda: nc.scalar.activation(out=Gt[:, 20:24], in_=Gt[:, 20:24], func=AF.Exp), R=[Gb], W=[Gb])
                    k.op("act", lambda: nc.scalar.activation(out=Gt[:, 24:28], in_=pgB[:, 0:4], func=AF.Exp), R=[pgBb], W=[Gb])
                    k.op("act", lambda: nc.scalar.activation(out=Gt[:, 28:32], in_=pgA[:, 0:4], func=AF.Exp), R=[pgAb], W=[Gb])
                    y, yb = yr.next()
                    for h in range(4):
                        rh, rhb = rhr.next()
                        k.op("pool", lambda: nc.gpsimd.tensor_scalar(out=rh[:], in0=tri, scalar1=Gt[:, 12 + h:13 + h], scalar2=None, op0=ALU.mult), R=[Gb, cstb], W=[rhb])
                        k.op("pe", lambda: nc.tensor.matmul(pBbc[:, 0:128], lhsT=ones, rhs=rh[:], start=True, stop=True), R=[rhb, cstb], W=[pBbcb])
                        Et, Eb = Er.next()
                        k.op("act", lambda: nc.scalar.activation(out=Et[:], in_=pBbc[:, 0:128], func=AF.Exp, bias=Gt[:, 16 + h:17 + h]), R=[pBbcb, Gb], W=[Eb])
                        k.op("pool", lambda: nc.gpsimd.tensor_tensor(out=Et[:], in0=Et[:], in1=tri, op=ALU.mult), R=[Eb, cstb], W=[Eb])
                        k.op("pe", lambda: nc.tensor.matmul(pQK[:, 0:128], lhsT=qk[:, 4 + h, :], rhs=qk[:, h, :], start=True, stop=True), R=[qkb], W=[pQKb])
                        ST, STb = STr.next()
                        k.op("dve", lambda: nc.vector.tensor_tensor(out=ST[:], in0=pQK[:, 0:128], in1=Et[:], op=ALU.mult), R=[pQKb, Eb], W=[STb])
                        k.op("pe", lambda: nc.tensor.matmul(pN1[:, 0:129], lhsT=ST[:], rhs=vt[:, h, :], start=True, stop=True), R=[STb, vb], W=[pN1b])
                        k.op("pe", lambda: nc.tensor.matmul(pN2[:, 0:129], lhsT=qk[:, h, :], rhs=CT[h][:], start=True, stop=True), R=[qkb, CTb[h]], W=[pN2b])
                        n1, n1b = n1r.next()
                        k.op("act", lambda: nc.scalar.activation(out=n1[:], in_=pN2[:, 0:129], func=AF.Copy, scale=Gt[:, 28 + h:29 + h]), R=[pN2b, Gb], W=[n1b])
                        n2, n2b = n2r.next()
                        k.op("dve", lambda: nc.vector.tensor_tensor(out=n2[:], in0=pN1[:, 0:129], in1=n1[:], op=ALU.add), R=[pN1b, n1b], W=[n2b])
                        sm, smb = smr.next()
                        k.op("act", lambda: nc.scalar.activation(out=sm[:, 0:1], in_=n2[:, 128:129], func=AF.Abs), R=[n2b], W=[smb])
                        k.op("dve", lambda: nc.vector.tensor_scalar_max(out=sm[:, 0:1], in0=sm[:, 0:1], scalar1=1.0), R=[smb], W=[smb])
                        k.op("dve", lambda: nc.vector.reciprocal(out=sm[:, 0:1], in_=sm[:, 0:1]), R=[smb], W=[smb])
                        hh, hhb = hhr.next()
                        k.op("dve", lambda: nc.vector.tensor_scalar(out=hh[:], in0=n2[:, 0:128], scalar1=sm[:, 0:1], scalar2=None, op0=ALU.mult), R=[n2b, smb], W=[hhb])
                        k.op("dve", lambda: nc.vector.bn_stats(out=sm[:, 2:2 + NSD], in_=hh[:]), R=[hhb], W=[smb])
                        k.op("dve", lambda: nc.vector.bn_aggr(out=sm[:, 10:12], in_=sm[:, 2:2 + NSD]), R=[smb], W=[smb])
                        rstd(sm[:, 12:13], sm[:, 11:12], 1.0, C_EPS, [smb], [smb])
                        k.op("dve", lambda: nc.vector.tensor_scalar(out=y[:, h * 128:(h + 1) * 128], in0=hh[:], scalar1=sm[:, 10:11], scalar2=sm[:, 12:13], op0=ALU.subtract, op1=ALU.mult),
                             R=[hhb, smb], W=[yb])
                        k.op("pe", lambda: nc.tensor.transpose(pKT[:, 0:128], qk[:, 4 + h, :], ident), R=[qkb, cstb], W=[pKTb])
                        kw, kwb = kwr.next()
                        k.op("act", lambda: nc.scalar.activation(out=kw[:], in_=pKT[:, 0:128], func=AF.Copy, scale=Gt[:, 20 + h:21 + h]), R=[pKTb, Gb], W=[kwb])
                        k.op("pe", lambda: nc.tensor.matmul(pdC[:, 0:129], lhsT=kw[:], rhs=vt[:, h, :], start=True, stop=True), R=[kwb, vb], W=[pdCb])
                        k.op("dve", lambda: nc.vector.scalar_tensor_tensor(out=CT[h][:], in0=CT[h][:], scalar=Gt[:, 24 + h:25 + h], in1=pdC[:, 0:129], op0=ALU.mult, op1=ALU.add),
                             R=[CTb[h], Gb, pdCb], W=[CTb[h]])
                    gg, ggb = ggr.next()
                    k.op("act", lambda: nc.scalar.activation(out=gg[:, 0:512], in_=oz[:, 0:512], func=AF.Sigmoid), R=[ozb], W=[ggb])
                    k.op("act", lambda: nc.scalar.activation(out=gg[:, 512:1024], in_=oz[:, 512:1024], func=AF.Silu), R=[ozb], W=[ggb])
                    k.op("pool", lambda: nc.gpsimd.tensor_tensor(out=gg[:, 0:512], in0=gg[:, 0:512], in1=gg[:, 512:1024], op=ALU.mult), R=[ggb], W=[ggb])
                    k.op("pool", lambda: nc.gpsimd.tensor_tensor(out=gg[:, 0:512], in0=gg[:, 0:512], in1=gbc[:], op=ALU.mult), R=[ggb, pb_], W=[ggb])
                    k.op("dve", lambda: nc.vector.tensor_tensor(out=y[:], in0=y[:], in1=gg[:, 0:512], op=ALU.mult), R=[yb, ggb], W=[yb])
                    mt, mtb = mtr.next()
                    for j in range(4):
                        k.op("pe", lambda: nc.tensor.transpose(pKT[:, 128 + j * 64:128 + j * 64 + 64] if False else pKT[:, 0:128], y[:, j * 128:(j + 1) * 128], ident), R=[yb, cstb], W=[pKTb])
                        k.op("act", lambda: nc.scalar.copy(out=mt[:, j, :], in_=pKT[:, 0:128]), R=[pKTb], W=[mtb])
                    k.dma("pool", mixT.rearrange("(cc p) t -> p cc t", p=128)[:, 0:4, t0:t0 + 128], mt[:], R=[mtb])
                k.barrier()

            with ExitStack() as es:
                SB = lambda name, shape: es.enter_context(sbt(nc, name, shape, F32))
                PS = lambda name: es.enter_context(pst(nc, name, [128, 512], F32))
                tri = cst[:, C_TRI:C_TRI + 128]
                wuq = SB("a_wuq", [128, 4, 1536]); wukv = SB("a_wukv", [128, 2, 2048]); gq = SB("a_gq", [128, 6])
                wb_ = Buf()
                k.dma("sp", wuq[:], mla_w_uq[l].rearrange("(c p) n -> p c n", p=128), W=[wb_])
                k.dma("sp", wukv[:], mla_w_ukv[l].rearrange("(c p) n -> p c n", p=128), W=[wb_])
                k.dma("sp", gq[:, 0:4], mla_q_norm_g[l].rearrange("(c p) -> p c", p=128), W=[wb_], slow=True)
                k.dma("sp", gq[:, 4:6], mla_kv_norm_g[l].rearrange("(c p) -> p c", p=128), W=[wb_], slow=True)
                xcr = Ring(es, nc, "a_xc", [128, 6, 128], F32, 2)
                sqr = Ring(es, nc, "a_sq", [128, 6, 128], F32, 2)
                rbr = Ring(es, nc, "a_rb", [128, 2, 128], F32, 2)
                cnr = Ring(es, nc, "a_cn", [128, 6, 128], F32, 2)
                csr = Ring(es, nc, "a_cs", [128, 64], F32, 2)
                kxr = Ring(es, nc, "a_kx", [128, 64], F32, 2)
                stq = Ring(es, nc, "a_stq", [128, 4, 128], BF16, 4)
                tmr = Ring(es, nc, "a_tm", [128, 8, 32], F32, 4)
                qrr = Ring(es, nc, "a_qr", [128, 8, 64], F32, 2)
                krr = Ring(es, nc, "a_kr", [128, 64], F32, 2)
                kst = Ring(es, nc, "a_kst", [64, 128], BF16, 2)
                var_ = Ring(es, nc, "a_va", [128, 8, 129], BF16, 2)
                for (vt, vb) in var_.items:
                    k.op("pool", lambda: nc.gpsimd.memset(vt[:, :, 128:129], 1.0), W=[vb])
                pS1 = PS("a_pS1"); pS2 = PS("a_pS2"); pS1b = Buf(); pS2b = Buf()
                pq = Ring(es, nc, "a_pq", [128, 512], F32, 4, psum=True)
                qn_v = qnT.rearrange("(h p) t -> p h t", p=128)
                kn_v = knT.rearrange("(h p) t -> p h t", p=128)
                qr_v = qrT.rearrange("(b p) t -> p b t", p=128)
                for c in range(NT):
                    t0 = c * 128
                    xc, xcb = xcr.next()
                    k.dma("sp", xc[:], pFM.rearrange("(ch p) t -> p ch t", p=128)[:, FM_CQ:FM_CQ + 6, t0:t0 + 128], W=[xcb])
                    cs_, csb = csr.next()
                    k.dma("sp", cs_[:], csd[t0:t0 + 128, :], W=[csb])
                    kx, kxb = kxr.next()
                    k.dma("sp", kx[:], pTM[t0:t0 + 128, TM_KR:TM_KR + 64], W=[kxb])
                    sq, sqb = sqr.next()
                    k.op("act", lambda: nc.scalar.activation(out=sq[:], in_=xc[:], func=AF.Square), R=[xcb], W=[sqb])
                    for ch in range(4):
                        k.op("pe", lambda: nc.tensor.matmul(pS1[:, 0:128], lhsT=ones, rhs=sq[:, ch, :], start=(ch == 0), stop=(ch == 3)), R=[sqb, cstb], W=[pS1b])
                    for ch in range(2):
                        k.op("pe", lambda: nc.tensor.matmul(pS2[:, 0:128], lhsT=ones, rhs=sq[:, 4 + ch, :], start=(ch == 0), stop=(ch == 1)), R=[sqb, cstb], W=[pS2b])
                    rb, rbb = rbr.next()
                    rstd(rb[:, 0, :], pS1[:, 0:128], 1.0 / 512, C_EPS, [pS1b], [rbb])
                    rstd(rb[:, 1, :], pS2[:, 0:128], 1.0 / 256, C_EPS, [pS2b], [rbb])
                    cn, cnb = cnr.next()
                    for ch in range(6):
                        k.op("dve", lambda: nc.vector.scalar_tensor_tensor(out=cn[:, ch, :], in0=xc[:, ch, :], scalar=gq[:, ch:ch + 1], in1=rb[:, 0 if ch < 4 else 1, :], op0=ALU.mult, op1=ALU.mult),
                             R=[xcb, wb_, rbb], W=[cnb])
                    for (W_, nch, c0_, hs, dst) in ((wuq, 4, 0, 192, qn_v), (wukv, 2, 4, 256, kn_v)):
                        for b in range(2):
                            p, pb = pq.next()
                            for hh_ in range(4):
                                h = b * 4 + hh_
                                for ch in range(nch):
                                    k.op("pe", lambda: nc.tensor.matmul(p[:, hh_ * 128:(hh_ + 1) * 128], lhsT=W_[:, ch, h * hs:h * hs + 128], rhs=cn[:, c0_ + ch, :],
                                                                        start=(ch == 0), stop=(ch == nch - 1)), R=[wb_, cnb], W=[pb])
                            st, sb = stq.next()
                            k.op("act", lambda: nc.scalar.copy(out=st[:], in_=p[:].rearrange("p (a b) -> p a b", a=4)), R=[pb], W=[sb])
                            k.dma("pool", dst[:, b * 4:(b + 1) * 4, t0:t0 + 128], st[:], R=[sb])
                    va, vab = var_.next()
                    for b in range(2):
                        p, pb = pq.next()
                        for ch in range(2):
                            k.op("pe", lambda: nc.tensor.matmul(p[:], lhsT=cn[:, 4 + ch, :], rhs=wukv[:, ch, :].rearrange("p (h d) -> p h d", d=256)[:, b * 4:(b + 1) * 4, 128:256],
                                                                start=(ch == 0), stop=(ch == 1)), R=[wb_, cnb], W=[pb])
                        k.op("act", lambda: nc.scalar.copy(out=va[:, b * 4:(b + 1) * 4, 0:128], in_=p[:].rearrange("p (a b) -> p a b", a=4)), R=[pb], W=[vab])
                    k.dma("pool", vaug.rearrange("h t d -> t h d")[t0:t0 + 128], va[:], R=[vab])
                    p, pb = pq.next()
                    for ch in range(4):
                        k.op("pe", lambda: nc.tensor.matmul(p[:], lhsT=cn[:, ch, :], rhs=wuq[:, ch, :].rearrange("p (h d) -> p h d", d=192)[:, :, 128:192],
                                                            start=(ch == 0), stop=(ch == 3)), R=[wb_, cnb], W=[pb])
                    pv = p[:].rearrange("p (h d) -> p h d", d=64)
                    sin8 = cs_[:, 0:32].unsqueeze(1).to_broadcast([128, 8, 32])
                    cos8 = cs_[:, 32:64].unsqueeze(1).to_broadcast([128, 8, 32])
                    qr, qrb = qrr.next()
                    t1, t1b = tmr.next(); t2, t2b = tmr.next(); t3, t3b = tmr.next(); t4, t4b = tmr.next()
                    k.op("dve", lambda: nc.vector.tensor_tensor(out=t1[:], in0=pv[:, :, 0:32], in1=cos8, op=ALU.mult), R=[pb, csb], W=[t1b])
                    k.op("dve", lambda: nc.vector.tensor_tensor(out=t2[:], in0=pv[:, :, 32:64], in1=sin8, op=ALU.mult), R=[pb, csb], W=[t2b])
                    k.op("dve", lambda: nc.vector.tensor_tensor(out=t3[:], in0=pv[:, :, 32:64], in1=cos8, op=ALU.mult), R=[pb, csb], W=[t3b])
                    k.op("dve", lambda: nc.vector.tensor_tensor(out=t4[:], in0=pv[:, :, 0:32], in1=sin8, op=ALU.mult), R=[pb, csb], W=[t4b])
                    k.op("pool", lambda: nc.gpsimd.tensor_tensor(out=qr[:, :, 0:32], in0=t1[:], in1=t2[:], op=ALU.subtract), R=[t1b, t2b], W=[qrb])
                    k.op("pool", lambda: nc.gpsimd.tensor_tensor(out=qr[:, :, 32:64], in0=t3[:], in1=t4[:], op=ALU.add), R=[t3b, t4b], W=[qrb])
                    p, pb = pq.next()
                    for b in range(4):
                        k.op("pe", lambda: nc.tensor.transpose(p[:, b * 128:(b + 1) * 128], qr[:, 2 * b:2 * b + 2, :].rearrange("p a d -> p (a d)"), ident), R=[qrb, cstb], W=[pb])
                    st, sb = stq.next()
                    k.op("act", lambda: nc.scalar.copy(out=st[:], in_=p[:].rearrange("p (a b) -> p a b", a=4)), R=[pb], W=[sb])
                    k.dma("pool", qr_v[:, :, t0:t0 + 128], st[:], R=[sb])
                    kr_, krb = krr.next()
                    t1, t1b = tmr.next(); t2, t2b = tmr.next()
                    k.op("dve", lambda: nc.vector.tensor_tensor(out=t1[:, 0, :], in0=kx[:, 0:32], in1=cs_[:, 32:64], op=ALU.mult), R=[kxb, csb], W=[t1b])
                    k.op("dve", lambda: nc.vector.tensor_tensor(out=t1[:, 1, :], in0=kx[:, 32:64], in1=cs_[:, 0:32], op=ALU.mult), R=[kxb, csb], W=[t1b])
                    k.op("dve", lambda: nc.vector.tensor_tensor(out=t2[:, 0, :], in0=kx[:, 32:64], in1=cs_[:, 32:64], op=ALU.mult), R=[kxb, csb], W=[t2b])
                    k.op("dve", lambda: nc.vector.tensor_tensor(out=t2[:, 1, :], in0=kx[:, 0:32], in1=cs_[:, 0:32], op=ALU.mult), R=[kxb, csb], W=[t2b])
                    k.op("pool", lambda: nc.gpsimd.tensor_tensor(out=kr_[:, 0:32], in0=t1[:, 0, :], in1=t1[:, 1, :], op=ALU.subtract), R=[t1b], W=[krb])
                    k.op("pool", lambda: nc.gpsimd.tensor_tensor(out=kr_[:, 32:64], in0=t2[:, 0, :], in1=t2[:, 1, :], op=ALU.add), R=[t2b], W=[krb])
                    p, pb = pq.next()
                    k.op("pe", lambda: nc.tensor.transpose(p[0:64, 0:128], kr_[:], ident), R=[krb, cstb], W=[pb])
                    ks, ksb = kst.next()
                    k.op("act", lambda: nc.scalar.copy(out=ks[:], in_=p[0:64, 0:128]), R=[pb], W=[ksb])
                    k.dma("pool", krT[:, t0:t0 + 128], ks[:], R=[ksb])
                k.barrier()

            with ExitStack() as es:
                SB = lambda name, shape: es.enter_context(sbt(nc, name, shape, F32))
                tri = cst[:, C_TRI:C_TRI + 128]
                SBh = lambda name, shape: es.enter_context(sbt(nc, name, shape, BF16))
                krt = SBh("b_krt", [64, T]); krtb = Buf()
                k.dma("sp", krt[:], krT, W=[krtb])
                knh = SBh("b_kn", [128, T]); qnh = SBh("b_qn", [128, T]); qrh = SBh("b_qr", [64, T])
                vah = SBh("b_va", [128, NT, 129]); zh = SB("b_z", [128, NT, 128])
                trib = SBh("b_tri", [128, 128]); tribb = Buf()
                k.op("dve", lambda: nc.vector.tensor_copy(out=trib[:], in_=tri), R=[cstb], W=[tribb])
                knb, qnb, qrb, vahb, zhb = [Buf() for _ in range(5)]
                Ptr = Ring(es, nc, "b_P", [128, 512], BF16, 3)
                smr = Ring(es, nc, "b_sm", [128, 2], F32, 2)
                ytr = Ring(es, nc, "b_y", [128, 128], F32, 2)
                szr = Ring(es, nc, "b_sz", [128, 128], F32, 2)
                sty = Ring(es, nc, "b_sty", [128, 128], BF16, 3)
                pST = Ring(es, nc, "b_pST", [128, 512], F32, 3, psum=True)
                pO = Ring(es, nc, "b_pO", [128, 512], F32, 2, psum=True)
                pX = Ring(es, nc, "b_pX", [128, 512], F32, 2, psum=True)
                SCL = float(192 ** -0.5)
                for h in range(8):
                    k.dma("sp", knh[:], knT[h * 128:(h + 1) * 128, :], W=[knb])
                    k.dma("sp", qnh[:], qnT[h * 128:(h + 1) * 128, :], W=[qnb])
                    k.dma("sp", qrh[:], qrT[h * 64:(h + 1) * 64, :], W=[qrb])
                    k.dma("sp", vah[:], vaug[h].rearrange("(n p) d -> p n d", p=128), W=[vahb])
                    k.dma("sp", zh[:], pTM[:, TM_MZ + h * 128:TM_MZ + (h + 1) * 128].rearrange("(n p) d -> p n d", p=128), W=[zhb])
                    for qt in range(NT):
                        qs = slice(qt * 128, (qt + 1) * 128)
                        po, pob_ = pO.next()
                        for kb in range(0, qt + 1, 4):
                            nk = min(4, qt + 1 - kb)
                            ps_, psb = pST.next()
                            for i in range(nk):
                                j = kb + i
                                js = slice(j * 128, (j + 1) * 128)
                                k.op("pe", lambda: nc.tensor.matmul(ps_[:, i * 128:(i + 1) * 128], lhsT=knh[:, js], rhs=qnh[:, qs], start=True, stop=False), R=[knb, qnb], W=[psb])
                                k.op("pe", lambda: nc.tensor.matmul(ps_[:, i * 128:(i + 1) * 128], lhsT=krt[:, js], rhs=qrh[:, qs], start=False, stop=True), R=[krtb, qrb], W=[psb])
                            Pt, Ptb = Ptr.next()
                            k.op("act", lambda: nc.scalar.activation(out=Pt[:, 0:nk * 128], in_=ps_[:, 0:nk * 128], func=AF.Exp, scale=SCL), R=[psb], W=[Ptb])
                            if kb + nk - 1 == qt:
                                i = nk - 1
                                k.op("pool", lambda: nc.gpsimd.tensor_tensor(out=Pt[:, i * 128:(i + 1) * 128], in0=Pt[:, i * 128:(i + 1) * 128], in1=trib[:], op=ALU.mult), R=[Ptb, tribb], W=[Ptb])
                            for i in range(nk):
                                j = kb + i
                                k.op("pe", lambda: nc.tensor.matmul(po[:, 0:129], lhsT=Pt[:, i * 128:(i + 1) * 128], rhs=vah[:, j, :], start=(j == 0), stop=(j == qt)), R=[Ptb, vahb], W=[pob_])
                        sm, smb = smr.next()
                        k.op("dve", lambda: nc.vector.reciprocal(out=sm[:, 0:1], in_=po[:, 128:129]), R=[pob_], W=[smb])
                        yt, ytb = ytr.next()
                        k.op("dve", lambda: nc.vector.tensor_scalar(out=yt[:], in0=po[:, 0:128], scalar1=sm[:, 0:1], scalar2=None, op0=ALU.mult), R=[pob_, smb], W=[ytb])
                        sz, szb = szr.next()
                        k.op("act", lambda: nc.scalar.activation(out=sz[:], in_=zh[:, qt, :], func=AF.Silu), R=[zhb], W=[szb])
                        k.op("pool", lambda: nc.gpsimd.tensor_tensor(out=yt[:], in0=yt[:], in1=sz[:], op=ALU.mult), R=[ytb, szb], W=[ytb])
                        px, pxb = pX.next()
                        k.op("pe", lambda: nc.tensor.transpose(px[:, 0:128], yt[:], ident), R=[ytb, cstb], W=[pxb])
                        st, sb = sty.next()
                        k.op("dve", lambda: nc.vector.tensor_copy(out=st[:], in_=px[:, 0:128]), R=[pxb], W=[sb])
                        k.dma("pool", mixT[512 + h * 128:512 + (h + 1) * 128, qs], st[:], R=[sb])
                k.barrier()

            NCH = T // 64
            with ExitStack() as es:
                SB = lambda name, shape: es.enter_context(sbt(nc, name, shape, F32))
                mu = SB("r_mu", [128, 13]); prm = SB("r_prm", [128, 6, 4]); w0bc = SB("r_w0", [128, 512])
                w2t = SB("r_w2", [64, 512]); a2t = SB("r_a2", [128, 512])
                v1t = SB("r_v1", [128, 4, 32]); v2t = SB("r_v2", [32, 512])
                prb = Buf()
                k.dma("sp", mu[:], rw_mu[l].rearrange("(c p) -> p c", p=128), W=[prb], slow=True)
                plist = [rw_a0[l], rw_k_k[l], rw_k_a[l], rw_r_k[l].rearrange("h d -> (h d)")]
                if l > 0:
                    plist.append(rw_v0[l - 1])
                for i_, src in enumerate(plist):
                    k.dma("sp", prm[:, i_, :], src.rearrange("(c p) -> p c", p=128), W=[prb], slow=True)
                k.dma("sp", w0bc[:], rw_w0[l].partition_broadcast(128), W=[prb])
                k.dma("sp", w2t[:], rw_w2[l], W=[prb])
                k.dma("sp", a2t[64:128, :], rw_a2[l], W=[prb])
                if l > 0:
                    k.dma("sp", v1t[:], rw_v1[l - 1].rearrange("(c p) n -> p c n", p=128), W=[prb])
                    k.dma("sp", v2t[:], rw_v2[l - 1], W=[prb])
                k.op("dve", lambda: nc.vector.tensor_scalar(out=prm[:, 5, :], in0=prm[:, 2, :], scalar1=-1.0, scalar2=1.0, op0=ALU.mult, op1=ALU.add), R=[prb], W=[prb])
                bc = lambda ap_: ap_.unsqueeze(2).to_broadcast([128, 4, 128])
                Xr = Ring(es, nc, "r_X", [128, 13, 129], F32, 2)
                dr = Ring(es, nc, "r_d", [128, 13, 128], F32, 1)
                xsr = Ring(es, nc, "r_xs", [128, 13, 128], F32, 2)
                twr = Ring(es, nc, "r_tw", [64, 128], F32, 2)
                ldr = Ring(es, nc, "r_ld", [128, 512], F32, 2)
                T4 = lambda name, n=1: Ring(es, nc, name, [128, 4, 128], F32, n)
                gir, ger, aar, kkr_, khr, bvr = T4("r_gi"), T4("r_ge"), T4("r_aa"), T4("r_kk"), T4("r_kh"), T4("r_bv")
                e1r, e2r, e3r, e4r = T4("r_e1"), T4("r_e2"), T4("r_e3"), T4("r_e4")
                tmpr = T4("r_tmp", 3)
                outr = T4("r_out", 4)
                vfr = T4("r_vf", 2)
                m1r = Ring(es, nc, "r_m1", [32, 128], F32, 2)
                tmo = Ring(es, nc, "r_tmo", [128, 512], F32, 3)
                rkro = Ring(es, nc, "r_rkr", [128, 8], F32, 2)
                glo = Ring(es, nc, "r_glo", [128, 4, 2], F32, 2)
                pr = Ring(es, nc, "r_ps", [128, 512], F32, 8, psum=True)
                fmv = lambda dt_: dt_.rearrange("(j p) t -> p j t", p=128)
                for c in range(NT):
                    t0 = c * 128
                    X, Xb = Xr.next()
                    src = pFM.rearrange("(ch p) t -> p ch t", p=128)
                    if c == 0:
                        k.op("pool", lambda: nc.gpsimd.memset(X[:, :, 0:1], 0.0), W=[Xb])
                        k.dma("sp", X[:, :, 1:129], src[:, FM_RW:FM_RW + 13, 0:128], W=[Xb])
                    else:
                        k.dma("sp", X[:], src[:, FM_RW:FM_RW + 13, t0 - 1:t0 + 128], W=[Xb])
                    d_, db = dr.next()
                    k.op("dve", lambda: nc.vector.tensor_tensor(out=d_[:], in0=X[:, :, 0:128], in1=X[:, :, 1:129], op=ALU.subtract), R=[Xb], W=[db])
                    xs_, xsb = xsr.next()
                    for ch in range(13):
                        k.op("dve", lambda: nc.vector.scalar_tensor_tensor(out=xs_[:, ch, :], in0=d_[:, ch, :], scalar=mu[:, ch:ch + 1], in1=X[:, ch, 1:129], op0=ALU.mult, op1=ALU.add),
                             R=[db, Xb, prb], W=[xsb])
                    rr = xs_[:, 0:4, :]; kx = xs_[:, 4:8, :]; vv = xs_[:, 8:12, :]
                    tw, twb = twr.next()
                    k.op("act", lambda: nc.scalar.activation(out=tw[:], in_=xs_[0:64, 12, :], func=AF.Tanh), R=[xsb], W=[twb])
                    pz, pzb = pr.next()
                    k.op("pe", lambda: nc.tensor.matmul(pz[:], lhsT=tw[:], rhs=w2t[:], start=True, stop=True), R=[twb, prb], W=[pzb])
                    ld, ldb = ldr.next()
                    k.op("dve", lambda: nc.vector.tensor_tensor(out=ld[:], in0=pz[:], in1=w0bc[:], op=ALU.add), R=[pzb, prb], W=[ldb])
                    k.op("act", lambda: nc.scalar.activation(out=ld[:], in_=ld[:], func=AF.Sigmoid), R=[ldb], W=[ldb])
                    k.op("pool", lambda: nc.gpsimd.tensor_scalar(out=ld[:], in0=ld[:], scalar1=float(-np.exp(-0.5)), scalar2=None, op0=ALU.mult), R=[ldb], W=[ldb])
                    pgi, pgib = pr.next(); pge, pgeb = pr.next()
                    for j in range(4):
                        k.op("pe", lambda: nc.tensor.matmul(pgi[:, j * 128:(j + 1) * 128], lhsT=ld[:, j * 128:(j + 1) * 128], rhs=cst[:, C_BI:C_BI + 128], start=True, stop=True), R=[ldb, cstb], W=[pgib])
                        k.op("pe", lambda: nc.tensor.matmul(pge[:, j * 128:(j + 1) * 128], lhsT=ld[:, j * 128:(j + 1) * 128], rhs=cst[:, C_BS:C_BS + 128], start=True, stop=True), R=[ldb, cstb], W=[pgeb])
                    gi, gib = gir.next(); ge, geb = ger.next()
                    k.op("act", lambda: nc.scalar.copy(out=gi[:], in_=pgi[:].rearrange("p (a b) -> p a b", a=4)), R=[pgib], W=[gib])
                    e1, e1b = e1r.next(); e2, e2b = e2r.next(); e3, e3b = e3r.next(); e4, e4b = e4r.next()
                    k.op("act", lambda: nc.scalar.activation(out=e1[:], in_=gi[:], func=AF.Exp), R=[gib], W=[e1b])
                    k.op("act", lambda: nc.scalar.activation(out=e2[:], in_=pge[:].rearrange("p (a b) -> p a b", a=4), func=AF.Exp), R=[pgeb], W=[e2b])
                    k.op("act", lambda: nc.scalar.activation(out=e3[:], in_=gi[:], func=AF.Exp, scale=-1.0), R=[gib], W=[e3b])
                    for j in range(4):
                        for hf in range(2):
                            k.op("act", lambda: nc.scalar.activation(out=e4[:, j, hf * 64:(hf + 1) * 64], in_=gi[:, j, hf * 64:(hf + 1) * 64], func=AF.Exp, scale=-1.0,
                                                                     bias=gi[:, j, hf * 64 + 63:hf * 64 + 64]), R=[gib], W=[e4b])
                    pa, pab = pr.next()
                    for j in range(4):
                        k.op("pe", lambda: nc.tensor.matmul(pa[:, j * 128:(j + 1) * 128], lhsT=a2t[64:128, j * 128:(j + 1) * 128], rhs=xs_[64:128, 12, :], start=True, stop=True), R=[xsb, prb], W=[pab])
                    aa, aab = aar.next()
                    for j in range(4):
                        k.op("act", lambda: nc.scalar.activation(out=aa[:, j, :], in_=pa[:, j * 128:(j + 1) * 128], func=AF.Sigmoid, bias=prm[:, 0, j:j + 1]), R=[pab, prb], W=[aab])
                    if l > 0:
                        pm, pmb = pr.next()
                        for ch in range(4):
                            k.op("pe", lambda: nc.tensor.matmul(pm[0:32, 0:128], lhsT=v1t[:, ch, :], rhs=xs_[:, 8 + ch, :], start=(ch == 0), stop=(ch == 3)), R=[xsb, prb], W=[pmb])
                        m1, m1b = m1r.next()
                        k.op("act", lambda: nc.scalar.copy(out=m1[:], in_=pm[0:32, 0:128]), R=[pmb], W=[m1b])
                        pm2, pm2b = pr.next()
                        for j in range(4):
                            k.op("pe", lambda: nc.tensor.matmul(pm2[:, j * 128:(j + 1) * 128], lhsT=v2t[:, j * 128:(j + 1) * 128], rhs=m1[:], start=True, stop=True), R=[m1b, prb], W=[pm2b])
                        gt_, gtb_ = tmpr.next()
                        for j in range(4):
                            k.op("act", lambda: nc.scalar.activation(out=gt_[:, j, :], in_=pm2[:, j * 128:(j + 1) * 128], func=AF.Sigmoid, bias=prm[:, 4, j:j + 1]), R=[pm2b, prb], W=[gtb_])
                        vf, vfb = vfr.next()
                        k.dma("sp", vf[:], fmv(vfirst)[:, :, t0:t0 + 128], W=[vfb])
                        k.op("dve", lambda: nc.vector.tensor_tensor(out=vf[:], in0=vf[:], in1=vv, op=ALU.subtract), R=[vfb, xsb], W=[vfb])
                        k.op("pool", lambda: nc.gpsimd.tensor_tensor(out=vf[:], in0=vf[:], in1=gt_[:], op=ALU.mult), R=[vfb, gtb_], W=[vfb])
                        k.op("dve", lambda: nc.vector.tensor_tensor(out=xs_[:, 8:12, :], in0=vv, in1=vf[:], op=ALU.add), R=[vfb, xsb], W=[xsb])
                    else:
                        k.dma("pool", fmv(vfirst)[:, :, t0:t0 + 128], vv, R=[xsb])
                    kk, kkb = kkr_.next()
                    k.op("dve", lambda: nc.vector.tensor_tensor(out=kk[:], in0=kx, in1=bc(prm[:, 1, :]), op=ALU.mult), R=[xsb, prb], W=[kkb])
                    sq_, sqb_ = tmpr.next()
                    k.op("act", lambda: nc.scalar.activation(out=sq_[:], in_=kk[:], func=AF.Square), R=[kkb], W=[sqb_])
                    pn, pnb = pr.next()
                    for j in range(4):
                        k.op("pe", lambda: nc.tensor.matmul(pn[:, j * 128:(j + 1) * 128], lhsT=cst[:, C_BD:C_BD + 128], rhs=sq_[:, j, :], start=True, stop=True), R=[sqb_, cstb], W=[pnb])
                    rn, rnb = tmpr.next()
                    k.op("act", lambda: nc.scalar.activation(out=rn[:], in_=pn[:].rearrange("p (a b) -> p a b", a=4), func=AF.Sqrt), R=[pnb], W=[rnb])
                    k.op("dve", lambda: nc.vector.tensor_scalar_max(out=rn[:], in0=rn[:], scalar1=1e-12), R=[rnb], W=[rnb])
                    k.op("dve", lambda: nc.vector.reciprocal(out=rn[:], in_=rn[:]), R=[rnb], W=[rnb])
                    k.op("pool", lambda: nc.gpsimd.tensor_tensor(out=kk[:], in0=kk[:], in1=rn[:], op=ALU.mult), R=[kkb, rnb], W=[kkb])
                    kh, khb = khr.next()
                    k.op("dve", lambda: nc.vector.tensor_tensor(out=kh[:], in0=aa[:], in1=bc(prm[:, 2, :]), op=ALU.mult), R=[aab, prb], W=[khb])
                    k.op("dve", lambda: nc.vector.tensor_tensor(out=kh[:], in0=kh[:], in1=bc(prm[:, 5, :]), op=ALU.add), R=[khb, prb], W=[khb])
                    k.op("dve", lambda: nc.vector.tensor_tensor(out=kh[:], in0=kh[:], in1=kx, op=ALU.mult), R=[khb, xsb], W=[khb])
                    bv, bvb = bvr.next()
                    k.op("pool", lambda: nc.gpsimd.tensor_tensor(out=bv[:], in0=kk[:], in1=aa[:], op=ALU.mult), R=[kkb, aab], W=[bvb])
                    def emit(dst, in0, in1, neg=False, R=()):
                        o, ob = outr.next()
                        k.op("dve", lambda: nc.vector.tensor_tensor(out=o[:], in0=in0, in1=in1, op=ALU.mult), R=list(R), W=[ob])
                        if neg:
                            k.op("pool", lambda: nc.gpsimd.tensor_scalar(out=o[:], in0=o[:], scalar1=-1.0, scalar2=None, op0=ALU.mult), R=[ob], W=[ob])
                        k.dma("pool", fmv(dst)[:, :, t0:t0 + 128], o[:], R=[ob])
                        return o, ob
                    emit(rwA, kk[:], e2[:], neg=True, R=[kkb, e2b])
                    emit(rwR, rr, e1[:], R=[xsb, e1b])
                    emit(rwB, bv[:], e3[:], R=[bvb, e3b])
                    emit(rwK, kh[:], e3[:], R=[khb, e3b])
                    for (dst, a_, ab_) in ((rwBp, bv, bvb), (rwKp, kh, khb), (rwV, None, None)):
                        if a_ is not None:
                            o, ob = outr.next()
                            k.op("dve", lambda: nc.vector.tensor_tensor(out=o[:], in0=a_[:], in1=e4[:], op=ALU.mult), R=[ab_, e4b], W=[ob])
                            srcv = o
                        else:
                            srcv, ob = xs_[:, 8:12, :], xsb
                        pt_, ptb_ = pr.next()
                        for j in range(4):
                            k.op("pe", lambda: nc.tensor.transpose(pt_[:, j * 128:(j + 1) * 128], srcv[:, j, :], ident), R=[ob, cstb], W=[ptb_])
                        to, tob = tmo.next()
                        k.op("act", lambda: nc.scalar.copy(out=to[:], in_=pt_[:]), R=[ptb_], W=[tob])
                        k.dma("pool", dst[t0:t0 + 128, :], to[:], R=[tob])
                    go_, gob = glo.next()
                    k.op("pool", lambda: nc.gpsimd.tensor_copy(out=go_[:, :, 0:1], in_=e1[:, :, 63:64]), R=[e1b], W=[gob])
                    k.op("pool", lambda: nc.gpsimd.tensor_copy(out=go_[:, :, 1:2], in_=e1[:, :, 127:128]), R=[e1b], W=[gob])
                    k.dma("pool", rwGL.rearrange("(j p) c -> p j c", p=128)[:, :, 2 * c:2 * c + 2], go_[:], R=[gob], slow=True)
                    pd_, pdb_ = tmpr.next()
                    k.op("dve", lambda: nc.vector.tensor_tensor(out=pd_[:], in0=rr, in1=kh[:], op=ALU.mult), R=[xsb, khb], W=[pdb_])
                    k.op("pool", lambda: nc.gpsimd.tensor_tensor(out=pd_[:], in0=pd_[:], in1=bc(prm[:, 3, :]), op=ALU.mult), R=[pdb_, prb], W=[pdb_])
                    pk, pkb = pr.next()
                    for j in range(4):
                        k.op("pe", lambda: nc.tensor.matmul(pk[:, 0:8], lhsT=pd_[:, j, :], rhs=cst[:, C_HS + 8 * j:C_HS + 8 * j + 8], start=(j == 0), stop=(j == 3)), R=[pdb_, cstb], W=[pkb])
                    ro, rob = rkro.next()
                    k.op("act", lambda: nc.scalar.copy(out=ro[:], in_=pk[:, 0:8]), R=[pkb], W=[rob])
                    k.dma("pool", rwRKR[t0:t0 + 128, :], ro[:], R=[rob])
                k.barrier()

            with ExitStack() as es:
                SB = lambda name, shape: es.enter_context(sbt(nc, name, shape, F32))
                GLt = SB("c_GL", [64, 8, NCH]); glb = Buf()
                k.dma("sp", GLt[:], rwGL.rearrange("(h q) c -> q h c", q=64), W=[glb])
                hv = lambda dt_: dt_.rearrange("(h q) t -> q h t", q=64)
                ARr = Ring(es, nc, "c_AR", [64, 8, 2, 64], F32, 3)
                BKr = Ring(es, nc, "c_BK", [64, 8, 2, 64], F32, 3)
                TMr = Ring(es, nc, "c_TM", [64, 3, 512], F32, 3)
                Hr = Ring(es, nc, "c_H", [64, 8, 64], F32, 2)
                A1r = Ring(es, nc, "c_A1", [64, 8, 128], F32, 2)
                A2r = Ring(es, nc, "c_A2", [64, 8, 128], F32, 2)
                Pr_ = Ring(es, nc, "c_P", [64, 8, 64], F32, 3)
                Ptr_ = Ring(es, nc, "c_Pt", [64, 8, 64], F32, 3)
                Lr = Ring(es, nc, "c_L", [64, 8, 64], F32, 3)
                Xsr = Ring(es, nc, "c_Xs", [64, 8, 64], F32, 2)
                Usr = Ring(es, nc, "c_Us", [64, 8, 64], F32, 2)
                Yr = Ring(es, nc, "c_Y", [64, 512], F32, 3)
                pr = Ring(es, nc, "c_ps", [128, 512], F32, 8, psum=True)
                m1 = cst[0:64, C_M1:C_M1 + 128].unsqueeze(1).to_broadcast([64, 4, 128])
                m3 = cst[0:64, C_M3:C_M3 + 64].unsqueeze(1).to_broadcast([64, 8, 64])
                i64b = cst[0:64, 0:64].unsqueeze(1).to_broadcast([64, 8, 64])
                H, Hb = Hr.next()
                k.op("pool", lambda: nc.gpsimd.memset(H[:], 0.0), W=[Hb])
                v8 = lambda p_: p_[0:64, :].rearrange("p (h d) -> p h d", h=8)
                for c in range(NCH):
                    cs_ = slice(c * 64, (c + 1) * 64)
                    AR, ARb = ARr.next(); BK, BKb = BKr.next(); TM_, TMb = TMr.next()
                    k.dma("sp", AR[:, :, 0, :], hv(rwA)[:, :, cs_], W=[ARb])
                    k.dma("sp", AR[:, :, 1, :], hv(rwR)[:, :, cs_], W=[ARb])
                    k.dma("sp", BK[:, :, 0, :], hv(rwB)[:, :, cs_], W=[BKb])
                    k.dma("sp", BK[:, :, 1, :], hv(rwK)[:, :, cs_], W=[BKb])
                    k.dma("sp", TM_[:, 0, :], rwBp[cs_, :], W=[TMb])
                    k.dma("sp", TM_[:, 1, :], rwKp[cs_, :], W=[TMb])
                    k.dma("sp", TM_[:, 2, :], rwV[cs_, :], W=[TMb])
                    A1, A1b = A1r.next(); A2, A2b = A2r.next()
                    for (A_, Ab_, which) in ((A1, A1b, 0), (A2, A2b, 1)):
                        for b in range(2):
                            p, pb = pr.next()
                            for hh_ in range(4):
                                h = b * 4 + hh_
                                k.op("pe", lambda: nc.tensor.matmul(p[0:64, hh_ * 128:(hh_ + 1) * 128], lhsT=BK[:, h, which, :], rhs=AR[:, h, :, :].rearrange("p a d -> p (a d)"), start=True, stop=True),
                                     R=[BKb, ARb], W=[pb])
                            k.op("dve", lambda: nc.vector.tensor_tensor(out=A_[:, b * 4:(b + 1) * 4, :], in0=p[0:64, :].rearrange("p (h d) -> p h d", h=4), in1=m1, op=ALU.mult), R=[pb, cstb], W=[Ab_])
                    p, pb = pr.next()
                    for h in range(8):
                        k.op("pe", lambda: nc.tensor.matmul(p[0:64, h * 64:(h + 1) * 64], lhsT=AR[:, h, 0, :], rhs=BK[:, h, 0, :], start=True, stop=True), R=[ARb, BKb], W=[pb])
                    Pt_, Ptb_ = Ptr_.next()
                    k.op("dve", lambda: nc.vector.tensor_tensor(out=Pt_[:], in0=v8(p), in1=m3, op=ALU.mult), R=[pb, cstb], W=[Ptb_])
                    P_, Pb_ = Pr_.next()
                    k.op("pool", lambda: nc.gpsimd.tensor_copy(out=P_[:], in_=A1[:, :, 0:64]), R=[A1b], W=[Pb_])
                    L_, Lb_ = Lr.next()
                    k.op("pool", lambda: nc.gpsimd.tensor_tensor(out=L_[:], in0=A1[:, :, 0:64], in1=i64b, op=ALU.add), R=[A1b, cstb], W=[Lb_])
                    for lev in range(5):
                        pa, pab = pr.next(); pb2, pb2b = pr.next()
                        for h in range(8):
                            k.op("pe", lambda: nc.tensor.matmul(pa[0:64, h * 64:(h + 1) * 64], lhsT=Pt_[:, h, :], rhs=P_[:, h, :], start=True, stop=True), R=[Ptb_, Pb_], W=[pab])
                        for h in range(8):
                            k.op("pe", lambda: nc.tensor.matmul(pb2[0:64, h * 64:(h + 1) * 64], lhsT=P_[:, h, :], rhs=Pt_[:, h, :], start=True, stop=True), R=[Ptb_, Pb_], W=[pb2b])
                        Pn, Pnb = Pr_.next(); Ptn, Ptnb = Ptr_.next()
                        k.op("act", lambda: nc.scalar.copy(out=Pn[:], in_=v8(pa)), R=[pab], W=[Pnb])
                        k.op("dve", lambda: nc.vector.tensor_copy(out=Ptn[:], in_=v8(pb2)), R=[pb2b], W=[Ptnb])
                        P_, Pb_, Pt_, Ptb_ = Pn, Pnb, Ptn, Ptnb
                        pc, pcb = pr.next()
                        for h in range(8):
                            k.op("pe", lambda: nc.tensor.matmul(pc[0:64, h * 64:(h + 1) * 64], lhsT=Pt_[:, h, :], rhs=L_[:, h, :], start=True, stop=True), R=[Ptb_, Lb_], W=[pcb])
                        Ln, Lnb = Lr.next()
                        k.op("dve", lambda: nc.vector.tensor_tensor(out=Ln[:], in0=L_[:], in1=v8(pc), op=ALU.add), R=[Lb_, pcb], W=[Lnb])
                        L_, Lb_ = Ln, Lnb
                    Vh = lambda h: TM_[:, 2, h * 64:(h + 1) * 64]
                    px, pxb = pr.next()
                    for h in range(8):
                        k.op("pe", lambda: nc.tensor.matmul(px[0:64, h * 64:(h + 1) * 64], lhsT=AR[:, h, 0, :], rhs=H[:, h, :], start=True, stop=False), R=[ARb, Hb], W=[pxb])
                        k.op("pe", lambda: nc.tensor.matmul(px[0:64, h * 64:(h + 1) * 64], lhsT=A2[:, h, 0:64], rhs=Vh(h), start=False, stop=True), R=[A2b, TMb], W=[pxb])
                    Xs, Xsb = Xsr.next()
                    k.op("act", lambda: nc.scalar.copy(out=Xs[:], in_=v8(px)), R=[pxb], W=[Xsb])
                    pu, pub = pr.next()
                    for h in range(8):
                        k.op("pe", lambda: nc.tensor.matmul(pu[0:64, h * 64:(h + 1) * 64], lhsT=L_[:, h, :], rhs=Xs[:, h, :], start=True, stop=True), R=[Lb_, Xsb], W=[pub])
                    Us, Usb = Usr.next()
                    k.op("dve", lambda: nc.vector.tensor_copy(out=Us[:], in_=v8(pu)), R=[pub], W=[Usb])
                    py, pyb = pr.next()
                    for h in range(8):
                        k.op("pe", lambda: nc.tensor.matmul(py[0:64, h * 64:(h + 1) * 64], lhsT=AR[:, h, 1, :], rhs=H[:, h, :], start=True, stop=False), R=[ARb, Hb], W=[pyb])
                        k.op("pe", lambda: nc.tensor.matmul(py[0:64, h * 64:(h + 1) * 64], lhsT=A1[:, h, 64:128], rhs=Us[:, h, :], start=False, stop=False), R=[A1b, Usb], W=[pyb])
                        k.op("pe", lambda: nc.tensor.matmul(py[0:64, h * 64:(h + 1) * 64], lhsT=A2[:, h, 64:128], rhs=Vh(h), start=False, stop=True), R=[A2b, TMb], W=[pyb])
                    Yt, Ytb = Yr.next()
                    k.op("act", lambda: nc.scalar.copy(out=Yt[:], in_=py[0:64, :]), R=[pyb], W=[Ytb])
                    k.dma("pool", rwY[cs_, :], Yt[:], R=[Ytb])
                    ph, phb = pr.next()
                    for h in range(8):
                        k.op("pe", lambda: nc.tensor.matmul(ph[0:64, h * 64:(h + 1) * 64], lhsT=TM_[:, 0, h * 64:(h + 1) * 64], rhs=Us[:, h, :], start=True, stop=False), R=[TMb, Usb], W=[phb])
                        k.op("pe", lambda: nc.tensor.matmul(ph[0:64, h * 64:(h + 1) * 64], lhsT=TM_[:, 1, h * 64:(h + 1) * 64], rhs=Vh(h), start=False, stop=True), R=[TMb], W=[phb])
                    Hn, Hnb = Hr.next()
                    k.op("dve", lambda: nc.vector.tensor_tensor(out=Hn[:], in0=H[:], in1=GLt[:, :, c:c + 1].to_broadcast([64, 8, 64]), op=ALU.mult), R=[Hb, glb], W=[Hnb])
                    k.op("dve", lambda: nc.vector.tensor_tensor(out=Hn[:], in0=Hn[:], in1=v8(ph), op=ALU.add), R=[Hnb, phb], W=[Hnb])
                    H, Hb = Hn, Hnb
                k.barrier()

            with ExitStack() as es:
                SB = lambda name, shape: es.enter_context(sbt(nc, name, shape, F32))
                lng = SB("e_g", [128, 512]); lnb = SB("e_b", [128, 512]); eb = Buf()
                k.dma("sp", lng[:], rw_ln_g[l].partition_broadcast(128), W=[eb])
                k.dma("sp", lnb[:], rw_ln_b[l].partition_broadcast(128), W=[eb])
                inr = Ring(es, nc, "e_in", [128, 3, 512], F32, 2)
                rkr_ = Ring(es, nc, "e_rk", [128, 8], F32, 2)
                sqr = Ring(es, nc, "e_sq", [128, 8, 64], F32, 2)
                str_ = Ring(es, nc, "e_st", [128, 4, 8], F32, 2)
                yr = Ring(es, nc, "e_y", [128, 8, 64], F32, 2)
                mtr = Ring(es, nc, "e_mt", [128, 4, 128], BF16, 2)
                pr = Ring(es, nc, "e_ps", [128, 512], F32, 2, psum=True)
                b8 = lambda ap_: ap_.unsqueeze(2).to_broadcast([128, 8, 64])
                v3 = lambda ap_: ap_.rearrange("p (h d) -> p h d", h=8)
                for c in range(NT):
                    t0 = c * 128
                    it, itb = inr.next()
                    k.dma("sp", it[:, 0, :], rwY[t0:t0 + 128, :], W=[itb])
                    k.dma("sp", it[:, 1, :], rwV[t0:t0 + 128, :], W=[itb])
                    k.dma("sp", it[:, 2, :], pTM[t0:t0 + 128, TM_RZ:TM_RZ + 512], W=[itb])
                    rk, rkb = rkr_.next()
                    k.dma("sp", rk[:], rwRKR[t0:t0 + 128, :], W=[rkb])
                    Y3 = v3(it[:, 0, :])
                    sq, sqb = sqr.next()
                    k.op("act", lambda: nc.scalar.activation(out=sq[:], in_=Y3, func=AF.Square), R=[itb], W=[sqb])
                    st, stb = str_.next()
                    k.op("dve", lambda: nc.vector.tensor_reduce(out=st[:, 0, :], in_=Y3, axis=AX.X, op=ALU.add), R=[itb], W=[stb])
                    k.op("dve", lambda: nc.vector.tensor_reduce(out=st[:, 1, :], in_=sq[:], axis=AX.X, op=ALU.add), R=[sqb], W=[stb])
                    k.op("dve", lambda: nc.vector.tensor_scalar(out=st[:, 0, :], in0=st[:, 0, :], scalar1=1.0 / 64, scalar2=None, op0=ALU.mult), R=[stb], W=[stb])
                    k.op("dve", lambda: nc.vector.tensor_tensor(out=st[:, 2, :], in0=st[:, 0, :], in1=st[:, 0, :], op=ALU.mult), R=[stb], W=[stb])
                    k.op("dve", lambda: nc.vector.scalar_tensor_tensor(out=st[:, 1, :], in0=st[:, 1, :], scalar=1.0 / 64, in1=st[:, 2, :], op0=ALU.mult, op1=ALU.subtract), R=[stb], W=[stb])
                    rstd(st[:, 3, :], st[:, 1, :], 1.0, C_GNEPS, [stb], [stb])
                    y, yb = yr.next()
                    k.op("dve", lambda: nc.vector.tensor_tensor(out=y[:], in0=Y3, in1=b8(st[:, 0, :]), op=ALU.subtract), R=[itb, stb], W=[yb])
                    k.op("dve", lambda: nc.vector.tensor_tensor(out=y[:], in0=y[:], in1=b8(st[:, 3, :]), op=ALU.mult), R=[yb, stb], W=[yb])
                    k.op("pool", lambda: nc.gpsimd.tensor_tensor(out=y[:], in0=y[:], in1=v3(lng[:]), op=ALU.mult), R=[yb, eb], W=[yb])
                    k.op("pool", lambda: nc.gpsimd.tensor_tensor(out=y[:], in0=y[:], in1=v3(lnb[:]), op=ALU.add), R=[yb, eb], W=[yb])
                    k.op("dve", lambda: nc.vector.tensor_tensor(out=sq[:], in0=v3(it[:, 1, :]), in1=b8(rk[:]), op=ALU.mult), R=[itb, rkb, sqb], W=[sqb])
                    k.op("dve", lambda: nc.vector.tensor_tensor(out=y[:], in0=y[:], in1=sq[:], op=ALU.add), R=[yb, sqb], W=[yb])
                    k.op("act", lambda: nc.scalar.activation(out=it[:, 2, :], in_=it[:, 2, :], func=AF.Silu), R=[itb], W=[itb])
                    k.op("dve", lambda: nc.vector.tensor_tensor(out=y[:], in0=y[:], in1=v3(it[:, 2, :]), op=ALU.mult), R=[yb, itb], W=[yb])
                    p, pb = pr.next()
                    yf = y[:].rearrange("p h d -> p (h d)")
                    for j in range(4):
                        k.op("pe", lambda: nc.tensor.transpose(p[:, j * 128:(j + 1) * 128], yf[:, j * 128:(j + 1) * 128], ident), R=[yb, cstb], W=[pb])
                    mt, mtb = mtr.next()
                    k.op("act", lambda: nc.scalar.copy(out=mt[:], in_=p[:].rearrange("p (a b) -> p a b", a=4)), R=[pb], W=[mtb])
                    k.dma("pool", mixT.rearrange("(cc p) t -> p cc t", p=128)[:, 12:16, t0:t0 + 128], mt[:], R=[mtb])
                k.barrier()

            with ExitStack() as es:
                mx = Ring(es, nc, "p5_m", [128, 16, G], BF16, 2)
                wfr = Ring(es, nc, "p5_wf", [128, 16, 512], F32, 2)
                wr = Ring(es, nc, "p5_w", [128, 16, 512], BF16, 2)
                xr = Ring(es, nc, "p5_x", [128, G], F32, 3)
                xo = Ring(es, nc, "p5_o", [128, G], F32, 3)
                ps = Ring(es, nc, "p5_ps", [128, 512], F32, 4, psum=True)
                for g in range(NG):
                    mt, mb = mx.next()
                    k.dma("sp", mt[:], mixT.rearrange("(cc p) t -> p cc t", p=128)[:, :, g * G:(g + 1) * G], W=[mb])
                    for nb in range(4):
                        wf, wfb = wfr.next()
                        k.dma("sp", wf[:], w_out[l].rearrange("(cc p) n -> p cc n", p=128)[:, :, nb * 512:(nb + 1) * 512], W=[wfb])
                        wt, wb = wr.next()
                        k.op("act", lambda: nc.scalar.copy(out=wt[:, 0:8, :], in_=wf[:, 0:8, :]), R=[wfb], W=[wb])
                        k.op("pool", lambda: nc.gpsimd.tensor_copy(out=wt[:, 8:16, :], in_=wf[:, 8:16, :]), R=[wfb], W=[wb])
                        for j in range(4):
                            n0 = nb * 512 + j * 128
                            xt, xb = xr.next()
                            k.dma("sp", xt[:], xT[n0:n0 + 128, g * G:(g + 1) * G], W=[xb])
                            p, pb = ps.next()
                            for cc in range(16):
                                k.op("pe", lambda: nc.tensor.matmul(p[:, :G], lhsT=wt[:, cc, j * 128:(j + 1) * 128], rhs=mt[:, cc, :],
                                                                    start=(cc == 0), stop=(cc == 15)), R=[wb, mb], W=[pb])
                            ot, ob = xo.next()
                            k.op("dve", lambda: nc.vector.tensor_tensor(out=ot[:], in0=p[:, :G], in1=xt[:], op=ALU.add), R=[pb, xb], W=[ob])
                            k.dma("pool", xT[n0:n0 + 128, g * G:(g + 1) * G], ot[:], R=[ob])
                k.barrier()

        with ExitStack() as es:
            gbc = es.enter_context(sbt(nc, "f_g", [128, D], F32))
            gbb = Buf()
            k.dma("sp", gbc[:], final_g.partition_broadcast(128), W=[gbb])
            xin = Ring(es, nc, "f_x", [128, 16, 128], F32, 2)
            xtm = Ring(es, nc, "f_t", [128, D], F32, 2)
            junk = es.enter_context(sbt(nc, "f_j", [128, D], F32))
            jb = Buf()
            ssq = Ring(es, nc, "f_s", [128, 2], F32, 2)
            ot = Ring(es, nc, "f_o", [128, D], F32, 2)
            ps = Ring(es, nc, "f_ps", [128, 512], F32, 8, psum=True)
            for t in range(NT):
                xt, xb = xin.next()
                k.dma("sp", xt[:], xT.rearrange("(kc p) t -> p kc t", p=128)[:, :, t * 128:(t + 1) * 128], W=[xb])
                xm, xmb = xtm.next()
                for q in range(4):
                    p, pb = ps.next()
                    for j in range(4):
                        kc = q * 4 + j
                        k.op("pe", lambda: nc.tensor.transpose(p[:, j * 128:(j + 1) * 128], xt[:, kc, :], ident), R=[xb, cstb], W=[pb])
                    if q % 2:
                        k.op("act", lambda: nc.scalar.copy(out=xm[:, q * 512:(q + 1) * 512], in_=p[:]), R=[pb], W=[xmb])
                    else:
                        k.op("dve", lambda: nc.vector.tensor_copy(out=xm[:, q * 512:(q + 1) * 512], in_=p[:]), R=[pb], W=[xmb])
                sq, sqb = ssq.next()
                k.op("act", lambda: nc.scalar.activation(out=junk[:], in_=xm[:], func=AF.Square, accum_out=sq[:, 0:1]), R=[xmb], W=[jb, sqb])
                rstd(sq[:, 1:2], sq[:, 0:1], 1.0 / D, C_EPS, [sqb], [sqb])
                o, ob = ot.next()
                k.op("dve", lambda: nc.vector.scalar_tensor_tensor(out=o[:], in0=xm[:], scalar=sq[:, 1:2], in1=gbc[:], op0=ALU.mult, op1=ALU.mult),
                     R=[xmb, sqb, gbb], W=[ob])
                k.dma("pool", out[t * 128:(t + 1) * 128, :], o[:], R=[ob])
            k.barrier()
    return nc


def MIXERS(env):
    pass


_CACHE = {}


WNAMES = ("norm_g", "w_in", "w_out", "final_norm_g", "ml_conv_w", "ml_conv_b", "ml_i_bias", "ml_f_bias", "ml_norm_g",
          "mla_q_norm_g", "mla_w_uq", "mla_kv_norm_g", "mla_w_ukv",
          "rw_mu", "rw_w0", "rw_w2", "rw_a0", "rw_a2", "rw_k_k", "rw_k_a", "rw_r_k", "rw_ln_g", "rw_ln_b")


def make_maps(inputs, T, depth):
    consts = make_consts()
    shared = {}
    for name in WNAMES:
        a = inputs[name]
        shared[name] = np.ascontiguousarray(a if name == "final_norm_g" else a[:depth])
    for name in ("rw_v0", "rw_v1", "rw_v2"):
        shared[name] = np.ascontiguousarray(inputs[name])
    in_maps = []
    for c in range(8):
        b = c % 4
        m = {"x": np.ascontiguousarray(inputs["x"][b, :T]), "positions": np.ascontiguousarray(inputs["positions"][b, :T]),
             "consts": consts}
        m.update(shared)
        in_maps.append(m)
    return in_maps


def kernel(**inputs):
    T = 4096
    depth = 4
    if "nc" not in _CACHE:
        _CACHE["nc"] = build(T, depth)
    nc = _CACHE["nc"]
    in_maps = make_maps(inputs, T, depth)
    res = run_bass_kernel_spmd(nc, in_maps, core_ids=list(range(8)))
    return np.stack([res.results[b]["out"] for b in range(4)], axis=0)
```

```python
import numpy as np
from contextlib import ExitStack
import concourse.bass as bass
import concourse.mybir as mybir
from concourse.bass_utils import run_bass_kernel_spmd

F32 = mybir.dt.float32
BF16 = mybir.dt.bfloat16
I32 = mybir.dt.int32
AF = mybir.ActivationFunctionType
ALU = mybir.AluOpType
AX = mybir.AxisListType

D = 2048
DIN = 6600
EPS = 1e-6
FM_RANGES = [(0, 1024), (2568, 3336), (4424, 6088)]
TM_RANGES = [(1024, 2568), (3336, 4424), (6088, 6600)]
TM_V, TM_I, TM_F, TM_O, TM_Z = 0, 512, 516, 520, 1032
TM_KR, TM_MZ, TM_RZ = 1544, 1608, 2632
NTM = 3144
FM_QK, FM_CQ, FM_CKV, FM_RW = 0, 8, 12, 14
NFM = 27


_UID = [0]


def sbt(nc, name, shape, dt):
    _UID[0] += 1
    return nc.sbuf_tensor("%s_u%d" % (name, _UID[0]), shape, dt)


def pst(nc, name, shape, dt):
    _UID[0] += 1
    return nc.psum_tensor("%s_u%d" % (name, _UID[0]), shape, dt)


class Buf:
    __slots__ = ("w", "r")

    def __init__(self):
        self.w = None
        self.r = {}


class KB:
    NDS = 24

    def __init__(self, nc):
        self.nc = nc
        self.E = {"pe": nc.tensor, "act": nc.scalar, "dve": nc.vector, "pool": nc.gpsimd, "sp": nc.sync}
        self.sem = {e: nc.alloc_semaphore("s_" + e) for e in ("pe", "act", "dve", "pool")}
        self.cnt = {e: 0 for e in self.sem}
        self.dsem = [nc.alloc_semaphore("d%d" % i) for i in range(self.NDS)]
        self.dcnt = [0] * self.NDS
        self.dnext = 0
        self.bar = nc.alloc_semaphore("bar")
        self.nbar = 0
        self.seen = {e: {} for e in self.E}

    def _semh(self, key):
        return self.sem[key] if isinstance(key, str) else self.dsem[key[1]]

    def _wait(self, e, key, val, same_ok=False):
        if val <= 0:
            return
        if key == e and (same_ok or e == "pe"):
            return
        if self.seen[e].get(key, 0) >= val:
            return
        self.E[e].wait_ge(self._semh(key), val)
        self.seen[e][key] = val

    def _deps(self, e, reads, writes):
        for b in reads:
            if b.w is not None:
                self._wait(e, b.w[0], b.w[1])
        for b in writes:
            if b.w is not None:
                self._wait(e, b.w[0], b.w[1])
            for key, val in b.r.items():
                self._wait(e, key, val, same_ok=True)

    def _mark(self, ev, reads, writes):
        for b in reads:
            if b.r.get(ev[0], 0) < ev[1]:
                b.r[ev[0]] = ev[1]
        for b in writes:
            b.w = ev
            b.r = {}

    def op(self, e, fn, R=(), W=()):
        self._deps(e, R, W)
        ins = fn()
        self.cnt[e] += 1
        ins.then_inc(self.sem[e], 1)
        self._mark((e, self.cnt[e]), R, W)

    def dma(self, q, out, in_, R=(), W=(), slow=False):
        i = self.dnext
        self.dnext = (i + 1) % self.NDS
        key = ("d", i)
        self._wait(q, key, self.dcnt[i])
        self._deps(q, R, W)
        if slow:
            ins = self.E[q].dma_start(out=out, in_=in_, allow_slow_non_contiguous=True)
        else:
            ins = self.E[q].dma_start(out=out, in_=in_)
        self.dcnt[i] += 16
        ins.then_inc(self.dsem[i], 16)
        self._mark((key, self.dcnt[i]), R, W)

    def barrier(self):
        sp = self.E["sp"]
        for e in self.sem:
            self._wait("sp", e, self.cnt[e])
        for i in range(self.NDS):
            self._wait("sp", ("d", i), self.dcnt[i])
        self.nbar += 1
        sp.sem_inc(self.bar, 1)
        for e in ("pe", "act", "dve", "pool"):
            self.E[e].wait_ge(self.bar, self.nbar)
            for k2 in self.sem:
                self.seen[e][k2] = self.cnt[k2]
            for i in range(self.NDS):
                self.seen[e][("d", i)] = self.dcnt[i]


class Ring:
    def __init__(self, es, nc, name, shape, dtype, n, psum=False):
        self.items = []
        for i in range(n):
            if psum:
                t = es.enter_context(pst(nc, "%s%d" % (name, i), shape, dtype))
            else:
                t = es.enter_context(sbt(nc, "%s%d" % (name, i), shape, dtype))
            self.items.append((t, Buf()))
        self.i = 0

    def next(self):
        it = self.items[self.i]
        self.i = (self.i + 1) % len(self.items)
        return it


def split_blocks(ranges, maxw=512):
    out = []
    for (a, b) in ranges:
        c = a
        while c < b:
            w = min(maxw, b - c)
            out.append((c, w))
            c += w
    return out


def make_consts():
    c = np.zeros((128, 1280), np.float32)
    c[:, 0:128] = np.eye(128, dtype=np.float32)
    j = np.arange(128)
    c[:, 128:256] = (j[:, None] <= j[None, :]).astype(np.float32)
    c[:, 256:384] = 1.0
    same = (j[:, None] // 64) == (j[None, :] // 64)
    c[:, 384:512] = ((j[:, None] <= j[None, :]) & same)
    c[:, 512:640] = ((j[:, None] < j[None, :]) & same)
    invf = np.power(10000.0, -np.arange(0, 64, 2, dtype=np.float32) / 64).astype(np.float32)
    c[:, 640:672] = invf[None, :]
    c[:, 672:674] = (j[:, None] // 64 == np.arange(2)[None, :])
    c[:, 700] = 1e-6
    c[:, 701] = 64e-5
    c[:, 702] = 1.0
    c[:, 703] = -0.5
    c[:, 704] = 0.0
    c[:, 705] = -np.pi
    c[:, 706] = 1e-24
    c[:, 707] = 1.0 / 128
    i64 = np.arange(64)
    c[:64, 768:832] = (i64[:, None] < i64[None, :])
    c[:64, 832:896] = (i64[:, None] <= i64[None, :])
    c[:64, 896:960] = (i64[:, None] > i64[None, :])
    for jj in range(4):
        c[:, 960 + jj * 8:968 + jj * 8] = ((2 * jj + j[:, None] // 64) == np.arange(8)[None, :])
    c[:, 1024:1152] = same
    return c


C_ID, C_TRI, C_ONE, C_BI, C_BS, C_IF, C_CI = 0, 128, 256, 384, 512, 640, 672
C_M1, C_M3, C_HS, C_BD = 768, 896, 960, 1024
C_EPS, C_GNEPS, C_1, C_MH, C_0, C_MPI, C_TINY, C_R128 = 700, 701, 702, 703, 704, 705, 706, 707


def build(T, depth, dbg=()):
    assert T % 128 == 0
    NT = T // 128
    SG = min(T, 1024)
    NSG = T // SG
    G = min(T, 512)
    NG = T // G
    nc = bass.Bass("TRN2", target_bir_lowering=False)

    def din(name, shape, dt=F32):
        return nc.dram_tensor(name, list(shape), dt, kind="ExternalInput").ap()

    def dscr(name, shape, dt=F32):
        kind = "ExternalOutput" if name in dbg else "Internal"
        return nc.dram_tensor(name, list(shape), dt, kind=kind).ap()

    x_in = din("x", [T, D])
    pos_in = din("positions", [T], I32)
    consts_in = din("consts", [128, 1280])
    norm_g = din("norm_g", [depth, D])
    w_in = din("w_in", [depth, D, DIN])
    w_out = din("w_out", [depth, D, D])
    final_g = din("final_norm_g", [D])
    ml_conv_w = din("ml_conv_w", [depth, 4, 1024]); ml_conv_b = din("ml_conv_b", [depth, 1024])
    mla_q_norm_g = din("mla_q_norm_g", [depth, 512]); mla_w_uq = din("mla_w_uq", [depth, 512, 1536])
    mla_kv_norm_g = din("mla_kv_norm_g", [depth, 256]); mla_w_ukv = din("mla_w_ukv", [depth, 256, 2048])
    rw_mu = din("rw_mu", [depth, 1664]); rw_w0 = din("rw_w0", [depth, 512]); rw_w2 = din("rw_w2", [depth, 64, 512])
    rw_a0 = din("rw_a0", [depth, 512]); rw_a2 = din("rw_a2", [depth, 64, 512])
    rw_v0 = din("rw_v0", [3, 512]); rw_v1 = din("rw_v1", [3, 512, 32]); rw_v2 = din("rw_v2", [3, 32, 512])
    rw_k_k = din("rw_k_k", [depth, 512]); rw_k_a = din("rw_k_a", [depth, 512]); rw_r_k = din("rw_r_k", [depth, 8, 64])
    rw_ln_g = din("rw_ln_g", [depth, 512]); rw_ln_b = din("rw_ln_b", [depth, 512])
    ml_i_bias = din("ml_i_bias", [depth, 4]); ml_f_bias = din("ml_f_bias", [depth, 4]); ml_norm_g = din("ml_norm_g", [depth, 512])
    out = nc.dram_tensor("out", [T, D], F32, kind="ExternalOutput").ap()

    xT = dscr("xT", [D, T])
    pFM = dscr("pFM", [NFM * 128, T])
    pTM = dscr("pTM", [T, NTM])
    mixT = dscr("mixT", [D, T], BF16)
    csd = dscr("cs", [T, 64])
    qnT = dscr("qnT", [1024, T], BF16); knT = dscr("knT", [1024, T], BF16); qrT = dscr("qrT", [512, T], BF16); krT = dscr("krT", [64, T], BF16)
    vaug = dscr("vaug", [8, T, 129], BF16)
    rwA = dscr("rwA", [512, T]); rwR = dscr("rwR", [512, T]); rwB = dscr("rwB", [512, T]); rwK = dscr("rwK", [512, T])
    rwBp = dscr("rwBp", [T, 512]); rwKp = dscr("rwKp", [T, 512]); rwV = dscr("rwV", [T, 512]); rwY = dscr("rwY", [T, 512])
    rwGL = dscr("rwGL", [512, T // 64]); rwRKR = dscr("rwRKR", [T, 8]); vfirst = dscr("vfirst", [512, T])

    fm_blocks = split_blocks(FM_RANGES)
    tm_blocks = split_blocks(TM_RANGES)

    with ExitStack() as top:
        k = KB(nc)
        cst = top.enter_context(sbt(nc, "cst", [128, 1280], F32))
        cstb = Buf()
        k.dma("sp", cst[:], consts_in, W=[cstb])
        ident = cst[:, C_ID:C_ID + 128]
        ones = cst[:, C_ONE:C_ONE + 128]
        onec = cst[:, C_ONE:C_ONE + 1]

        def rstd(o, i, scale, epscol, R, W, np_=128):
            k.op("act", lambda: nc.scalar.activation(out=o, in_=i, func=AF.Sqrt, bias=cst[:np_, epscol:epscol + 1], scale=scale), R=list(R) + [cstb], W=W)
            k.op("dve", lambda: nc.vector.reciprocal(out=o, in_=o), R=W, W=W)

        with ExitStack() as es:
            xin = Ring(es, nc, "i_x", [128, D], F32, 2)
            ps = Ring(es, nc, "i_ps", [128, 512], F32, 4, psum=True)
            stg = Ring(es, nc, "i_st", [128, 16, 128], F32, 2)
            for t in range(NT):
                xt, xb = xin.next()
                k.dma("sp", xt[:], x_in[t * 128:(t + 1) * 128, :], W=[xb])
                st, sb = stg.next()
                for q in range(4):
                    p, pb = ps.next()
                    for j in range(4):
                        kc = q * 4 + j
                        k.op("pe", lambda: nc.tensor.transpose(p[:, j * 128:(j + 1) * 128], xt[:, kc * 128:(kc + 1) * 128], ident),
                             R=[xb, cstb], W=[pb])
                    eng = "act" if q % 2 else "dve"
                    if eng == "act":
                        k.op("act", lambda: nc.scalar.copy(out=st[:, q * 4:(q + 1) * 4, :], in_=p[:].rearrange("p (a b) -> p a b", a=4)), R=[pb], W=[sb])
                    else:
                        k.op("dve", lambda: nc.vector.tensor_copy(out=st[:, q * 4:(q + 1) * 4, :], in_=p[:].rearrange("p (a b) -> p a b", a=4)), R=[pb], W=[sb])
                k.dma("pool", xT.rearrange("(kc p) t -> p kc t", p=128)[:, :, t * 128:(t + 1) * 128], st[:], R=[sb])
            posi = es.enter_context(sbt(nc, "i_pi", [128, NT], I32))
            posf = es.enter_context(sbt(nc, "i_pf", [128, NT], F32))
            pob = Buf()
            k.dma("sp", posi[:], pos_in.rearrange("(n p) -> p n", p=128), W=[pob], slow=True)
            k.op("dve", lambda: nc.vector.tensor_copy(out=posf[:], in_=posi[:]), R=[pob], W=[pob])
            angr = Ring(es, nc, "i_ang", [128, 64], F32, 2)
            tir = Ring(es, nc, "i_ti", [128, 64], I32, 2)
            tfr = Ring(es, nc, "i_tf", [128, 64], F32, 2)
            TWO_PI = float(2 * np.pi)
            C1 = 6.28125
            C2 = float(2 * np.pi - 6.28125)
            for t in range(NT):
                an, anb = angr.next()
                ti, tib = tir.next()
                tf, tfb = tfr.next()
                k.op("dve", lambda: nc.vector.tensor_scalar(out=an[:, 0:32], in0=cst[:, C_IF:C_IF + 32], scalar1=posf[:, t:t + 1], scalar2=None, op0=ALU.mult), R=[pob, cstb], W=[anb])
                k.op("dve", lambda: nc.vector.tensor_scalar(out=an[:, 32:64], in0=an[:, 0:32], scalar1=float(np.pi / 2), scalar2=None, op0=ALU.add), R=[anb], W=[anb])
                k.op("dve", lambda: nc.vector.tensor_scalar(out=tf[:], in0=an[:], scalar1=float(1 / (2 * np.pi)), scalar2=None, op0=ALU.mult), R=[anb], W=[tfb])
                k.op("dve", lambda: nc.vector.tensor_copy(out=ti[:], in_=tf[:]), R=[tfb], W=[tib])
                k.op("dve", lambda: nc.vector.tensor_copy(out=tf[:], in_=ti[:]), R=[tib], W=[tfb])
                k.op("dve", lambda: nc.vector.scalar_tensor_tensor(out=an[:], in0=tf[:], scalar=-C1, in1=an[:], op0=ALU.mult, op1=ALU.add), R=[tfb, anb], W=[anb])
                k.op("dve", lambda: nc.vector.scalar_tensor_tensor(out=an[:], in0=tf[:], scalar=-C2, in1=an[:], op0=ALU.mult, op1=ALU.add), R=[tfb, anb], W=[anb])
                k.op("dve", lambda: nc.vector.tensor_scalar(out=tf[:], in0=an[:], scalar1=float(np.pi), scalar2=-TWO_PI, op0=ALU.is_gt, op1=ALU.mult), R=[anb], W=[tfb])
                k.op("dve", lambda: nc.vector.tensor_tensor(out=an[:], in0=an[:], in1=tf[:], op=ALU.add), R=[tfb, anb], W=[anb])
                k.op("dve", lambda: nc.vector.tensor_scalar(out=tf[:], in0=an[:], scalar1=float(-np.pi), scalar2=TWO_PI, op0=ALU.is_lt, op1=ALU.mult), R=[anb], W=[tfb])
                k.op("dve", lambda: nc.vector.tensor_tensor(out=an[:], in0=an[:], in1=tf[:], op=ALU.add), R=[tfb, anb], W=[anb])
                k.op("act", lambda: nc.scalar.activation(out=an[:], in_=an[:], func=AF.Sin), R=[anb], W=[anb])
                k.dma("pool", csd[t * 128:(t + 1) * 128, :], an[:], R=[anb])
            k.barrier()

        for l in range(depth):
            with ExitStack() as es:
                gcol = es.enter_context(sbt(nc, "p1_g", [128, 16], F32))
                gb = Buf()
                k.dma("sp", gcol[:], norm_g[l].rearrange("(kc p) -> p kc", p=128), W=[gb], slow=True)
                hT = es.enter_context(sbt(nc, "p1_hT", [128, 16, SG], BF16))
                hb = Buf()
                xs = Ring(es, nc, "p1_xs", [128, SG], F32, 2)
                sqr = Ring(es, nc, "p1_sq", [128, SG], F32, 2)
                wr = Ring(es, nc, "p1_w", [128, 16, 512], F32, 2)
                wcr = Ring(es, nc, "p1_wc", [128, 16, 512], BF16, 2)
                wcn = [0]
                stg = Ring(es, nc, "p1_st", [128, 512], F32, 4)
                rbc = es.enter_context(sbt(nc, "p1_rbc", [128, SG], F32))
                rbcb = Buf()
                rtm = es.enter_context(sbt(nc, "p1_rtm", [128, SG // 128], F32))
                rtmb = Buf()
                ps = Ring(es, nc, "p1_ps", [128, 512], F32, 4, psum=True)
                pbc = Ring(es, nc, "p1_pbc", [128, 512], F32, SG // G, psum=True)
                ptm = es.enter_context(pst(nc, "p1_ptm", [128, 512], F32))
                ptmb = Buf()
                def p1_loadw(cc0, nb):
                    wf, wfb = wr.next()
                    k.dma("sp", wf[:, :, :nb], w_in[l].rearrange("(kc p) n -> p kc n", p=128)[:, :, cc0:cc0 + nb], W=[wfb])
                    wc, wcb = wcr.next()
                    wcn[0] += 1
                    for hf in range(2):
                        if (wcn[0] + hf) % 2:
                            k.op("act", lambda: nc.scalar.copy(out=wc[:, hf * 8:(hf + 1) * 8, :nb], in_=wf[:, hf * 8:(hf + 1) * 8, :nb]), R=[wfb], W=[wcb])
                        else:
                            k.op("dve", lambda: nc.vector.tensor_copy(out=wc[:, hf * 8:(hf + 1) * 8, :nb], in_=wf[:, hf * 8:(hf + 1) * 8, :nb]), R=[wfb], W=[wcb])
                    return wc, wcb

                for sg in range(NSG):
                    c0 = sg * SG
                    pbcs = [pbc.next() for _ in range(SG // G)]
                    for kc in range(16):
                        xt, xb = xs.next()
                        k.dma("sp", xt[:], xT[kc * 128:(kc + 1) * 128, c0:c0 + SG], W=[xb])
                        sq, sqb = sqr.next()
                        k.op("act", lambda: nc.scalar.activation(out=sq[:], in_=xt[:], func=AF.Square), R=[xb], W=[sqb])
                        k.op("dve", lambda: nc.vector.tensor_scalar(out=hT[:, kc, :], in0=xt[:], scalar1=gcol[:, kc:kc + 1], scalar2=None, op0=ALU.mult),
                             R=[xb, gb], W=[hb])
                        for s in range(SG // G):
                            pp, ppb = pbcs[s]
                            k.op("pe", lambda: nc.tensor.matmul(pp[:, :G], lhsT=ones, rhs=sq[:, s * G:(s + 1) * G], start=(kc == 0), stop=(kc == 15)),
                                 R=[sqb, cstb], W=[ppb])
                    for s in range(SG // G):
                        pp, ppb = pbcs[s]
                        rstd(rbc[:, s * G:(s + 1) * G], pp[:, :G], 1.0 / D, C_EPS, [ppb], [rbcb])
                    for t in range(SG // 128):
                        k.op("pe", lambda: nc.tensor.matmul(ptm[:, t:t + 1], lhsT=rbc[:, t * 128:(t + 1) * 128], rhs=cst[:, C_R128:C_R128 + 1], start=True, stop=True),
                             R=[rbcb, cstb], W=[ptmb])
                    k.op("dve", lambda: nc.vector.tensor_copy(out=rtm[:], in_=ptm[:, :SG // 128]), R=[ptmb], W=[rtmb])
                    fmrow = 0
                    for (cc0, nb) in fm_blocks:
                        wt, wb = p1_loadw(cc0, nb)
                        for j in range(nb // 128):
                            for s in range(SG // G):
                                p, pb = ps.next()
                                for kc in range(16):
                                    k.op("pe", lambda: nc.tensor.matmul(p[:, :G], lhsT=wt[:, kc, j * 128:(j + 1) * 128], rhs=hT[:, kc, s * G:(s + 1) * G],
                                                                        start=(kc == 0), stop=(kc == 15)), R=[wb, hb], W=[pb])
                                st, sb = stg.next()
                                k.op("dve", lambda: nc.vector.tensor_tensor(out=st[:, :G], in0=p[:, :G], in1=rbc[:, s * G:(s + 1) * G], op=ALU.mult),
                                     R=[pb, rbcb], W=[sb])
                                k.dma("pool", pFM[fmrow * 128:(fmrow + 1) * 128, c0 + s * G:c0 + (s + 1) * G], st[:, :G], R=[sb])
                            fmrow += 1
                    tmcol = 0
                    for (cc0, nb) in tm_blocks:
                        wt, wb = p1_loadw(cc0, nb)
                        for t in range(SG // 128):
                            p, pb = ps.next()
                            for kc in range(16):
                                k.op("pe", lambda: nc.tensor.matmul(p[:, :nb], lhsT=hT[:, kc, t * 128:(t + 1) * 128], rhs=wt[:, kc, :nb],
                                                                    start=(kc == 0), stop=(kc == 15)), R=[wb, hb], W=[pb])
                            st, sb = stg.next()
                            k.op("act", lambda: nc.scalar.activation(out=st[:, :nb], in_=p[:, :nb], func=AF.Copy, scale=rtm[:, t:t + 1]),
                                 R=[pb, rtmb], W=[sb])
                            k.dma("pool", pTM[c0 + t * 128:c0 + (t + 1) * 128, tmcol:tmcol + nb], st[:, :nb], R=[sb])
                        tmcol += nb
                k.barrier()


            with ExitStack() as es:
                SB = lambda name, shape: es.enter_context(sbt(nc, name, shape, F32))
                PS = lambda name: es.enter_context(pst(nc, name, [128, 512], F32))
                tri = cst[:, C_TRI:C_TRI + 128]
                cw = SB("m_cw", [128, 4, 8]); cb = SB("m_cb", [128, 8]); ibfb = SB("m_ib", [128, 8]); gbc = SB("m_g", [128, 512])
                pb_ = Buf()
                for j in range(4):
                    k.dma("sp", cw[:, j, :], ml_conv_w[l, j].rearrange("(c p) -> p c", p=128), W=[pb_], slow=True)
                k.dma("sp", cb[:], ml_conv_b[l].rearrange("(c p) -> p c", p=128), W=[pb_], slow=True)
                k.dma("sp", ibfb[:, 0:4], ml_i_bias[l].partition_broadcast(128), W=[pb_])
                k.dma("sp", ibfb[:, 4:8], ml_f_bias[l].partition_broadcast(128), W=[pb_])
                k.dma("sp", gbc[:], ml_norm_g[l].partition_broadcast(128), W=[pb_])
                CT = [SB("m_CT%d" % h, [128, 129]) for h in range(4)]
                CTb = [Buf() for h in range(4)]
                for h in range(4):
                    k.op("pool", lambda: nc.gpsimd.memset(CT[h][:], 0.0), W=[CTb[h]])
                qkr = Ring(es, nc, "m_qkr", [128, 8, 131], F32, 2)
                accr = Ring(es, nc, "m_acc", [128, 8, 128], F32, 2)
                qkt = Ring(es, nc, "m_qk", [128, 8, 128], F32, 2)
                vr = Ring(es, nc, "m_v", [128, 4, 129], F32, 2)
                for (vt, vb) in vr.items:
                    k.op("pool", lambda: nc.gpsimd.memset(vt[:, :, 128:129], 1.0), W=[vb])
                gtr = Ring(es, nc, "m_gt", [128, 8], F32, 2)
                Gr = Ring(es, nc, "m_G", [128, 32], F32, 2)
                ozr = Ring(es, nc, "m_oz", [128, 1024], F32, 2)
                rhr = Ring(es, nc, "m_rh", [128, 128], F32, 2)
                Er = Ring(es, nc, "m_E", [128, 128], F32, 2)
                STr = Ring(es, nc, "m_ST", [128, 128], F32, 2)
                n1r = Ring(es, nc, "m_n1", [128, 129], F32, 2)
                n2r = Ring(es, nc, "m_n2", [128, 129], F32, 2)
                smr = Ring(es, nc, "m_sm", [128, 16], F32, 2)
                hhr = Ring(es, nc, "m_hh", [128, 128], F32, 2)
                kwr = Ring(es, nc, "m_kw", [128, 128], F32, 2)
                yr = Ring(es, nc, "m_y", [128, 512], F32, 2)
                ggr = Ring(es, nc, "m_gg", [128, 1024], F32, 2)
                mtr = Ring(es, nc, "m_mt", [128, 4, 128], BF16, 2)
                pgA = PS("m_pgA"); pgB = PS("m_pgB"); pBbc = PS("m_pB"); pQK = PS("m_pQK")
                pN1 = PS("m_pN1"); pN2 = PS("m_pN2"); pKT = PS("m_pKT"); pdC = PS("m_pdC")
                pgAb, pgBb, pBbcb, pQKb, pN1b, pN2b, pKTb, pdCb = [Buf() for _ in range(8)]
                NSD = nc.vector.BN_STATS_DIM
                for c in range(NT):
                    t0 = c * 128
                    X, Xb = qkr.next()
                    src = pFM.rearrange("(ch p) t -> p ch t", p=128)
                    if c == 0:
                        k.op("pool", lambda: nc.gpsimd.memset(X[:, :, 0:3], 0.0), W=[Xb])
                        k.dma("sp", X[:, :, 3:131], src[:, 0:8, 0:128], W=[Xb])
                    else:
                        k.dma("sp", X[:], src[:, 0:8, t0 - 3:t0 + 128], W=[Xb])
                    vt, vb = vr.next()
                    k.dma("sp", vt[:, :, 0:128], pTM[t0:t0 + 128, TM_V:TM_V + 512].rearrange("p (h d) -> p h d", h=4), W=[vb])
                    gt, gtb = gtr.next()
                    k.dma("sp", gt[:], pTM[t0:t0 + 128, TM_I:TM_I + 8], W=[gtb])
                    oz, ozb = ozr.next()
                    k.dma("sp", oz[:], pTM[t0:t0 + 128, TM_O:TM_O + 1024], W=[ozb])
                    acc, accb = accr.next()
                    qk, qkb = qkt.next()
                    for ch in range(8):
                        eng, E_ = ("dve", nc.vector)
                        k.op(eng, lambda: E_.tensor_scalar(out=acc[:, ch, :], in0=X[:, ch, 0:128], scalar1=cw[:, 0, ch:ch + 1], scalar2=cb[:, ch:ch + 1], op0=ALU.mult, op1=ALU.add),
                             R=[Xb, pb_], W=[accb])
                        for j in range(1, 4):
                            k.op(eng, lambda: E_.scalar_tensor_tensor(out=acc[:, ch, :], in0=X[:, ch, j:j + 128], scalar=cw[:, j, ch:ch + 1], in1=acc[:, ch, :], op0=ALU.mult, op1=ALU.add),
                                 R=[Xb, pb_, accb], W=[accb])
                    k.op("act", lambda: nc.scalar.activation(out=qk[:], in_=acc[:], func=AF.Silu), R=[accb], W=[qkb])
                    k.op("pool", lambda: nc.gpsimd.tensor_scalar(out=qk[:, 0:4, :], in0=qk[:, 0:4, :], scalar1=float(128 ** -0.5), scalar2=None, op0=ALU.mult), R=[qkb], W=[qkb])
                    Gt, Gb = Gr.next()
                    k.op("dve", lambda: nc.vector.tensor_tensor(out=Gt[:, 0:8], in0=gt[:], in1=ibfb[:], op=ALU.add), R=[gtb, pb_], W=[Gb])
                    k.op("act", lambda: nc.scalar.activation(out=Gt[:, 8:12], in_=Gt[:, 4:8], func=AF.Exp, scale=-1.0), R=[Gb], W=[Gb])
                    k.op("act", lambda: nc.scalar.activation(out=Gt[:, 12:16], in_=Gt[:, 8:12], func=AF.Ln, bias=cst[:, C_1:C_1 + 1]), R=[Gb, cstb], W=[Gb])
                    k.op("dve", lambda: nc.vector.tensor_scalar(out=Gt[:, 12:16], in0=Gt[:, 12:16], scalar1=-1.0, scalar2=None, op0=ALU.mult), R=[Gb], W=[Gb])
                    k.op("pe", lambda: nc.tensor.matmul(pgA[:, 0:4], lhsT=tri, rhs=Gt[:, 12:16], start=True, stop=True), R=[Gb, cstb], W=[pgAb])
                    k.op("pe", lambda: nc.tensor.matmul(pgB[:, 0:4], lhsT=ones, rhs=Gt[:, 12:16], start=True, stop=True), R=[Gb, cstb], W=[pgBb])
                    k.op("dve", lambda: nc.vector.tensor_tensor(out=Gt[:, 16:20], in0=Gt[:, 0:4], in1=pgA[:, 0:4], op=ALU.subtract), R=[Gb, pgAb], W=[Gb])
                    k.op("dve", lambda: nc.vector.tensor_tensor(out=Gt[:, 20:24], in0=Gt[:, 16:20], in1=pgB[:, 0:4], op=ALU.add), R=[Gb, pgBb], W=[Gb])
                    k.op("act", lambda: nc.scalar.activation(out=Gt[:, 20:24], in_=Gt[:, 20:24], func=AF.Exp), R=[Gb], W=[Gb])
                    k.op("act", lambda: nc.scalar.activation(out=Gt[:, 24:28], in_=pgB[:, 0:4], func=AF.Exp), R=[pgBb], W=[Gb])
                    k.op("act", lambda: nc.scalar.activation(out=Gt[:, 28:32], in_=pgA[:, 0:4], func=AF.Exp), R=[pgAb], W=[Gb])
                    y, yb = yr.next()
                    for h in range(4):
                        rh, rhb = rhr.next()
                        k.op("pool", lambda: nc.gpsimd.tensor_scalar(out=rh[:], in0=tri, scalar1=Gt[:, 12 + h:13 + h], scalar2=None, op0=ALU.mult), R=[Gb, cstb], W=[rhb])
                        k.op("pe", lambda: nc.tensor.matmul(pBbc[:, 0:128], lhsT=ones, rhs=rh[:], start=True, stop=True), R=[rhb, cstb], W=[pBbcb])
                        Et, Eb = Er.next()
                        k.op("act", lambda: nc.scalar.activation(out=Et[:], in_=pBbc[:, 0:128], func=AF.Exp, bias=Gt[:, 16 + h:17 + h]), R=[pBbcb, Gb], W=[Eb])
                        k.op("pool", lambda: nc.gpsimd.tensor_tensor(out=Et[:], in0=Et[:], in1=tri, op=ALU.mult), R=[Eb, cstb], W=[Eb])
                        k.op("pe", lambda: nc.tensor.matmul(pQK[:, 0:128], lhsT=qk[:, 4 + h, :], rhs=qk[:, h, :], start=True, stop=True), R=[qkb], W=[pQKb])
                        ST, STb = STr.next()
                        k.op("dve", lambda: nc.vector.tensor_tensor(out=ST[:], in0=pQK[:, 0:128], in1=Et[:], op=ALU.mult), R=[pQKb, Eb], W=[STb])
                        k.op("pe", lambda: nc.tensor.matmul(pN1[:, 0:129], lhsT=ST[:], rhs=vt[:, h, :], start=True, stop=True), R=[STb, vb], W=[pN1b])
                        k.op("pe", lambda: nc.tensor.matmul(pN2[:, 0:129], lhsT=qk[:, h, :], rhs=CT[h][:], start=True, stop=True), R=[qkb, CTb[h]], W=[pN2b])
                        n1, n1b = n1r.next()
                        k.op("act", lambda: nc.scalar.activation(out=n1[:], in_=pN2[:, 0:129], func=AF.Copy, scale=Gt[:, 28 + h:29 + h]), R=[pN2b, Gb], W=[n1b])
                        n2, n2b = n2r.next()
                        k.op("dve", lambda: nc.vector.tensor_tensor(out=n2[:], in0=pN1[:, 0:129], in1=n1[:], op=ALU.add), R=[pN1b, n1b], W=[n2b])
                        sm, smb = smr.next()
                        k.op("act", lambda: nc.scalar.activation(out=sm[:, 0:1], in_=n2[:, 128:129], func=AF.Abs), R=[n2b], W=[smb])
                        k.op("dve", lambda: nc.vector.tensor_scalar_max(out=sm[:, 0:1], in0=sm[:, 0:1], scalar1=1.0), R=[smb], W=[smb])
                        k.op("dve", lambda: nc.vector.reciprocal(out=sm[:, 0:1], in_=sm[:, 0:1]), R=[smb], W=[smb])
                        hh, hhb = hhr.next()
                        k.op("dve", lambda: nc.vector.tensor_scalar(out=hh[:], in0=n2[:, 0:128], scalar1=sm[:, 0:1], scalar2=None, op0=ALU.mult), R=[n2b, smb], W=[hhb])
                        k.op("dve", lambda: nc.vector.bn_stats(out=sm[:, 2:2 + NSD], in_=hh[:]), R=[hhb], W=[smb])
                        k.op("dve", lambda: nc.vector.bn_aggr(out=sm[:, 10:12], in_=sm[:, 2:2 + NSD]), R=[smb], W=[smb])
                        rstd(sm[:, 12:13], sm[:, 11:12], 1.0, C_EPS, [smb], [smb])
                        k.op("dve", lambda: nc.vector.tensor_scalar(out=y[:, h * 128:(h + 1) * 128], in0=hh[:], scalar1=sm[:, 10:11], scalar2=sm[:, 12:13], op0=ALU.subtract, op1=ALU.mult),
                             R=[hhb, smb], W=[yb])
                        k.op("pe", lambda: nc.tensor.transpose(pKT[:, 0:128], qk[:, 4 + h, :], ident), R=[qkb, cstb], W=[pKTb])
                        kw, kwb = kwr.next()
                        k.op("act", lambda: nc.scalar.activation(out=kw[:], in_=pKT[:, 0:128], func=AF.Copy, scale=Gt[:, 20 + h:21 + h]), R=[pKTb, Gb], W=[kwb])
                        k.op("pe", lambda: nc.tensor.matmul(pdC[:, 0:129], lhsT=kw[:], rhs=vt[:, h, :], start=True, stop=True), R=[kwb, vb], W=[pdCb])
                        k.op("dve", lambda: nc.vector.scalar_tensor_tensor(out=CT[h][:], in0=CT[h][:], scalar=Gt[:, 24 + h:25 + h], in1=pdC[:, 0:129], op0=ALU.mult, op1=ALU.add),
                             R=[CTb[h], Gb, pdCb], W=[CTb[h]])
                    gg, ggb = ggr.next()
                    k.op("act", lambda: nc.scalar.activation(out=gg[:, 0:512], in_=oz[:, 0:512], func=AF.Sigmoid), R=[ozb], W=[ggb])
                    k.op("act", lambda: nc.scalar.activation(out=gg[:, 512:1024], in_=oz[:, 512:1024], func=AF.Silu), R=[ozb], W=[ggb])
                    k.op("pool", lambda: nc.gpsimd.tensor_tensor(out=gg[:, 0:512], in0=gg[:, 0:512], in1=gg[:, 512:1024], op=ALU.mult), R=[ggb], W=[ggb])
                    k.op("pool", lambda: nc.gpsimd.tensor_tensor(out=gg[:, 0:512], in0=gg[:, 0:512], in1=gbc[:], op=ALU.mult), R=[ggb, pb_], W=[ggb])
                    k.op("dve", lambda: nc.vector.tensor_tensor(out=y[:], in0=y[:], in1=gg[:, 0:512], op=ALU.mult), R=[yb, ggb], W=[yb])
                    mt, mtb = mtr.next()
                    for j in range(4):
                        k.op("pe", lambda: nc.tensor.transpose(pKT[:, 128 + j * 64:128 + j * 64 + 64] if False else pKT[:, 0:128], y[:, j * 128:(j + 1) * 128], ident), R=[yb, cstb], W=[pKTb])
                        k.op("act", lambda: nc.scalar.copy(out=mt[:, j, :], in_=pKT[:, 0:128]), R=[pKTb], W=[mtb])
                    k.dma("pool", mixT.rearrange("(cc p) t -> p cc t", p=128)[:, 0:4, t0:t0 + 128], mt[:], R=[mtb])
                k.barrier()

            with ExitStack() as es:
                SB = lambda name, shape: es.enter_context(sbt(nc, name, shape, F32))
                PS = lambda name: es.enter_context(pst(nc, name, [128, 512], F32))
                tri = cst[:, C_TRI:C_TRI + 128]
                wuq = SB("a_wuq", [128, 4, 1536]); wukv = SB("a_wukv", [128, 2, 2048]); gq = SB("a_gq", [128, 6])
                wb_ = Buf()
                k.dma("sp", wuq[:], mla_w_uq[l].rearrange("(c p) n -> p c n", p=128), W=[wb_])
                k.dma("sp", wukv[:], mla_w_ukv[l].rearrange("(c p) n -> p c n", p=128), W=[wb_])
                k.dma("sp", gq[:, 0:4], mla_q_norm_g[l].rearrange("(c p) -> p c", p=128), W=[wb_], slow=True)
                k.dma("sp", gq[:, 4:6], mla_kv_norm_g[l].rearrange("(c p) -> p c", p=128), W=[wb_], slow=True)
                xcr = Ring(es, nc, "a_xc", [128, 6, 128], F32, 2)
                sqr = Ring(es, nc, "a_sq", [128, 6, 128], F32, 2)
                rbr = Ring(es, nc, "a_rb", [128, 2, 128], F32, 2)
                cnr = Ring(es, nc, "a_cn", [128, 6, 128], F32, 2)
                csr = Ring(es, nc, "a_cs", [128, 64], F32, 2)
                kxr = Ring(es, nc, "a_kx", [128, 64], F32, 2)
                stq = Ring(es, nc, "a_stq", [128, 4, 128], BF16, 4)
                tmr = Ring(es, nc, "a_tm", [128, 8, 32], F32, 4)
                qrr = Ring(es, nc, "a_qr", [128, 8, 64], F32, 2)
                krr = Ring(es, nc, "a_kr", [128, 64], F32, 2)
                kst = Ring(es, nc, "a_kst", [64, 128], BF16, 2)
                var_ = Ring(es, nc, "a_va", [128, 8, 129], BF16, 2)
                for (vt, vb) in var_.items:
                    k.op("pool", lambda: nc.gpsimd.memset(vt[:, :, 128:129], 1.0), W=[vb])
                pS1 = PS("a_pS1"); pS2 = PS("a_pS2"); pS1b = Buf(); pS2b = Buf()
                pq = Ring(es, nc, "a_pq", [128, 512], F32, 4, psum=True)
                qn_v = qnT.rearrange("(h p) t -> p h t", p=128)
                kn_v = knT.rearrange("(h p) t -> p h t", p=128)
                qr_v = qrT.rearrange("(b p) t -> p b t", p=128)
                for c in range(NT):
                    t0 = c * 128
                    xc, xcb = xcr.next()
                    k.dma("sp", xc[:], pFM.rearrange("(ch p) t -> p ch t", p=128)[:, FM_CQ:FM_CQ + 6, t0:t0 + 128], W=[xcb])
                    cs_, csb = csr.next()
                    k.dma("sp", cs_[:], csd[t0:t0 + 128, :], W=[csb])
                    kx, kxb = kxr.next()
                    k.dma("sp", kx[:], pTM[t0:t0 + 128, TM_KR:TM_KR + 64], W=[kxb])
                    sq, sqb = sqr.next()
                    k.op("act", lambda: nc.scalar.activation(out=sq[:], in_=xc[:], func=AF.Square), R=[xcb], W=[sqb])
                    for ch in range(4):
                        k.op("pe", lambda: nc.tensor.matmul(pS1[:, 0:128], lhsT=ones, rhs=sq[:, ch, :], start=(ch == 0), stop=(ch == 3)), R=[sqb, cstb], W=[pS1b])
                    for ch in range(2):
                        k.op("pe", lambda: nc.tensor.matmul(pS2[:, 0:128], lhsT=ones, rhs=sq[:, 4 + ch, :], start=(ch == 0), stop=(ch == 1)), R=[sqb, cstb], W=[pS2b])
                    rb, rbb = rbr.next()
                    rstd(rb[:, 0, :], pS1[:, 0:128], 1.0 / 512, C_EPS, [pS1b], [rbb])
                    rstd(rb[:, 1, :], pS2[:, 0:128], 1.0 / 256, C_EPS, [pS2b], [rbb])
                    cn, cnb = cnr.next()
                    for ch in range(6):
                        k.op("dve", lambda: nc.vector.scalar_tensor_tensor(out=cn[:, ch, :], in0=xc[:, ch, :], scalar=gq[:, ch:ch + 1], in1=rb[:, 0 if ch < 4 else 1, :], op0=ALU.mult, op1=ALU.mult),
                             R=[xcb, wb_, rbb], W=[cnb])
                    for (W_, nch, c0_, hs, dst) in ((wuq, 4, 0, 192, qn_v), (wukv, 2, 4, 256, kn_v)):
                        for b in range(2):
                            p, pb = pq.next()
                            for hh_ in range(4):
                                h = b * 4 + hh_
                                for ch in range(nch):
                                    k.op("pe", lambda: nc.tensor.matmul(p[:, hh_ * 128:(hh_ + 1) * 128], lhsT=W_[:, ch, h * hs:h * hs + 128], rhs=cn[:, c0_ + ch, :],
                                                                        start=(ch == 0), stop=(ch == nch - 1)), R=[wb_, cnb], W=[pb])
                            st, sb = stq.next()
                            k.op("act", lambda: nc.scalar.copy(out=st[:], in_=p[:].rearrange("p (a b) -> p a b", a=4)), R=[pb], W=[sb])
                            k.dma("pool", dst[:, b * 4:(b + 1) * 4, t0:t0 + 128], st[:], R=[sb])
                    va, vab = var_.next()
                    for b in range(2):
                        p, pb = pq.next()
                        for ch in range(2):
                            k.op("pe", lambda: nc.tensor.matmul(p[:], lhsT=cn[:, 4 + ch, :], rhs=wukv[:, ch, :].rearrange("p (h d) -> p h d", d=256)[:, b * 4:(b + 1) * 4, 128:256],
                                                                start=(ch == 0), stop=(ch == 1)), R=[wb_, cnb], W=[pb])
                        k.op("act", lambda: nc.scalar.copy(out=va[:, b * 4:(b + 1) * 4, 0:128], in_=p[:].rearrange("p (a b) -> p a b", a=4)), R=[pb], W=[vab])
                    k.dma("pool", vaug.rearrange("h t d -> t h d")[t0:t0 + 128], va[:], R=[vab])
                    p, pb = pq.next()
                    for ch in range(4):
                        k.op("pe", lambda: nc.tensor.matmul(p[:], lhsT=cn[:, ch, :], rhs=wuq[:, ch, :].rearrange("p (h d) -> p h d", d=192)[:, :, 128:192],
                                                            start=(ch == 0), stop=(ch == 3)), R=[wb_, cnb], W=[pb])
                    pv = p[:].rearrange("p (h d) -> p h d", d=64)
                    sin8 = cs_[:, 0:32].unsqueeze(1).to_broadcast([128, 8, 32])
                    cos8 = cs_[:, 32:64].unsqueeze(1).to_broadcast([128, 8, 32])
                    qr, qrb = qrr.next()
                    t1, t1b = tmr.next(); t2, t2b = tmr.next(); t3, t3b = tmr.next(); t4, t4b = tmr.next()
                    k.op("dve", lambda: nc.vector.tensor_tensor(out=t1[:], in0=pv[:, :, 0:32], in1=cos8, op=ALU.mult), R=[pb, csb], W=[t1b])
                    k.op("dve", lambda: nc.vector.tensor_tensor(out=t2[:], in0=pv[:, :, 32:64], in1=sin8, op=ALU.mult), R=[pb, csb], W=[t2b])
                    k.op("dve", lambda: nc.vector.tensor_tensor(out=t3[:], in0=pv[:, :, 32:64], in1=cos8, op=ALU.mult), R=[pb, csb], W=[t3b])
                    k.op("dve", lambda: nc.vector.tensor_tensor(out=t4[:], in0=pv[:, :, 0:32], in1=sin8, op=ALU.mult), R=[pb, csb], W=[t4b])
                    k.op("pool", lambda: nc.gpsimd.tensor_tensor(out=qr[:, :, 0:32], in0=t1[:], in1=t2[:], op=ALU.subtract), R=[t1b, t2b], W=[qrb])
                    k.op("pool", lambda: nc.gpsimd.tensor_tensor(out=qr[:, :, 32:64], in0=t3[:], in1=t4[:], op=ALU.add), R=[t3b, t4b], W=[qrb])
                    p, pb = pq.next()
                    for b in range(4):
                        k.op("pe", lambda: nc.tensor.transpose(p[:, b * 128:(b + 1) * 128], qr[:, 2 * b:2 * b + 2, :].rearrange("p a d -> p (a d)"), ident), R=[qrb, cstb], W=[pb])
                    st, sb = stq.next()
                    k.op("act", lambda: nc.scalar.copy(out=st[:], in_=p[:].rearrange("p (a b) -> p a b", a=4)), R=[pb], W=[sb])
                    k.dma("pool", qr_v[:, :, t0:t0 + 128], st[:], R=[sb])
                    kr_, krb = krr.next()
                    t1, t1b = tmr.next(); t2, t2b = tmr.next()
                    k.op("dve", lambda: nc.vector.tensor_tensor(out=t1[:, 0, :], in0=kx[:, 0:32], in1=cs_[:, 32:64], op=ALU.mult), R=[kxb, csb], W=[t1b])
                    k.op("dve", lambda: nc.vector.tensor_tensor(out=t1[:, 1, :], in0=kx[:, 32:64], in1=cs_[:, 0:32], op=ALU.mult), R=[kxb, csb], W=[t1b])
                    k.op("dve", lambda: nc.vector.tensor_tensor(out=t2[:, 0, :], in0=kx[:, 32:64], in1=cs_[:, 32:64], op=ALU.mult), R=[kxb, csb], W=[t2b])
                    k.op("dve", lambda: nc.vector.tensor_tensor(out=t2[:, 1, :], in0=kx[:, 0:32], in1=cs_[:, 0:32], op=ALU.mult), R=[kxb, csb], W=[t2b])
                    k.op("pool", lambda: nc.gpsimd.tensor_tensor(out=kr_[:, 0:32], in0=t1[:, 0, :], in1=t1[:, 1, :], op=ALU.subtract), R=[t1b], W=[krb])
                    k.op("pool", lambda: nc.gpsimd.tensor_tensor(out=kr_[:, 32:64], in0=t2[:, 0, :], in1=t2[:, 1, :], op=ALU.add), R=[t2b], W=[krb])
                    p, pb = pq.next()
                    k.op("pe", lambda: nc.tensor.transpose(p[0:64, 0:128], kr_[:], ident), R=[krb, cstb], W=[pb])
                    ks, ksb = kst.next()
                    k.op("act", lambda: nc.scalar.copy(out=ks[:], in_=p[0:64, 0:128]), R=[pb], W=[ksb])
                    k.dma("pool", krT[:, t0:t0 + 128], ks[:], R=[ksb])
                k.barrier()

            with ExitStack() as es:
                SB = lambda name, shape: es.enter_context(sbt(nc, name, shape, F32))
                tri = cst[:, C_TRI:C_TRI + 128]
                SBh = lambda name, shape: es.enter_context(sbt(nc, name, shape, BF16))
                krt = SBh("b_krt", [64, T]); krtb = Buf()
                k.dma("sp", krt[:], krT, W=[krtb])
                knh = SBh("b_kn", [128, T]); qnh = SBh("b_qn", [128, T]); qrh = SBh("b_qr", [64, T])
                vah = SBh("b_va", [128, NT, 129]); zh = SB("b_z", [128, NT, 128])
                trib = SBh("b_tri", [128, 128]); tribb = Buf()
                k.op("dve", lambda: nc.vector.tensor_copy(out=trib[:], in_=tri), R=[cstb], W=[tribb])
                knb, qnb, qrb, vahb, zhb = [Buf() for _ in range(5)]
                Ptr = Ring(es, nc, "b_P", [128, 512], BF16, 3)
                smr = Ring(es, nc, "b_sm", [128, 2], F32, 2)
                ytr = Ring(es, nc, "b_y", [128, 128], F32, 2)
                szr = Ring(es, nc, "b_sz", [128, 128], F32, 2)
                sty = Ring(es, nc, "b_sty", [128, 128], BF16, 3)
                pST = Ring(es, nc, "b_pST", [128, 512], F32, 3, psum=True)
                pO = Ring(es, nc, "b_pO", [128, 512], F32, 2, psum=True)
                pX = Ring(es, nc, "b_pX", [128, 512], F32, 2, psum=True)
                SCL = float(192 ** -0.5)
                hcur = [0]

                def epilogue(po, pob_, qt, qs):
                    h = hcur[0]
                    sm, smb = smr.next()
                    k.op("dve", lambda: nc.vector.reciprocal(out=sm[:, 0:1], in_=po[:, 128:129]), R=[pob_], W=[smb])
                    yt, ytb = ytr.next()
                    k.op("dve", lambda: nc.vector.tensor_scalar(out=yt[:], in0=po[:, 0:128], scalar1=sm[:, 0:1], scalar2=None, op0=ALU.mult), R=[pob_, smb], W=[ytb])
                    sz, szb = szr.next()
                    k.op("act", lambda: nc.scalar.activation(out=sz[:], in_=zh[:, qt, :], func=AF.Silu), R=[zhb], W=[szb])
                    k.op("pool", lambda: nc.gpsimd.tensor_tensor(out=yt[:], in0=yt[:], in1=sz[:], op=ALU.mult), R=[ytb, szb], W=[ytb])
                    px, pxb = pX.next()
                    k.op("pe", lambda: nc.tensor.transpose(px[:, 0:128], yt[:], ident), R=[ytb, cstb], W=[pxb])
                    st, sb = sty.next()
                    k.op("dve", lambda: nc.vector.tensor_copy(out=st[:], in_=px[:, 0:128]), R=[pxb], W=[sb])
                    k.dma("pool", mixT[512 + h * 128:512 + (h + 1) * 128, qs], st[:], R=[sb])

                for h in range(8):
                    hcur[0] = h
                    k.dma("sp", knh[:], knT[h * 128:(h + 1) * 128, :], W=[knb])
                    k.dma("sp", qnh[:], qnT[h * 128:(h + 1) * 128, :], W=[qnb])
                    k.dma("sp", qrh[:], qrT[h * 64:(h + 1) * 64, :], W=[qrb])
                    k.dma("sp", vah[:], vaug[h].rearrange("(n p) d -> p n d", p=128), W=[vahb])
                    k.dma("sp", zh[:], pTM[:, TM_MZ + h * 128:TM_MZ + (h + 1) * 128].rearrange("(n p) d -> p n d", p=128), W=[zhb])
                    pend = [None]

                    def flush_pv():
                        if pend[0] is None:
                            return
                        (po_, pob2, Pt_, Ptb2, kb_, nk_, qt_, fin) = pend[0]
                        pend[0] = None
                        for i in range(nk_):
                            j = kb_ + i
                            k.op("pe", lambda: nc.tensor.matmul(po_[:, 0:129], lhsT=Pt_[:, i * 128:(i + 1) * 128], rhs=vah[:, j, :], start=(j == 0), stop=(j == qt_)), R=[Ptb2, vahb], W=[pob2])
                        if fin is not None:
                            fin()

                    for qt in range(NT):
                        qs = slice(qt * 128, (qt + 1) * 128)
                        po, pob_ = pO.next()
                        for kb in range(0, qt + 1, 4):
                            nk = min(4, qt + 1 - kb)
                            ps_, psb = pST.next()
                            for i in range(nk):
                                j = kb + i
                                js = slice(j * 128, (j + 1) * 128)
                                k.op("pe", lambda: nc.tensor.matmul(ps_[:, i * 128:(i + 1) * 128], lhsT=knh[:, js], rhs=qnh[:, qs], start=True, stop=False), R=[knb, qnb], W=[psb])
                                k.op("pe", lambda: nc.tensor.matmul(ps_[:, i * 128:(i + 1) * 128], lhsT=krt[:, js], rhs=qrh[:, qs], start=False, stop=True), R=[krtb, qrb], W=[psb])
                            flush_pv()
                            Pt, Ptb = Ptr.next()
                            k.op("act", lambda: nc.scalar.activation(out=Pt[:, 0:nk * 128], in_=ps_[:, 0:nk * 128], func=AF.Exp, scale=SCL), R=[psb], W=[Ptb])
                            last = (kb + nk - 1 == qt)
                            if last:
                                i = nk - 1
                                k.op("pool", lambda: nc.gpsimd.tensor_tensor(out=Pt[:, i * 128:(i + 1) * 128], in0=Pt[:, i * 128:(i + 1) * 128], in1=trib[:], op=ALU.mult), R=[Ptb, tribb], W=[Ptb])
                            pend[0] = (po, pob_, Pt, Ptb, kb, nk, qt, (lambda po=po, pob_=pob_, qt=qt, qs=qs: epilogue(po, pob_, qt, qs)) if last else None)
                    flush_pv()

                k.barrier()

            NCH = T // 64
            with ExitStack() as es:
                SB = lambda name, shape: es.enter_context(sbt(nc, name, shape, F32))
                mu = SB("r_mu", [128, 13]); prm = SB("r_prm", [128, 6, 4]); w0bc = SB("r_w0", [128, 512])
                w2t = SB("r_w2", [64, 512]); a2t = SB("r_a2", [128, 512])
                v1t = SB("r_v1", [128, 4, 32]); v2t = SB("r_v2", [32, 512])
                prb = Buf()
                k.dma("sp", mu[:], rw_mu[l].rearrange("(c p) -> p c", p=128), W=[prb], slow=True)
                plist = [rw_a0[l], rw_k_k[l], rw_k_a[l], rw_r_k[l].rearrange("h d -> (h d)")]
                if l > 0:
                    plist.append(rw_v0[l - 1])
                for i_, src in enumerate(plist):
                    k.dma("sp", prm[:, i_, :], src.rearrange("(c p) -> p c", p=128), W=[prb], slow=True)
                k.dma("sp", w0bc[:], rw_w0[l].partition_broadcast(128), W=[prb])
                k.dma("sp", w2t[:], rw_w2[l], W=[prb])
                k.dma("sp", a2t[64:128, :], rw_a2[l], W=[prb])
                if l > 0:
                    k.dma("sp", v1t[:], rw_v1[l - 1].rearrange("(c p) n -> p c n", p=128), W=[prb])
                    k.dma("sp", v2t[:], rw_v2[l - 1], W=[prb])
                k.op("dve", lambda: nc.vector.tensor_scalar(out=prm[:, 5, :], in0=prm[:, 2, :], scalar1=-1.0, scalar2=1.0, op0=ALU.mult, op1=ALU.add), R=[prb], W=[prb])
                bc = lambda ap_: ap_.unsqueeze(2).to_broadcast([128, 4, 128])
                Xr = Ring(es, nc, "r_X", [128, 13, 129], F32, 2)
                dr = Ring(es, nc, "r_d", [128, 13, 128], F32, 1)
                xsr = Ring(es, nc, "r_xs", [128, 13, 128], F32, 2)
                twr = Ring(es, nc, "r_tw", [64, 128], F32, 2)
                ldr = Ring(es, nc, "r_ld", [128, 512], F32, 2)
                T4 = lambda name, n=1: Ring(es, nc, name, [128, 4, 128], F32, n)
                gir, ger, aar, kkr_, khr, bvr = T4("r_gi"), T4("r_ge"), T4("r_aa"), T4("r_kk"), T4("r_kh"), T4("r_bv")
                e1r, e2r, e3r, e4r = T4("r_e1"), T4("r_e2"), T4("r_e3"), T4("r_e4")
                tmpr = T4("r_tmp", 3)
                outr = T4("r_out", 4)
                vfr = T4("r_vf", 2)
                m1r = Ring(es, nc, "r_m1", [32, 128], F32, 2)
                tmo = Ring(es, nc, "r_tmo", [128, 512], F32, 3)
                rkro = Ring(es, nc, "r_rkr", [128, 8], F32, 2)
                glo = Ring(es, nc, "r_glo", [128, 4, 2], F32, 2)
                pr = Ring(es, nc, "r_ps", [128, 512], F32, 8, psum=True)
                fmv = lambda dt_: dt_.rearrange("(j p) t -> p j t", p=128)
                for c in range(NT):
                    t0 = c * 128
                    X, Xb = Xr.next()
                    src = pFM.rearrange("(ch p) t -> p ch t", p=128)
                    if c == 0:
                        k.op("pool", lambda: nc.gpsimd.memset(X[:, :, 0:1], 0.0), W=[Xb])
                        k.dma("sp", X[:, :, 1:129], src[:, FM_RW:FM_RW + 13, 0:128], W=[Xb])
                    else:
                        k.dma("sp", X[:], src[:, FM_RW:FM_RW + 13, t0 - 1:t0 + 128], W=[Xb])
                    d_, db = dr.next()
                    k.op("dve", lambda: nc.vector.tensor_tensor(out=d_[:], in0=X[:, :, 0:128], in1=X[:, :, 1:129], op=ALU.subtract), R=[Xb], W=[db])
                    xs_, xsb = xsr.next()
                    for ch in range(13):
                        k.op("dve", lambda: nc.vector.scalar_tensor_tensor(out=xs_[:, ch, :], in0=d_[:, ch, :], scalar=mu[:, ch:ch + 1], in1=X[:, ch, 1:129], op0=ALU.mult, op1=ALU.add),
                             R=[db, Xb, prb], W=[xsb])
                    rr = xs_[:, 0:4, :]; kx = xs_[:, 4:8, :]; vv = xs_[:, 8:12, :]
                    tw, twb = twr.next()
                    k.op("act", lambda: nc.scalar.activation(out=tw[:], in_=xs_[0:64, 12, :], func=AF.Tanh), R=[xsb], W=[twb])
                    pz, pzb = pr.next()
                    k.op("pe", lambda: nc.tensor.matmul(pz[:], lhsT=tw[:], rhs=w2t[:], start=True, stop=True), R=[twb, prb], W=[pzb])
                    ld, ldb = ldr.next()
                    k.op("dve", lambda: nc.vector.tensor_tensor(out=ld[:], in0=pz[:], in1=w0bc[:], op=ALU.add), R=[pzb, prb], W=[ldb])
                    k.op("act", lambda: nc.scalar.activation(out=ld[:], in_=ld[:], func=AF.Sigmoid), R=[ldb], W=[ldb])
                    k.op("pool", lambda: nc.gpsimd.tensor_scalar(out=ld[:], in0=ld[:], scalar1=float(-np.exp(-0.5)), scalar2=None, op0=ALU.mult), R=[ldb], W=[ldb])
                    pgi, pgib = pr.next(); pge, pgeb = pr.next()
                    for j in range(4):
                        k.op("pe", lambda: nc.tensor.matmul(pgi[:, j * 128:(j + 1) * 128], lhsT=ld[:, j * 128:(j + 1) * 128], rhs=cst[:, C_BI:C_BI + 128], start=True, stop=True), R=[ldb, cstb], W=[pgib])
                        k.op("pe", lambda: nc.tensor.matmul(pge[:, j * 128:(j + 1) * 128], lhsT=ld[:, j * 128:(j + 1) * 128], rhs=cst[:, C_BS:C_BS + 128], start=True, stop=True), R=[ldb, cstb], W=[pgeb])
                    gi, gib = gir.next(); ge, geb = ger.next()
                    k.op("act", lambda: nc.scalar.copy(out=gi[:], in_=pgi[:].rearrange("p (a b) -> p a b", a=4)), R=[pgib], W=[gib])
                    e1, e1b = e1r.next(); e2, e2b = e2r.next(); e3, e3b = e3r.next(); e4, e4b = e4r.next()
                    k.op("act", lambda: nc.scalar.activation(out=e1[:], in_=gi[:], func=AF.Exp), R=[gib], W=[e1b])
                    k.op("act", lambda: nc.scalar.activation(out=e2[:], in_=pge[:].rearrange("p (a b) -> p a b", a=4), func=AF.Exp), R=[pgeb], W=[e2b])
                    k.op("act", lambda: nc.scalar.activation(out=e3[:], in_=gi[:], func=AF.Exp, scale=-1.0), R=[gib], W=[e3b])
                    for j in range(4):
                        for hf in range(2):
                            k.op("act", lambda: nc.scalar.activation(out=e4[:, j, hf * 64:(hf + 1) * 64], in_=gi[:, j, hf * 64:(hf + 1) * 64], func=AF.Exp, scale=-1.0,
                                                                     bias=gi[:, j, hf * 64 + 63:hf * 64 + 64]), R=[gib], W=[e4b])
                    pa, pab = pr.next()
                    for j in range(4):
                        k.op("pe", lambda: nc.tensor.matmul(pa[:, j * 128:(j + 1) * 128], lhsT=a2t[64:128, j * 128:(j + 1) * 128], rhs=xs_[64:128, 12, :], start=True, stop=True), R=[xsb, prb], W=[pab])
                    aa, aab = aar.next()
                    for j in range(4):
                        k.op("act", lambda: nc.scalar.activation(out=aa[:, j, :], in_=pa[:, j * 128:(j + 1) * 128], func=AF.Sigmoid, bias=prm[:, 0, j:j + 1]), R=[pab, prb], W=[aab])
                    if l > 0:
                        pm, pmb = pr.next()
                        for ch in range(4):
                            k.op("pe", lambda: nc.tensor.matmul(pm[0:32, 0:128], lhsT=v1t[:, ch, :], rhs=xs_[:, 8 + ch, :], start=(ch == 0), stop=(ch == 3)), R=[xsb, prb], W=[pmb])
                        m1, m1b = m1r.next()
                        k.op("act", lambda: nc.scalar.copy(out=m1[:], in_=pm[0:32, 0:128]), R=[pmb], W=[m1b])
                        pm2, pm2b = pr.next()
                        for j in range(4):
                            k.op("pe", lambda: nc.tensor.matmul(pm2[:, j * 128:(j + 1) * 128], lhsT=v2t[:, j * 128:(j + 1) * 128], rhs=m1[:], start=True, stop=True), R=[m1b, prb], W=[pm2b])
                        gt_, gtb_ = tmpr.next()
                        for j in range(4):
                            k.op("act", lambda: nc.scalar.activation(out=gt_[:, j, :], in_=pm2[:, j * 128:(j + 1) * 128], func=AF.Sigmoid, bias=prm[:, 4, j:j + 1]), R=[pm2b, prb], W=[gtb_])
                        vf, vfb = vfr.next()
                        k.dma("sp", vf[:], fmv(vfirst)[:, :, t0:t0 + 128], W=[vfb])
                        k.op("dve", lambda: nc.vector.tensor_tensor(out=vf[:], in0=vf[:], in1=vv, op=ALU.subtract), R=[vfb, xsb], W=[vfb])
                        k.op("pool", lambda: nc.gpsimd.tensor_tensor(out=vf[:], in0=vf[:], in1=gt_[:], op=ALU.mult), R=[vfb, gtb_], W=[vfb])
                        k.op("dve", lambda: nc.vector.tensor_tensor(out=xs_[:, 8:12, :], in0=vv, in1=vf[:], op=ALU.add), R=[vfb, xsb], W=[xsb])
                    else:
                        k.dma("pool", fmv(vfirst)[:, :, t0:t0 + 128], vv, R=[xsb])
                    kk, kkb = kkr_.next()
                    k.op("dve", lambda: nc.vector.tensor_tensor(out=kk[:], in0=kx, in1=bc(prm[:, 1, :]), op=ALU.mult), R=[xsb, prb], W=[kkb])
                    sq_, sqb_ = tmpr.next()
                    k.op("act", lambda: nc.scalar.activation(out=sq_[:], in_=kk[:], func=AF.Square), R=[kkb], W=[sqb_])
                    pn, pnb = pr.next()
                    for j in range(4):
                        k.op("pe", lambda: nc.tensor.matmul(pn[:, j * 128:(j + 1) * 128], lhsT=cst[:, C_BD:C_BD + 128], rhs=sq_[:, j, :], start=True, stop=True), R=[sqb_, cstb], W=[pnb])
                    rn, rnb = tmpr.next()
                    k.op("act", lambda: nc.scalar.activation(out=rn[:], in_=pn[:].rearrange("p (a b) -> p a b", a=4), func=AF.Sqrt), R=[pnb], W=[rnb])
                    k.op("dve", lambda: nc.vector.tensor_scalar_max(out=rn[:], in0=rn[:], scalar1=1e-12), R=[rnb], W=[rnb])
                    k.op("dve", lambda: nc.vector.reciprocal(out=rn[:], in_=rn[:]), R=[rnb], W=[rnb])
                    k.op("pool", lambda: nc.gpsimd.tensor_tensor(out=kk[:], in0=kk[:], in1=rn[:], op=ALU.mult), R=[kkb, rnb], W=[kkb])
                    kh, khb = khr.next()
                    k.op("dve", lambda: nc.vector.tensor_tensor(out=kh[:], in0=aa[:], in1=bc(prm[:, 2, :]), op=ALU.mult), R=[aab, prb], W=[khb])
                    k.op("dve", lambda: nc.vector.tensor_tensor(out=kh[:], in0=kh[:], in1=bc(prm[:, 5, :]), op=ALU.add), R=[khb, prb], W=[khb])
                    k.op("dve", lambda: nc.vector.tensor_tensor(out=kh[:], in0=kh[:], in1=kx, op=ALU.mult), R=[khb, xsb], W=[khb])
                    bv, bvb = bvr.next()
                    k.op("pool", lambda: nc.gpsimd.tensor_tensor(out=bv[:], in0=kk[:], in1=aa[:], op=ALU.mult), R=[kkb, aab], W=[bvb])
                    def emit(dst, in0, in1, neg=False, R=()):
                        o, ob = outr.next()
                        k.op("dve", lambda: nc.vector.tensor_tensor(out=o[:], in0=in0, in1=in1, op=ALU.mult), R=list(R), W=[ob])
                        if neg:
                            k.op("pool", lambda: nc.gpsimd.tensor_scalar(out=o[:], in0=o[:], scalar1=-1.0, scalar2=None, op0=ALU.mult), R=[ob], W=[ob])
                        k.dma("pool", fmv(dst)[:, :, t0:t0 + 128], o[:], R=[ob])
                        return o, ob
                    emit(rwA, kk[:], e2[:], neg=True, R=[kkb, e2b])
                    emit(rwR, rr, e1[:], R=[xsb, e1b])
                    emit(rwB, bv[:], e3[:], R=[bvb, e3b])
                    emit(rwK, kh[:], e3[:], R=[khb, e3b])
                    for (dst, a_, ab_) in ((rwBp, bv, bvb), (rwKp, kh, khb), (rwV, None, None)):
                        if a_ is not None:
                            o, ob = outr.next()
                            k.op("dve", lambda: nc.vector.tensor_tensor(out=o[:], in0=a_[:], in1=e4[:], op=ALU.mult), R=[ab_, e4b], W=[ob])
                            srcv = o
                        else:
                            srcv, ob = xs_[:, 8:12, :], xsb
                        pt_, ptb_ = pr.next()
                        for j in range(4):
                            k.op("pe", lambda: nc.tensor.transpose(pt_[:, j * 128:(j + 1) * 128], srcv[:, j, :], ident), R=[ob, cstb], W=[ptb_])
                        to, tob = tmo.next()
                        k.op("act", lambda: nc.scalar.copy(out=to[:], in_=pt_[:]), R=[ptb_], W=[tob])
                        k.dma("pool", dst[t0:t0 + 128, :], to[:], R=[tob])
                    go_, gob = glo.next()
                    k.op("pool", lambda: nc.gpsimd.tensor_copy(out=go_[:, :, 0:1], in_=e1[:, :, 63:64]), R=[e1b], W=[gob])
                    k.op("pool", lambda: nc.gpsimd.tensor_copy(out=go_[:, :, 1:2], in_=e1[:, :, 127:128]), R=[e1b], W=[gob])
                    k.dma("pool", rwGL.rearrange("(j p) c -> p j c", p=128)[:, :, 2 * c:2 * c + 2], go_[:], R=[gob], slow=True)
                    pd_, pdb_ = tmpr.next()
                    k.op("dve", lambda: nc.vector.tensor_tensor(out=pd_[:], in0=rr, in1=kh[:], op=ALU.mult), R=[xsb, khb], W=[pdb_])
                    k.op("pool", lambda: nc.gpsimd.tensor_tensor(out=pd_[:], in0=pd_[:], in1=bc(prm[:, 3, :]), op=ALU.mult), R=[pdb_, prb], W=[pdb_])
                    pk, pkb = pr.next()
                    for j in range(4):
                        k.op("pe", lambda: nc.tensor.matmul(pk[:, 0:8], lhsT=pd_[:, j, :], rhs=cst[:, C_HS + 8 * j:C_HS + 8 * j + 8], start=(j == 0), stop=(j == 3)), R=[pdb_, cstb], W=[pkb])
                    ro, rob = rkro.next()
                    k.op("act", lambda: nc.scalar.copy(out=ro[:], in_=pk[:, 0:8]), R=[pkb], W=[rob])
                    k.dma("pool", rwRKR[t0:t0 + 128, :], ro[:], R=[rob])
                k.barrier()

            with ExitStack() as es:
                SB = lambda name, shape: es.enter_context(sbt(nc, name, shape, F32))
                GLt = SB("c_GL", [64, 8, NCH]); glb = Buf()
                k.dma("sp", GLt[:], rwGL.rearrange("(h q) c -> q h c", q=64), W=[glb])
                hv = lambda dt_: dt_.rearrange("(h q) t -> q h t", q=64)
                ARr = Ring(es, nc, "c_AR", [64, 8, 2, 64], F32, 3)
                BKr = Ring(es, nc, "c_BK", [64, 8, 2, 64], F32, 3)
                TMr = Ring(es, nc, "c_TM", [64, 3, 512], F32, 3)
                Hr = Ring(es, nc, "c_H", [64, 8, 64], F32, 2)
                A1r = Ring(es, nc, "c_A1", [64, 8, 128], F32, 2)
                A2r = Ring(es, nc, "c_A2", [64, 8, 128], F32, 2)
                Pr_ = Ring(es, nc, "c_P", [64, 8, 64], F32, 3)
                Ptr_ = Ring(es, nc, "c_Pt", [64, 8, 64], F32, 3)
                Lr = Ring(es, nc, "c_L", [64, 8, 64], F32, 3)
                Xsr = Ring(es, nc, "c_Xs", [64, 8, 64], F32, 2)
                Usr = Ring(es, nc, "c_Us", [64, 8, 64], F32, 2)
                Yr = Ring(es, nc, "c_Y", [64, 512], F32, 3)
                pr = Ring(es, nc, "c_ps", [128, 512], F32, 8, psum=True)
                m1 = cst[0:64, C_M1:C_M1 + 128].unsqueeze(1).to_broadcast([64, 4, 128])
                m3 = cst[0:64, C_M3:C_M3 + 64].unsqueeze(1).to_broadcast([64, 8, 64])
                i64b = cst[0:64, 0:64].unsqueeze(1).to_broadcast([64, 8, 64])
                H, Hb = Hr.next()
                k.op("pool", lambda: nc.gpsimd.memset(H[:], 0.0), W=[Hb])
                v8 = lambda p_: p_[0:64, :].rearrange("p (h d) -> p h d", h=8)
                for c in range(NCH):
                    cs_ = slice(c * 64, (c + 1) * 64)
                    AR, ARb = ARr.next(); BK, BKb = BKr.next(); TM_, TMb = TMr.next()
                    k.dma("sp", AR[:, :, 0, :], hv(rwA)[:, :, cs_], W=[ARb])
                    k.dma("sp", AR[:, :, 1, :], hv(rwR)[:, :, cs_], W=[ARb])
                    k.dma("sp", BK[:, :, 0, :], hv(rwB)[:, :, cs_], W=[BKb])
                    k.dma("sp", BK[:, :, 1, :], hv(rwK)[:, :, cs_], W=[BKb])
                    k.dma("sp", TM_[:, 0, :], rwBp[cs_, :], W=[TMb])
                    k.dma("sp", TM_[:, 1, :], rwKp[cs_, :], W=[TMb])
                    k.dma("sp", TM_[:, 2, :], rwV[cs_, :], W=[TMb])
                    A1, A1b = A1r.next(); A2, A2b = A2r.next()
                    for (A_, Ab_, which) in ((A1, A1b, 0), (A2, A2b, 1)):
                        for b in range(2):
                            p, pb = pr.next()
                            for hh_ in range(4):
                                h = b * 4 + hh_
                                k.op("pe", lambda: nc.tensor.matmul(p[0:64, hh_ * 128:(hh_ + 1) * 128], lhsT=BK[:, h, which, :], rhs=AR[:, h, :, :].rearrange("p a d -> p (a d)"), start=True, stop=True),
                                     R=[BKb, ARb], W=[pb])
                            k.op("dve", lambda: nc.vector.tensor_tensor(out=A_[:, b * 4:(b + 1) * 4, :], in0=p[0:64, :].rearrange("p (h d) -> p h d", h=4), in1=m1, op=ALU.mult), R=[pb, cstb], W=[Ab_])
                    p, pb = pr.next()
                    for h in range(8):
                        k.op("pe", lambda: nc.tensor.matmul(p[0:64, h * 64:(h + 1) * 64], lhsT=AR[:, h, 0, :], rhs=BK[:, h, 0, :], start=True, stop=True), R=[ARb, BKb], W=[pb])
                    Pt_, Ptb_ = Ptr_.next()
                    k.op("dve", lambda: nc.vector.tensor_tensor(out=Pt_[:], in0=v8(p), in1=m3, op=ALU.mult), R=[pb, cstb], W=[Ptb_])
                    P_, Pb_ = Pr_.next()
                    k.op("pool", lambda: nc.gpsimd.tensor_copy(out=P_[:], in_=A1[:, :, 0:64]), R=[A1b], W=[Pb_])
                    L_, Lb_ = Lr.next()
                    k.op("pool", lambda: nc.gpsimd.tensor_tensor(out=L_[:], in0=A1[:, :, 0:64], in1=i64b, op=ALU.add), R=[A1b, cstb], W=[Lb_])
                    for lev in range(5):
                        pa, pab = pr.next(); pb2, pb2b = pr.next()
                        for h in range(8):
                            k.op("pe", lambda: nc.tensor.matmul(pa[0:64, h * 64:(h + 1) * 64], lhsT=Pt_[:, h, :], rhs=P_[:, h, :], start=True, stop=True), R=[Ptb_, Pb_], W=[pab])
                        for h in range(8):
                            k.op("pe", lambda: nc.tensor.matmul(pb2[0:64, h * 64:(h + 1) * 64], lhsT=P_[:, h, :], rhs=Pt_[:, h, :], start=True, stop=True), R=[Ptb_, Pb_], W=[pb2b])
                        Pn, Pnb = Pr_.next(); Ptn, Ptnb = Ptr_.next()
                        k.op("act", lambda: nc.scalar.copy(out=Pn[:], in_=v8(pa)), R=[pab], W=[Pnb])
                        k.op("dve", lambda: nc.vector.tensor_copy(out=Ptn[:], in_=v8(pb2)), R=[pb2b], W=[Ptnb])
                        P_, Pb_, Pt_, Ptb_ = Pn, Pnb, Ptn, Ptnb
                        pc, pcb = pr.next()
                        for h in range(8):
                            k.op("pe", lambda: nc.tensor.matmul(pc[0:64, h * 64:(h + 1) * 64], lhsT=Pt_[:, h, :], rhs=L_[:, h, :], start=True, stop=True), R=[Ptb_, Lb_], W=[pcb])
                        Ln, Lnb = Lr.next()
                        k.op("dve", lambda: nc.vector.tensor_tensor(out=Ln[:], in0=L_[:], in1=v8(pc), op=ALU.add), R=[Lb_, pcb], W=[Lnb])
                        L_, Lb_ = Ln, Lnb
                    Vh = lambda h: TM_[:, 2, h * 64:(h + 1) * 64]
                    px, pxb = pr.next()
                    for h in range(8):
                        k.op("pe", lambda: nc.tensor.matmul(px[0:64, h * 64:(h + 1) * 64], lhsT=AR[:, h, 0, :], rhs=H[:, h, :], start=True, stop=False), R=[ARb, Hb], W=[pxb])
                        k.op("pe", lambda: nc.tensor.matmul(px[0:64, h * 64:(h + 1) * 64], lhsT=A2[:, h, 0:64], rhs=Vh(h), start=False, stop=True), R=[A2b, TMb], W=[pxb])
                    Xs, Xsb = Xsr.next()
                    k.op("act", lambda: nc.scalar.copy(out=Xs[:], in_=v8(px)), R=[pxb], W=[Xsb])
                    pu, pub = pr.next()
                    for h in range(8):
                        k.op("pe", lambda: nc.tensor.matmul(pu[0:64, h * 64:(h + 1) * 64], lhsT=L_[:, h, :], rhs=Xs[:, h, :], start=True, stop=True), R=[Lb_, Xsb], W=[pub])
                    Us, Usb = Usr.next()
                    k.op("dve", lambda: nc.vector.tensor_copy(out=Us[:], in_=v8(pu)), R=[pub], W=[Usb])
                    py, pyb = pr.next()
                    for h in range(8):
                        k.op("pe", lambda: nc.tensor.matmul(py[0:64, h * 64:(h + 1) * 64], lhsT=AR[:, h, 1, :], rhs=H[:, h, :], start=True, stop=False), R=[ARb, Hb], W=[pyb])
                        k.op("pe", lambda: nc.tensor.matmul(py[0:64, h * 64:(h + 1) * 64], lhsT=A1[:, h, 64:128], rhs=Us[:, h, :], start=False, stop=False), R=[A1b, Usb], W=[pyb])
                        k.op("pe", lambda: nc.tensor.matmul(py[0:64, h * 64:(h + 1) * 64], lhsT=A2[:, h, 64:128], rhs=Vh(h), start=False, stop=True), R=[A2b, TMb], W=[pyb])
                    Yt, Ytb = Yr.next()
                    k.op("act", lambda: nc.scalar.copy(out=Yt[:], in_=py[0:64, :]), R=[pyb], W=[Ytb])
                    k.dma("pool", rwY[cs_, :], Yt[:], R=[Ytb])
                    ph, phb = pr.next()
                    for h in range(8):
                        k.op("pe", lambda: nc.tensor.matmul(ph[0:64, h * 64:(h + 1) * 64], lhsT=TM_[:, 0, h * 64:(h + 1) * 64], rhs=Us[:, h, :], start=True, stop=False), R=[TMb, Usb], W=[phb])
                        k.op("pe", lambda: nc.tensor.matmul(ph[0:64, h * 64:(h + 1) * 64], lhsT=TM_[:, 1, h * 64:(h + 1) * 64], rhs=Vh(h), start=False, stop=True), R=[TMb], W=[phb])
                    Hn, Hnb = Hr.next()
                    k.op("dve", lambda: nc.vector.tensor_tensor(out=Hn[:], in0=H[:], in1=GLt[:, :, c:c + 1].to_broadcast([64, 8, 64]), op=ALU.mult), R=[Hb, glb], W=[Hnb])
                    k.op("dve", lambda: nc.vector.tensor_tensor(out=Hn[:], in0=Hn[:], in1=v8(ph), op=ALU.add), R=[Hnb, phb], W=[Hnb])
                    H, Hb = Hn, Hnb
                k.barrier()

            with ExitStack() as es:
                SB = lambda name, shape: es.enter_context(sbt(nc, name, shape, F32))
                lng = SB("e_g", [128, 512]); lnb = SB("e_b", [128, 512]); eb = Buf()
                k.dma("sp", lng[:], rw_ln_g[l].partition_broadcast(128), W=[eb])
                k.dma("sp", lnb[:], rw_ln_b[l].partition_broadcast(128), W=[eb])
                inr = Ring(es, nc, "e_in", [128, 3, 512], F32, 2)
                rkr_ = Ring(es, nc, "e_rk", [128, 8], F32, 2)
                sqr = Ring(es, nc, "e_sq", [128, 8, 64], F32, 2)
                str_ = Ring(es, nc, "e_st", [128, 4, 8], F32, 2)
                yr = Ring(es, nc, "e_y", [128, 8, 64], F32, 2)
                mtr = Ring(es, nc, "e_mt", [128, 4, 128], BF16, 2)
                pr = Ring(es, nc, "e_ps", [128, 512], F32, 2, psum=True)
                b8 = lambda ap_: ap_.unsqueeze(2).to_broadcast([128, 8, 64])
                v3 = lambda ap_: ap_.rearrange("p (h d) -> p h d", h=8)
                for c in range(NT):
                    t0 = c * 128
                    it, itb = inr.next()
                    k.dma("sp", it[:, 0, :], rwY[t0:t0 + 128, :], W=[itb])
                    k.dma("sp", it[:, 1, :], rwV[t0:t0 + 128, :], W=[itb])
                    k.dma("sp", it[:, 2, :], pTM[t0:t0 + 128, TM_RZ:TM_RZ + 512], W=[itb])
                    rk, rkb = rkr_.next()
                    k.dma("sp", rk[:], rwRKR[t0:t0 + 128, :], W=[rkb])
                    Y3 = v3(it[:, 0, :])
                    sq, sqb = sqr.next()
                    k.op("act", lambda: nc.scalar.activation(out=sq[:], in_=Y3, func=AF.Square), R=[itb], W=[sqb])
                    st, stb = str_.next()
                    k.op("dve", lambda: nc.vector.tensor_reduce(out=st[:, 0, :], in_=Y3, axis=AX.X, op=ALU.add), R=[itb], W=[stb])
                    k.op("dve", lambda: nc.vector.tensor_reduce(out=st[:, 1, :], in_=sq[:], axis=AX.X, op=ALU.add), R=[sqb], W=[stb])
                    k.op("dve", lambda: nc.vector.tensor_scalar(out=st[:, 0, :], in0=st[:, 0, :], scalar1=1.0 / 64, scalar2=None, op0=ALU.mult), R=[stb], W=[stb])
                    k.op("dve", lambda: nc.vector.tensor_tensor(out=st[:, 2, :], in0=st[:, 0, :], in1=st[:, 0, :], op=ALU.mult), R=[stb], W=[stb])
                    k.op("dve", lambda: nc.vector.scalar_tensor_tensor(out=st[:, 1, :], in0=st[:, 1, :], scalar=1.0 / 64, in1=st[:, 2, :], op0=ALU.mult, op1=ALU.subtract), R=[stb], W=[stb])
                    rstd(st[:, 3, :], st[:, 1, :], 1.0, C_GNEPS, [stb], [stb])
                    y, yb = yr.next()
                    k.op("dve", lambda: nc.vector.tensor_tensor(out=y[:], in0=Y3, in1=b8(st[:, 0, :]), op=ALU.subtract), R=[itb, stb], W=[yb])
                    k.op("dve", lambda: nc.vector.tensor_tensor(out=y[:], in0=y[:], in1=b8(st[:, 3, :]), op=ALU.mult), R=[yb, stb], W=[yb])
                    k.op("pool", lambda: nc.gpsimd.tensor_tensor(out=y[:], in0=y[:], in1=v3(lng[:]), op=ALU.mult), R=[yb, eb], W=[yb])
                    k.op("pool", lambda: nc.gpsimd.tensor_tensor(out=y[:], in0=y[:], in1=v3(lnb[:]), op=ALU.add), R=[yb, eb], W=[yb])
                    k.op("dve", lambda: nc.vector.tensor_tensor(out=sq[:], in0=v3(it[:, 1, :]), in1=b8(rk[:]), op=ALU.mult), R=[itb, rkb, sqb], W=[sqb])
                    k.op("dve", lambda: nc.vector.tensor_tensor(out=y[:], in0=y[:], in1=sq[:], op=ALU.add), R=[yb, sqb], W=[yb])
                    k.op("act", lambda: nc.scalar.activation(out=it[:, 2, :], in_=it[:, 2, :], func=AF.Silu), R=[itb], W=[itb])
                    k.op("dve", lambda: nc.vector.tensor_tensor(out=y[:], in0=y[:], in1=v3(it[:, 2, :]), op=ALU.mult), R=[yb, itb], W=[yb])
                    p, pb = pr.next()
                    yf = y[:].rearrange("p h d -> p (h d)")
                    for j in range(4):
                        k.op("pe", lambda: nc.tensor.transpose(p[:, j * 128:(j + 1) * 128], yf[:, j * 128:(j + 1) * 128], ident), R=[yb, cstb], W=[pb])
                    mt, mtb = mtr.next()
                    k.op("act", lambda: nc.scalar.copy(out=mt[:], in_=p[:].rearrange("p (a b) -> p a b", a=4)), R=[pb], W=[mtb])
                    k.dma("pool", mixT.rearrange("(cc p) t -> p cc t", p=128)[:, 12:16, t0:t0 + 128], mt[:], R=[mtb])
                k.barrier()

            with ExitStack() as es:
                mx = Ring(es, nc, "p5_m", [128, 16, G], BF16, 2)
                wfr = Ring(es, nc, "p5_wf", [128, 16, 512], F32, 2)
                wr = Ring(es, nc, "p5_w", [128, 16, 512], BF16, 2)
                xr = Ring(es, nc, "p5_x", [128, G], F32, 3)
                xo = Ring(es, nc, "p5_o", [128, G], F32, 3)
                ps = Ring(es, nc, "p5_ps", [128, 512], F32, 4, psum=True)
                for g in range(NG):
                    mt, mb = mx.next()
                    k.dma("sp", mt[:], mixT.rearrange("(cc p) t -> p cc t", p=128)[:, :, g * G:(g + 1) * G], W=[mb])
                    for nb in range(4):
                        wf, wfb = wfr.next()
                        k.dma("sp", wf[:], w_out[l].rearrange("(cc p) n -> p cc n", p=128)[:, :, nb * 512:(nb + 1) * 512], W=[wfb])
                        wt, wb = wr.next()
                        k.op("act", lambda: nc.scalar.copy(out=wt[:, 0:8, :], in_=wf[:, 0:8, :]), R=[wfb], W=[wb])
                        k.op("pool", lambda: nc.gpsimd.tensor_copy(out=wt[:, 8:16, :], in_=wf[:, 8:16, :]), R=[wfb], W=[wb])
                        for j in range(4):
                            n0 = nb * 512 + j * 128
                            xt, xb = xr.next()
                            k.dma("sp", xt[:], xT[n0:n0 + 128, g * G:(g + 1) * G], W=[xb])
                            p, pb = ps.next()
                            for cc in range(16):
                                k.op("pe", lambda: nc.tensor.matmul(p[:, :G], lhsT=wt[:, cc, j * 128:(j + 1) * 128], rhs=mt[:, cc, :],
                                                                    start=(cc == 0), stop=(cc == 15)), R=[wb, mb], W=[pb])
                            ot, ob = xo.next()
                            k.op("dve", lambda: nc.vector.tensor_tensor(out=ot[:], in0=p[:, :G], in1=xt[:], op=ALU.add), R=[pb, xb], W=[ob])
                            k.dma("pool", xT[n0:n0 + 128, g * G:(g + 1) * G], ot[:], R=[ob])
                k.barrier()

        with ExitStack() as es:
            gbc = es.enter_context(sbt(nc, "f_g", [128, D], F32))
            gbb = Buf()
            k.dma("sp", gbc[:], final_g.partition_broadcast(128), W=[gbb])
            xin = Ring(es, nc, "f_x", [128, 16, 128], F32, 2)
            xtm = Ring(es, nc, "f_t", [128, D], F32, 2)
            junk = es.enter_context(sbt(nc, "f_j", [128, D], F32))
            jb = Buf()
            ssq = Ring(es, nc, "f_s", [128, 2], F32, 2)
            ot = Ring(es, nc, "f_o", [128, D], F32, 2)
            ps = Ring(es, nc, "f_ps", [128, 512], F32, 8, psum=True)
            for t in range(NT):
                xt, xb = xin.next()
                k.dma("sp", xt[:], xT.rearrange("(kc p) t -> p kc t", p=128)[:, :, t * 128:(t + 1) * 128], W=[xb])
                xm, xmb = xtm.next()
                for q in range(4):
                    p, pb = ps.next()
                    for j in range(4):
                        kc = q * 4 + j
                        k.op("pe", lambda: nc.tensor.transpose(p[:, j * 128:(j + 1) * 128], xt[:, kc, :], ident), R=[xb, cstb], W=[pb])
                    if q % 2:
                        k.op("act", lambda: nc.scalar.copy(out=xm[:, q * 512:(q + 1) * 512], in_=p[:]), R=[pb], W=[xmb])
                    else:
                        k.op("dve", lambda: nc.vector.tensor_copy(out=xm[:, q * 512:(q + 1) * 512], in_=p[:]), R=[pb], W=[xmb])
                sq, sqb = ssq.next()
                k.op("act", lambda: nc.scalar.activation(out=junk[:], in_=xm[:], func=AF.Square, accum_out=sq[:, 0:1]), R=[xmb], W=[jb, sqb])
                rstd(sq[:, 1:2], sq[:, 0:1], 1.0 / D, C_EPS, [sqb], [sqb])
                o, ob = ot.next()
                k.op("dve", lambda: nc.vector.scalar_tensor_tensor(out=o[:], in0=xm[:], scalar=sq[:, 1:2], in1=gbc[:], op0=ALU.mult, op1=ALU.mult),
                     R=[xmb, sqb, gbb], W=[ob])
                k.dma("pool", out[t * 128:(t + 1) * 128, :], o[:], R=[ob])
            k.barrier()
    return nc


def MIXERS(env):
    pass


_CACHE = {}


WNAMES = ("norm_g", "w_in", "w_out", "final_norm_g", "ml_conv_w", "ml_conv_b", "ml_i_bias", "ml_f_bias", "ml_norm_g",
          "mla_q_norm_g", "mla_w_uq", "mla_kv_norm_g", "mla_w_ukv",
          "rw_mu", "rw_w0", "rw_w2", "rw_a0", "rw_a2", "rw_k_k", "rw_k_a", "rw_r_k", "rw_ln_g", "rw_ln_b")


def make_maps(inputs, T, depth):
    consts = make_consts()
    shared = {}
    for name in WNAMES:
        a = inputs[name]
        shared[name] = np.ascontiguousarray(a if name == "final_norm_g" else a[:depth])
    for name in ("rw_v0", "rw_v1", "rw_v2"):
        shared[name] = np.ascontiguousarray(inputs[name])
    in_maps = []
    for c in range(8):
        b = c % 4
        m = {"x": np.ascontiguousarray(inputs["x"][b, :T]), "positions": np.ascontiguousarray(inputs["positions"][b, :T]),
             "consts": consts}
        m.update(shared)
        in_maps.append(m)
    return in_maps


def kernel(**inputs):
    T = 4096
    depth = 4
    if "nc" not in _CACHE:
        _CACHE["nc"] = build(T, depth)
    nc = _CACHE["nc"]
    in_maps = make_maps(inputs, T, depth)
    res = run_bass_kernel_spmd(nc, in_maps, core_ids=list(range(8)))
    return np.stack([res.results[b]["out"] for b in range(4)], axis=0)
```

```python
import numpy as np
from contextlib import ExitStack
import concourse.bass as bass
import concourse.mybir as mybir
from concourse.bass_utils import run_bass_kernel_spmd

F32 = mybir.dt.float32
BF16 = mybir.dt.bfloat16
I32 = mybir.dt.int32
AF = mybir.ActivationFunctionType
ALU = mybir.AluOpType
AX = mybir.AxisListType

D = 2048
DIN = 6600
EPS = 1e-6
FM_RANGES = [(0, 1024), (2568, 3336), (4424, 6088)]
TM_RANGES = [(1024, 2568), (3336, 4424), (6088, 6600)]
TM_V, TM_I, TM_F, TM_O, TM_Z = 0, 512, 516, 520, 1032
TM_KR, TM_MZ, TM_RZ = 1544, 1608, 2632
NTM = 3144
FM_QK, FM_CQ, FM_CKV, FM_RW = 0, 8, 12, 14
NFM = 27


_UID = [0]


def sbt(nc, name, shape, dt):
    _UID[0] += 1
    return nc.sbuf_tensor("%s_u%d" % (name, _UID[0]), shape, dt)


def pst(nc, name, shape, dt):
    _UID[0] += 1
    return nc.psum_tensor("%s_u%d" % (name, _UID[0]), shape, dt)


class Buf:
    __slots__ = ("w", "r")

    def __init__(self):
        self.w = None
        self.r = {}


class KB:
    NDS = 24

    def __init__(self, nc):
        self.nc = nc
        self.E = {"pe": nc.tensor, "act": nc.scalar, "dve": nc.vector, "pool": nc.gpsimd, "sp": nc.sync}
        self.sem = {e: nc.alloc_semaphore("s_" + e) for e in ("pe", "act", "dve", "pool")}
        self.cnt = {e: 0 for e in self.sem}
        self.dsem = [nc.alloc_semaphore("d%d" % i) for i in range(self.NDS)]
        self.dcnt = [0] * self.NDS
        self.dnext = 0
        self.bar = nc.alloc_semaphore("bar")
        self.nbar = 0
        self.seen = {e: {} for e in self.E}

    def _semh(self, key):
        return self.sem[key] if isinstance(key, str) else self.dsem[key[1]]

    def _wait(self, e, key, val, same_ok=False):
        if val <= 0:
            return
        if key == e and (same_ok or e == "pe"):
            return
        if self.seen[e].get(key, 0) >= val:
            return
        self.E[e].wait_ge(self._semh(key), val)
        self.seen[e][key] = val

    def _deps(self, e, reads, writes):
        for b in reads:
            if b.w is not None:
                self._wait(e, b.w[0], b.w[1])
        for b in writes:
            if b.w is not None:
                self._wait(e, b.w[0], b.w[1])
            for key, val in b.r.items():
                self._wait(e, key, val, same_ok=True)

    def _mark(self, ev, reads, writes):
        for b in reads:
            if b.r.get(ev[0], 0) < ev[1]:
                b.r[ev[0]] = ev[1]
        for b in writes:
            b.w = ev
            b.r = {}

    def op(self, e, fn, R=(), W=()):
        self._deps(e, R, W)
        ins = fn()
        self.cnt[e] += 1
        ins.then_inc(self.sem[e], 1)
        self._mark((e, self.cnt[e]), R, W)

    def dma(self, q, out, in_, R=(), W=(), slow=False):
        i = self.dnext
        self.dnext = (i + 1) % self.NDS
        key = ("d", i)
        self._wait(q, key, self.dcnt[i])
        self._deps(q, R, W)
        if slow:
            ins = self.E[q].dma_start(out=out, in_=in_, allow_slow_non_contiguous=True)
        else:
            ins = self.E[q].dma_start(out=out, in_=in_)
        self.dcnt[i] += 16
        ins.then_inc(self.dsem[i], 16)
        self._mark((key, self.dcnt[i]), R, W)

    def barrier(self):
        sp = self.E["sp"]
        for e in self.sem:
            self._wait("sp", e, self.cnt[e])
        for i in range(self.NDS):
            self._wait("sp", ("d", i), self.dcnt[i])
        self.nbar += 1
        sp.sem_inc(self.bar, 1)
        for e in ("pe", "act", "dve", "pool"):
            self.E[e].wait_ge(self.bar, self.nbar)
            for k2 in self.sem:
                self.seen[e][k2] = self.cnt[k2]
            for i in range(self.NDS):
                self.seen[e][("d", i)] = self.dcnt[i]


class Ring:
    def __init__(self, es, nc, name, shape, dtype, n, psum=False):
        self.items = []
        for i in range(n):
            if psum:
                t = es.enter_context(pst(nc, "%s%d" % (name, i), shape, dtype))
            else:
                t = es.enter_context(sbt(nc, "%s%d" % (name, i), shape, dtype))
            self.items.append((t, Buf()))
        self.i = 0

    def next(self):
        it = self.items[self.i]
        self.i = (self.i + 1) % len(self.items)
        return it


def split_blocks(ranges, maxw=512):
    out = []
    for (a, b) in ranges:
        c = a
        while c < b:
            w = min(maxw, b - c)
            out.append((c, w))
            c += w
    return out


def make_consts():
    c = np.zeros((128, 1280), np.float32)
    c[:, 0:128] = np.eye(128, dtype=np.float32)
    j = np.arange(128)
    c[:, 128:256] = (j[:, None] <= j[None, :]).astype(np.float32)
    c[:, 256:384] = 1.0
    same = (j[:, None] // 64) == (j[None, :] // 64)
    c[:, 384:512] = ((j[:, None] <= j[None, :]) & same)
    c[:, 512:640] = ((j[:, None] < j[None, :]) & same)
    invf = np.power(10000.0, -np.arange(0, 64, 2, dtype=np.float32) / 64).astype(np.float32)
    c[:, 640:672] = invf[None, :]
    c[:, 672:674] = (j[:, None] // 64 == np.arange(2)[None, :])
    c[:, 700] = 1e-6
    c[:, 701] = 64e-5
    c[:, 702] = 1.0
    c[:, 703] = -0.5
    c[:, 704] = 0.0
    c[:, 705] = -np.pi
    c[:, 706] = 1e-24
    c[:, 707] = 1.0 / 128
    i64 = np.arange(64)
    c[:64, 768:832] = (i64[:, None] < i64[None, :])
    c[:64, 832:896] = (i64[:, None] <= i64[None, :])
    c[:64, 896:960] = (i64[:, None] > i64[None, :])
    for jj in range(4):
        c[:, 960 + jj * 8:968 + jj * 8] = ((2 * jj + j[:, None] // 64) == np.arange(8)[None, :])
    c[:, 1024:1152] = same
    return c


C_ID, C_TRI, C_ONE, C_BI, C_BS, C_IF, C_CI = 0, 128, 256, 384, 512, 640, 672
C_M1, C_M3, C_HS, C_BD = 768, 896, 960, 1024
C_EPS, C_GNEPS, C_1, C_MH, C_0, C_MPI, C_TINY, C_R128 = 700, 701, 702, 703, 704, 705, 706, 707


def build(T, depth, dbg=()):
    assert T % 128 == 0
    NT = T // 128
    SG = min(T, 1024)
    NSG = T // SG
    G = min(T, 512)
    NG = T // G
    nc = bass.Bass("TRN2", target_bir_lowering=False)

    def din(name, shape, dt=F32):
        return nc.dram_tensor(name, list(shape), dt, kind="ExternalInput").ap()

    def dscr(name, shape, dt=F32):
        kind = "ExternalOutput" if name in dbg else "Internal"
        return nc.dram_tensor(name, list(shape), dt, kind=kind).ap()

    x_in = din("x", [T, D])
    pos_in = din("positions", [T], I32)
    consts_in = din("consts", [128, 1280])
    norm_g = din("norm_g", [depth, D])
    w_in = din("w_in", [depth, D, DIN])
    w_out = din("w_out", [depth, D, D])
    final_g = din("final_norm_g", [D])
    ml_conv_w = din("ml_conv_w", [depth, 4, 1024]); ml_conv_b = din("ml_conv_b", [depth, 1024])
    mla_q_norm_g = din("mla_q_norm_g", [depth, 512]); mla_w_uq = din("mla_w_uq", [depth, 512, 1536])
    mla_kv_norm_g = din("mla_kv_norm_g", [depth, 256]); mla_w_ukv = din("mla_w_ukv", [depth, 256, 2048])
    rw_mu = din("rw_mu", [depth, 1664]); rw_w0 = din("rw_w0", [depth, 512]); rw_w2 = din("rw_w2", [depth, 64, 512])
    rw_a0 = din("rw_a0", [depth, 512]); rw_a2 = din("rw_a2", [depth, 64, 512])
    rw_v0 = din("rw_v0", [3, 512]); rw_v1 = din("rw_v1", [3, 512, 32]); rw_v2 = din("rw_v2", [3, 32, 512])
    rw_k_k = din("rw_k_k", [depth, 512]); rw_k_a = din("rw_k_a", [depth, 512]); rw_r_k = din("rw_r_k", [depth, 8, 64])
    rw_ln_g = din("rw_ln_g", [depth, 512]); rw_ln_b = din("rw_ln_b", [depth, 512])
    ml_i_bias = din("ml_i_bias", [depth, 4]); ml_f_bias = din("ml_f_bias", [depth, 4]); ml_norm_g = din("ml_norm_g", [depth, 512])
    out = nc.dram_tensor("out", [T, D], F32, kind="ExternalOutput").ap()

    xT = dscr("xT", [D, T])
    pFM = dscr("pFM", [NFM * 128, T])
    pTM = dscr("pTM", [T, NTM])
    mixT = dscr("mixT", [D, T], BF16)
    csd = dscr("cs", [T, 64])
    qnT = dscr("qnT", [1024, T], BF16); knT = dscr("knT", [1024, T], BF16); qrT = dscr("qrT", [512, T], BF16); krT = dscr("krT", [64, T], BF16)
    vaug = dscr("vaug", [8, T, 129], BF16)
    rwA = dscr("rwA", [512, T]); rwR = dscr("rwR", [512, T]); rwB = dscr("rwB", [512, T]); rwK = dscr("rwK", [512, T])
    rwBp = dscr("rwBp", [T, 512]); rwKp = dscr("rwKp", [T, 512]); rwV = dscr("rwV", [T, 512]); rwY = dscr("rwY", [T, 512])
    rwGL = dscr("rwGL", [512, T // 64]); rwRKR = dscr("rwRKR", [T, 8]); vfirst = dscr("vfirst", [512, T])

    fm_blocks = split_blocks(FM_RANGES)
    tm_blocks = split_blocks(TM_RANGES)

    with ExitStack() as top:
        k = KB(nc)
        cst = top.enter_context(sbt(nc, "cst", [128, 1280], F32))
        cstb = Buf()
        k.dma("sp", cst[:], consts_in, W=[cstb])
        ident = cst[:, C_ID:C_ID + 128]
        ones = cst[:, C_ONE:C_ONE + 128]
        onec = cst[:, C_ONE:C_ONE + 1]

        def rstd(o, i, scale, epscol, R, W, np_=128):
            k.op("act", lambda: nc.scalar.activation(out=o, in_=i, func=AF.Sqrt, bias=cst[:np_, epscol:epscol + 1], scale=scale), R=list(R) + [cstb], W=W)
            k.op("dve", lambda: nc.vector.reciprocal(out=o, in_=o), R=W, W=W)

        with ExitStack() as es:
            xin = Ring(es, nc, "i_x", [128, D], F32, 2)
            ps = Ring(es, nc, "i_ps", [128, 512], F32, 4, psum=True)
            stg = Ring(es, nc, "i_st", [128, 16, 128], F32, 2)
            for t in range(NT):
                xt, xb = xin.next()
                k.dma("sp", xt[:], x_in[t * 128:(t + 1) * 128, :], W=[xb])
                st, sb = stg.next()
                for q in range(4):
                    p, pb = ps.next()
                    for j in range(4):
                        kc = q * 4 + j
                        k.op("pe", lambda: nc.tensor.transpose(p[:, j * 128:(j + 1) * 128], xt[:, kc * 128:(kc + 1) * 128], ident),
                             R=[xb, cstb], W=[pb])
                    eng = "act" if q % 2 else "dve"
                    if eng == "act":
                        k.op("act", lambda: nc.scalar.copy(out=st[:, q * 4:(q + 1) * 4, :], in_=p[:].rearrange("p (a b) -> p a b", a=4)), R=[pb], W=[sb])
                    else:
                        k.op("dve", lambda: nc.vector.tensor_copy(out=st[:, q * 4:(q + 1) * 4, :], in_=p[:].rearrange("p (a b) -> p a b", a=4)), R=[pb], W=[sb])
                k.dma("pool", xT.rearrange("(kc p) t -> p kc t", p=128)[:, :, t * 128:(t + 1) * 128], st[:], R=[sb])
            posi = es.enter_context(sbt(nc, "i_pi", [128, NT], I32))
            posf = es.enter_context(sbt(nc, "i_pf", [128, NT], F32))
            pob = Buf()
            k.dma("sp", posi[:], pos_in.rearrange("(n p) -> p n", p=128), W=[pob], slow=True)
            k.op("dve", lambda: nc.vector.tensor_copy(out=posf[:], in_=posi[:]), R=[pob], W=[pob])
            angr = Ring(es, nc, "i_ang", [128, 64], F32, 2)
            tir = Ring(es, nc, "i_ti", [128, 64], I32, 2)
            tfr = Ring(es, nc, "i_tf", [128, 64], F32, 2)
            TWO_PI = float(2 * np.pi)
            C1 = 6.28125
            C2 = float(2 * np.pi - 6.28125)
            for t in range(NT):
                an, anb = angr.next()
                ti, tib = tir.next()
                tf, tfb = tfr.next()
                k.op("dve", lambda: nc.vector.tensor_scalar(out=an[:, 0:32], in0=cst[:, C_IF:C_IF + 32], scalar1=posf[:, t:t + 1], scalar2=None, op0=ALU.mult), R=[pob, cstb], W=[anb])
                k.op("dve", lambda: nc.vector.tensor_scalar(out=an[:, 32:64], in0=an[:, 0:32], scalar1=float(np.pi / 2), scalar2=None, op0=ALU.add), R=[anb], W=[anb])
                k.op("dve", lambda: nc.vector.tensor_scalar(out=tf[:], in0=an[:], scalar1=float(1 / (2 * np.pi)), scalar2=None, op0=ALU.mult), R=[anb], W=[tfb])
                k.op("dve", lambda: nc.vector.tensor_copy(out=ti[:], in_=tf[:]), R=[tfb], W=[tib])
                k.op("dve", lambda: nc.vector.tensor_copy(out=tf[:], in_=ti[:]), R=[tib], W=[tfb])
                k.op("dve", lambda: nc.vector.scalar_tensor_tensor(out=an[:], in0=tf[:], scalar=-C1, in1=an[:], op0=ALU.mult, op1=ALU.add), R=[tfb, anb], W=[anb])
                k.op("dve", lambda: nc.vector.scalar_tensor_tensor(out=an[:], in0=tf[:], scalar=-C2, in1=an[:], op0=ALU.mult, op1=ALU.add), R=[tfb, anb], W=[anb])
                k.op("dve", lambda: nc.vector.tensor_scalar(out=tf[:], in0=an[:], scalar1=float(np.pi), scalar2=-TWO_PI, op0=ALU.is_gt, op1=ALU.mult), R=[anb], W=[tfb])
                k.op("dve", lambda: nc.vector.tensor_tensor(out=an[:], in0=an[:], in1=tf[:], op=ALU.add), R=[tfb, anb], W=[anb])
                k.op("dve", lambda: nc.vector.tensor_scalar(out=tf[:], in0=an[:], scalar1=float(-np.pi), scalar2=TWO_PI, op0=ALU.is_lt, op1=ALU.mult), R=[anb], W=[tfb])
                k.op("dve", lambda: nc.vector.tensor_tensor(out=an[:], in0=an[:], in1=tf[:], op=ALU.add), R=[tfb, anb], W=[anb])
                k.op("act", lambda: nc.scalar.activation(out=an[:], in_=an[:], func=AF.Sin), R=[anb], W=[anb])
                k.dma("pool", csd[t * 128:(t + 1) * 128, :], an[:], R=[anb])
            k.barrier()

        for l in range(depth):
            with ExitStack() as es:
                gcol = es.enter_context(sbt(nc, "p1_g", [128, 16], F32))
                gb = Buf()
                k.dma("sp", gcol[:], norm_g[l].rearrange("(kc p) -> p kc", p=128), W=[gb], slow=True)
                hT = es.enter_context(sbt(nc, "p1_hT", [128, 16, SG], BF16))
                hb = Buf()
                xs = Ring(es, nc, "p1_xs", [128, SG], F32, 2)
                sqr = Ring(es, nc, "p1_sq", [128, SG], F32, 2)
                wr = Ring(es, nc, "p1_w", [128, 16, 512], F32, 2)
                wcr = Ring(es, nc, "p1_wc", [128, 16, 512], BF16, 2)
                wcn = [0]
                stg = Ring(es, nc, "p1_st", [128, 512], F32, 4)
                rbc = es.enter_context(sbt(nc, "p1_rbc", [128, SG], F32))
                rbcb = Buf()
                rtm = es.enter_context(sbt(nc, "p1_rtm", [128, SG // 128], F32))
                rtmb = Buf()
                ps = Ring(es, nc, "p1_ps", [128, 512], F32, 4, psum=True)
                pbc = Ring(es, nc, "p1_pbc", [128, 512], F32, SG // G, psum=True)
                ptm = es.enter_context(pst(nc, "p1_ptm", [128, 512], F32))
                ptmb = Buf()
                def p1_loadw(cc0, nb):
                    wf, wfb = wr.next()
                    k.dma("sp", wf[:, :, :nb], w_in[l].rearrange("(kc p) n -> p kc n", p=128)[:, :, cc0:cc0 + nb], W=[wfb])
                    wc, wcb = wcr.next()
                    wcn[0] += 1
                    for hf in range(2):
                        if (wcn[0] + hf) % 2:
                            k.op("act", lambda: nc.scalar.copy(out=wc[:, hf * 8:(hf + 1) * 8, :nb], in_=wf[:, hf * 8:(hf + 1) * 8, :nb]), R=[wfb], W=[wcb])
                        else:
                            k.op("dve", lambda: nc.vector.tensor_copy(out=wc[:, hf * 8:(hf + 1) * 8, :nb], in_=wf[:, hf * 8:(hf + 1) * 8, :nb]), R=[wfb], W=[wcb])
                    return wc, wcb

                for sg in range(NSG):
                    c0 = sg * SG
                    pbcs = [pbc.next() for _ in range(SG // G)]
                    for kc in range(16):
                        xt, xb = xs.next()
                        k.dma("sp", xt[:], xT[kc * 128:(kc + 1) * 128, c0:c0 + SG], W=[xb])
                        sq, sqb = sqr.next()
                        k.op("act", lambda: nc.scalar.activation(out=sq[:], in_=xt[:], func=AF.Square), R=[xb], W=[sqb])
                        k.op("dve", lambda: nc.vector.tensor_scalar(out=hT[:, kc, :], in0=xt[:], scalar1=gcol[:, kc:kc + 1], scalar2=None, op0=ALU.mult),
                             R=[xb, gb], W=[hb])
                        for s in range(SG // G):
                            pp, ppb = pbcs[s]
                            k.op("pe", lambda: nc.tensor.matmul(pp[:, :G], lhsT=ones, rhs=sq[:, s * G:(s + 1) * G], start=(kc == 0), stop=(kc == 15)),
                                 R=[sqb, cstb], W=[ppb])
                    for s in range(SG // G):
                        pp, ppb = pbcs[s]
                        rstd(rbc[:, s * G:(s + 1) * G], pp[:, :G], 1.0 / D, C_EPS, [ppb], [rbcb])
                    for t in range(SG // 128):
                        k.op("pe", lambda: nc.tensor.matmul(ptm[:, t:t + 1], lhsT=rbc[:, t * 128:(t + 1) * 128], rhs=cst[:, C_R128:C_R128 + 1], start=True, stop=True),
                             R=[rbcb, cstb], W=[ptmb])
                    k.op("dve", lambda: nc.vector.tensor_copy(out=rtm[:], in_=ptm[:, :SG // 128]), R=[ptmb], W=[rtmb])
                    fmrow = 0
                    for (cc0, nb) in fm_blocks:
                        wt, wb = p1_loadw(cc0, nb)
                        for j in range(nb // 128):
                            for s in range(SG // G):
                                p, pb = ps.next()
                                for kc in range(16):
                                    k.op("pe", lambda: nc.tensor.matmul(p[:, :G], lhsT=wt[:, kc, j * 128:(j + 1) * 128], rhs=hT[:, kc, s * G:(s + 1) * G],
                                                                        start=(kc == 0), stop=(kc == 15)), R=[wb, hb], W=[pb])
                                st, sb = stg.next()
                                k.op("dve", lambda: nc.vector.tensor_tensor(out=st[:, :G], in0=p[:, :G], in1=rbc[:, s * G:(s + 1) * G], op=ALU.mult),
                                     R=[pb, rbcb], W=[sb])
                                k.dma("pool", pFM[fmrow * 128:(fmrow + 1) * 128, c0 + s * G:c0 + (s + 1) * G], st[:, :G], R=[sb])
                            fmrow += 1
                    tmcol = 0
                    for (cc0, nb) in tm_blocks:
                        wt, wb = p1_loadw(cc0, nb)
                        for t in range(SG // 128):
                            p, pb = ps.next()
                            for kc in range(16):
                                k.op("pe", lambda: nc.tensor.matmul(p[:, :nb], lhsT=hT[:, kc, t * 128:(t + 1) * 128], rhs=wt[:, kc, :nb],
                                                                    start=(kc == 0), stop=(kc == 15)), R=[wb, hb], W=[pb])
                            st, sb = stg.next()
                            k.op("act", lambda: nc.scalar.activation(out=st[:, :nb], in_=p[:, :nb], func=AF.Copy, scale=rtm[:, t:t + 1]),
                                 R=[pb, rtmb], W=[sb])
                            k.dma("pool", pTM[c0 + t * 128:c0 + (t + 1) * 128, tmcol:tmcol + nb], st[:, :nb], R=[sb])
                        tmcol += nb
                k.barrier()


            with ExitStack() as es:
                SB = lambda name, shape: es.enter_context(sbt(nc, name, shape, F32))
                PS = lambda name: es.enter_context(pst(nc, name, [128, 512], F32))
                tri = cst[:, C_TRI:C_TRI + 128]
                cw = SB("m_cw", [128, 4, 8]); cb = SB("m_cb", [128, 8]); ibfb = SB("m_ib", [128, 8]); gbc = SB("m_g", [128, 512])
                pb_ = Buf()
                for j in range(4):
                    k.dma("sp", cw[:, j, :], ml_conv_w[l, j].rearrange("(c p) -> p c", p=128), W=[pb_], slow=True)
                k.dma("sp", cb[:], ml_conv_b[l].rearrange("(c p) -> p c", p=128), W=[pb_], slow=True)
                k.dma("sp", ibfb[:, 0:4], ml_i_bias[l].partition_broadcast(128), W=[pb_])
                k.dma("sp", ibfb[:, 4:8], ml_f_bias[l].partition_broadcast(128), W=[pb_])
                k.dma("sp", gbc[:], ml_norm_g[l].partition_broadcast(128), W=[pb_])
                CT = [SB("m_CT%d" % h, [128, 129]) for h in range(4)]
                CTb = [Buf() for h in range(4)]
                for h in range(4):
                    k.op("pool", lambda: nc.gpsimd.memset(CT[h][:], 0.0), W=[CTb[h]])
                qkr = Ring(es, nc, "m_qkr", [128, 8, 131], F32, 2)
                accr = Ring(es, nc, "m_acc", [128, 8, 128], F32, 2)
                qkt = Ring(es, nc, "m_qk", [128, 8, 128], F32, 2)
                vr = Ring(es, nc, "m_v", [128, 4, 129], F32, 2)
                for (vt, vb) in vr.items:
                    k.op("pool", lambda: nc.gpsimd.memset(vt[:, :, 128:129], 1.0), W=[vb])
                gtr = Ring(es, nc, "m_gt", [128, 8], F32, 2)
                Gr = Ring(es, nc, "m_G", [128, 32], F32, 2)
                ozr = Ring(es, nc, "m_oz", [128, 1024], F32, 2)
                rhr = Ring(es, nc, "m_rh", [128, 128], F32, 2)
                Er = Ring(es, nc, "m_E", [128, 128], F32, 2)
                STr = Ring(es, nc, "m_ST", [128, 128], F32, 2)
                n1r = Ring(es, nc, "m_n1", [128, 129], F32, 2)
                n2r = Ring(es, nc, "m_n2", [128, 129], F32, 2)
                smr = Ring(es, nc, "m_sm", [128, 16], F32, 2)
                hhr = Ring(es, nc, "m_hh", [128, 128], F32, 2)
                kwr = Ring(es, nc, "m_kw", [128, 128], F32, 2)
                yr = Ring(es, nc, "m_y", [128, 512], F32, 2)
                ggr = Ring(es, nc, "m_gg", [128, 1024], F32, 2)
                mtr = Ring(es, nc, "m_mt", [128, 4, 128], BF16, 2)
                pgA = PS("m_pgA"); pgB = PS("m_pgB"); pBbc = PS("m_pB"); pQK = PS("m_pQK")
                pN1 = PS("m_pN1"); pN2 = PS("m_pN2"); pKT = PS("m_pKT"); pdC = PS("m_pdC")
                pgAb, pgBb, pBbcb, pQKb, pN1b, pN2b, pKTb, pdCb = [Buf() for _ in range(8)]
                NSD = nc.vector.BN_STATS_DIM
                for c in range(NT):
                    t0 = c * 128
                    X, Xb = qkr.next()
                    src = pFM.rearrange("(ch p) t -> p ch t", p=128)
                    if c == 0:
                        k.op("pool", lambda: nc.gpsimd.memset(X[:, :, 0:3], 0.0), W=[Xb])
                        k.dma("sp", X[:, :, 3:131], src[:, 0:8, 0:128], W=[Xb])
                    else:
                        k.dma("sp", X[:], src[:, 0:8, t0 - 3:t0 + 128], W=[Xb])
                    vt, vb = vr.next()
                    k.dma("sp", vt[:, :, 0:128], pTM[t0:t0 + 128, TM_V:TM_V + 512].rearrange("p (h d) -> p h d", h=4), W=[vb])
                    gt, gtb = gtr.next()
                    k.dma("sp", gt[:], pTM[t0:t0 + 128, TM_I:TM_I + 8], W=[gtb])
                    oz, ozb = ozr.next()
                    k.dma("sp", oz[:], pTM[t0:t0 + 128, TM_O:TM_O + 1024], W=[ozb])
                    acc, accb = accr.next()
                    qk, qkb = qkt.next()
                    for ch in range(8):
                        eng, E_ = ("dve", nc.vector)
                        k.op(eng, lambda: E_.tensor_scalar(out=acc[:, ch, :], in0=X[:, ch, 0:128], scalar1=cw[:, 0, ch:ch + 1], scalar2=cb[:, ch:ch + 1], op0=ALU.mult, op1=ALU.add),
                             R=[Xb, pb_], W=[accb])
                        for j in range(1, 4):
                            k.op(eng, lambda: E_.scalar_tensor_tensor(out=acc[:, ch, :], in0=X[:, ch, j:j + 128], scalar=cw[:, j, ch:ch + 1], in1=acc[:, ch, :], op0=ALU.mult, op1=ALU.add),
                                 R=[Xb, pb_, accb], W=[accb])
                    k.op("act", lambda: nc.scalar.activation(out=qk[:], in_=acc[:], func=AF.Silu), R=[accb], W=[qkb])
                    k.op("pool", lambda: nc.gpsimd.tensor_scalar(out=qk[:, 0:4, :], in0=qk[:, 0:4, :], scalar1=float(128 ** -0.5), scalar2=None, op0=ALU.mult), R=[qkb], W=[qkb])
                    Gt, Gb = Gr.next()
                    k.op("dve", lambda: nc.vector.tensor_tensor(out=Gt[:, 0:8], in0=gt[:], in1=ibfb[:], op=ALU.add), R=[gtb, pb_], W=[Gb])
                    k.op("act", lambda: nc.scalar.activation(out=Gt[:, 8:12], in_=Gt[:, 4:8], func=AF.Exp, scale=-1.0), R=[Gb], W=[Gb])
                    k.op("act", lambda: nc.scalar.activation(out=Gt[:, 12:16], in_=Gt[:, 8:12], func=AF.Ln, bias=cst[:, C_1:C_1 + 1]), R=[Gb, cstb], W=[Gb])
                    k.op("dve", lambda: nc.vector.tensor_scalar(out=Gt[:, 12:16], in0=Gt[:, 12:16], scalar1=-1.0, scalar2=None, op0=ALU.mult), R=[Gb], W=[Gb])
                    k.op("pe", lambda: nc.tensor.matmul(pgA[:, 0:4], lhsT=tri, rhs=Gt[:, 12:16], start=True, stop=True), R=[Gb, cstb], W=[pgAb])
                    k.op("pe", lambda: nc.tensor.matmul(pgB[:, 0:4], lhsT=ones, rhs=Gt[:, 12:16], start=True, stop=True), R=[Gb, cstb], W=[pgBb])
                    k.op("dve", lambda: nc.vector.tensor_tensor(out=Gt[:, 16:20], in0=Gt[:, 0:4], in1=pgA[:, 0:4], op=ALU.subtract), R=[Gb, pgAb], W=[Gb])
                    k.op("dve", lambda: nc.vector.tensor_tensor(out=Gt[:, 20:24], in0=Gt[:, 16:20], in1=pgB[:, 0:4], op=ALU.add), R=[Gb, pgBb], W=[Gb])
                    k.op("act", lambda: nc.scalar.activation(out=Gt[:, 20:24], in_=Gt[:, 20:24], func=AF.Exp), R=[Gb], W=[Gb])
                    k.op("act", lambda: nc.scalar.activation(out=Gt[:, 24:28], in_=pgB[:, 0:4], func=AF.Exp), R=[pgBb], W=[Gb])
                    k.op("act", lambda: nc.scalar.activation(out=Gt[:, 28:32], in_=pgA[:, 0:4], func=AF.Exp), R=[pgAb], W=[Gb])
                    y, yb = yr.next()
                    for h in range(4):
                        rh, rhb = rhr.next()
                        k.op("pool", lambda: nc.gpsimd.tensor_scalar(out=rh[:], in0=tri, scalar1=Gt[:, 12 + h:13 + h], scalar2=None, op0=ALU.mult), R=[Gb, cstb], W=[rhb])
                        k.op("pe", lambda: nc.tensor.matmul(pBbc[:, 0:128], lhsT=ones, rhs=rh[:], start=True, stop=True), R=[rhb, cstb], W=[pBbcb])
                        Et, Eb = Er.next()
                        k.op("act", lambda: nc.scalar.activation(out=Et[:], in_=pBbc[:, 0:128], func=AF.Exp, bias=Gt[:, 16 + h:17 + h]), R=[pBbcb, Gb], W=[Eb])
                        k.op("pool", lambda: nc.gpsimd.tensor_tensor(out=Et[:], in0=Et[:], in1=tri, op=ALU.mult), R=[Eb, cstb], W=[Eb])
                        k.op("pe", lambda: nc.tensor.matmul(pQK[:, 0:128], lhsT=qk[:, 4 + h, :], rhs=qk[:, h, :], start=True, stop=True), R=[qkb], W=[pQKb])
                        ST, STb = STr.next()
                        k.op("dve", lambda: nc.vector.tensor_tensor(out=ST[:], in0=pQK[:, 0:128], in1=Et[:], op=ALU.mult), R=[pQKb, Eb], W=[STb])
                        k.op("pe", lambda: nc.tensor.matmul(pN1[:, 0:129], lhsT=ST[:], rhs=vt[:, h, :], start=True, stop=True), R=[STb, vb], W=[pN1b])
                        k.op("pe", lambda: nc.tensor.matmul(pN2[:, 0:129], lhsT=qk[:, h, :], rhs=CT[h][:], start=True, stop=True), R=[qkb, CTb[h]], W=[pN2b])
                        n1, n1b = n1r.next()
                        k.op("act", lambda: nc.scalar.activation(out=n1[:], in_=pN2[:, 0:129], func=AF.Copy, scale=Gt[:, 28 + h:29 + h]), R=[pN2b, Gb], W=[n1b])
                        n2, n2b = n2r.next()
                        k.op("dve", lambda: nc.vector.tensor_tensor(out=n2[:], in0=pN1[:, 0:129], in1=n1[:], op=ALU.add), R=[pN1b, n1b], W=[n2b])
                        sm, smb = smr.next()
                        k.op("act", lambda: nc.scalar.activation(out=sm[:, 0:1], in_=n2[:, 128:129], func=AF.Abs), R=[n2b], W=[smb])
                        k.op("dve", lambda: nc.vector.tensor_scalar_max(out=sm[:, 0:1], in0=sm[:, 0:1], scalar1=1.0), R=[smb], W=[smb])
                        k.op("dve", lambda: nc.vector.reciprocal(out=sm[:, 0:1], in_=sm[:, 0:1]), R=[smb], W=[smb])
                        hh, hhb = hhr.next()
                        k.op("dve", lambda: nc.vector.tensor_scalar(out=hh[:], in0=n2[:, 0:128], scalar1=sm[:, 0:1], scalar2=None, op0=ALU.mult), R=[n2b, smb], W=[hhb])
                        k.op("dve", lambda: nc.vector.bn_stats(out=sm[:, 2:2 + NSD], in_=hh[:]), R=[hhb], W=[smb])
                        k.op("dve", lambda: nc.vector.bn_aggr(out=sm[:, 10:12], in_=sm[:, 2:2 + NSD]), R=[smb], W=[smb])
                        rstd(sm[:, 12:13], sm[:, 11:12], 1.0, C_EPS, [smb], [smb])
                        k.op("dve", lambda: nc.vector.tensor_scalar(out=y[:, h * 128:(h + 1) * 128], in0=hh[:], scalar1=sm[:, 10:11], scalar2=sm[:, 12:13], op0=ALU.subtract, op1=ALU.mult),
                             R=[hhb, smb], W=[yb])
                        k.op("pe", lambda: nc.tensor.transpose(pKT[:, 0:128], qk[:, 4 + h, :], ident), R=[qkb, cstb], W=[pKTb])
                        kw, kwb = kwr.next()
                        k.op("act", lambda: nc.scalar.activation(out=kw[:], in_=pKT[:, 0:128], func=AF.Copy, scale=Gt[:, 20 + h:21 + h]), R=[pKTb, Gb], W=[kwb])
                        k.op("pe", lambda: nc.tensor.matmul(pdC[:, 0:129], lhsT=kw[:], rhs=vt[:, h, :], start=True, stop=True), R=[kwb, vb], W=[pdCb])
                        k.op("dve", lambda: nc.vector.scalar_tensor_tensor(out=CT[h][:], in0=CT[h][:], scalar=Gt[:, 24 + h:25 + h], in1=pdC[:, 0:129], op0=ALU.mult, op1=ALU.add),
                             R=[CTb[h], Gb, pdCb], W=[CTb[h]])
                    gg, ggb = ggr.next()
                    k.op("act", lambda: nc.scalar.activation(out=gg[:, 0:512], in_=oz[:, 0:512], func=AF.Sigmoid), R=[ozb], W=[ggb])
                    k.op("act", lambda: nc.scalar.activation(out=gg[:, 512:1024], in_=oz[:, 512:1024], func=AF.Silu), R=[ozb], W=[ggb])
                    k.op("pool", lambda: nc.gpsimd.tensor_tensor(out=gg[:, 0:512], in0=gg[:, 0:512], in1=gg[:, 512:1024], op=ALU.mult), R=[ggb], W=[ggb])
                    k.op("pool", lambda: nc.gpsimd.tensor_tensor(out=gg[:, 0:512], in0=gg[:, 0:512], in1=gbc[:], op=ALU.mult), R=[ggb, pb_], W=[ggb])
                    k.op("dve", lambda: nc.vector.tensor_tensor(out=y[:], in0=y[:], in1=gg[:, 0:512], op=ALU.mult), R=[yb, ggb], W=[yb])
                    mt, mtb = mtr.next()
                    for j in range(4):
                        k.op("pe", lambda: nc.tensor.transpose(pKT[:, 128 + j * 64:128 + j * 64 + 64] if False else pKT[:, 0:128], y[:, j * 128:(j + 1) * 128], ident), R=[yb, cstb], W=[pKTb])
                        k.op("act", lambda: nc.scalar.copy(out=mt[:, j, :], in_=pKT[:, 0:128]), R=[pKTb], W=[mtb])
                    k.dma("pool", mixT.rearrange("(cc p) t -> p cc t", p=128)[:, 0:4, t0:t0 + 128], mt[:], R=[mtb])
                k.barrier()

            with ExitStack() as es:
                SB = lambda name, shape: es.enter_context(sbt(nc, name, shape, F32))
                PS = lambda name: es.enter_context(pst(nc, name, [128, 512], F32))
                tri = cst[:, C_TRI:C_TRI + 128]
                wuq = SB("a_wuq", [128, 4, 1536]); wukv = SB("a_wukv", [128, 2, 2048]); gq = SB("a_gq", [128, 6])
                wb_ = Buf()
                k.dma("sp", wuq[:], mla_w_uq[l].rearrange("(c p) n -> p c n", p=128), W=[wb_])
                k.dma("sp", wukv[:], mla_w_ukv[l].rearrange("(c p) n -> p c n", p=128), W=[wb_])
                k.dma("sp", gq[:, 0:4], mla_q_norm_g[l].rearrange("(c p) -> p c", p=128), W=[wb_], slow=True)
                k.dma("sp", gq[:, 4:6], mla_kv_norm_g[l].rearrange("(c p) -> p c", p=128), W=[wb_], slow=True)
                xcr = Ring(es, nc, "a_xc", [128, 6, 128], F32, 2)
                sqr = Ring(es, nc, "a_sq", [128, 6, 128], F32, 2)
                rbr = Ring(es, nc, "a_rb", [128, 2, 128], F32, 2)
                cnr = Ring(es, nc, "a_cn", [128, 6, 128], F32, 2)
                csr = Ring(es, nc, "a_cs", [128, 64], F32, 2)
                kxr = Ring(es, nc, "a_kx", [128, 64], F32, 2)
                stq = Ring(es, nc, "a_stq", [128, 4, 128], BF16, 4)
                tmr = Ring(es, nc, "a_tm", [128, 8, 32], F32, 4)
                qrr = Ring(es, nc, "a_qr", [128, 8, 64], F32, 2)
                krr = Ring(es, nc, "a_kr", [128, 64], F32, 2)
                kst = Ring(es, nc, "a_kst", [64, 128], BF16, 2)
                var_ = Ring(es, nc, "a_va", [128, 8, 129], BF16, 2)
                for (vt, vb) in var_.items:
                    k.op("pool", lambda: nc.gpsimd.memset(vt[:, :, 128:129], 1.0), W=[vb])
                pS1 = PS("a_pS1"); pS2 = PS("a_pS2"); pS1b = Buf(); pS2b = Buf()
                pq = Ring(es, nc, "a_pq", [128, 512], F32, 4, psum=True)
                qn_v = qnT.rearrange("(h p) t -> p h t", p=128)
                kn_v = knT.rearrange("(h p) t -> p h t", p=128)
                qr_v = qrT.rearrange("(b p) t -> p b t", p=128)
                for c in range(NT):
                    t0 = c * 128
                    xc, xcb = xcr.next()
                    k.dma("sp", xc[:], pFM.rearrange("(ch p) t -> p ch t", p=128)[:, FM_CQ:FM_CQ + 6, t0:t0 + 128], W=[xcb])
                    cs_, csb = csr.next()
                    k.dma("sp", cs_[:], csd[t0:t0 + 128, :], W=[csb])
                    kx, kxb = kxr.next()
                    k.dma("sp", kx[:], pTM[t0:t0 + 128, TM_KR:TM_KR + 64], W=[kxb])
                    sq, sqb = sqr.next()
                    k.op("act", lambda: nc.scalar.activation(out=sq[:], in_=xc[:], func=AF.Square), R=[xcb], W=[sqb])
                    for ch in range(4):
                        k.op("pe", lambda: nc.tensor.matmul(pS1[:, 0:128], lhsT=ones, rhs=sq[:, ch, :], start=(ch == 0), stop=(ch == 3)), R=[sqb, cstb], W=[pS1b])
                    for ch in range(2):
                        k.op("pe", lambda: nc.tensor.matmul(pS2[:, 0:128], lhsT=ones, rhs=sq[:, 4 + ch, :], start=(ch == 0), stop=(ch == 1)), R=[sqb, cstb], W=[pS2b])
                    rb, rbb = rbr.next()
                    rstd(rb[:, 0, :], pS1[:, 0:128], 1.0 / 512, C_EPS, [pS1b], [rbb])
                    rstd(rb[:, 1, :], pS2[:, 0:128], 1.0 / 256, C_EPS, [pS2b], [rbb])
                    cn, cnb = cnr.next()
                    for ch in range(6):
                        k.op("dve", lambda: nc.vector.scalar_tensor_tensor(out=cn[:, ch, :], in0=xc[:, ch, :], scalar=gq[:, ch:ch + 1], in1=rb[:, 0 if ch < 4 else 1, :], op0=ALU.mult, op1=ALU.mult),
                             R=[xcb, wb_, rbb], W=[cnb])
                    for (W_, nch, c0_, hs, dst) in ((wuq, 4, 0, 192, qn_v), (wukv, 2, 4, 256, kn_v)):
                        for b in range(2):
                            p, pb = pq.next()
                            for hh_ in range(4):
                                h = b * 4 + hh_
                                for ch in range(nch):
                                    k.op("pe", lambda: nc.tensor.matmul(p[:, hh_ * 128:(hh_ + 1) * 128], lhsT=W_[:, ch, h * hs:h * hs + 128], rhs=cn[:, c0_ + ch, :],
                                                                        start=(ch == 0), stop=(ch == nch - 1)), R=[wb_, cnb], W=[pb])
                            st, sb = stq.next()
                            k.op("act", lambda: nc.scalar.copy(out=st[:], in_=p[:].rearrange("p (a b) -> p a b", a=4)), R=[pb], W=[sb])
                            k.dma("pool", dst[:, b * 4:(b + 1) * 4, t0:t0 + 128], st[:], R=[sb])
                    va, vab = var_.next()
                    for b in range(2):
                        p, pb = pq.next()
                        for ch in range(2):
                            k.op("pe", lambda: nc.tensor.matmul(p[:], lhsT=cn[:, 4 + ch, :], rhs=wukv[:, ch, :].rearrange("p (h d) -> p h d", d=256)[:, b * 4:(b + 1) * 4, 128:256],
                                                                start=(ch == 0), stop=(ch == 1)), R=[wb_, cnb], W=[pb])
                        k.op("act", lambda: nc.scalar.copy(out=va[:, b * 4:(b + 1) * 4, 0:128], in_=p[:].rearrange("p (a b) -> p a b", a=4)), R=[pb], W=[vab])
                    k.dma("pool", vaug.rearrange("h t d -> t h d")[t0:t0 + 128], va[:], R=[vab])
                    p, pb = pq.next()
                    for ch in range(4):
                        k.op("pe", lambda: nc.tensor.matmul(p[:], lhsT=cn[:, ch, :], rhs=wuq[:, ch, :].rearrange("p (h d) -> p h d", d=192)[:, :, 128:192],
                                                            start=(ch == 0), stop=(ch == 3)), R=[wb_, cnb], W=[pb])
                    pv = p[:].rearrange("p (h d) -> p h d", d=64)
                    sin8 = cs_[:, 0:32].unsqueeze(1).to_broadcast([128, 8, 32])
                    cos8 = cs_[:, 32:64].unsqueeze(1).to_broadcast([128, 8, 32])
                    qr, qrb = qrr.next()
                    t1, t1b = tmr.next(); t2, t2b = tmr.next(); t3, t3b = tmr.next(); t4, t4b = tmr.next()
                    k.op("dve", lambda: nc.vector.tensor_tensor(out=t1[:], in0=pv[:, :, 0:32], in1=cos8, op=ALU.mult), R=[pb, csb], W=[t1b])
                    k.op("dve", lambda: nc.vector.tensor_tensor(out=t2[:], in0=pv[:, :, 32:64], in1=sin8, op=ALU.mult), R=[pb, csb], W=[t2b])
                    k.op("dve", lambda: nc.vector.tensor_tensor(out=t3[:], in0=pv[:, :, 32:64], in1=cos8, op=ALU.mult), R=[pb, csb], W=[t3b])
                    k.op("dve", lambda: nc.vector.tensor_tensor(out=t4[:], in0=pv[:, :, 0:32], in1=sin8, op=ALU.mult), R=[pb, csb], W=[t4b])
                    k.op("pool", lambda: nc.gpsimd.tensor_tensor(out=qr[:, :, 0:32], in0=t1[:], in1=t2[:], op=ALU.subtract), R=[t1b, t2b], W=[qrb])
                    k.op("pool", lambda: nc.gpsimd.tensor_tensor(out=qr[:, :, 32:64], in0=t3[:], in1=t4[:], op=ALU.add), R=[t3b, t4b], W=[qrb])
                    p, pb = pq.next()
                    for b in range(4):
                        k.op("pe", lambda: nc.tensor.transpose(p[:, b * 128:(b + 1) * 128], qr[:, 2 * b:2 * b + 2, :].rearrange("p a d -> p (a d)"), ident), R=[qrb, cstb], W=[pb])
                    st, sb = stq.next()
                    k.op("act", lambda: nc.scalar.copy(out=st[:], in_=p[:].rearrange("p (a b) -> p a b", a=4)), R=[pb], W=[sb])
                    k.dma("pool", qr_v[:, :, t0:t0 + 128], st[:], R=[sb])
                    kr_, krb = krr.next()
                    t1, t1b = tmr.next(); t2, t2b = tmr.next()
                    k.op("dve", lambda: nc.vector.tensor_tensor(out=t1[:, 0, :], in0=kx[:, 0:32], in1=cs_[:, 32:64], op=ALU.mult), R=[kxb, csb], W=[t1b])
                    k.op("dve", lambda: nc.vector.tensor_tensor(out=t1[:, 1, :], in0=kx[:, 32:64], in1=cs_[:, 0:32], op=ALU.mult), R=[kxb, csb], W=[t1b])
                    k.op("dve", lambda: nc.vector.tensor_tensor(out=t2[:, 0, :], in0=kx[:, 32:64], in1=cs_[:, 32:64], op=ALU.mult), R=[kxb, csb], W=[t2b])
                    k.op("dve", lambda: nc.vector.tensor_tensor(out=t2[:, 1, :], in0=kx[:, 0:32], in1=cs_[:, 0:32], op=ALU.mult), R=[kxb, csb], W=[t2b])
                    k.op("pool", lambda: nc.gpsimd.tensor_tensor(out=kr_[:, 0:32], in0=t1[:, 0, :], in1=t1[:, 1, :], op=ALU.subtract), R=[t1b], W=[krb])
                    k.op("pool", lambda: nc.gpsimd.tensor_tensor(out=kr_[:, 32:64], in0=t2[:, 0, :], in1=t2[:, 1, :], op=ALU.add), R=[t2b], W=[krb])
                    p, pb = pq.next()
                    k.op("pe", lambda: nc.tensor.transpose(p[0:64, 0:128], kr_[:], ident), R=[krb, cstb], W=[pb])
                    ks, ksb = kst.next()
                    k.op("act", lambda: nc.scalar.copy(out=ks[:], in_=p[0:64, 0:128]), R=[pb], W=[ksb])
                    k.dma("pool", krT[:, t0:t0 + 128], ks[:], R=[ksb])
                k.barrier()

            with ExitStack() as es:
                SB = lambda name, shape: es.enter_context(sbt(nc, name, shape, F32))
                tri = cst[:, C_TRI:C_TRI + 128]
                SBh = lambda name, shape: es.enter_context(sbt(nc, name, shape, BF16))
                krt = SBh("b_krt", [64, T]); krtb = Buf()
                k.dma("sp", krt[:], krT, W=[krtb])
                knR = Ring(es, nc, "b_kn", [128, T], BF16, 2); qnR = Ring(es, nc, "b_qn", [128, T], BF16, 2); qrR = Ring(es, nc, "b_qr", [64, T], BF16, 2)
                vaR = Ring(es, nc, "b_va", [128, NT, 129], BF16, 2); zR = Ring(es, nc, "b_z", [128, NT, 128], F32, 2)
                trib = SBh("b_tri", [128, 128]); tribb = Buf()
                k.op("dve", lambda: nc.vector.tensor_copy(out=trib[:], in_=tri), R=[cstb], W=[tribb])
                HD = {}
                Ptr = Ring(es, nc, "b_P", [128, 512], BF16, 3)
                smr = Ring(es, nc, "b_sm", [128, 2], F32, 2)
                ytr = Ring(es, nc, "b_y", [128, 128], F32, 2)
                szr = Ring(es, nc, "b_sz", [128, 128], F32, 2)
                sty = Ring(es, nc, "b_sty", [128, 128], BF16, 3)
                pST = Ring(es, nc, "b_pST", [128, 512], F32, 3, psum=True)
                pO = Ring(es, nc, "b_pO", [128, 512], F32, 2, psum=True)
                pX = Ring(es, nc, "b_pX", [128, 512], F32, 2, psum=True)
                SCL = float(192 ** -0.5)
                hcur = [0]

                def epilogue(po, pob_, qt, qs):
                    h = hcur[0]
                    sm, smb = smr.next()
                    k.op("dve", lambda: nc.vector.reciprocal(out=sm[:, 0:1], in_=po[:, 128:129]), R=[pob_], W=[smb])
                    yt, ytb = ytr.next()
                    k.op("dve", lambda: nc.vector.tensor_scalar(out=yt[:], in0=po[:, 0:128], scalar1=sm[:, 0:1], scalar2=None, op0=ALU.mult), R=[pob_, smb], W=[ytb])
                    sz, szb = szr.next()
                    k.op("act", lambda: nc.scalar.activation(out=sz[:], in_=HD["zh"][:, qt, :], func=AF.Silu), R=[HD["zhb"]], W=[szb])
                    k.op("pool", lambda: nc.gpsimd.tensor_tensor(out=yt[:], in0=yt[:], in1=sz[:], op=ALU.mult), R=[ytb, szb], W=[ytb])
                    px, pxb = pX.next()
                    k.op("pe", lambda: nc.tensor.transpose(px[:, 0:128], yt[:], ident), R=[ytb, cstb], W=[pxb])
                    st, sb = sty.next()
                    k.op("dve", lambda: nc.vector.tensor_copy(out=st[:], in_=px[:, 0:128]), R=[pxb], W=[sb])
                    k.dma("pool", mixT[512 + h * 128:512 + (h + 1) * 128, qs], st[:], R=[sb])

                for h in range(8):
                    hcur[0] = h
                    (knh, knb), (qnh, qnb), (qrh, qrb), (vah, vahb), (zh, zhb) = knR.next(), qnR.next(), qrR.next(), vaR.next(), zR.next()
                    HD["zh"], HD["zhb"] = zh, zhb
                    k.dma("sp", knh[:], knT[h * 128:(h + 1) * 128, :], W=[knb])
                    k.dma("sp", qnh[:], qnT[h * 128:(h + 1) * 128, :], W=[qnb])
                    k.dma("sp", qrh[:], qrT[h * 64:(h + 1) * 64, :], W=[qrb])
                    k.dma("sp", vah[:], vaug[h].rearrange("(n p) d -> p n d", p=128), W=[vahb])
                    k.dma("sp", zh[:], pTM[:, TM_MZ + h * 128:TM_MZ + (h + 1) * 128].rearrange("(n p) d -> p n d", p=128), W=[zhb])
                    pend = [None]

                    def flush_pv():
                        if pend[0] is None:
                            return
                        (po_, pob2, Pt_, Ptb2, kb_, nk_, qt_, fin) = pend[0]
                        pend[0] = None
                        for i in range(nk_):
                            j = kb_ + i
                            k.op("pe", lambda: nc.tensor.matmul(po_[:, 0:129], lhsT=Pt_[:, i * 128:(i + 1) * 128], rhs=vah[:, j, :], start=(j == 0), stop=(j == qt_)), R=[Ptb2, vahb], W=[pob2])
                        if fin is not None:
                            fin()

                    for qt in range(NT):
                        qs = slice(qt * 128, (qt + 1) * 128)
                        po, pob_ = pO.next()
                        for kb in range(0, qt + 1, 4):
                            nk = min(4, qt + 1 - kb)
                            ps_, psb = pST.next()
                            for i in range(nk):
                                j = kb + i
                                js = slice(j * 128, (j + 1) * 128)
                                k.op("pe", lambda: nc.tensor.matmul(ps_[:, i * 128:(i + 1) * 128], lhsT=knh[:, js], rhs=qnh[:, qs], start=True, stop=False), R=[knb, qnb], W=[psb])
                                k.op("pe", lambda: nc.tensor.matmul(ps_[:, i * 128:(i + 1) * 128], lhsT=krt[:, js], rhs=qrh[:, qs], start=False, stop=True), R=[krtb, qrb], W=[psb])
                            flush_pv()
                            Pt, Ptb = Ptr.next()
                            k.op("act", lambda: nc.scalar.activation(out=Pt[:, 0:nk * 128], in_=ps_[:, 0:nk * 128], func=AF.Exp, scale=SCL), R=[psb], W=[Ptb])
                            last = (kb + nk - 1 == qt)
                            if last:
                                i = nk - 1
                                k.op("pool", lambda: nc.gpsimd.tensor_tensor(out=Pt[:, i * 128:(i + 1) * 128], in0=Pt[:, i * 128:(i + 1) * 128], in1=trib[:], op=ALU.mult), R=[Ptb, tribb], W=[Ptb])
                            pend[0] = (po, pob_, Pt, Ptb, kb, nk, qt, (lambda po=po, pob_=pob_, qt=qt, qs=qs: epilogue(po, pob_, qt, qs)) if last else None)
                    flush_pv()

                k.barrier()

            NCH = T // 64
            with ExitStack() as es:
                SB = lambda name, shape: es.enter_context(sbt(nc, name, shape, F32))
                mu = SB("r_mu", [128, 13]); prm = SB("r_prm", [128, 6, 4]); w0bc = SB("r_w0", [128, 512])
                w2t = SB("r_w2", [64, 512]); a2t = SB("r_a2", [128, 512])
                v1t = SB("r_v1", [128, 4, 32]); v2t = SB("r_v2", [32, 512])
                prb = Buf()
                k.dma("sp", mu[:], rw_mu[l].rearrange("(c p) -> p c", p=128), W=[prb], slow=True)
                plist = [rw_a0[l], rw_k_k[l], rw_k_a[l], rw_r_k[l].rearrange("h d -> (h d)")]
                if l > 0:
                    plist.append(rw_v0[l - 1])
                for i_, src in enumerate(plist):
                    k.dma("sp", prm[:, i_, :], src.rearrange("(c p) -> p c", p=128), W=[prb], slow=True)
                k.dma("sp", w0bc[:], rw_w0[l].partition_broadcast(128), W=[prb])
                k.dma("sp", w2t[:], rw_w2[l], W=[prb])
                k.dma("sp", a2t[64:128, :], rw_a2[l], W=[prb])
                if l > 0:
                    k.dma("sp", v1t[:], rw_v1[l - 1].rearrange("(c p) n -> p c n", p=128), W=[prb])
                    k.dma("sp", v2t[:], rw_v2[l - 1], W=[prb])
                k.op("dve", lambda: nc.vector.tensor_scalar(out=prm[:, 5, :], in0=prm[:, 2, :], scalar1=-1.0, scalar2=1.0, op0=ALU.mult, op1=ALU.add), R=[prb], W=[prb])
                bc = lambda ap_: ap_.unsqueeze(2).to_broadcast([128, 4, 128])
                Xr = Ring(es, nc, "r_X", [128, 13, 129], F32, 2)
                dr = Ring(es, nc, "r_d", [128, 13, 128], F32, 1)
                xsr = Ring(es, nc, "r_xs", [128, 13, 128], F32, 2)
                twr = Ring(es, nc, "r_tw", [64, 128], F32, 2)
                ldr = Ring(es, nc, "r_ld", [128, 512], F32, 2)
                T4 = lambda name, n=1: Ring(es, nc, name, [128, 4, 128], F32, n)
                gir, ger, aar, kkr_, khr, bvr = T4("r_gi"), T4("r_ge"), T4("r_aa"), T4("r_kk"), T4("r_kh"), T4("r_bv")
                e1r, e2r, e3r, e4r = T4("r_e1"), T4("r_e2"), T4("r_e3"), T4("r_e4")
                tmpr = T4("r_tmp", 3)
                outr = T4("r_out", 4)
                vfr = T4("r_vf", 2)
                m1r = Ring(es, nc, "r_m1", [32, 128], F32, 2)
                tmo = Ring(es, nc, "r_tmo", [128, 512], F32, 3)
                rkro = Ring(es, nc, "r_rkr", [128, 8], F32, 2)
                glo = Ring(es, nc, "r_glo", [128, 4, 2], F32, 2)
                pr = Ring(es, nc, "r_ps", [128, 512], F32, 8, psum=True)
                fmv = lambda dt_: dt_.rearrange("(j p) t -> p j t", p=128)
                for c in range(NT):
                    t0 = c * 128
                    X, Xb = Xr.next()
                    src = pFM.rearrange("(ch p) t -> p ch t", p=128)
                    if c == 0:
                        k.op("pool", lambda: nc.gpsimd.memset(X[:, :, 0:1], 0.0), W=[Xb])
                        k.dma("sp", X[:, :, 1:129], src[:, FM_RW:FM_RW + 13, 0:128], W=[Xb])
                    else:
                        k.dma("sp", X[:], src[:, FM_RW:FM_RW + 13, t0 - 1:t0 + 128], W=[Xb])
                    d_, db = dr.next()
                    k.op("dve", lambda: nc.vector.tensor_tensor(out=d_[:], in0=X[:, :, 0:128], in1=X[:, :, 1:129], op=ALU.subtract), R=[Xb], W=[db])
                    xs_, xsb = xsr.next()
                    for ch in range(13):
                        k.op("dve", lambda: nc.vector.scalar_tensor_tensor(out=xs_[:, ch, :], in0=d_[:, ch, :], scalar=mu[:, ch:ch + 1], in1=X[:, ch, 1:129], op0=ALU.mult, op1=ALU.add),
                             R=[db, Xb, prb], W=[xsb])
                    rr = xs_[:, 0:4, :]; kx = xs_[:, 4:8, :]; vv = xs_[:, 8:12, :]
                    tw, twb = twr.next()
                    k.op("act", lambda: nc.scalar.activation(out=tw[:], in_=xs_[0:64, 12, :], func=AF.Tanh), R=[xsb], W=[twb])
                    pz, pzb = pr.next()
                    k.op("pe", lambda: nc.tensor.matmul(pz[:], lhsT=tw[:], rhs=w2t[:], start=True, stop=True), R=[twb, prb], W=[pzb])
                    ld, ldb = ldr.next()
                    k.op("dve", lambda: nc.vector.tensor_tensor(out=ld[:], in0=pz[:], in1=w0bc[:], op=ALU.add), R=[pzb, prb], W=[ldb])
                    k.op("act", lambda: nc.scalar.activation(out=ld[:], in_=ld[:], func=AF.Sigmoid), R=[ldb], W=[ldb])
                    k.op("pool", lambda: nc.gpsimd.tensor_scalar(out=ld[:], in0=ld[:], scalar1=float(-np.exp(-0.5)), scalar2=None, op0=ALU.mult), R=[ldb], W=[ldb])
                    pgi, pgib = pr.next(); pge, pgeb = pr.next()
                    for j in range(4):
                        k.op("pe", lambda: nc.tensor.matmul(pgi[:, j * 128:(j + 1) * 128], lhsT=ld[:, j * 128:(j + 1) * 128], rhs=cst[:, C_BI:C_BI + 128], start=True, stop=True), R=[ldb, cstb], W=[pgib])
                        k.op("pe", lambda: nc.tensor.matmul(pge[:, j * 128:(j + 1) * 128], lhsT=ld[:, j * 128:(j + 1) * 128], rhs=cst[:, C_BS:C_BS + 128], start=True, stop=True), R=[ldb, cstb], W=[pgeb])
                    gi, gib = gir.next(); ge, geb = ger.next()
                    k.op("act", lambda: nc.scalar.copy(out=gi[:], in_=pgi[:].rearrange("p (a b) -> p a b", a=4)), R=[pgib], W=[gib])
                    e1, e1b = e1r.next(); e2, e2b = e2r.next(); e3, e3b = e3r.next(); e4, e4b = e4r.next()
                    k.op("act", lambda: nc.scalar.activation(out=e1[:], in_=gi[:], func=AF.Exp), R=[gib], W=[e1b])
                    k.op("act", lambda: nc.scalar.activation(out=e2[:], in_=pge[:].rearrange("p (a b) -> p a b", a=4), func=AF.Exp), R=[pgeb], W=[e2b])
                    k.op("act", lambda: nc.scalar.activation(out=e3[:], in_=gi[:], func=AF.Exp, scale=-1.0), R=[gib], W=[e3b])
                    for j in range(4):
                        for hf in range(2):
                            k.op("act", lambda: nc.scalar.activation(out=e4[:, j, hf * 64:(hf + 1) * 64], in_=gi[:, j, hf * 64:(hf + 1) * 64], func=AF.Exp, scale=-1.0,
                                                                     bias=gi[:, j, hf * 64 + 63:hf * 64 + 64]), R=[gib], W=[e4b])
                    pa, pab = pr.next()
                    for j in range(4):
                        k.op("pe", lambda: nc.tensor.matmul(pa[:, j * 128:(j + 1) * 128], lhsT=a2t[64:128, j * 128:(j + 1) * 128], rhs=xs_[64:128, 12, :], start=True, stop=True), R=[xsb, prb], W=[pab])
                    aa, aab = aar.next()
                    for j in range(4):
                        k.op("act", lambda: nc.scalar.activation(out=aa[:, j, :], in_=pa[:, j * 128:(j + 1) * 128], func=AF.Sigmoid, bias=prm[:, 0, j:j + 1]), R=[pab, prb], W=[aab])
                    if l > 0:
                        pm, pmb = pr.next()
                        for ch in range(4):
                            k.op("pe", lambda: nc.tensor.matmul(pm[0:32, 0:128], lhsT=v1t[:, ch, :], rhs=xs_[:, 8 + ch, :], start=(ch == 0), stop=(ch == 3)), R=[xsb, prb], W=[pmb])
                        m1, m1b = m1r.next()
                        k.op("act", lambda: nc.scalar.copy(out=m1[:], in_=pm[0:32, 0:128]), R=[pmb], W=[m1b])
                        pm2, pm2b = pr.next()
                        for j in range(4):
                            k.op("pe", lambda: nc.tensor.matmul(pm2[:, j * 128:(j + 1) * 128], lhsT=v2t[:, j * 128:(j + 1) * 128], rhs=m1[:], start=True, stop=True), R=[m1b, prb], W=[pm2b])
                        gt_, gtb_ = tmpr.next()
                        for j in range(4):
                            k.op("act", lambda: nc.scalar.activation(out=gt_[:, j, :], in_=pm2[:, j * 128:(j + 1) * 128], func=AF.Sigmoid, bias=prm[:, 4, j:j + 1]), R=[pm2b, prb], W=[gtb_])
                        vf, vfb = vfr.next()
                        k.dma("sp", vf[:], fmv(vfirst)[:, :, t0:t0 + 128], W=[vfb])
                        k.op("dve", lambda: nc.vector.tensor_tensor(out=vf[:], in0=vf[:], in1=vv, op=ALU.subtract), R=[vfb, xsb], W=[vfb])
                        k.op("pool", lambda: nc.gpsimd.tensor_tensor(out=vf[:], in0=vf[:], in1=gt_[:], op=ALU.mult), R=[vfb, gtb_], W=[vfb])
                        k.op("dve", lambda: nc.vector.tensor_tensor(out=xs_[:, 8:12, :], in0=vv, in1=vf[:], op=ALU.add), R=[vfb, xsb], W=[xsb])
                    else:
                        k.dma("pool", fmv(vfirst)[:, :, t0:t0 + 128], vv, R=[xsb])
                    kk, kkb = kkr_.next()
                    k.op("dve", lambda: nc.vector.tensor_tensor(out=kk[:], in0=kx, in1=bc(prm[:, 1, :]), op=ALU.mult), R=[xsb, prb], W=[kkb])
                    sq_, sqb_ = tmpr.next()
                    k.op("act", lambda: nc.scalar.activation(out=sq_[:], in_=kk[:], func=AF.Square), R=[kkb], W=[sqb_])
                    pn, pnb = pr.next()
                    for j in range(4):
                        k.op("pe", lambda: nc.tensor.matmul(pn[:, j * 128:(j + 1) * 128], lhsT=cst[:, C_BD:C_BD + 128], rhs=sq_[:, j, :], start=True, stop=True), R=[sqb_, cstb], W=[pnb])
                    rn, rnb = tmpr.next()
                    k.op("act", lambda: nc.scalar.activation(out=rn[:], in_=pn[:].rearrange("p (a b) -> p a b", a=4), func=AF.Sqrt), R=[pnb], W=[rnb])
                    k.op("dve", lambda: nc.vector.tensor_scalar_max(out=rn[:], in0=rn[:], scalar1=1e-12), R=[rnb], W=[rnb])
                    k.op("dve", lambda: nc.vector.reciprocal(out=rn[:], in_=rn[:]), R=[rnb], W=[rnb])
                    k.op("pool", lambda: nc.gpsimd.tensor_tensor(out=kk[:], in0=kk[:], in1=rn[:], op=ALU.mult), R=[kkb, rnb], W=[kkb])
                    kh, khb = khr.next()
                    k.op("dve", lambda: nc.vector.tensor_tensor(out=kh[:], in0=aa[:], in1=bc(prm[:, 2, :]), op=ALU.mult), R=[aab, prb], W=[khb])
                    k.op("dve", lambda: nc.vector.tensor_tensor(out=kh[:], in0=kh[:], in1=bc(prm[:, 5, :]), op=ALU.add), R=[khb, prb], W=[khb])
                    k.op("dve", lambda: nc.vector.tensor_tensor(out=kh[:], in0=kh[:], in1=kx, op=ALU.mult), R=[khb, xsb], W=[khb])
                    bv, bvb = bvr.next()
                    k.op("pool", lambda: nc.gpsimd.tensor_tensor(out=bv[:], in0=kk[:], in1=aa[:], op=ALU.mult), R=[kkb, aab], W=[bvb])
                    def emit(dst, in0, in1, neg=False, R=()):
                        o, ob = outr.next()
                        k.op("dve", lambda: nc.vector.tensor_tensor(out=o[:], in0=in0, in1=in1, op=ALU.mult), R=list(R), W=[ob])
                        if neg:
                            k.op("pool", lambda: nc.gpsimd.tensor_scalar(out=o[:], in0=o[:], scalar1=-1.0, scalar2=None, op0=ALU.mult), R=[ob], W=[ob])
                        k.dma("pool", fmv(dst)[:, :, t0:t0 + 128], o[:], R=[ob])
                        return o, ob
                    emit(rwA, kk[:], e2[:], neg=True, R=[kkb, e2b])
                    emit(rwR, rr, e1[:], R=[xsb, e1b])
                    emit(rwB, bv[:], e3[:], R=[bvb, e3b])
                    emit(rwK, kh[:], e3[:], R=[khb, e3b])
                    for (dst, a_, ab_) in ((rwBp, bv, bvb), (rwKp, kh, khb), (rwV, None, None)):
                        if a_ is not None:
                            o, ob = outr.next()
                            k.op("dve", lambda: nc.vector.tensor_tensor(out=o[:], in0=a_[:], in1=e4[:], op=ALU.mult), R=[ab_, e4b], W=[ob])
                            srcv = o
                        else:
                            srcv, ob = xs_[:, 8:12, :], xsb
                        pt_, ptb_ = pr.next()
                        for j in range(4):
                            k.op("pe", lambda: nc.tensor.transpose(pt_[:, j * 128:(j + 1) * 128], srcv[:, j, :], ident), R=[ob, cstb], W=[ptb_])
                        to, tob = tmo.next()
                        k.op("act", lambda: nc.scalar.copy(out=to[:], in_=pt_[:]), R=[ptb_], W=[tob])
                        k.dma("pool", dst[t0:t0 + 128, :], to[:], R=[tob])
                    go_, gob = glo.next()
                    k.op("pool", lambda: nc.gpsimd.tensor_copy(out=go_[:, :, 0:1], in_=e1[:, :, 63:64]), R=[e1b], W=[gob])
                    k.op("pool", lambda: nc.gpsimd.tensor_copy(out=go_[:, :, 1:2], in_=e1[:, :, 127:128]), R=[e1b], W=[gob])
                    k.dma("pool", rwGL.rearrange("(j p) c -> p j c", p=128)[:, :, 2 * c:2 * c + 2], go_[:], R=[gob], slow=True)
                    pd_, pdb_ = tmpr.next()
                    k.op("dve", lambda: nc.vector.tensor_tensor(out=pd_[:], in0=rr, in1=kh[:], op=ALU.mult), R=[xsb, khb], W=[pdb_])
                    k.op("pool", lambda: nc.gpsimd.tensor_tensor(out=pd_[:], in0=pd_[:], in1=bc(prm[:, 3, :]), op=ALU.mult), R=[pdb_, prb], W=[pdb_])
                    pk, pkb = pr.next()
                    for j in range(4):
                        k.op("pe", lambda: nc.tensor.matmul(pk[:, 0:8], lhsT=pd_[:, j, :], rhs=cst[:, C_HS + 8 * j:C_HS + 8 * j + 8], start=(j == 0), stop=(j == 3)), R=[pdb_, cstb], W=[pkb])
                    ro, rob = rkro.next()
                    k.op("act", lambda: nc.scalar.copy(out=ro[:], in_=pk[:, 0:8]), R=[pkb], W=[rob])
                    k.dma("pool", rwRKR[t0:t0 + 128, :], ro[:], R=[rob])
                k.barrier()

            with ExitStack() as es:
                SB = lambda name, shape: es.enter_context(sbt(nc, name, shape, F32))
                GLt = SB("c_GL", [64, 8, NCH]); glb = Buf()
                k.dma("sp", GLt[:], rwGL.rearrange("(h q) c -> q h c", q=64), W=[glb])
                hv = lambda dt_: dt_.rearrange("(h q) t -> q h t", q=64)
                ARr = Ring(es, nc, "c_AR", [64, 8, 2, 64], F32, 3)
                BKr = Ring(es, nc, "c_BK", [64, 8, 2, 64], F32, 3)
                TMr = Ring(es, nc, "c_TM", [64, 3, 512], F32, 3)
                Hr = Ring(es, nc, "c_H", [64, 8, 64], F32, 2)
                A1r = Ring(es, nc, "c_A1", [64, 8, 128], F32, 2)
                A2r = Ring(es, nc, "c_A2", [64, 8, 128], F32, 2)
                Pr_ = Ring(es, nc, "c_P", [64, 8, 64], F32, 3)
                Ptr_ = Ring(es, nc, "c_Pt", [64, 8, 64], F32, 3)
                Lr = Ring(es, nc, "c_L", [64, 8, 64], F32, 3)
                Xsr = Ring(es, nc, "c_Xs", [64, 8, 64], F32, 2)
                Usr = Ring(es, nc, "c_Us", [64, 8, 64], F32, 2)
                Yr = Ring(es, nc, "c_Y", [64, 512], F32, 3)
                pr = Ring(es, nc, "c_ps", [128, 512], F32, 8, psum=True)
                m1 = cst[0:64, C_M1:C_M1 + 128].unsqueeze(1).to_broadcast([64, 4, 128])
                m3 = cst[0:64, C_M3:C_M3 + 64].unsqueeze(1).to_broadcast([64, 8, 64])
                i64b = cst[0:64, 0:64].unsqueeze(1).to_broadcast([64, 8, 64])
                H, Hb = Hr.next()
                k.op("pool", lambda: nc.gpsimd.memset(H[:], 0.0), W=[Hb])
                v8 = lambda p_: p_[0:64, :].rearrange("p (h d) -> p h d", h=8)
                for c in range(NCH):
                    cs_ = slice(c * 64, (c + 1) * 64)
                    AR, ARb = ARr.next(); BK, BKb = BKr.next(); TM_, TMb = TMr.next()
                    k.dma("sp", AR[:, :, 0, :], hv(rwA)[:, :, cs_], W=[ARb])
                    k.dma("sp", AR[:, :, 1, :], hv(rwR)[:, :, cs_], W=[ARb])
                    k.dma("sp", BK[:, :, 0, :], hv(rwB)[:, :, cs_], W=[BKb])
                    k.dma("sp", BK[:, :, 1, :], hv(rwK)[:, :, cs_], W=[BKb])
                    k.dma("sp", TM_[:, 0, :], rwBp[cs_, :], W=[TMb])
                    k.dma("sp", TM_[:, 1, :], rwKp[cs_, :], W=[TMb])
                    k.dma("sp", TM_[:, 2, :], rwV[cs_, :], W=[TMb])
                    A1, A1b = A1r.next(); A2, A2b = A2r.next()
                    for (A_, Ab_, which) in ((A1, A1b, 0), (A2, A2b, 1)):
                        for b in range(2):
                            p, pb = pr.next()
                            for hh_ in range(4):
                                h = b * 4 + hh_
                                k.op("pe", lambda: nc.tensor.matmul(p[0:64, hh_ * 128:(hh_ + 1) * 128], lhsT=BK[:, h, which, :], rhs=AR[:, h, :, :].rearrange("p a d -> p (a d)"), start=True, stop=True),
                                     R=[BKb, ARb], W=[pb])
                            k.op("dve", lambda: nc.vector.tensor_tensor(out=A_[:, b * 4:(b + 1) * 4, :], in0=p[0:64, :].rearrange("p (h d) -> p h d", h=4), in1=m1, op=ALU.mult), R=[pb, cstb], W=[Ab_])
                    p, pb = pr.next()
                    for h in range(8):
                        k.op("pe", lambda: nc.tensor.matmul(p[0:64, h * 64:(h + 1) * 64], lhsT=AR[:, h, 0, :], rhs=BK[:, h, 0, :], start=True, stop=True), R=[ARb, BKb], W=[pb])
                    Pt_, Ptb_ = Ptr_.next()
                    k.op("dve", lambda: nc.vector.tensor_tensor(out=Pt_[:], in0=v8(p), in1=m3, op=ALU.mult), R=[pb, cstb], W=[Ptb_])
                    P_, Pb_ = Pr_.next()
                    k.op("pool", lambda: nc.gpsimd.tensor_copy(out=P_[:], in_=A1[:, :, 0:64]), R=[A1b], W=[Pb_])
                    L_, Lb_ = Lr.next()
                    k.op("pool", lambda: nc.gpsimd.tensor_tensor(out=L_[:], in0=A1[:, :, 0:64], in1=i64b, op=ALU.add), R=[A1b, cstb], W=[Lb_])
                    for lev in range(5):
                        pa, pab = pr.next(); pb2, pb2b = pr.next()
                        for h in range(8):
                            if lev == 4:
                                break
                            k.op("pe", lambda: nc.tensor.matmul(pa[0:64, h * 64:(h + 1) * 64], lhsT=Pt_[:, h, :], rhs=P_[:, h, :], start=True, stop=True), R=[Ptb_, Pb_], W=[pab])
                        for h in range(8):
                            k.op("pe", lambda: nc.tensor.matmul(pb2[0:64, h * 64:(h + 1) * 64], lhsT=P_[:, h, :], rhs=Pt_[:, h, :], start=True, stop=True), R=[Ptb_, Pb_], W=[pb2b])
                        Pn, Pnb = Pr_.next(); Ptn, Ptnb = Ptr_.next()
                        if lev < 4:
                            k.op("act", lambda: nc.scalar.copy(out=Pn[:], in_=v8(pa)), R=[pab], W=[Pnb])
                        k.op("dve", lambda: nc.vector.tensor_copy(out=Ptn[:], in_=v8(pb2)), R=[pb2b], W=[Ptnb])
                        P_, Pb_, Pt_, Ptb_ = Pn, Pnb, Ptn, Ptnb
                        pc, pcb = pr.next()
                        for h in range(8):
                            k.op("pe", lambda: nc.tensor.matmul(pc[0:64, h * 64:(h + 1) * 64], lhsT=Pt_[:, h, :], rhs=L_[:, h, :], start=True, stop=True), R=[Ptb_, Lb_], W=[pcb])
                        Ln, Lnb = Lr.next()
                        k.op("dve", lambda: nc.vector.tensor_tensor(out=Ln[:], in0=L_[:], in1=v8(pc), op=ALU.add), R=[Lb_, pcb], W=[Lnb])
                        L_, Lb_ = Ln, Lnb
                    Vh = lambda h: TM_[:, 2, h * 64:(h + 1) * 64]
                    px, pxb = pr.next()
                    for h in range(8):
                        k.op("pe", lambda: nc.tensor.matmul(px[0:64, h * 64:(h + 1) * 64], lhsT=AR[:, h, 0, :], rhs=H[:, h, :], start=True, stop=False), R=[ARb, Hb], W=[pxb])
                        k.op("pe", lambda: nc.tensor.matmul(px[0:64, h * 64:(h + 1) * 64], lhsT=A2[:, h, 0:64], rhs=Vh(h), start=False, stop=True), R=[A2b, TMb], W=[pxb])
                    Xs, Xsb = Xsr.next()
                    k.op("act", lambda: nc.scalar.copy(out=Xs[:], in_=v8(px)), R=[pxb], W=[Xsb])
                    pu, pub = pr.next()
                    for h in range(8):
                        k.op("pe", lambda: nc.tensor.matmul(pu[0:64, h * 64:(h + 1) * 64], lhsT=L_[:, h, :], rhs=Xs[:, h, :], start=True, stop=True), R=[Lb_, Xsb], W=[pub])
                    Us, Usb = Usr.next()
                    k.op("dve", lambda: nc.vector.tensor_copy(out=Us[:], in_=v8(pu)), R=[pub], W=[Usb])
                    py, pyb = pr.next()
                    for h in range(8):
                        k.op("pe", lambda: nc.tensor.matmul(py[0:64, h * 64:(h + 1) * 64], lhsT=AR[:, h, 1, :], rhs=H[:, h, :], start=True, stop=False), R=[ARb, Hb], W=[pyb])
                        k.op("pe", lambda: nc.tensor.matmul(py[0:64, h * 64:(h + 1) * 64], lhsT=A1[:, h, 64:128], rhs=Us[:, h, :], start=False, stop=False), R=[A1b, Usb], W=[pyb])
                        k.op("pe", lambda: nc.tensor.matmul(py[0:64, h * 64:(h + 1) * 64], lhsT=A2[:, h, 64:128], rhs=Vh(h), start=False, stop=True), R=[A2b, TMb], W=[pyb])
                    Yt, Ytb = Yr.next()
                    k.op("act", lambda: nc.scalar.copy(out=Yt[:], in_=py[0:64, :]), R=[pyb], W=[Ytb])
                    k.dma("pool", rwY[cs_, :], Yt[:], R=[Ytb])
                    ph, phb = pr.next()
                    for h in range(8):
                        k.op("pe", lambda: nc.tensor.matmul(ph[0:64, h * 64:(h + 1) * 64], lhsT=TM_[:, 0, h * 64:(h + 1) * 64], rhs=Us[:, h, :], start=True, stop=False), R=[TMb, Usb], W=[phb])
                        k.op("pe", lambda: nc.tensor.matmul(ph[0:64, h * 64:(h + 1) * 64], lhsT=TM_[:, 1, h * 64:(h + 1) * 64], rhs=Vh(h), start=False, stop=True), R=[TMb], W=[phb])
                    Hn, Hnb = Hr.next()
                    k.op("dve", lambda: nc.vector.tensor_tensor(out=Hn[:], in0=H[:], in1=GLt[:, :, c:c + 1].to_broadcast([64, 8, 64]), op=ALU.mult), R=[Hb, glb], W=[Hnb])
                    k.op("dve", lambda: nc.vector.tensor_tensor(out=Hn[:], in0=Hn[:], in1=v8(ph), op=ALU.add), R=[Hnb, phb], W=[Hnb])
                    H, Hb = Hn, Hnb
                k.barrier()

            with ExitStack() as es:
                SB = lambda name, shape: es.enter_context(sbt(nc, name, shape, F32))
                lng = SB("e_g", [128, 512]); lnb = SB("e_b", [128, 512]); eb = Buf()
                k.dma("sp", lng[:], rw_ln_g[l].partition_broadcast(128), W=[eb])
                k.dma("sp", lnb[:], rw_ln_b[l].partition_broadcast(128), W=[eb])
                inr = Ring(es, nc, "e_in", [128, 3, 512], F32, 2)
                rkr_ = Ring(es, nc, "e_rk", [128, 8], F32, 2)
                sqr = Ring(es, nc, "e_sq", [128, 8, 64], F32, 2)
                str_ = Ring(es, nc, "e_st", [128, 4, 8], F32, 2)
                yr = Ring(es, nc, "e_y", [128, 8, 64], F32, 2)
                mtr = Ring(es, nc, "e_mt", [128, 4, 128], BF16, 2)
                pr = Ring(es, nc, "e_ps", [128, 512], F32, 2, psum=True)
                b8 = lambda ap_: ap_.unsqueeze(2).to_broadcast([128, 8, 64])
                v3 = lambda ap_: ap_.rearrange("p (h d) -> p h d", h=8)
                for c in range(NT):
                    t0 = c * 128
                    it, itb = inr.next()
                    k.dma("sp", it[:, 0, :], rwY[t0:t0 + 128, :], W=[itb])
                    k.dma("sp", it[:, 1, :], rwV[t0:t0 + 128, :], W=[itb])
                    k.dma("sp", it[:, 2, :], pTM[t0:t0 + 128, TM_RZ:TM_RZ + 512], W=[itb])
                    rk, rkb = rkr_.next()
                    k.dma("sp", rk[:], rwRKR[t0:t0 + 128, :], W=[rkb])
                    Y3 = v3(it[:, 0, :])
                    sq, sqb = sqr.next()
                    k.op("act", lambda: nc.scalar.activation(out=sq[:], in_=Y3, func=AF.Square), R=[itb], W=[sqb])
                    st, stb = str_.next()
                    k.op("dve", lambda: nc.vector.tensor_reduce(out=st[:, 0, :], in_=Y3, axis=AX.X, op=ALU.add), R=[itb], W=[stb])
                    k.op("dve", lambda: nc.vector.tensor_reduce(out=st[:, 1, :], in_=sq[:], axis=AX.X, op=ALU.add), R=[sqb], W=[stb])
                    k.op("dve", lambda: nc.vector.tensor_scalar(out=st[:, 0, :], in0=st[:, 0, :], scalar1=1.0 / 64, scalar2=None, op0=ALU.mult), R=[stb], W=[stb])
                    k.op("dve", lambda: nc.vector.tensor_tensor(out=st[:, 2, :], in0=st[:, 0, :], in1=st[:, 0, :], op=ALU.mult), R=[stb], W=[stb])
                    k.op("dve", lambda: nc.vector.scalar_tensor_tensor(out=st[:, 1, :], in0=st[:, 1, :], scalar=1.0 / 64, in1=st[:, 2, :], op0=ALU.mult, op1=ALU.subtract), R=[stb], W=[stb])
                    rstd(st[:, 3, :], st[:, 1, :], 1.0, C_GNEPS, [stb], [stb])
                    y, yb = yr.next()
                    k.op("dve", lambda: nc.vector.tensor_tensor(out=y[:], in0=Y3, in1=b8(st[:, 0, :]), op=ALU.subtract), R=[itb, stb], W=[yb])
                    k.op("dve", lambda: nc.vector.tensor_tensor(out=y[:], in0=y[:], in1=b8(st[:, 3, :]), op=ALU.mult), R=[yb, stb], W=[yb])
                    k.op("pool", lambda: nc.gpsimd.tensor_tensor(out=y[:], in0=y[:], in1=v3(lng[:]), op=ALU.mult), R=[yb, eb], W=[yb])
                    k.op("pool", lambda: nc.gpsimd.tensor_tensor(out=y[:], in0=y[:], in1=v3(lnb[:]), op=ALU.add), R=[yb, eb], W=[yb])
                    k.op("dve", lambda: nc.vector.tensor_tensor(out=sq[:], in0=v3(it[:, 1, :]), in1=b8(rk[:]), op=ALU.mult), R=[itb, rkb, sqb], W=[sqb])
                    k.op("dve", lambda: nc.vector.tensor_tensor(out=y[:], in0=y[:], in1=sq[:], op=ALU.add), R=[yb, sqb], W=[yb])
                    k.op("act", lambda: nc.scalar.activation(out=it[:, 2, :], in_=it[:, 2, :], func=AF.Silu), R=[itb], W=[itb])
                    k.op("dve", lambda: nc.vector.tensor_tensor(out=y[:], in0=y[:], in1=v3(it[:, 2, :]), op=ALU.mult), R=[yb, itb], W=[yb])
                    p, pb = pr.next()
                    yf = y[:].rearrange("p h d -> p (h d)")
                    for j in range(4):
                        k.op("pe", lambda: nc.tensor.transpose(p[:, j * 128:(j + 1) * 128], yf[:, j * 128:(j + 1) * 128], ident), R=[yb, cstb], W=[pb])
                    mt, mtb = mtr.next()
                    k.op("act", lambda: nc.scalar.copy(out=mt[:], in_=p[:].rearrange("p (a b) -> p a b", a=4)), R=[pb], W=[mtb])
                    k.dma("pool", mixT.rearrange("(cc p) t -> p cc t", p=128)[:, 12:16, t0:t0 + 128], mt[:], R=[mtb])
                k.barrier()

            with ExitStack() as es:
                mx = Ring(es, nc, "p5_m", [128, 16, G], BF16, 2)
                wfr = Ring(es, nc, "p5_wf", [128, 16, 512], F32, 2)
                wr = Ring(es, nc, "p5_w", [128, 16, 512], BF16, 2)
                xr = Ring(es, nc, "p5_x", [128, G], F32, 3)
                xo = Ring(es, nc, "p5_o", [128, G], F32, 3)
                ps = Ring(es, nc, "p5_ps", [128, 512], F32, 4, psum=True)
                for g in range(NG):
                    mt, mb = mx.next()
                    k.dma("sp", mt[:], mixT.rearrange("(cc p) t -> p cc t", p=128)[:, :, g * G:(g + 1) * G], W=[mb])
                    for nb in range(4):
                        wf, wfb = wfr.next()
                        k.dma("sp", wf[:], w_out[l].rearrange("(cc p) n -> p cc n", p=128)[:, :, nb * 512:(nb + 1) * 512], W=[wfb])
                        wt, wb = wr.next()
                        k.op("act", lambda: nc.scalar.copy(out=wt[:, 0:8, :], in_=wf[:, 0:8, :]), R=[wfb], W=[wb])
                        k.op("pool", lambda: nc.gpsimd.tensor_copy(out=wt[:, 8:16, :], in_=wf[:, 8:16, :]), R=[wfb], W=[wb])
                        for j in range(4):
                            n0 = nb * 512 + j * 128
                            xt, xb = xr.next()
                            k.dma("sp", xt[:], xT[n0:n0 + 128, g * G:(g + 1) * G], W=[xb])
                            p, pb = ps.next()
                            for cc in range(16):
                                k.op("pe", lambda: nc.tensor.matmul(p[:, :G], lhsT=wt[:, cc, j * 128:(j + 1) * 128], rhs=mt[:, cc, :],
                                                                    start=(cc == 0), stop=(cc == 15)), R=[wb, mb], W=[pb])
                            ot, ob = xo.next()
                            k.op("dve", lambda: nc.vector.tensor_tensor(out=ot[:], in0=p[:, :G], in1=xt[:], op=ALU.add), R=[pb, xb], W=[ob])
                            k.dma("pool", xT[n0:n0 + 128, g * G:(g + 1) * G], ot[:], R=[ob])
                k.barrier()

        with ExitStack() as es:
            gbc = es.enter_context(sbt(nc, "f_g", [128, D], F32))
            gbb = Buf()
            k.dma("sp", gbc[:], final_g.partition_broadcast(128), W=[gbb])
            xin = Ring(es, nc, "f_x", [128, 16, 128], F32, 2)
            xtm = Ring(es, nc, "f_t", [128, D], F32, 2)
            junk = es.enter_context(sbt(nc, "f_j", [128, D], F32))
            jb = Buf()
            ssq = Ring(es, nc, "f_s", [128, 2], F32, 2)
            ot = Ring(es, nc, "f_o", [128, D], F32, 2)
            ps = Ring(es, nc, "f_ps", [128, 512], F32, 8, psum=True)
            for t in range(NT):
                xt, xb = xin.next()
                k.dma("sp", xt[:], xT.rearrange("(kc p) t -> p kc t", p=128)[:, :, t * 128:(t + 1) * 128], W=[xb])
                xm, xmb = xtm.next()
                for q in range(4):
                    p, pb = ps.next()
                    for j in range(4):
                        kc = q * 4 + j
                        k.op("pe", lambda: nc.tensor.transpose(p[:, j * 128:(j + 1) * 128], xt[:, kc, :], ident), R=[xb, cstb], W=[pb])
                    if q % 2:
                        k.op("act", lambda: nc.scalar.copy(out=xm[:, q * 512:(q + 1) * 512], in_=p[:]), R=[pb], W=[xmb])
                    else:
                        k.op("dve", lambda: nc.vector.tensor_copy(out=xm[:, q * 512:(q + 1) * 512], in_=p[:]), R=[pb], W=[xmb])
                sq, sqb = ssq.next()
                k.op("act", lambda: nc.scalar.activation(out=junk[:], in_=xm[:], func=AF.Square, accum_out=sq[:, 0:1]), R=[xmb], W=[jb, sqb])
                rstd(sq[:, 1:2], sq[:, 0:1], 1.0 / D, C_EPS, [sqb], [sqb])
                o, ob = ot.next()
                k.op("dve", lambda: nc.vector.scalar_tensor_tensor(out=o[:], in0=xm[:], scalar=sq[:, 1:2], in1=gbc[:], op0=ALU.mult, op1=ALU.mult),
                     R=[xmb, sqb, gbb], W=[ob])
                k.dma("pool", out[t * 128:(t + 1) * 128, :], o[:], R=[ob])
            k.barrier()
    return nc


def MIXERS(env):
    pass


_CACHE = {}


WNAMES = ("norm_g", "w_in", "w_out", "final_norm_g", "ml_conv_w", "ml_conv_b", "ml_i_bias", "ml_f_bias", "ml_norm_g",
          "mla_q_norm_g", "mla_w_uq", "mla_kv_norm_g", "mla_w_ukv",
          "rw_mu", "rw_w0", "rw_w2", "rw_a0", "rw_a2", "rw_k_k", "rw_k_a", "rw_r_k", "rw_ln_g", "rw_ln_b")


def make_maps(inputs, T, depth):
    consts = make_consts()
    shared = {}
    for name in WNAMES:
        a = inputs[name]
        shared[name] = np.ascontiguousarray(a if name == "final_norm_g" else a[:depth])
    for name in ("rw_v0", "rw_v1", "rw_v2"):
        shared[name] = np.ascontiguousarray(inputs[name])
    in_maps = []
    for c in range(8):
        b = c % 4
        m = {"x": np.ascontiguousarray(inputs["x"][b, :T]), "positions": np.ascontiguousarray(inputs["positions"][b, :T]),
             "consts": consts}
        m.update(shared)
        in_maps.append(m)
    return in_maps


def kernel(**inputs):
    T = 4096
    depth = 4
    if "nc" not in _CACHE:
        _CACHE["nc"] = build(T, depth)
    nc = _CACHE["nc"]
    in_maps = make_maps(inputs, T, depth)
    res = run_bass_kernel_spmd(nc, in_maps, core_ids=list(range(8)))
    return np.stack([res.results[b]["out"] for b in range(4)], axis=0)
```

```python
import numpy as np
from contextlib import ExitStack
import concourse.bass as bass
import concourse.mybir as mybir
from concourse.bass_utils import run_bass_kernel_spmd

F32 = mybir.dt.float32
BF16 = mybir.dt.bfloat16
I32 = mybir.dt.int32
AF = mybir.ActivationFunctionType
ALU = mybir.AluOpType
AX = mybir.AxisListType

D = 2048
DIN = 6600
EPS = 1e-6
FM_RANGES = [(0, 1024), (2568, 3336), (4424, 6088)]
TM_RANGES = [(1024, 2568), (3336, 4424), (6088, 6600)]
TM_V, TM_I, TM_F, TM_O, TM_Z = 0, 512, 516, 520, 1032
TM_KR, TM_MZ, TM_RZ = 1544, 1608, 2632
NTM = 3144
FM_QK, FM_CQ, FM_CKV, FM_RW = 0, 8, 12, 14
NFM = 27


_UID = [0]


def sbt(nc, name, shape, dt):
    _UID[0] += 1
    return nc.sbuf_tensor("%s_u%d" % (name, _UID[0]), shape, dt)


def pst(nc, name, shape, dt):
    _UID[0] += 1
    return nc.psum_tensor("%s_u%d" % (name, _UID[0]), shape, dt)


class Buf:
    __slots__ = ("w", "r")

    def __init__(self):
        self.w = None
        self.r = {}


class KB:
    NDS = 48

    def __init__(self, nc):
        self.nc = nc
        self.E = {"pe": nc.tensor, "act": nc.scalar, "dve": nc.vector, "pool": nc.gpsimd, "sp": nc.sync}
        self.sem = {e: nc.alloc_semaphore("s_" + e) for e in ("pe", "act", "dve", "pool")}
        self.cnt = {e: 0 for e in self.sem}
        self.dsem = [nc.alloc_semaphore("d%d" % i) for i in range(self.NDS)]
        self.dcnt = [0] * self.NDS
        self.dnext = 0
        self.bar = nc.alloc_semaphore("bar")
        self.nbar = 0
        self.seen = {e: {} for e in self.E}

    def _semh(self, key):
        return self.sem[key] if isinstance(key, str) else self.dsem[key[1]]

    def _wait(self, e, key, val, same_ok=False):
        if val <= 0:
            return
        if key == e and (same_ok or e == "pe"):
            return
        if self.seen[e].get(key, 0) >= val:
            return
        self.E[e].wait_ge(self._semh(key), val)
        self.seen[e][key] = val

    def _deps(self, e, reads, writes):
        for b in reads:
            if b.w is not None:
                self._wait(e, b.w[0], b.w[1])
        for b in writes:
            if b.w is not None:
                self._wait(e, b.w[0], b.w[1])
            for key, val in b.r.items():
                self._wait(e, key, val, same_ok=True)

    def _mark(self, ev, reads, writes):
        for b in reads:
            if b.r.get(ev[0], 0) < ev[1]:
                b.r[ev[0]] = ev[1]
        for b in writes:
            b.w = ev
            b.r = {}

    def op(self, e, fn, R=(), W=()):
        self._deps(e, R, W)
        ins = fn()
        self.cnt[e] += 1
        ins.then_inc(self.sem[e], 1)
        self._mark((e, self.cnt[e]), R, W)

    def dma(self, q, out, in_, R=(), W=(), slow=False):
        i = self.dnext
        self.dnext = (i + 1) % self.NDS
        key = ("d", i)
        self._wait(q, key, self.dcnt[i])
        self._deps(q, R, W)
        if slow:
            ins = self.E[q].dma_start(out=out, in_=in_, allow_slow_non_contiguous=True)
        else:
            ins = self.E[q].dma_start(out=out, in_=in_)
        self.dcnt[i] += 16
        ins.then_inc(self.dsem[i], 16)
        self._mark((key, self.dcnt[i]), R, W)

    def barrier(self):
        sp = self.E["sp"]
        for e in self.sem:
            self._wait("sp", e, self.cnt[e])
        for i in range(self.NDS):
            self._wait("sp", ("d", i), self.dcnt[i])
        self.nbar += 1
        sp.sem_inc(self.bar, 1)
        for e in ("pe", "act", "dve", "pool"):
            self.E[e].wait_ge(self.bar, self.nbar)
            for k2 in self.sem:
                self.seen[e][k2] = self.cnt[k2]
            for i in range(self.NDS):
                self.seen[e][("d", i)] = self.dcnt[i]


class Ring:
    def __init__(self, es, nc, name, shape, dtype, n, psum=False):
        self.items = []
        for i in range(n):
            if psum:
                t = es.enter_context(pst(nc, "%s%d" % (name, i), shape, dtype))
            else:
                t = es.enter_context(sbt(nc, "%s%d" % (name, i), shape, dtype))
            self.items.append((t, Buf()))
        self.i = 0

    def next(self):
        it = self.items[self.i]
        self.i = (self.i + 1) % len(self.items)
        return it


def split_blocks(ranges, maxw=512):
    out = []
    for (a, b) in ranges:
        c = a
        while c < b:
            w = min(maxw, b - c)
            out.append((c, w))
            c += w
    return out


def make_consts():
    c = np.zeros((128, 1280), np.float32)
    c[:, 0:128] = np.eye(128, dtype=np.float32)
    j = np.arange(128)
    c[:, 128:256] = (j[:, None] <= j[None, :]).astype(np.float32)
    c[:, 256:384] = 1.0
    same = (j[:, None] // 64) == (j[None, :] // 64)
    c[:, 384:512] = ((j[:, None] <= j[None, :]) & same)
    c[:, 512:640] = ((j[:, None] < j[None, :]) & same)
    invf = np.power(10000.0, -np.arange(0, 64, 2, dtype=np.float32) / 64).astype(np.float32)
    c[:, 640:672] = invf[None, :]
    c[:, 672:674] = (j[:, None] // 64 == np.arange(2)[None, :])
    c[:, 700] = 1e-6
    c[:, 701] = 64e-5
    c[:, 702] = 1.0
    c[:, 703] = -0.5
    c[:, 704] = 0.0
    c[:, 705] = -np.pi
    c[:, 706] = 1e-24
    c[:, 707] = 1.0 / 128
    i64 = np.arange(64)
    c[:64, 768:832] = (i64[:, None] < i64[None, :])
    c[:64, 832:896] = (i64[:, None] <= i64[None, :])
    c[:64, 896:960] = (i64[:, None] > i64[None, :])
    for jj in range(4):
        c[:, 960 + jj * 8:968 + jj * 8] = ((2 * jj + j[:, None] // 64) == np.arange(8)[None, :])
    c[:, 1024:1152] = same
    return c


C_ID, C_TRI, C_ONE, C_BI, C_BS, C_IF, C_CI = 0, 128, 256, 384, 512, 640, 672
C_M1, C_M3, C_HS, C_BD = 768, 896, 960, 1024
C_EPS, C_GNEPS, C_1, C_MH, C_0, C_MPI, C_TINY, C_R128 = 700, 701, 702, 703, 704, 705, 706, 707


def build(T, depth, dbg=()):
    assert T % 128 == 0
    NT = T // 128
    SG = min(T, 1024)
    NSG = T // SG
    G = min(T, 512)
    NG = T // G
    nc = bass.Bass("TRN2", target_bir_lowering=False)

    def din(name, shape, dt=F32):
        return nc.dram_tensor(name, list(shape), dt, kind="ExternalInput").ap()

    def dscr(name, shape, dt=F32):
        kind = "ExternalOutput" if name in dbg else "Internal"
        return nc.dram_tensor(name, list(shape), dt, kind=kind).ap()

    x_in = din("x", [T, D])
    pos_in = din("positions", [T], I32)
    consts_in = din("consts", [128, 1280])
    norm_g = din("norm_g", [depth, D])
    w_in = din("w_in", [depth, D, DIN])
    w_out = din("w_out", [depth, D, D])
    final_g = din("final_norm_g", [D])
    ml_conv_w = din("ml_conv_w", [depth, 4, 1024]); ml_conv_b = din("ml_conv_b", [depth, 1024])
    mla_q_norm_g = din("mla_q_norm_g", [depth, 512]); mla_w_uq = din("mla_w_uq", [depth, 512, 1536])
    mla_kv_norm_g = din("mla_kv_norm_g", [depth, 256]); mla_w_ukv = din("mla_w_ukv", [depth, 256, 2048])
    rw_mu = din("rw_mu", [depth, 1664]); rw_w0 = din("rw_w0", [depth, 512]); rw_w2 = din("rw_w2", [depth, 64, 512])
    rw_a0 = din("rw_a0", [depth, 512]); rw_a2 = din("rw_a2", [depth, 64, 512])
    rw_v0 = din("rw_v0", [3, 512]); rw_v1 = din("rw_v1", [3, 512, 32]); rw_v2 = din("rw_v2", [3, 32, 512])
    rw_k_k = din("rw_k_k", [depth, 512]); rw_k_a = din("rw_k_a", [depth, 512]); rw_r_k = din("rw_r_k", [depth, 8, 64])
    rw_ln_g = din("rw_ln_g", [depth, 512]); rw_ln_b = din("rw_ln_b", [depth, 512])
    ml_i_bias = din("ml_i_bias", [depth, 4]); ml_f_bias = din("ml_f_bias", [depth, 4]); ml_norm_g = din("ml_norm_g", [depth, 512])
    out = nc.dram_tensor("out", [T, D], F32, kind="ExternalOutput").ap()

    xT = dscr("xT", [D, T])
    pFM = dscr("pFM", [NFM * 128, T])
    pTM = dscr("pTM", [T, NTM])
    mixT = dscr("mixT", [D, T], BF16)
    csd = dscr("cs", [T, 64])
    qnT = dscr("qnT", [1024, T], BF16); knT = dscr("knT", [1024, T], BF16); qrT = dscr("qrT", [512, T], BF16); krT = dscr("krT", [64, T], BF16)
    vaug = dscr("vaug", [8, T, 129], BF16)
    rwA = dscr("rwA", [512, T]); rwR = dscr("rwR", [512, T]); rwB = dscr("rwB", [512, T]); rwK = dscr("rwK", [512, T])
    rwBp = dscr("rwBp", [T, 512]); rwKp = dscr("rwKp", [T, 512]); rwV = dscr("rwV", [T, 512]); rwY = dscr("rwY", [T, 512])
    rwGL = dscr("rwGL", [512, T // 64]); rwRKR = dscr("rwRKR", [T, 8]); vfirst = dscr("vfirst", [512, T])

    fm_blocks = split_blocks(FM_RANGES)
    tm_blocks = split_blocks(TM_RANGES)

    with ExitStack() as top:
        k = KB(nc)
        cst = top.enter_context(sbt(nc, "cst", [128, 1280], F32))
        cstb = Buf()
        k.dma("sp", cst[:], consts_in, W=[cstb])
        ident = cst[:, C_ID:C_ID + 128]
        ones = cst[:, C_ONE:C_ONE + 128]
        onec = cst[:, C_ONE:C_ONE + 1]

        def rstd(o, i, scale, epscol, R, W, np_=128):
            k.op("act", lambda: nc.scalar.activation(out=o, in_=i, func=AF.Sqrt, bias=cst[:np_, epscol:epscol + 1], scale=scale), R=list(R) + [cstb], W=W)
            k.op("dve", lambda: nc.vector.reciprocal(out=o, in_=o), R=W, W=W)

        with ExitStack() as es:
            xin = Ring(es, nc, "i_x", [128, D], F32, 2)
            ps = Ring(es, nc, "i_ps", [128, 512], F32, 4, psum=True)
            stg = Ring(es, nc, "i_st", [128, 16, 128], F32, 2)
            for t in range(NT):
                xt, xb = xin.next()
                k.dma("sp", xt[:], x_in[t * 128:(t + 1) * 128, :], W=[xb])
                st, sb = stg.next()
                for q in range(4):
                    p, pb = ps.next()
                    for j in range(4):
                        kc = q * 4 + j
                        k.op("pe", lambda: nc.tensor.transpose(p[:, j * 128:(j + 1) * 128], xt[:, kc * 128:(kc + 1) * 128], ident),
                             R=[xb, cstb], W=[pb])
                    eng = "act" if q % 2 else "dve"
                    if eng == "act":
                        k.op("act", lambda: nc.scalar.copy(out=st[:, q * 4:(q + 1) * 4, :], in_=p[:].rearrange("p (a b) -> p a b", a=4)), R=[pb], W=[sb])
                    else:
                        k.op("dve", lambda: nc.vector.tensor_copy(out=st[:, q * 4:(q + 1) * 4, :], in_=p[:].rearrange("p (a b) -> p a b", a=4)), R=[pb], W=[sb])
                k.dma("pool", xT.rearrange("(kc p) t -> p kc t", p=128)[:, :, t * 128:(t + 1) * 128], st[:], R=[sb])
            posi = es.enter_context(sbt(nc, "i_pi", [128, NT], I32))
            posf = es.enter_context(sbt(nc, "i_pf", [128, NT], F32))
            pob = Buf()
            k.dma("sp", posi[:], pos_in.rearrange("(n p) -> p n", p=128), W=[pob], slow=True)
            k.op("dve", lambda: nc.vector.tensor_copy(out=posf[:], in_=posi[:]), R=[pob], W=[pob])
            angr = Ring(es, nc, "i_ang", [128, 64], F32, 2)
            tir = Ring(es, nc, "i_ti", [128, 64], I32, 2)
            tfr = Ring(es, nc, "i_tf", [128, 64], F32, 2)
            TWO_PI = float(2 * np.pi)
            C1 = 6.28125
            C2 = float(2 * np.pi - 6.28125)
            for t in range(NT):
                an, anb = angr.next()
                ti, tib = tir.next()
                tf, tfb = tfr.next()
                k.op("dve", lambda: nc.vector.tensor_scalar(out=an[:, 0:32], in0=cst[:, C_IF:C_IF + 32], scalar1=posf[:, t:t + 1], scalar2=None, op0=ALU.mult), R=[pob, cstb], W=[anb])
                k.op("dve", lambda: nc.vector.tensor_scalar(out=an[:, 32:64], in0=an[:, 0:32], scalar1=float(np.pi / 2), scalar2=None, op0=ALU.add), R=[anb], W=[anb])
                k.op("dve", lambda: nc.vector.tensor_scalar(out=tf[:], in0=an[:], scalar1=float(1 / (2 * np.pi)), scalar2=None, op0=ALU.mult), R=[anb], W=[tfb])
                k.op("dve", lambda: nc.vector.tensor_copy(out=ti[:], in_=tf[:]), R=[tfb], W=[tib])
                k.op("dve", lambda: nc.vector.tensor_copy(out=tf[:], in_=ti[:]), R=[tib], W=[tfb])
                k.op("dve", lambda: nc.vector.scalar_tensor_tensor(out=an[:], in0=tf[:], scalar=-C1, in1=an[:], op0=ALU.mult, op1=ALU.add), R=[tfb, anb], W=[anb])
                k.op("dve", lambda: nc.vector.scalar_tensor_tensor(out=an[:], in0=tf[:], scalar=-C2, in1=an[:], op0=ALU.mult, op1=ALU.add), R=[tfb, anb], W=[anb])
                k.op("dve", lambda: nc.vector.tensor_scalar(out=tf[:], in0=an[:], scalar1=float(np.pi), scalar2=-TWO_PI, op0=ALU.is_gt, op1=ALU.mult), R=[anb], W=[tfb])
                k.op("dve", lambda: nc.vector.tensor_tensor(out=an[:], in0=an[:], in1=tf[:], op=ALU.add), R=[tfb, anb], W=[anb])
                k.op("dve", lambda: nc.vector.tensor_scalar(out=tf[:], in0=an[:], scalar1=float(-np.pi), scalar2=TWO_PI, op0=ALU.is_lt, op1=ALU.mult), R=[anb], W=[tfb])
                k.op("dve", lambda: nc.vector.tensor_tensor(out=an[:], in0=an[:], in1=tf[:], op=ALU.add), R=[tfb, anb], W=[anb])
                k.op("act", lambda: nc.scalar.activation(out=an[:], in_=an[:], func=AF.Sin), R=[anb], W=[anb])
                k.dma("pool", csd[t * 128:(t + 1) * 128, :], an[:], R=[anb])
            k.barrier()

        for l in range(depth):
            with ExitStack() as es:
                gcol = es.enter_context(sbt(nc, "p1_g", [128, 16], F32))
                gb = Buf()
                k.dma("sp", gcol[:], norm_g[l].rearrange("(kc p) -> p kc", p=128), W=[gb], slow=True)
                hT = es.enter_context(sbt(nc, "p1_hT", [128, 16, SG], BF16))
                hb = Buf()
                xs = Ring(es, nc, "p1_xs", [128, SG], F32, 2)
                sqr = Ring(es, nc, "p1_sq", [128, SG], F32, 2)
                wr = Ring(es, nc, "p1_w", [128, 16, 512], F32, 3)
                wcr = Ring(es, nc, "p1_wc", [128, 16, 512], BF16, 2)
                wcn = [0]
                stg = Ring(es, nc, "p1_st", [128, 512], F32, 4)
                rbc = es.enter_context(sbt(nc, "p1_rbc", [128, SG], F32))
                rbcb = Buf()
                rtm = es.enter_context(sbt(nc, "p1_rtm", [128, SG // 128], F32))
                rtmb = Buf()
                ps = Ring(es, nc, "p1_ps", [128, 512], F32, 4, psum=True)
                pbc = Ring(es, nc, "p1_pbc", [128, 512], F32, SG // G, psum=True)
                ptm = es.enter_context(pst(nc, "p1_ptm", [128, 512], F32))
                ptmb = Buf()
                def p1_loadw(cc0, nb):
                    wf, wfb = wr.next()
                    k.dma("sp", wf[:, :, :nb], w_in[l].rearrange("(kc p) n -> p kc n", p=128)[:, :, cc0:cc0 + nb], W=[wfb])
                    wc, wcb = wcr.next()
                    wcn[0] += 1
                    for hf in range(2):
                        if (wcn[0] + hf) % 2:
                            k.op("act", lambda: nc.scalar.copy(out=wc[:, hf * 8:(hf + 1) * 8, :nb], in_=wf[:, hf * 8:(hf + 1) * 8, :nb]), R=[wfb], W=[wcb])
                        else:
                            k.op("dve", lambda: nc.vector.tensor_copy(out=wc[:, hf * 8:(hf + 1) * 8, :nb], in_=wf[:, hf * 8:(hf + 1) * 8, :nb]), R=[wfb], W=[wcb])
                    return wc, wcb

                for sg in range(NSG):
                    c0 = sg * SG
                    pbcs = [pbc.next() for _ in range(SG // G)]
                    for kc in range(16):
                        xt, xb = xs.next()
                        k.dma("sp", xt[:], xT[kc * 128:(kc + 1) * 128, c0:c0 + SG], W=[xb])
                        sq, sqb = sqr.next()
                        k.op("act", lambda: nc.scalar.activation(out=sq[:], in_=xt[:], func=AF.Square), R=[xb], W=[sqb])
                        k.op("dve", lambda: nc.vector.tensor_scalar(out=hT[:, kc, :], in0=xt[:], scalar1=gcol[:, kc:kc + 1], scalar2=None, op0=ALU.mult),
                             R=[xb, gb], W=[hb])
                        for s in range(SG // G):
                            pp, ppb = pbcs[s]
                            k.op("pe", lambda: nc.tensor.matmul(pp[:, :G], lhsT=ones, rhs=sq[:, s * G:(s + 1) * G], start=(kc == 0), stop=(kc == 15)),
                                 R=[sqb, cstb], W=[ppb])
                    for s in range(SG // G):
                        pp, ppb = pbcs[s]
                        rstd(rbc[:, s * G:(s + 1) * G], pp[:, :G], 1.0 / D, C_EPS, [ppb], [rbcb])
                    for t in range(SG // 128):
                        k.op("pe", lambda: nc.tensor.matmul(ptm[:, t:t + 1], lhsT=rbc[:, t * 128:(t + 1) * 128], rhs=cst[:, C_R128:C_R128 + 1], start=True, stop=True),
                             R=[rbcb, cstb], W=[ptmb])
                    k.op("dve", lambda: nc.vector.tensor_copy(out=rtm[:], in_=ptm[:, :SG // 128]), R=[ptmb], W=[rtmb])
                    fmrow = 0
                    for (cc0, nb) in fm_blocks:
                        wt, wb = p1_loadw(cc0, nb)
                        for j in range(nb // 128):
                            for s in range(SG // G):
                                p, pb = ps.next()
                                for kc in range(16):
                                    k.op("pe", lambda: nc.tensor.matmul(p[:, :G], lhsT=wt[:, kc, j * 128:(j + 1) * 128], rhs=hT[:, kc, s * G:(s + 1) * G],
                                                                        start=(kc == 0), stop=(kc == 15)), R=[wb, hb], W=[pb])
                                st, sb = stg.next()
                                k.op("dve", lambda: nc.vector.tensor_tensor(out=st[:, :G], in0=p[:, :G], in1=rbc[:, s * G:(s + 1) * G], op=ALU.mult),
                                     R=[pb, rbcb], W=[sb])
                                k.dma("pool", pFM[fmrow * 128:(fmrow + 1) * 128, c0 + s * G:c0 + (s + 1) * G], st[:, :G], R=[sb])
                            fmrow += 1
                    tmcol = 0
                    for (cc0, nb) in tm_blocks:
                        wt, wb = p1_loadw(cc0, nb)
                        for t in range(SG // 128):
                            p, pb = ps.next()
                            for kc in range(16):
                                k.op("pe", lambda: nc.tensor.matmul(p[:, :nb], lhsT=hT[:, kc, t * 128:(t + 1) * 128], rhs=wt[:, kc, :nb],
                                                                    start=(kc == 0), stop=(kc == 15)), R=[wb, hb], W=[pb])
                            st, sb = stg.next()
                            k.op("act", lambda: nc.scalar.activation(out=st[:, :nb], in_=p[:, :nb], func=AF.Copy, scale=rtm[:, t:t + 1]),
                                 R=[pb, rtmb], W=[sb])
                            k.dma("pool", pTM[c0 + t * 128:c0 + (t + 1) * 128, tmcol:tmcol + nb], st[:, :nb], R=[sb])
                        tmcol += nb
                k.barrier()


            with ExitStack() as es:
                SB = lambda name, shape: es.enter_context(sbt(nc, name, shape, F32))
                PS = lambda name: es.enter_context(pst(nc, name, [128, 512], F32))
                tri = cst[:, C_TRI:C_TRI + 128]
                cw = SB("m_cw", [128, 4, 8]); cb = SB("m_cb", [128, 8]); ibfb = SB("m_ib", [128, 8]); gbc = SB("m_g", [128, 512])
                pb_ = Buf()
                for j in range(4):
                    k.dma("sp", cw[:, j, :], ml_conv_w[l, j].rearrange("(c p) -> p c", p=128), W=[pb_], slow=True)
                k.dma("sp", cb[:], ml_conv_b[l].rearrange("(c p) -> p c", p=128), W=[pb_], slow=True)
                k.dma("sp", ibfb[:, 0:4], ml_i_bias[l].partition_broadcast(128), W=[pb_])
                k.dma("sp", ibfb[:, 4:8], ml_f_bias[l].partition_broadcast(128), W=[pb_])
                k.dma("sp", gbc[:], ml_norm_g[l].partition_broadcast(128), W=[pb_])
                CT = [SB("m_CT%d" % h, [128, 129]) for h in range(4)]
                CTb = [Buf() for h in range(4)]
                for h in range(4):
                    k.op("pool", lambda: nc.gpsimd.memset(CT[h][:], 0.0), W=[CTb[h]])
                qkr = Ring(es, nc, "m_qkr", [128, 8, 131], F32, 2)
                accr = Ring(es, nc, "m_acc", [128, 8, 128], F32, 2)
                qkt = Ring(es, nc, "m_qk", [128, 8, 128], F32, 2)
                vr = Ring(es, nc, "m_v", [128, 4, 129], F32, 2)
                for (vt, vb) in vr.items:
                    k.op("pool", lambda: nc.gpsimd.memset(vt[:, :, 128:129], 1.0), W=[vb])
                gtr = Ring(es, nc, "m_gt", [128, 8], F32, 2)
                Gr = Ring(es, nc, "m_G", [128, 32], F32, 2)
                ozr = Ring(es, nc, "m_oz", [128, 1024], F32, 2)
                rhr = Ring(es, nc, "m_rh", [128, 128], F32, 2)
                Er = Ring(es, nc, "m_E", [128, 128], F32, 2)
                STr = Ring(es, nc, "m_ST", [128, 128], F32, 2)
                n1r = Ring(es, nc, "m_n1", [128, 129], F32, 2)
                n2r = Ring(es, nc, "m_n2", [128, 129], F32, 2)
                smr = Ring(es, nc, "m_sm", [128, 16], F32, 2)
                hhr = Ring(es, nc, "m_hh", [128, 128], F32, 2)
                kwr = Ring(es, nc, "m_kw", [128, 128], F32, 2)
                yr = Ring(es, nc, "m_y", [128, 512], F32, 2)
                ggr = Ring(es, nc, "m_gg", [128, 1024], F32, 2)
                mtr = Ring(es, nc, "m_mt", [128, 4, 128], BF16, 2)
                pgA = PS("m_pgA"); pgB = PS("m_pgB"); pBbc = PS("m_pB"); pQK = PS("m_pQK")
                pN1 = PS("m_pN1"); pN2 = PS("m_pN2"); pKT = PS("m_pKT"); pdC = PS("m_pdC")
                pgAb, pgBb, pBbcb, pQKb, pN1b, pN2b, pKTb, pdCb = [Buf() for _ in range(8)]
                NSD = nc.vector.BN_STATS_DIM
                for c in range(NT):
                    t0 = c * 128
                    X, Xb = qkr.next()
                    src = pFM.rearrange("(ch p) t -> p ch t", p=128)
                    if c == 0:
                        k.op("pool", lambda: nc.gpsimd.memset(X[:, :, 0:3], 0.0), W=[Xb])
                        k.dma("sp", X[:, :, 3:131], src[:, 0:8, 0:128], W=[Xb])
                    else:
                        k.dma("sp", X[:], src[:, 0:8, t0 - 3:t0 + 128], W=[Xb])
                    vt, vb = vr.next()
                    k.dma("sp", vt[:, :, 0:128], pTM[t0:t0 + 128, TM_V:TM_V + 512].rearrange("p (h d) -> p h d", h=4), W=[vb])
                    gt, gtb = gtr.next()
                    k.dma("sp", gt[:], pTM[t0:t0 + 128, TM_I:TM_I + 8], W=[gtb])
                    oz, ozb = ozr.next()
                    k.dma("sp", oz[:], pTM[t0:t0 + 128, TM_O:TM_O + 1024], W=[ozb])
                    acc, accb = accr.next()
                    qk, qkb = qkt.next()
                    for ch in range(8):
                        eng, E_ = ("dve", nc.vector)
                        k.op(eng, lambda: E_.tensor_scalar(out=acc[:, ch, :], in0=X[:, ch, 0:128], scalar1=cw[:, 0, ch:ch + 1], scalar2=cb[:, ch:ch + 1], op0=ALU.mult, op1=ALU.add),
                             R=[Xb, pb_], W=[accb])
                        for j in range(1, 4):
                            k.op(eng, lambda: E_.scalar_tensor_tensor(out=acc[:, ch, :], in0=X[:, ch, j:j + 128], scalar=cw[:, j, ch:ch + 1], in1=acc[:, ch, :], op0=ALU.mult, op1=ALU.add),
                                 R=[Xb, pb_, accb], W=[accb])
                    k.op("act", lambda: nc.scalar.activation(out=qk[:], in_=acc[:], func=AF.Silu), R=[accb], W=[qkb])
                    k.op("pool", lambda: nc.gpsimd.tensor_scalar(out=qk[:, 0:4, :], in0=qk[:, 0:4, :], scalar1=float(128 ** -0.5), scalar2=None, op0=ALU.mult), R=[qkb], W=[qkb])
                    Gt, Gb = Gr.next()
                    k.op("dve", lambda: nc.vector.tensor_tensor(out=Gt[:, 0:8], in0=gt[:], in1=ibfb[:], op=ALU.add), R=[gtb, pb_], W=[Gb])
                    k.op("act", lambda: nc.scalar.activation(out=Gt[:, 8:12], in_=Gt[:, 4:8], func=AF.Exp, scale=-1.0), R=[Gb], W=[Gb])
                    k.op("act", lambda: nc.scalar.activation(out=Gt[:, 12:16], in_=Gt[:, 8:12], func=AF.Ln, bias=cst[:, C_1:C_1 + 1]), R=[Gb, cstb], W=[Gb])
                    k.op("dve", lambda: nc.vector.tensor_scalar(out=Gt[:, 12:16], in0=Gt[:, 12:16], scalar1=-1.0, scalar2=None, op0=ALU.mult), R=[Gb], W=[Gb])
                    k.op("pe", lambda: nc.tensor.matmul(pgA[:, 0:4], lhsT=tri, rhs=Gt[:, 12:16], start=True, stop=True), R=[Gb, cstb], W=[pgAb])
                    k.op("pe", lambda: nc.tensor.matmul(pgB[:, 0:4], lhsT=ones, rhs=Gt[:, 12:16], start=True, stop=True), R=[Gb, cstb], W=[pgBb])
                    k.op("dve", lambda: nc.vector.tensor_tensor(out=Gt[:, 16:20], in0=Gt[:, 0:4], in1=pgA[:, 0:4], op=ALU.subtract), R=[Gb, pgAb], W=[Gb])
                    k.op("dve", lambda: nc.vector.tensor_tensor(out=Gt[:, 20:24], in0=Gt[:, 16:20], in1=pgB[:, 0:4], op=ALU.add), R=[Gb, pgBb], W=[Gb])
                    k.op("act", lambda: nc.scalar.activation(out=Gt[:, 20:24], in_=Gt[:, 20:24], func=AF.Exp), R=[Gb], W=[Gb])
                    k.op("act", lambda: nc.scalar.activation(out=Gt[:, 24:28], in_=pgB[:, 0:4], func=AF.Exp), R=[pgBb], W=[Gb])
                    k.op("act", lambda: nc.scalar.activation(out=Gt[:, 28:32], in_=pgA[:, 0:4], func=AF.Exp), R=[pgAb], W=[Gb])
                    y, yb = yr.next()
                    for h in range(4):
                        rh, rhb = rhr.next()
                        k.op("pool", lambda: nc.gpsimd.tensor_scalar(out=rh[:], in0=tri, scalar1=Gt[:, 12 + h:13 + h], scalar2=None, op0=ALU.mult), R=[Gb, cstb], W=[rhb])
                        k.op("pe", lambda: nc.tensor.matmul(pBbc[:, 0:128], lhsT=ones, rhs=rh[:], start=True, stop=True), R=[rhb, cstb], W=[pBbcb])
                        Et, Eb = Er.next()
                        k.op("act", lambda: nc.scalar.activation(out=Et[:], in_=pBbc[:, 0:128], func=AF.Exp, bias=Gt[:, 16 + h:17 + h]), R=[pBbcb, Gb], W=[Eb])
                        k.op("pool", lambda: nc.gpsimd.tensor_tensor(out=Et[:], in0=Et[:], in1=tri, op=ALU.mult), R=[Eb, cstb], W=[Eb])
                        k.op("pe", lambda: nc.tensor.matmul(pQK[:, 0:128], lhsT=qk[:, 4 + h, :], rhs=qk[:, h, :], start=True, stop=True), R=[qkb], W=[pQKb])
                        ST, STb = STr.next()
                        k.op("dve", lambda: nc.vector.tensor_tensor(out=ST[:], in0=pQK[:, 0:128], in1=Et[:], op=ALU.mult), R=[pQKb, Eb], W=[STb])
                        k.op("pe", lambda: nc.tensor.matmul(pN1[:, 0:129], lhsT=ST[:], rhs=vt[:, h, :], start=True, stop=True), R=[STb, vb], W=[pN1b])
                        k.op("pe", lambda: nc.tensor.matmul(pN2[:, 0:129], lhsT=qk[:, h, :], rhs=CT[h][:], start=True, stop=True), R=[qkb, CTb[h]], W=[pN2b])
                        n1, n1b = n1r.next()
                        k.op("act", lambda: nc.scalar.activation(out=n1[:], in_=pN2[:, 0:129], func=AF.Copy, scale=Gt[:, 28 + h:29 + h]), R=[pN2b, Gb], W=[n1b])
                        n2, n2b = n2r.next()
                        k.op("dve", lambda: nc.vector.tensor_tensor(out=n2[:], in0=pN1[:, 0:129], in1=n1[:], op=ALU.add), R=[pN1b, n1b], W=[n2b])
                        sm, smb = smr.next()
                        k.op("act", lambda: nc.scalar.activation(out=sm[:, 0:1], in_=n2[:, 128:129], func=AF.Abs), R=[n2b], W=[smb])
                        k.op("dve", lambda: nc.vector.tensor_scalar_max(out=sm[:, 0:1], in0=sm[:, 0:1], scalar1=1.0), R=[smb], W=[smb])
                        k.op("dve", lambda: nc.vector.reciprocal(out=sm[:, 0:1], in_=sm[:, 0:1]), R=[smb], W=[smb])
                        hh, hhb = hhr.next()
                        k.op("dve", lambda: nc.vector.tensor_scalar(out=hh[:], in0=n2[:, 0:128], scalar1=sm[:, 0:1], scalar2=None, op0=ALU.mult), R=[n2b, smb], W=[hhb])
                        k.op("dve", lambda: nc.vector.bn_stats(out=sm[:, 2:2 + NSD], in_=hh[:]), R=[hhb], W=[smb])
                        k.op("dve", lambda: nc.vector.bn_aggr(out=sm[:, 10:12], in_=sm[:, 2:2 + NSD]), R=[smb], W=[smb])
                        rstd(sm[:, 12:13], sm[:, 11:12], 1.0, C_EPS, [smb], [smb])
                        k.op("dve", lambda: nc.vector.tensor_scalar(out=y[:, h * 128:(h + 1) * 128], in0=hh[:], scalar1=sm[:, 10:11], scalar2=sm[:, 12:13], op0=ALU.subtract, op1=ALU.mult),
                             R=[hhb, smb], W=[yb])
                        k.op("pe", lambda: nc.tensor.transpose(pKT[:, 0:128], qk[:, 4 + h, :], ident), R=[qkb, cstb], W=[pKTb])
                        kw, kwb = kwr.next()
                        k.op("act", lambda: nc.scalar.activation(out=kw[:], in_=pKT[:, 0:128], func=AF.Copy, scale=Gt[:, 20 + h:21 + h]), R=[pKTb, Gb], W=[kwb])
                        k.op("pe", lambda: nc.tensor.matmul(pdC[:, 0:129], lhsT=kw[:], rhs=vt[:, h, :], start=True, stop=True), R=[kwb, vb], W=[pdCb])
                        k.op("dve", lambda: nc.vector.scalar_tensor_tensor(out=CT[h][:], in0=CT[h][:], scalar=Gt[:, 24 + h:25 + h], in1=pdC[:, 0:129], op0=ALU.mult, op1=ALU.add),
                             R=[CTb[h], Gb, pdCb], W=[CTb[h]])
                    gg, ggb = ggr.next()
                    k.op("act", lambda: nc.scalar.activation(out=gg[:, 0:512], in_=oz[:, 0:512], func=AF.Sigmoid), R=[ozb], W=[ggb])
                    k.op("act", lambda: nc.scalar.activation(out=gg[:, 512:1024], in_=oz[:, 512:1024], func=AF.Silu), R=[ozb], W=[ggb])
                    k.op("pool", lambda: nc.gpsimd.tensor_tensor(out=gg[:, 0:512], in0=gg[:, 0:512], in1=gg[:, 512:1024], op=ALU.mult), R=[ggb], W=[ggb])
                    k.op("pool", lambda: nc.gpsimd.tensor_tensor(out=gg[:, 0:512], in0=gg[:, 0:512], in1=gbc[:], op=ALU.mult), R=[ggb, pb_], W=[ggb])
                    k.op("dve", lambda: nc.vector.tensor_tensor(out=y[:], in0=y[:], in1=gg[:, 0:512], op=ALU.mult), R=[yb, ggb], W=[yb])
                    mt, mtb = mtr.next()
                    for j in range(4):
                        k.op("pe", lambda: nc.tensor.transpose(pKT[:, 128 + j * 64:128 + j * 64 + 64] if False else pKT[:, 0:128], y[:, j * 128:(j + 1) * 128], ident), R=[yb, cstb], W=[pKTb])
                        k.op("act", lambda: nc.scalar.copy(out=mt[:, j, :], in_=pKT[:, 0:128]), R=[pKTb], W=[mtb])
                    k.dma("pool", mixT.rearrange("(cc p) t -> p cc t", p=128)[:, 0:4, t0:t0 + 128], mt[:], R=[mtb])
                k.barrier()

            with ExitStack() as es:
                SB = lambda name, shape: es.enter_context(sbt(nc, name, shape, F32))
                PS = lambda name: es.enter_context(pst(nc, name, [128, 512], F32))
                tri = cst[:, C_TRI:C_TRI + 128]
                wuq = SB("a_wuq", [128, 4, 1536]); wukv = SB("a_wukv", [128, 2, 2048]); gq = SB("a_gq", [128, 6])
                wb_ = Buf()
                k.dma("sp", wuq[:], mla_w_uq[l].rearrange("(c p) n -> p c n", p=128), W=[wb_])
                k.dma("sp", wukv[:], mla_w_ukv[l].rearrange("(c p) n -> p c n", p=128), W=[wb_])
                k.dma("sp", gq[:, 0:4], mla_q_norm_g[l].rearrange("(c p) -> p c", p=128), W=[wb_], slow=True)
                k.dma("sp", gq[:, 4:6], mla_kv_norm_g[l].rearrange("(c p) -> p c", p=128), W=[wb_], slow=True)
                xcr = Ring(es, nc, "a_xc", [128, 6, 128], F32, 2)
                sqr = Ring(es, nc, "a_sq", [128, 6, 128], F32, 2)
                rbr = Ring(es, nc, "a_rb", [128, 2, 128], F32, 2)
                cnr = Ring(es, nc, "a_cn", [128, 6, 128], F32, 2)
                csr = Ring(es, nc, "a_cs", [128, 64], F32, 2)
                kxr = Ring(es, nc, "a_kx", [128, 64], F32, 2)
                stq = Ring(es, nc, "a_stq", [128, 4, 128], BF16, 4)
                tmr = Ring(es, nc, "a_tm", [128, 8, 32], F32, 4)
                qrr = Ring(es, nc, "a_qr", [128, 8, 64], F32, 2)
                krr = Ring(es, nc, "a_kr", [128, 64], F32, 2)
                kst = Ring(es, nc, "a_kst", [64, 128], BF16, 2)
                var_ = Ring(es, nc, "a_va", [128, 8, 129], BF16, 2)
                for (vt, vb) in var_.items:
                    k.op("pool", lambda: nc.gpsimd.memset(vt[:, :, 128:129], 1.0), W=[vb])
                pS1 = PS("a_pS1"); pS2 = PS("a_pS2"); pS1b = Buf(); pS2b = Buf()
                pq = Ring(es, nc, "a_pq", [128, 512], F32, 4, psum=True)
                qn_v = qnT.rearrange("(h p) t -> p h t", p=128)
                kn_v = knT.rearrange("(h p) t -> p h t", p=128)
                qr_v = qrT.rearrange("(b p) t -> p b t", p=128)
                for c in range(NT):
                    t0 = c * 128
                    xc, xcb = xcr.next()
                    k.dma("sp", xc[:], pFM.rearrange("(ch p) t -> p ch t", p=128)[:, FM_CQ:FM_CQ + 6, t0:t0 + 128], W=[xcb])
                    cs_, csb = csr.next()
                    k.dma("sp", cs_[:], csd[t0:t0 + 128, :], W=[csb])
                    kx, kxb = kxr.next()
                    k.dma("sp", kx[:], pTM[t0:t0 + 128, TM_KR:TM_KR + 64], W=[kxb])
                    sq, sqb = sqr.next()
                    k.op("act", lambda: nc.scalar.activation(out=sq[:], in_=xc[:], func=AF.Square), R=[xcb], W=[sqb])
                    for ch in range(4):
                        k.op("pe", lambda: nc.tensor.matmul(pS1[:, 0:128], lhsT=ones, rhs=sq[:, ch, :], start=(ch == 0), stop=(ch == 3)), R=[sqb, cstb], W=[pS1b])
                    for ch in range(2):
                        k.op("pe", lambda: nc.tensor.matmul(pS2[:, 0:128], lhsT=ones, rhs=sq[:, 4 + ch, :], start=(ch == 0), stop=(ch == 1)), R=[sqb, cstb], W=[pS2b])
                    rb, rbb = rbr.next()
                    rstd(rb[:, 0, :], pS1[:, 0:128], 1.0 / 512, C_EPS, [pS1b], [rbb])
                    rstd(rb[:, 1, :], pS2[:, 0:128], 1.0 / 256, C_EPS, [pS2b], [rbb])
                    cn, cnb = cnr.next()
                    for ch in range(6):
                        k.op("dve", lambda: nc.vector.scalar_tensor_tensor(out=cn[:, ch, :], in0=xc[:, ch, :], scalar=gq[:, ch:ch + 1], in1=rb[:, 0 if ch < 4 else 1, :], op0=ALU.mult, op1=ALU.mult),
                             R=[xcb, wb_, rbb], W=[cnb])
                    for (W_, nch, c0_, hs, dst) in ((wuq, 4, 0, 192, qn_v), (wukv, 2, 4, 256, kn_v)):
                        for b in range(2):
                            p, pb = pq.next()
                            for hh_ in range(4):
                                h = b * 4 + hh_
                                for ch in range(nch):
                                    k.op("pe", lambda: nc.tensor.matmul(p[:, hh_ * 128:(hh_ + 1) * 128], lhsT=W_[:, ch, h * hs:h * hs + 128], rhs=cn[:, c0_ + ch, :],
                                                                        start=(ch == 0), stop=(ch == nch - 1)), R=[wb_, cnb], W=[pb])
                            st, sb = stq.next()
                            k.op("act", lambda: nc.scalar.copy(out=st[:], in_=p[:].rearrange("p (a b) -> p a b", a=4)), R=[pb], W=[sb])
                            k.dma("pool", dst[:, b * 4:(b + 1) * 4, t0:t0 + 128], st[:], R=[sb])
                    va, vab = var_.next()
                    for b in range(2):
                        p, pb = pq.next()
                        for ch in range(2):
                            k.op("pe", lambda: nc.tensor.matmul(p[:], lhsT=cn[:, 4 + ch, :], rhs=wukv[:, ch, :].rearrange("p (h d) -> p h d", d=256)[:, b * 4:(b + 1) * 4, 128:256],
                                                                start=(ch == 0), stop=(ch == 1)), R=[wb_, cnb], W=[pb])
                        k.op("act", lambda: nc.scalar.copy(out=va[:, b * 4:(b + 1) * 4, 0:128], in_=p[:].rearrange("p (a b) -> p a b", a=4)), R=[pb], W=[vab])
                    k.dma("pool", vaug.rearrange("h t d -> t h d")[t0:t0 + 128], va[:], R=[vab])
                    p, pb = pq.next()
                    for ch in range(4):
                        k.op("pe", lambda: nc.tensor.matmul(p[:], lhsT=cn[:, ch, :], rhs=wuq[:, ch, :].rearrange("p (h d) -> p h d", d=192)[:, :, 128:192],
                                                            start=(ch == 0), stop=(ch == 3)), R=[wb_, cnb], W=[pb])
                    pv = p[:].rearrange("p (h d) -> p h d", d=64)
                    sin8 = cs_[:, 0:32].unsqueeze(1).to_broadcast([128, 8, 32])
                    cos8 = cs_[:, 32:64].unsqueeze(1).to_broadcast([128, 8, 32])
                    qr, qrb = qrr.next()
                    t1, t1b = tmr.next(); t2, t2b = tmr.next(); t3, t3b = tmr.next(); t4, t4b = tmr.next()
                    k.op("dve", lambda: nc.vector.tensor_tensor(out=t1[:], in0=pv[:, :, 0:32], in1=cos8, op=ALU.mult), R=[pb, csb], W=[t1b])
                    k.op("dve", lambda: nc.vector.tensor_tensor(out=t2[:], in0=pv[:, :, 32:64], in1=sin8, op=ALU.mult), R=[pb, csb], W=[t2b])
                    k.op("dve", lambda: nc.vector.tensor_tensor(out=t3[:], in0=pv[:, :, 32:64], in1=cos8, op=ALU.mult), R=[pb, csb], W=[t3b])
                    k.op("dve", lambda: nc.vector.tensor_tensor(out=t4[:], in0=pv[:, :, 0:32], in1=sin8, op=ALU.mult), R=[pb, csb], W=[t4b])
                    k.op("pool", lambda: nc.gpsimd.tensor_tensor(out=qr[:, :, 0:32], in0=t1[:], in1=t2[:], op=ALU.subtract), R=[t1b, t2b], W=[qrb])
                    k.op("pool", lambda: nc.gpsimd.tensor_tensor(out=qr[:, :, 32:64], in0=t3[:], in1=t4[:], op=ALU.add), R=[t3b, t4b], W=[qrb])
                    p, pb = pq.next()
                    for b in range(4):
                        k.op("pe", lambda: nc.tensor.transpose(p[:, b * 128:(b + 1) * 128], qr[:, 2 * b:2 * b + 2, :].rearrange("p a d -> p (a d)"), ident), R=[qrb, cstb], W=[pb])
                    st, sb = stq.next()
                    k.op("act", lambda: nc.scalar.copy(out=st[:], in_=p[:].rearrange("p (a b) -> p a b", a=4)), R=[pb], W=[sb])
                    k.dma("pool", qr_v[:, :, t0:t0 + 128], st[:], R=[sb])
                    kr_, krb = krr.next()
                    t1, t1b = tmr.next(); t2, t2b = tmr.next()
                    k.op("dve", lambda: nc.vector.tensor_tensor(out=t1[:, 0, :], in0=kx[:, 0:32], in1=cs_[:, 32:64], op=ALU.mult), R=[kxb, csb], W=[t1b])
                    k.op("dve", lambda: nc.vector.tensor_tensor(out=t1[:, 1, :], in0=kx[:, 32:64], in1=cs_[:, 0:32], op=ALU.mult), R=[kxb, csb], W=[t1b])
                    k.op("dve", lambda: nc.vector.tensor_tensor(out=t2[:, 0, :], in0=kx[:, 32:64], in1=cs_[:, 32:64], op=ALU.mult), R=[kxb, csb], W=[t2b])
                    k.op("dve", lambda: nc.vector.tensor_tensor(out=t2[:, 1, :], in0=kx[:, 0:32], in1=cs_[:, 0:32], op=ALU.mult), R=[kxb, csb], W=[t2b])
                    k.op("pool", lambda: nc.gpsimd.tensor_tensor(out=kr_[:, 0:32], in0=t1[:, 0, :], in1=t1[:, 1, :], op=ALU.subtract), R=[t1b], W=[krb])
                    k.op("pool", lambda: nc.gpsimd.tensor_tensor(out=kr_[:, 32:64], in0=t2[:, 0, :], in1=t2[:, 1, :], op=ALU.add), R=[t2b], W=[krb])
                    p, pb = pq.next()
                    k.op("pe", lambda: nc.tensor.transpose(p[0:64, 0:128], kr_[:], ident), R=[krb, cstb], W=[pb])
                    ks, ksb = kst.next()
                    k.op("act", lambda: nc.scalar.copy(out=ks[:], in_=p[0:64, 0:128]), R=[pb], W=[ksb])
                    k.dma("pool", krT[:, t0:t0 + 128], ks[:], R=[ksb])
                k.barrier()

            with ExitStack() as es:
                SB = lambda name, shape: es.enter_context(sbt(nc, name, shape, F32))
                tri = cst[:, C_TRI:C_TRI + 128]
                SBh = lambda name, shape: es.enter_context(sbt(nc, name, shape, BF16))
                krt = SBh("b_krt", [64, T]); krtb = Buf()
                k.dma("sp", krt[:], krT, W=[krtb])
                knR = Ring(es, nc, "b_kn", [128, T], BF16, 2); qnR = Ring(es, nc, "b_qn", [128, T], BF16, 2); qrR = Ring(es, nc, "b_qr", [64, T], BF16, 2)
                vaR = Ring(es, nc, "b_va", [128, NT, 129], BF16, 2); zR = Ring(es, nc, "b_z", [128, NT, 128], F32, 2)
                trib = SBh("b_tri", [128, 128]); tribb = Buf()
                k.op("dve", lambda: nc.vector.tensor_copy(out=trib[:], in_=tri), R=[cstb], W=[tribb])
                HD = {}
                Ptr = Ring(es, nc, "b_P", [128, 512], BF16, 3)
                smr = Ring(es, nc, "b_sm", [128, 2], F32, 2)
                ytr = Ring(es, nc, "b_y", [128, 128], F32, 2)
                szr = Ring(es, nc, "b_sz", [128, 128], F32, 2)
                sty = Ring(es, nc, "b_sty", [128, 128], BF16, 3)
                pST = Ring(es, nc, "b_pST", [128, 512], F32, 3, psum=True)
                pO = Ring(es, nc, "b_pO", [128, 512], F32, 2, psum=True)
                pX = Ring(es, nc, "b_pX", [128, 512], F32, 2, psum=True)
                SCL = float(192 ** -0.5)
                hcur = [0]

                def epilogue(po, pob_, qt, qs):
                    h = hcur[0]
                    sm, smb = smr.next()
                    k.op("dve", lambda: nc.vector.reciprocal(out=sm[:, 0:1], in_=po[:, 128:129]), R=[pob_], W=[smb])
                    yt, ytb = ytr.next()
                    k.op("dve", lambda: nc.vector.tensor_scalar(out=yt[:], in0=po[:, 0:128], scalar1=sm[:, 0:1], scalar2=None, op0=ALU.mult), R=[pob_, smb], W=[ytb])
                    sz, szb = szr.next()
                    k.op("act", lambda: nc.scalar.activation(out=sz[:], in_=HD["zh"][:, qt, :], func=AF.Silu), R=[HD["zhb"]], W=[szb])
                    k.op("pool", lambda: nc.gpsimd.tensor_tensor(out=yt[:], in0=yt[:], in1=sz[:], op=ALU.mult), R=[ytb, szb], W=[ytb])
                    px, pxb = pX.next()
                    k.op("pe", lambda: nc.tensor.transpose(px[:, 0:128], yt[:], ident), R=[ytb, cstb], W=[pxb])
                    st, sb = sty.next()
                    k.op("dve", lambda: nc.vector.tensor_copy(out=st[:], in_=px[:, 0:128]), R=[pxb], W=[sb])
                    k.dma("pool", mixT[512 + h * 128:512 + (h + 1) * 128, qs], st[:], R=[sb])

                for h in range(8):
                    hcur[0] = h
                    (knh, knb), (qnh, qnb), (qrh, qrb), (vah, vahb), (zh, zhb) = knR.next(), qnR.next(), qrR.next(), vaR.next(), zR.next()
                    HD["zh"], HD["zhb"] = zh, zhb
                    k.dma("sp", knh[:], knT[h * 128:(h + 1) * 128, :], W=[knb])
                    k.dma("sp", qnh[:], qnT[h * 128:(h + 1) * 128, :], W=[qnb])
                    k.dma("sp", qrh[:], qrT[h * 64:(h + 1) * 64, :], W=[qrb])
                    k.dma("sp", vah[:], vaug[h].rearrange("(n p) d -> p n d", p=128), W=[vahb])
                    k.dma("sp", zh[:], pTM[:, TM_MZ + h * 128:TM_MZ + (h + 1) * 128].rearrange("(n p) d -> p n d", p=128), W=[zhb])
                    pend = [None]

                    def flush_pv():
                        if pend[0] is None:
                            return
                        (po_, pob2, Pt_, Ptb2, kb_, nk_, qt_, fin) = pend[0]
                        pend[0] = None
                        for i in range(nk_):
                            j = kb_ + i
                            k.op("pe", lambda: nc.tensor.matmul(po_[:, 0:129], lhsT=Pt_[:, i * 128:(i + 1) * 128], rhs=vah[:, j, :], start=(j == 0), stop=(j == qt_)), R=[Ptb2, vahb], W=[pob2])
                        if fin is not None:
                            fin()

                    for qt in range(NT):
                        qs = slice(qt * 128, (qt + 1) * 128)
                        po, pob_ = pO.next()
                        for kb in range(0, qt + 1, 4):
                            nk = min(4, qt + 1 - kb)
                            ps_, psb = pST.next()
                            for i in range(nk):
                                j = kb + i
                                js = slice(j * 128, (j + 1) * 128)
                                k.op("pe", lambda: nc.tensor.matmul(ps_[:, i * 128:(i + 1) * 128], lhsT=knh[:, js], rhs=qnh[:, qs], start=True, stop=False), R=[knb, qnb], W=[psb])
                                k.op("pe", lambda: nc.tensor.matmul(ps_[:, i * 128:(i + 1) * 128], lhsT=krt[:, js], rhs=qrh[:, qs], start=False, stop=True), R=[krtb, qrb], W=[psb])
                            flush_pv()
                            Pt, Ptb = Ptr.next()
                            k.op("act", lambda: nc.scalar.activation(out=Pt[:, 0:nk * 128], in_=ps_[:, 0:nk * 128], func=AF.Exp, scale=SCL), R=[psb], W=[Ptb])
                            last = (kb + nk - 1 == qt)
                            if last:
                                i = nk - 1
                                k.op("pool", lambda: nc.gpsimd.tensor_tensor(out=Pt[:, i * 128:(i + 1) * 128], in0=Pt[:, i * 128:(i + 1) * 128], in1=trib[:], op=ALU.mult), R=[Ptb, tribb], W=[Ptb])
                            pend[0] = (po, pob_, Pt, Ptb, kb, nk, qt, (lambda po=po, pob_=pob_, qt=qt, qs=qs: epilogue(po, pob_, qt, qs)) if last else None)
                    flush_pv()

                k.barrier()

            NCH = T // 64
            with ExitStack() as es:
                SB = lambda name, shape: es.enter_context(sbt(nc, name, shape, F32))
                mu = SB("r_mu", [128, 13]); prm = SB("r_prm", [128, 6, 4]); w0bc = SB("r_w0", [128, 512])
                w2t = SB("r_w2", [64, 512]); a2t = SB("r_a2", [128, 512])
                v1t = SB("r_v1", [128, 4, 32]); v2t = SB("r_v2", [32, 512])
                prb = Buf()
                k.dma("sp", mu[:], rw_mu[l].rearrange("(c p) -> p c", p=128), W=[prb], slow=True)
                plist = [rw_a0[l], rw_k_k[l], rw_k_a[l], rw_r_k[l].rearrange("h d -> (h d)")]
                if l > 0:
                    plist.append(rw_v0[l - 1])
                for i_, src in enumerate(plist):
                    k.dma("sp", prm[:, i_, :], src.rearrange("(c p) -> p c", p=128), W=[prb], slow=True)
                k.dma("sp", w0bc[:], rw_w0[l].partition_broadcast(128), W=[prb])
                k.dma("sp", w2t[:], rw_w2[l], W=[prb])
                k.dma("sp", a2t[64:128, :], rw_a2[l], W=[prb])
                if l > 0:
                    k.dma("sp", v1t[:], rw_v1[l - 1].rearrange("(c p) n -> p c n", p=128), W=[prb])
                    k.dma("sp", v2t[:], rw_v2[l - 1], W=[prb])
                k.op("dve", lambda: nc.vector.tensor_scalar(out=prm[:, 5, :], in0=prm[:, 2, :], scalar1=-1.0, scalar2=1.0, op0=ALU.mult, op1=ALU.add), R=[prb], W=[prb])
                bc = lambda ap_: ap_.unsqueeze(2).to_broadcast([128, 4, 128])
                Xr = Ring(es, nc, "r_X", [128, 13, 129], F32, 2)
                dr = Ring(es, nc, "r_d", [128, 13, 128], F32, 2)
                xsr = Ring(es, nc, "r_xs", [128, 13, 128], F32, 2)
                twr = Ring(es, nc, "r_tw", [64, 128], F32, 2)
                ldr = Ring(es, nc, "r_ld", [128, 512], F32, 2)
                T4 = lambda name, n=2: Ring(es, nc, name, [128, 4, 128], F32, n)
                gir, ger, aar, kkr_, khr, bvr = T4("r_gi"), T4("r_ge"), T4("r_aa"), T4("r_kk"), T4("r_kh"), T4("r_bv")
                e1r, e2r, e3r, e4r = T4("r_e1"), T4("r_e2"), T4("r_e3"), T4("r_e4")
                tmpr = T4("r_tmp", 3)
                outr = T4("r_out", 4)
                vfr = T4("r_vf", 2)
                m1r = Ring(es, nc, "r_m1", [32, 128], F32, 2)
                tmo = Ring(es, nc, "r_tmo", [128, 512], F32, 3)
                rkro = Ring(es, nc, "r_rkr", [128, 8], F32, 2)
                glo = Ring(es, nc, "r_glo", [128, 4, 2], F32, 2)
                pr = Ring(es, nc, "r_ps", [128, 512], F32, 8, psum=True)
                fmv = lambda dt_: dt_.rearrange("(j p) t -> p j t", p=128)
                for c in range(NT):
                    t0 = c * 128
                    X, Xb = Xr.next()
                    src = pFM.rearrange("(ch p) t -> p ch t", p=128)
                    if c == 0:
                        k.op("pool", lambda: nc.gpsimd.memset(X[:, :, 0:1], 0.0), W=[Xb])
                        k.dma("sp", X[:, :, 1:129], src[:, FM_RW:FM_RW + 13, 0:128], W=[Xb])
                    else:
                        k.dma("sp", X[:], src[:, FM_RW:FM_RW + 13, t0 - 1:t0 + 128], W=[Xb])
                    d_, db = dr.next()
                    k.op("dve", lambda: nc.vector.tensor_tensor(out=d_[:], in0=X[:, :, 0:128], in1=X[:, :, 1:129], op=ALU.subtract), R=[Xb], W=[db])
                    xs_, xsb = xsr.next()
                    for ch in range(13):
                        k.op("dve", lambda: nc.vector.scalar_tensor_tensor(out=xs_[:, ch, :], in0=d_[:, ch, :], scalar=mu[:, ch:ch + 1], in1=X[:, ch, 1:129], op0=ALU.mult, op1=ALU.add),
                             R=[db, Xb, prb], W=[xsb])
                    rr = xs_[:, 0:4, :]; kx = xs_[:, 4:8, :]; vv = xs_[:, 8:12, :]
                    tw, twb = twr.next()
                    k.op("act", lambda: nc.scalar.activation(out=tw[:], in_=xs_[0:64, 12, :], func=AF.Tanh), R=[xsb], W=[twb])
                    pz, pzb = pr.next()
                    k.op("pe", lambda: nc.tensor.matmul(pz[:], lhsT=tw[:], rhs=w2t[:], start=True, stop=True), R=[twb, prb], W=[pzb])
                    ld, ldb = ldr.next()
                    k.op("dve", lambda: nc.vector.tensor_tensor(out=ld[:], in0=pz[:], in1=w0bc[:], op=ALU.add), R=[pzb, prb], W=[ldb])
                    k.op("act", lambda: nc.scalar.activation(out=ld[:], in_=ld[:], func=AF.Sigmoid), R=[ldb], W=[ldb])
                    k.op("pool", lambda: nc.gpsimd.tensor_scalar(out=ld[:], in0=ld[:], scalar1=float(-np.exp(-0.5)), scalar2=None, op0=ALU.mult), R=[ldb], W=[ldb])
                    pgi, pgib = pr.next(); pge, pgeb = pr.next()
                    for j in range(4):
                        k.op("pe", lambda: nc.tensor.matmul(pgi[:, j * 128:(j + 1) * 128], lhsT=ld[:, j * 128:(j + 1) * 128], rhs=cst[:, C_BI:C_BI + 128], start=True, stop=True), R=[ldb, cstb], W=[pgib])
                        k.op("pe", lambda: nc.tensor.matmul(pge[:, j * 128:(j + 1) * 128], lhsT=ld[:, j * 128:(j + 1) * 128], rhs=cst[:, C_BS:C_BS + 128], start=True, stop=True), R=[ldb, cstb], W=[pgeb])
                    gi, gib = gir.next(); ge, geb = ger.next()
                    k.op("act", lambda: nc.scalar.copy(out=gi[:], in_=pgi[:].rearrange("p (a b) -> p a b", a=4)), R=[pgib], W=[gib])
                    e1, e1b = e1r.next(); e2, e2b = e2r.next(); e3, e3b = e3r.next(); e4, e4b = e4r.next()
                    k.op("act", lambda: nc.scalar.activation(out=e1[:], in_=gi[:], func=AF.Exp), R=[gib], W=[e1b])
                    k.op("act", lambda: nc.scalar.activation(out=e2[:], in_=pge[:].rearrange("p (a b) -> p a b", a=4), func=AF.Exp), R=[pgeb], W=[e2b])
                    k.op("act", lambda: nc.scalar.activation(out=e3[:], in_=gi[:], func=AF.Exp, scale=-1.0), R=[gib], W=[e3b])
                    for j in range(4):
                        for hf in range(2):
                            k.op("act", lambda: nc.scalar.activation(out=e4[:, j, hf * 64:(hf + 1) * 64], in_=gi[:, j, hf * 64:(hf + 1) * 64], func=AF.Exp, scale=-1.0,
                                                                     bias=gi[:, j, hf * 64 + 63:hf * 64 + 64]), R=[gib], W=[e4b])
                    pa, pab = pr.next()
                    for j in range(4):
                        k.op("pe", lambda: nc.tensor.matmul(pa[:, j * 128:(j + 1) * 128], lhsT=a2t[64:128, j * 128:(j + 1) * 128], rhs=xs_[64:128, 12, :], start=True, stop=True), R=[xsb, prb], W=[pab])
                    aa, aab = aar.next()
                    for j in range(4):
                        k.op("act", lambda: nc.scalar.activation(out=aa[:, j, :], in_=pa[:, j * 128:(j + 1) * 128], func=AF.Sigmoid, bias=prm[:, 0, j:j + 1]), R=[pab, prb], W=[aab])
                    if l > 0:
                        pm, pmb = pr.next()
                        for ch in range(4):
                            k.op("pe", lambda: nc.tensor.matmul(pm[0:32, 0:128], lhsT=v1t[:, ch, :], rhs=xs_[:, 8 + ch, :], start=(ch == 0), stop=(ch == 3)), R=[xsb, prb], W=[pmb])
                        m1, m1b = m1r.next()
                        k.op("act", lambda: nc.scalar.copy(out=m1[:], in_=pm[0:32, 0:128]), R=[pmb], W=[m1b])
                        pm2, pm2b = pr.next()
                        for j in range(4):
                            k.op("pe", lambda: nc.tensor.matmul(pm2[:, j * 128:(j + 1) * 128], lhsT=v2t[:, j * 128:(j + 1) * 128], rhs=m1[:], start=True, stop=True), R=[m1b, prb], W=[pm2b])
                        gt_, gtb_ = tmpr.next()
                        for j in range(4):
                            k.op("act", lambda: nc.scalar.activation(out=gt_[:, j, :], in_=pm2[:, j * 128:(j + 1) * 128], func=AF.Sigmoid, bias=prm[:, 4, j:j + 1]), R=[pm2b, prb], W=[gtb_])
                        vf, vfb = vfr.next()
                        k.dma("sp", vf[:], fmv(vfirst)[:, :, t0:t0 + 128], W=[vfb])
                        k.op("dve", lambda: nc.vector.tensor_tensor(out=vf[:], in0=vf[:], in1=vv, op=ALU.subtract), R=[vfb, xsb], W=[vfb])
                        k.op("pool", lambda: nc.gpsimd.tensor_tensor(out=vf[:], in0=vf[:], in1=gt_[:], op=ALU.mult), R=[vfb, gtb_], W=[vfb])
                        k.op("dve", lambda: nc.vector.tensor_tensor(out=xs_[:, 8:12, :], in0=vv, in1=vf[:], op=ALU.add), R=[vfb, xsb], W=[xsb])
                    else:
                        k.dma("pool", fmv(vfirst)[:, :, t0:t0 + 128], vv, R=[xsb])
                    kk, kkb = kkr_.next()
                    k.op("dve", lambda: nc.vector.tensor_tensor(out=kk[:], in0=kx, in1=bc(prm[:, 1, :]), op=ALU.mult), R=[xsb, prb], W=[kkb])
                    sq_, sqb_ = tmpr.next()
                    k.op("act", lambda: nc.scalar.activation(out=sq_[:], in_=kk[:], func=AF.Square), R=[kkb], W=[sqb_])
                    pn, pnb = pr.next()
                    for j in range(4):
                        k.op("pe", lambda: nc.tensor.matmul(pn[:, j * 128:(j + 1) * 128], lhsT=cst[:, C_BD:C_BD + 128], rhs=sq_[:, j, :], start=True, stop=True), R=[sqb_, cstb], W=[pnb])
                    rn, rnb = tmpr.next()
                    k.op("act", lambda: nc.scalar.activation(out=rn[:], in_=pn[:].rearrange("p (a b) -> p a b", a=4), func=AF.Sqrt), R=[pnb], W=[rnb])
                    k.op("dve", lambda: nc.vector.tensor_scalar_max(out=rn[:], in0=rn[:], scalar1=1e-12), R=[rnb], W=[rnb])
                    k.op("dve", lambda: nc.vector.reciprocal(out=rn[:], in_=rn[:]), R=[rnb], W=[rnb])
                    k.op("pool", lambda: nc.gpsimd.tensor_tensor(out=kk[:], in0=kk[:], in1=rn[:], op=ALU.mult), R=[kkb, rnb], W=[kkb])
                    kh, khb = khr.next()
                    k.op("dve", lambda: nc.vector.tensor_tensor(out=kh[:], in0=aa[:], in1=bc(prm[:, 2, :]), op=ALU.mult), R=[aab, prb], W=[khb])
                    k.op("dve", lambda: nc.vector.tensor_tensor(out=kh[:], in0=kh[:], in1=bc(prm[:, 5, :]), op=ALU.add), R=[khb, prb], W=[khb])
                    k.op("dve", lambda: nc.vector.tensor_tensor(out=kh[:], in0=kh[:], in1=kx, op=ALU.mult), R=[khb, xsb], W=[khb])
                    bv, bvb = bvr.next()
                    k.op("pool", lambda: nc.gpsimd.tensor_tensor(out=bv[:], in0=kk[:], in1=aa[:], op=ALU.mult), R=[kkb, aab], W=[bvb])
                    def emit(dst, in0, in1, neg=False, R=()):
                        o, ob = outr.next()
                        k.op("dve", lambda: nc.vector.tensor_tensor(out=o[:], in0=in0, in1=in1, op=ALU.mult), R=list(R), W=[ob])
                        if neg:
                            k.op("pool", lambda: nc.gpsimd.tensor_scalar(out=o[:], in0=o[:], scalar1=-1.0, scalar2=None, op0=ALU.mult), R=[ob], W=[ob])
                        k.dma("pool", fmv(dst)[:, :, t0:t0 + 128], o[:], R=[ob])
                        return o, ob
                    emit(rwA, kk[:], e2[:], neg=True, R=[kkb, e2b])
                    emit(rwR, rr, e1[:], R=[xsb, e1b])
                    emit(rwB, bv[:], e3[:], R=[bvb, e3b])
                    emit(rwK, kh[:], e3[:], R=[khb, e3b])
                    for (dst, a_, ab_) in ((rwBp, bv, bvb), (rwKp, kh, khb), (rwV, None, None)):
                        if a_ is not None:
                            o, ob = outr.next()
                            k.op("dve", lambda: nc.vector.tensor_tensor(out=o[:], in0=a_[:], in1=e4[:], op=ALU.mult), R=[ab_, e4b], W=[ob])
                            srcv = o
                        else:
                            srcv, ob = xs_[:, 8:12, :], xsb
                        pt_, ptb_ = pr.next()
                        for j in range(4):
                            k.op("pe", lambda: nc.tensor.transpose(pt_[:, j * 128:(j + 1) * 128], srcv[:, j, :], ident), R=[ob, cstb], W=[ptb_])
                        to, tob = tmo.next()
                        k.op("act", lambda: nc.scalar.copy(out=to[:], in_=pt_[:]), R=[ptb_], W=[tob])
                        k.dma("pool", dst[t0:t0 + 128, :], to[:], R=[tob])
                    go_, gob = glo.next()
                    k.op("pool", lambda: nc.gpsimd.tensor_copy(out=go_[:, :, 0:1], in_=e1[:, :, 63:64]), R=[e1b], W=[gob])
                    k.op("pool", lambda: nc.gpsimd.tensor_copy(out=go_[:, :, 1:2], in_=e1[:, :, 127:128]), R=[e1b], W=[gob])
                    k.dma("pool", rwGL.rearrange("(j p) c -> p j c", p=128)[:, :, 2 * c:2 * c + 2], go_[:], R=[gob], slow=True)
                    pd_, pdb_ = tmpr.next()
                    k.op("dve", lambda: nc.vector.tensor_tensor(out=pd_[:], in0=rr, in1=kh[:], op=ALU.mult), R=[xsb, khb], W=[pdb_])
                    k.op("pool", lambda: nc.gpsimd.tensor_tensor(out=pd_[:], in0=pd_[:], in1=bc(prm[:, 3, :]), op=ALU.mult), R=[pdb_, prb], W=[pdb_])
                    pk, pkb = pr.next()
                    for j in range(4):
                        k.op("pe", lambda: nc.tensor.matmul(pk[:, 0:8], lhsT=pd_[:, j, :], rhs=cst[:, C_HS + 8 * j:C_HS + 8 * j + 8], start=(j == 0), stop=(j == 3)), R=[pdb_, cstb], W=[pkb])
                    ro, rob = rkro.next()
                    k.op("act", lambda: nc.scalar.copy(out=ro[:], in_=pk[:, 0:8]), R=[pkb], W=[rob])
                    k.dma("pool", rwRKR[t0:t0 + 128, :], ro[:], R=[rob])
                k.barrier()

            with ExitStack() as es:
                SB = lambda name, shape: es.enter_context(sbt(nc, name, shape, F32))
                GLt = SB("c_GL", [64, 8, NCH]); glb = Buf()
                k.dma("sp", GLt[:], rwGL.rearrange("(h q) c -> q h c", q=64), W=[glb])
                hv = lambda dt_: dt_.rearrange("(h q) t -> q h t", q=64)
                ARr = Ring(es, nc, "c_AR", [64, 8, 2, 64], F32, 3)
                BKr = Ring(es, nc, "c_BK", [64, 8, 2, 64], F32, 3)
                TMr = Ring(es, nc, "c_TM", [64, 3, 512], F32, 3)
                Hr = Ring(es, nc, "c_H", [64, 8, 64], F32, 2)
                A1r = Ring(es, nc, "c_A1", [64, 8, 128], F32, 2)
                A2r = Ring(es, nc, "c_A2", [64, 8, 128], F32, 2)
                Pr_ = Ring(es, nc, "c_P", [64, 8, 64], F32, 3)
                Ptr_ = Ring(es, nc, "c_Pt", [64, 8, 64], F32, 3)
                Lr = Ring(es, nc, "c_L", [64, 8, 64], F32, 3)
                Xsr = Ring(es, nc, "c_Xs", [64, 8, 64], F32, 2)
                Usr = Ring(es, nc, "c_Us", [64, 8, 64], F32, 2)
                Yr = Ring(es, nc, "c_Y", [64, 512], F32, 3)
                pr = Ring(es, nc, "c_ps", [128, 512], F32, 8, psum=True)
                m1 = cst[0:64, C_M1:C_M1 + 128].unsqueeze(1).to_broadcast([64, 4, 128])
                m3 = cst[0:64, C_M3:C_M3 + 64].unsqueeze(1).to_broadcast([64, 8, 64])
                i64b = cst[0:64, 0:64].unsqueeze(1).to_broadcast([64, 8, 64])
                H, Hb = Hr.next()
                k.op("pool", lambda: nc.gpsimd.memset(H[:], 0.0), W=[Hb])
                v8 = lambda p_: p_[0:64, :].rearrange("p (h d) -> p h d", h=8)
                for c in range(NCH):
                    cs_ = slice(c * 64, (c + 1) * 64)
                    AR, ARb = ARr.next(); BK, BKb = BKr.next(); TM_, TMb = TMr.next()
                    k.dma("sp", AR[:, :, 0, :], hv(rwA)[:, :, cs_], W=[ARb])
                    k.dma("sp", AR[:, :, 1, :], hv(rwR)[:, :, cs_], W=[ARb])
                    k.dma("sp", BK[:, :, 0, :], hv(rwB)[:, :, cs_], W=[BKb])
                    k.dma("sp", BK[:, :, 1, :], hv(rwK)[:, :, cs_], W=[BKb])
                    k.dma("sp", TM_[:, 0, :], rwBp[cs_, :], W=[TMb])
                    k.dma("sp", TM_[:, 1, :], rwKp[cs_, :], W=[TMb])
                    k.dma("sp", TM_[:, 2, :], rwV[cs_, :], W=[TMb])
                    A1, A1b = A1r.next(); A2, A2b = A2r.next()
                    for (A_, Ab_, which) in ((A1, A1b, 0), (A2, A2b, 1)):
                        for b in range(2):
                            p, pb = pr.next()
                            for hh_ in range(4):
                                h = b * 4 + hh_
                                k.op("pe", lambda: nc.tensor.matmul(p[0:64, hh_ * 128:(hh_ + 1) * 128], lhsT=BK[:, h, which, :], rhs=AR[:, h, :, :].rearrange("p a d -> p (a d)"), start=True, stop=True),
                                     R=[BKb, ARb], W=[pb])
                            k.op("dve", lambda: nc.vector.tensor_tensor(out=A_[:, b * 4:(b + 1) * 4, :], in0=p[0:64, :].rearrange("p (h d) -> p h d", h=4), in1=m1, op=ALU.mult), R=[pb, cstb], W=[Ab_])
                    p, pb = pr.next()
                    for h in range(8):
                        k.op("pe", lambda: nc.tensor.matmul(p[0:64, h * 64:(h + 1) * 64], lhsT=AR[:, h, 0, :], rhs=BK[:, h, 0, :], start=True, stop=True), R=[ARb, BKb], W=[pb])
                    Pt_, Ptb_ = Ptr_.next()
                    k.op("dve", lambda: nc.vector.tensor_tensor(out=Pt_[:], in0=v8(p), in1=m3, op=ALU.mult), R=[pb, cstb], W=[Ptb_])
                    P_, Pb_ = Pr_.next()
                    k.op("pool", lambda: nc.gpsimd.tensor_copy(out=P_[:], in_=A1[:, :, 0:64]), R=[A1b], W=[Pb_])
                    L_, Lb_ = Lr.next()
                    k.op("pool", lambda: nc.gpsimd.tensor_tensor(out=L_[:], in0=A1[:, :, 0:64], in1=i64b, op=ALU.add), R=[A1b, cstb], W=[Lb_])
                    for lev in range(5):
                        pa, pab = pr.next(); pb2, pb2b = pr.next()
                        for h in range(8):
                            if lev == 4:
                                break
                            k.op("pe", lambda: nc.tensor.matmul(pa[0:64, h * 64:(h + 1) * 64], lhsT=Pt_[:, h, :], rhs=P_[:, h, :], start=True, stop=True), R=[Ptb_, Pb_], W=[pab])
                        for h in range(8):
                            k.op("pe", lambda: nc.tensor.matmul(pb2[0:64, h * 64:(h + 1) * 64], lhsT=P_[:, h, :], rhs=Pt_[:, h, :], start=True, stop=True), R=[Ptb_, Pb_], W=[pb2b])
                        Pn, Pnb = Pr_.next(); Ptn, Ptnb = Ptr_.next()
                        if lev < 4:
                            k.op("act", lambda: nc.scalar.copy(out=Pn[:], in_=v8(pa)), R=[pab], W=[Pnb])
                        k.op("dve", lambda: nc.vector.tensor_copy(out=Ptn[:], in_=v8(pb2)), R=[pb2b], W=[Ptnb])
                        P_, Pb_, Pt_, Ptb_ = Pn, Pnb, Ptn, Ptnb
                        pc, pcb = pr.next()
                        for h in range(8):
                            k.op("pe", lambda: nc.tensor.matmul(pc[0:64, h * 64:(h + 1) * 64], lhsT=Pt_[:, h, :], rhs=L_[:, h, :], start=True, stop=True), R=[Ptb_, Lb_], W=[pcb])
                        Ln, Lnb = Lr.next()
                        k.op("dve", lambda: nc.vector.tensor_tensor(out=Ln[:], in0=L_[:], in1=v8(pc), op=ALU.add), R=[Lb_, pcb], W=[Lnb])
                        L_, Lb_ = Ln, Lnb
                    Vh = lambda h: TM_[:, 2, h * 64:(h + 1) * 64]
                    px, pxb = pr.next()
                    for h in range(8):
                        k.op("pe", lambda: nc.tensor.matmul(px[0:64, h * 64:(h + 1) * 64], lhsT=AR[:, h, 0, :], rhs=H[:, h, :], start=True, stop=False), R=[ARb, Hb], W=[pxb])
                        k.op("pe", lambda: nc.tensor.matmul(px[0:64, h * 64:(h + 1) * 64], lhsT=A2[:, h, 0:64], rhs=Vh(h), start=False, stop=True), R=[A2b, TMb], W=[pxb])
                    Xs, Xsb = Xsr.next()
                    k.op("act", lambda: nc.scalar.copy(out=Xs[:], in_=v8(px)), R=[pxb], W=[Xsb])
                    pu, pub = pr.next()
                    for h in range(8):
                        k.op("pe", lambda: nc.tensor.matmul(pu[0:64, h * 64:(h + 1) * 64], lhsT=L_[:, h, :], rhs=Xs[:, h, :], start=True, stop=True), R=[Lb_, Xsb], W=[pub])
                    Us, Usb = Usr.next()
                    k.op("dve", lambda: nc.vector.tensor_copy(out=Us[:], in_=v8(pu)), R=[pub], W=[Usb])
                    py, pyb = pr.next()
                    for h in range(8):
                        k.op("pe", lambda: nc.tensor.matmul(py[0:64, h * 64:(h + 1) * 64], lhsT=AR[:, h, 1, :], rhs=H[:, h, :], start=True, stop=False), R=[ARb, Hb], W=[pyb])
                        k.op("pe", lambda: nc.tensor.matmul(py[0:64, h * 64:(h + 1) * 64], lhsT=A1[:, h, 64:128], rhs=Us[:, h, :], start=False, stop=False), R=[A1b, Usb], W=[pyb])
                        k.op("pe", lambda: nc.tensor.matmul(py[0:64, h * 64:(h + 1) * 64], lhsT=A2[:, h, 64:128], rhs=Vh(h), start=False, stop=True), R=[A2b, TMb], W=[pyb])
                    Yt, Ytb = Yr.next()
                    k.op("act", lambda: nc.scalar.copy(out=Yt[:], in_=py[0:64, :]), R=[pyb], W=[Ytb])
                    k.dma("pool", rwY[cs_, :], Yt[:], R=[Ytb])
                    ph, phb = pr.next()
                    for h in range(8):
                        k.op("pe", lambda: nc.tensor.matmul(ph[0:64, h * 64:(h + 1) * 64], lhsT=TM_[:, 0, h * 64:(h + 1) * 64], rhs=Us[:, h, :], start=True, stop=False), R=[TMb, Usb], W=[phb])
                        k.op("pe", lambda: nc.tensor.matmul(ph[0:64, h * 64:(h + 1) * 64], lhsT=TM_[:, 1, h * 64:(h + 1) * 64], rhs=Vh(h), start=False, stop=True), R=[TMb], W=[phb])
                    Hn, Hnb = Hr.next()
                    k.op("dve", lambda: nc.vector.tensor_tensor(out=Hn[:], in0=H[:], in1=GLt[:, :, c:c + 1].to_broadcast([64, 8, 64]), op=ALU.mult), R=[Hb, glb], W=[Hnb])
                    k.op("dve", lambda: nc.vector.tensor_tensor(out=Hn[:], in0=Hn[:], in1=v8(ph), op=ALU.add), R=[Hnb, phb], W=[Hnb])
                    H, Hb = Hn, Hnb
                k.barrier()

            with ExitStack() as es:
                SB = lambda name, shape: es.enter_context(sbt(nc, name, shape, F32))
                lng = SB("e_g", [128, 512]); lnb = SB("e_b", [128, 512]); eb = Buf()
                k.dma("sp", lng[:], rw_ln_g[l].partition_broadcast(128), W=[eb])
                k.dma("sp", lnb[:], rw_ln_b[l].partition_broadcast(128), W=[eb])
                inr = Ring(es, nc, "e_in", [128, 3, 512], F32, 2)
                rkr_ = Ring(es, nc, "e_rk", [128, 8], F32, 2)
                sqr = Ring(es, nc, "e_sq", [128, 8, 64], F32, 2)
                str_ = Ring(es, nc, "e_st", [128, 4, 8], F32, 2)
                yr = Ring(es, nc, "e_y", [128, 8, 64], F32, 2)
                mtr = Ring(es, nc, "e_mt", [128, 4, 128], BF16, 2)
                pr = Ring(es, nc, "e_ps", [128, 512], F32, 2, psum=True)
                b8 = lambda ap_: ap_.unsqueeze(2).to_broadcast([128, 8, 64])
                v3 = lambda ap_: ap_.rearrange("p (h d) -> p h d", h=8)
                for c in range(NT):
                    t0 = c * 128
                    it, itb = inr.next()
                    k.dma("sp", it[:, 0, :], rwY[t0:t0 + 128, :], W=[itb])
                    k.dma("sp", it[:, 1, :], rwV[t0:t0 + 128, :], W=[itb])
                    k.dma("sp", it[:, 2, :], pTM[t0:t0 + 128, TM_RZ:TM_RZ + 512], W=[itb])
                    rk, rkb = rkr_.next()
                    k.dma("sp", rk[:], rwRKR[t0:t0 + 128, :], W=[rkb])
                    Y3 = v3(it[:, 0, :])
                    sq, sqb = sqr.next()
                    k.op("act", lambda: nc.scalar.activation(out=sq[:], in_=Y3, func=AF.Square), R=[itb], W=[sqb])
                    st, stb = str_.next()
                    k.op("dve", lambda: nc.vector.tensor_reduce(out=st[:, 0, :], in_=Y3, axis=AX.X, op=ALU.add), R=[itb], W=[stb])
                    k.op("dve", lambda: nc.vector.tensor_reduce(out=st[:, 1, :], in_=sq[:], axis=AX.X, op=ALU.add), R=[sqb], W=[stb])
                    k.op("dve", lambda: nc.vector.tensor_scalar(out=st[:, 0, :], in0=st[:, 0, :], scalar1=1.0 / 64, scalar2=None, op0=ALU.mult), R=[stb], W=[stb])
                    k.op("dve", lambda: nc.vector.tensor_tensor(out=st[:, 2, :], in0=st[:, 0, :], in1=st[:, 0, :], op=ALU.mult), R=[stb], W=[stb])
                    k.op("dve", lambda: nc.vector.scalar_tensor_tensor(out=st[:, 1, :], in0=st[:, 1, :], scalar=1.0 / 64, in1=st[:, 2, :], op0=ALU.mult, op1=ALU.subtract), R=[stb], W=[stb])
                    rstd(st[:, 3, :], st[:, 1, :], 1.0, C_GNEPS, [stb], [stb])
                    y, yb = yr.next()
                    k.op("dve", lambda: nc.vector.tensor_tensor(out=y[:], in0=Y3, in1=b8(st[:, 0, :]), op=ALU.subtract), R=[itb, stb], W=[yb])
                    k.op("dve", lambda: nc.vector.tensor_tensor(out=y[:], in0=y[:], in1=b8(st[:, 3, :]), op=ALU.mult), R=[yb, stb], W=[yb])
                    k.op("pool", lambda: nc.gpsimd.tensor_tensor(out=y[:], in0=y[:], in1=v3(lng[:]), op=ALU.mult), R=[yb, eb], W=[yb])
                    k.op("pool", lambda: nc.gpsimd.tensor_tensor(out=y[:], in0=y[:], in1=v3(lnb[:]), op=ALU.add), R=[yb, eb], W=[yb])
                    k.op("dve", lambda: nc.vector.tensor_tensor(out=sq[:], in0=v3(it[:, 1, :]), in1=b8(rk[:]), op=ALU.mult), R=[itb, rkb, sqb], W=[sqb])
                    k.op("dve", lambda: nc.vector.tensor_tensor(out=y[:], in0=y[:], in1=sq[:], op=ALU.add), R=[yb, sqb], W=[yb])
                    k.op("act", lambda: nc.scalar.activation(out=it[:, 2, :], in_=it[:, 2, :], func=AF.Silu), R=[itb], W=[itb])
                    k.op("dve", lambda: nc.vector.tensor_tensor(out=y[:], in0=y[:], in1=v3(it[:, 2, :]), op=ALU.mult), R=[yb, itb], W=[yb])
                    p, pb = pr.next()
                    yf = y[:].rearrange("p h d -> p (h d)")
                    for j in range(4):
                        k.op("pe", lambda: nc.tensor.transpose(p[:, j * 128:(j + 1) * 128], yf[:, j * 128:(j + 1) * 128], ident), R=[yb, cstb], W=[pb])
                    mt, mtb = mtr.next()
                    k.op("act", lambda: nc.scalar.copy(out=mt[:], in_=p[:].rearrange("p (a b) -> p a b", a=4)), R=[pb], W=[mtb])
                    k.dma("pool", mixT.rearrange("(cc p) t -> p cc t", p=128)[:, 12:16, t0:t0 + 128], mt[:], R=[mtb])
                k.barrier()

            with ExitStack() as es:
                mx = Ring(es, nc, "p5_m", [128, 16, G], BF16, 2)
                wfr = Ring(es, nc, "p5_wf", [128, 16, 512], F32, 2)
                wr = Ring(es, nc, "p5_w", [128, 16, 512], BF16, 2)
                xr = Ring(es, nc, "p5_x", [128, G], F32, 3)
                xo = Ring(es, nc, "p5_o", [128, G], F32, 3)
                ps = Ring(es, nc, "p5_ps", [128, 512], F32, 4, psum=True)
                for g in range(NG):
                    mt, mb = mx.next()
                    k.dma("sp", mt[:], mixT.rearrange("(cc p) t -> p cc t", p=128)[:, :, g * G:(g + 1) * G], W=[mb])
                    for nb in range(4):
                        wf, wfb = wfr.next()
                        k.dma("sp", wf[:], w_out[l].rearrange("(cc p) n -> p cc n", p=128)[:, :, nb * 512:(nb + 1) * 512], W=[wfb])
                        wt, wb = wr.next()
                        k.op("act", lambda: nc.scalar.copy(out=wt[:, 0:8, :], in_=wf[:, 0:8, :]), R=[wfb], W=[wb])
                        k.op("pool", lambda: nc.gpsimd.tensor_copy(out=wt[:, 8:16, :], in_=wf[:, 8:16, :]), R=[wfb], W=[wb])
                        for j in range(4):
                            n0 = nb * 512 + j * 128
                            xt, xb = xr.next()
                            k.dma("sp", xt[:], xT[n0:n0 + 128, g * G:(g + 1) * G], W=[xb])
                            p, pb = ps.next()
                            for cc in range(16):
                                k.op("pe", lambda: nc.tensor.matmul(p[:, :G], lhsT=wt[:, cc, j * 128:(j + 1) * 128], rhs=mt[:, cc, :],
                                                                    start=(cc == 0), stop=(cc == 15)), R=[wb, mb], W=[pb])
                            ot, ob = xo.next()
                            k.op("dve", lambda: nc.vector.tensor_tensor(out=ot[:], in0=p[:, :G], in1=xt[:], op=ALU.add), R=[pb, xb], W=[ob])
                            k.dma("pool", xT[n0:n0 + 128, g * G:(g + 1) * G], ot[:], R=[ob])
                k.barrier()

        with ExitStack() as es:
            gbc = es.enter_context(sbt(nc, "f_g", [128, D], F32))
            gbb = Buf()
            k.dma("sp", gbc[:], final_g.partition_broadcast(128), W=[gbb])
            xin = Ring(es, nc, "f_x", [128, 16, 128], F32, 2)
            xtm = Ring(es, nc, "f_t", [128, D], F32, 2)
            junk = es.enter_context(sbt(nc, "f_j", [128, D], F32))
            jb = Buf()
            ssq = Ring(es, nc, "f_s", [128, 2], F32, 2)
            ot = Ring(es, nc, "f_o", [128, D], F32, 2)
            ps = Ring(es, nc, "f_ps", [128, 512], F32, 8, psum=True)
            for t in range(NT):
                xt, xb = xin.next()
                k.dma("sp", xt[:], xT.rearrange("(kc p) t -> p kc t", p=128)[:, :, t * 128:(t + 1) * 128], W=[xb])
                xm, xmb = xtm.next()
                for q in range(4):
                    p, pb = ps.next()
                    for j in range(4):
                        kc = q * 4 + j
                        k.op("pe", lambda: nc.tensor.transpose(p[:, j * 128:(j + 1) * 128], xt[:, kc, :], ident), R=[xb, cstb], W=[pb])
                    if q % 2:
                        k.op("act", lambda: nc.scalar.copy(out=xm[:, q * 512:(q + 1) * 512], in_=p[:]), R=[pb], W=[xmb])
                    else:
                        k.op("dve", lambda: nc.vector.tensor_copy(out=xm[:, q * 512:(q + 1) * 512], in_=p[:]), R=[pb], W=[xmb])
                sq, sqb = ssq.next()
                k.op("act", lambda: nc.scalar.activation(out=junk[:], in_=xm[:], func=AF.Square, accum_out=sq[:, 0:1]), R=[xmb], W=[jb, sqb])
                rstd(sq[:, 1:2], sq[:, 0:1], 1.0 / D, C_EPS, [sqb], [sqb])
                o, ob = ot.next()
                k.op("dve", lambda: nc.vector.scalar_tensor_tensor(out=o[:], in0=xm[:], scalar=sq[:, 1:2], in1=gbc[:], op0=ALU.mult, op1=ALU.mult),
                     R=[xmb, sqb, gbb], W=[ob])
                k.dma("pool", out[t * 128:(t + 1) * 128, :], o[:], R=[ob])
            k.barrier()
    return nc


def MIXERS(env):
    pass


_CACHE = {}


WNAMES = ("norm_g", "w_in", "w_out", "final_norm_g", "ml_conv_w", "ml_conv_b", "ml_i_bias", "ml_f_bias", "ml_norm_g",
          "mla_q_norm_g", "mla_w_uq", "mla_kv_norm_g", "mla_w_ukv",
          "rw_mu", "rw_w0", "rw_w2", "rw_a0", "rw_a2", "rw_k_k", "rw_k_a", "rw_r_k", "rw_ln_g", "rw_ln_b")


def make_maps(inputs, T, depth):
    consts = make_consts()
    shared = {}
    for name in WNAMES:
        a = inputs[name]
        shared[name] = np.ascontiguousarray(a if name == "final_norm_g" else a[:depth])
    for name in ("rw_v0", "rw_v1", "rw_v2"):
        shared[name] = np.ascontiguousarray(inputs[name])
    in_maps = []
    for c in range(8):
        b = c % 4
        m = {"x": np.ascontiguousarray(inputs["x"][b, :T]), "positions": np.ascontiguousarray(inputs["positions"][b, :T]),
             "consts": consts}
        m.update(shared)
        in_maps.append(m)
    return in_maps


def kernel(**inputs):
    T = 4096
    depth = 4
    if "nc" not in _CACHE:
        _CACHE["nc"] = build(T, depth)
    nc = _CACHE["nc"]
    in_maps = make_maps(inputs, T, depth)
    res = run_bass_kernel_spmd(nc, in_maps, core_ids=list(range(8)))
    return np.stack([res.results[b]["out"] for b in range(4)], axis=0)
```
